# Optimizing a Trainium2 kernel written in Bass

```python
import math
import jax, jax.numpy as jnp
from jax import lax
import numpy as np

D_MODEL = 1024
BATCH = 4
SEQ = 4096
DEPTH = 4

GRID_W = 64
CTX_LEN = 256
N_BRANCH = 4
BRANCH_W = 512
RET_HEADS = 4
RET_DK = 128
RET_DV = 128
RET_CHUNK = 128
HY_ORDER = 2
HY_EMB = 33
HY_HID = 64
HY_FAST_PCT = 0.3
HY_SLOW_PCT = 1.5
HY_TARGET = 1e-2
ATT_HEADS = 4
ATT_KV_HEADS = 2
HEAD_DIM = 128
Q_BLOCK = 128
ROPE_THETA = 10000.0
FF_DENSE = 2816
N_EXPERTS = 8
TOP_K = 2
FF_EXPERT = 3584
MOE_BLOCK = 256
N_MOD = 6
ALPHA = (2 * DEPTH) ** 0.25
BETA = (8 * DEPTH) ** -0.25

A_W = 3 * BRANCH_W
R_W = 2 * RET_HEADS * RET_DK + 2 * RET_HEADS * RET_DV
H_W = (HY_ORDER + 1) * BRANCH_W
D_W = (ATT_HEADS + 2 * ATT_KV_HEADS) * HEAD_DIM
IN_W = A_W + R_W + H_W + D_W
IN_SPLITS = (A_W, A_W + R_W, A_W + R_W + H_W)
HY_FILT_OUT = HY_ORDER * 2 * BRANCH_W

kernel_name = "hybrid_dit_gated_branch_block"


def layer_norm(x, g, b, eps=1e-6):
    xf = x.astype(jnp.float32)
    mu = jnp.mean(xf, -1, keepdims=True)
    var = jnp.mean(jnp.square(xf - mu), -1, keepdims=True)
    return ((xf - mu) * lax.rsqrt(var + eps) * g + b).astype(x.dtype)


def rms_norm(x, g, eps=1e-6):
    xf = x.astype(jnp.float32)
    return (xf * lax.rsqrt(jnp.mean(xf * xf, -1, keepdims=True) + eps) * g).astype(x.dtype)


def head_norm(o, eps=1e-6):
    mu = jnp.mean(o, -1, keepdims=True)
    var = jnp.mean(jnp.square(o - mu), -1, keepdims=True)
    return (o - mu) * lax.rsqrt(var + eps)


def heads(z, n):
    b, l, _ = z.shape
    return z.reshape(b, l, n, -1).transpose(0, 2, 1, 3)


def merge_heads(z):
    b, n, l, d = z.shape
    return z.transpose(0, 2, 1, 3).reshape(b, l, n * d)


def flip(a):
    return a[:, :, ::-1]


def dwconv3(x, w, b):
    xp = jnp.pad(x, ((0, 0), (1, 1), (0, 0)))
    return xp[:, :-2] * w[0] + xp[:, 1:-1] * w[1] + xp[:, 2:] * w[2] + b


def axial_rope_tables(rows):
    row = jnp.repeat(jnp.arange(rows, dtype=jnp.float32), GRID_W)
    col = jnp.tile(jnp.arange(GRID_W, dtype=jnp.float32), rows)
    n_freq = HEAD_DIM // 4
    inv = ROPE_THETA ** (-jnp.arange(n_freq, dtype=jnp.float32) / n_freq)
    ang = jnp.stack([row[:, None] * inv, col[:, None] * inv], axis=1)
    return jnp.cos(ang), jnp.sin(ang)


def apply_rope(x, cos, sin):
    b, h, l, _ = x.shape
    xr = x.astype(jnp.float32).reshape(b, h, l, 2, 2, HEAD_DIM // 4)
    x1, x2 = xr[..., 0, :], xr[..., 1, :]
    o1 = x1 * cos - x2 * sin
    o2 = x2 * cos + x1 * sin
    return jnp.stack([o1, o2], axis=-2).reshape(x.shape).astype(x.dtype)


def short_conv_mixer(z, w, b):
    bg, cg, xv = jnp.split(z, 3, axis=-1)
    return bg * dwconv3(cg * xv, w, b)


def retention_scan(q, k, v, log_gamma, state0):
    b, h, l, _ = q.shape
    n = l // RET_CHUNK
    i = jnp.arange(RET_CHUNK, dtype=jnp.float32)
    lg = log_gamma.astype(jnp.float32)[:, None]
    rel = i[:, None] - i[None, :]
    intra = jnp.where(rel >= 0, jnp.exp(lg[..., None] * jnp.maximum(rel, 0.0)), 0.0)
    q_dec = jnp.exp(lg * (i + 1.0))[:, :, None]
    k_dec = jnp.exp(lg * (RET_CHUNK - 1.0 - i))[:, :, None]
    c_dec = jnp.exp(lg[:, 0] * RET_CHUNK)[:, None, None]

    def to_chunks(a):
        return a.reshape(b, h, n, RET_CHUNK, a.shape[-1]).transpose(2, 0, 1, 3, 4)

    def step(state, inp):
        qc, kc, vc = inp
        s = jnp.einsum('bhid,bhjd->bhij', qc, kc) * intra
        o = jnp.einsum('bhij,bhjv->bhiv', s, vc) + jnp.einsum('bhid,bhdv->bhiv', qc * q_dec, state)
        state = c_dec * state + jnp.einsum('bhjd,bhjv->bhdv', kc * k_dec, vc)
        return state, o

    state, o = lax.scan(step, state0, (to_chunks(q), to_chunks(k), to_chunks(v)))
    return o.transpose(1, 2, 0, 3, 4).reshape(b, h, l, v.shape[-1]), state


def retention_out(o, g):
    return jax.nn.silu(g) * merge_heads(head_norm(o)).astype(g.dtype)


def retention_mixer(z, zc, cos, sin, log_gamma, need_ctx):
    hk, hv = RET_HEADS * RET_DK, RET_HEADS * RET_DV

    def prep(zz, rotate):
        q, k, v, g = jnp.split(zz, (hk, 2 * hk, 2 * hk + hv), axis=-1)
        q = heads(q, RET_HEADS).astype(jnp.float32)
        k = heads(k, RET_HEADS).astype(jnp.float32) * RET_DK ** -0.5
        v = heads(v, RET_HEADS).astype(jnp.float32)
        if rotate:
            q, k = apply_rope(q, cos, sin), apply_rope(k, cos, sin)
        return q, k, v, g

    q, k, v, g = prep(z, True)
    qc, kc, vc, gc = prep(zc, False)
    zero = jnp.zeros((z.shape[0], RET_HEADS, RET_DK, RET_DV), jnp.float32)
    oc_f, st_f = retention_scan(qc, kc, vc, log_gamma[0], zero)
    oc_b, st_b = retention_scan(flip(qc), flip(kc), flip(vc), log_gamma[1], zero)
    o_f, _ = retention_scan(q, k, v, log_gamma[0], st_f)
    o_b, _ = retention_scan(flip(q), flip(k), flip(v), log_gamma[1], st_b)
    y = retention_out(o_f + flip(o_b), g)
    yc = retention_out(oc_f + flip(oc_b), gc) if need_ctx else None
    return y, yc


def hyena_filters(l, w1, b1, w2, b2, w3, freq):
    t = jnp.linspace(0.0, 1.0, l, dtype=jnp.float32)[:, None]
    bands = (HY_EMB - 1) // 2
    w = 2.0 * math.pi * jnp.arange(l, dtype=jnp.float32) / l
    f = jnp.linspace(1e-4, bands - 1, bands, dtype=jnp.float32)
    ang = w[:, None] * f[None, :]
    feats = jnp.concatenate([t, jnp.cos(ang), -jnp.sin(ang)], axis=-1)
    hdn = jnp.sin(freq[0] * (feats @ w1 + b1))
    hdn = jnp.sin(freq[1] * (hdn @ w2 + b2))
    filt = (hdn @ w3).astype(jnp.float32)
    max_decay = math.log(HY_TARGET) / HY_FAST_PCT
    min_decay = math.log(HY_TARGET) / HY_SLOW_PCT
    deltas = jnp.linspace(min_decay, max_decay, HY_FILT_OUT, dtype=jnp.float32)
    filt = filt * jnp.exp(-t * jnp.abs(deltas))
    return filt.reshape(l, HY_ORDER, 2, BRANCH_W).transpose(1, 2, 0, 3)


def bidir_fftconv(u, hf, hb):
    l = u.shape[1]
    filt = jnp.concatenate([hf, jnp.zeros_like(hf[:1]), hb[:0:-1]], axis=0)
    ff = jnp.fft.rfft(filt, axis=0)
    uf = jnp.fft.rfft(u, n=2 * l, axis=1)
    return jnp.fft.irfft(uf * ff, n=2 * l, axis=1)[:, :l]


def hyena_mixer(z, conv_w, conv_b, filt, skip):
    z = dwconv3(z, conv_w, conv_b)
    v, x1, x2 = jnp.split(z, 3, axis=-1)
    y = v.astype(jnp.float32)
    for o, gate in enumerate((x1, x2)):
        y = gate.astype(jnp.float32) * (bidir_fftconv(y, filt[o, 0], filt[o, 1]) + skip[o] * y)
    return y.astype(z.dtype)


def attention_mixer(z, zc, cos, sin, gq, gk, need_ctx):
    qw, kw = ATT_HEADS * HEAD_DIM, ATT_KV_HEADS * HEAD_DIM
    grp = ATT_HEADS // ATT_KV_HEADS
    scale = HEAD_DIM ** -0.5

    def prep(zz):
        q, k, v = jnp.split(zz, (qw, qw + kw), axis=-1)
        q = rms_norm(heads(q, ATT_HEADS), gq)
        k = rms_norm(heads(k, ATT_KV_HEADS), gk)
        return q, k, heads(v, ATT_KV_HEADS)

    def group(a):
        b, _, l, d = a.shape
        return a.reshape(b, ATT_KV_HEADS, grp, l, d)

    def attend(qg, keys, vals):
        s = jnp.einsum('bkgqd,bksd->bkgqs', qg, keys).astype(jnp.float32) * scale
        p = jax.nn.softmax(s, axis=-1).astype(vals.dtype)
        return jnp.einsum('bkgqs,bksd->bkgqd', p, vals)

    q, k, v = prep(z)
    qc, kc, vc = prep(zc)
    q, k = apply_rope(q, cos, sin), apply_rope(k, cos, sin)
    b, _, l, _ = q.shape
    nb = l // Q_BLOCK
    k_all = jnp.concatenate([k, kc], axis=2)
    v_all = jnp.concatenate([v, vc], axis=2)
    qb = group(q).reshape(b, ATT_KV_HEADS, grp, nb, Q_BLOCK, HEAD_DIM).transpose(3, 0, 1, 2, 4, 5)
    ob = lax.map(lambda qi: attend(qi, k_all, v_all), qb)
    y = merge_heads(ob.transpose(1, 2, 3, 0, 4, 5).reshape(b, ATT_HEADS, l, HEAD_DIM))
    if not need_ctx:
        return y, None
    oc = attend(group(qc), kc, vc)
    yc = merge_heads(oc.reshape(b, ATT_HEADS, zc.shape[1], HEAD_DIM))
    return y, yc


def merge_branches(u, ys, w_gate, b_gate, w_branch, w_o):
    m = 0.0
    for i, y in enumerate(ys):
        m = m + jax.nn.sigmoid(u @ w_gate[i] + b_gate[i]) * (y @ w_branch[i])
    return m @ w_o


def swiglu(u, w1, w3, w2):
    return (jax.nn.silu(u @ w1) * (u @ w3)) @ w2


def moe_ffn(t, w_r, w1, w3, w2):
    n_tok = t.shape[0]
    n_pair = n_tok * TOP_K
    logits = (t @ w_r).astype(jnp.float32)
    top_v, top_i = lax.top_k(logits, TOP_K)
    wts = jax.nn.softmax(top_v, axis=-1)
    e_flat = top_i.reshape(-1)
    tok = jnp.repeat(jnp.arange(n_tok, dtype=jnp.int32), TOP_K)
    order = jnp.argsort(e_flat)
    e_s, tok_s, w_s = e_flat[order], tok[order], wts.reshape(-1)[order]
    counts = jnp.bincount(e_flat, length=N_EXPERTS)
    padded = (counts + MOE_BLOCK - 1) // MOE_BLOCK * MOE_BLOCK
    start = jnp.cumsum(counts) - counts
    pad_end = jnp.cumsum(padded)
    pad_start = pad_end - padded
    dest = pad_start[e_s] + jnp.arange(n_pair, dtype=jnp.int32) - start[e_s]
    n_blk = -(-n_pair // MOE_BLOCK) + N_EXPERTS
    row_tok = jnp.zeros((n_blk * MOE_BLOCK,), jnp.int32).at[dest].set(tok_s)
    blk_e = jnp.minimum(jnp.searchsorted(pad_end, jnp.arange(n_blk) * MOE_BLOCK, side='right'), N_EXPERTS - 1)
    xb = t[row_tok].reshape(n_blk, MOE_BLOCK, t.shape[-1])
    yb = lax.map(lambda a: swiglu(a[0], w1[a[1]], w3[a[1]], w2[a[1]]), (xb, blk_e))
    yb = yb.reshape(n_blk * MOE_BLOCK, t.shape[-1])
    return jnp.zeros_like(t).at[tok_s].add(yb[dest] * w_s[:, None].astype(t.dtype))


def setup_inputs(seed: int = 0) -> dict:
    key = jax.random.key(seed)
    ks = iter(jax.random.split(key, 48))
    D = D_MODEL
    n_dense, n_moe = (DEPTH + 1) // 2, DEPTH // 2

    def nrm(shape, s):
        return jax.random.normal(next(ks), shape, jnp.float32) * s

    base = np.log(-np.log(1.0 - 2.0 ** (-5.0 - np.arange(RET_HEADS))))
    return {
        "x": nrm((BATCH, SEQ, D), 1.0),
        "c": nrm((BATCH, D), 1.0),
        "ctx": nrm((BATCH, CTX_LEN, D), 1.0),
        "c_ctx": nrm((D,), 1.0),
        "w_mod": nrm((DEPTH, D, N_MOD * D), D ** -0.5),
        "b_mod": nrm((DEPTH, N_MOD * D), 0.02),
        "w_in": nrm((DEPTH, D, IN_W), D ** -0.5),
        "conv_a_w": nrm((DEPTH, 3, BRANCH_W), 3 ** -0.5),
        "conv_a_b": nrm((DEPTH, BRANCH_W), 0.02),
        "ret_decay": jnp.asarray(base, jnp.float32) + nrm((DEPTH, 2, RET_HEADS), 0.1),
        "hy_conv_w": nrm((DEPTH, 3, H_W), 3 ** -0.5),
        "hy_conv_b": nrm((DEPTH, H_W), 0.02),
        "hy_w1": nrm((DEPTH, HY_EMB, HY_HID), HY_EMB ** -0.5),
        "hy_b1": nrm((DEPTH, HY_HID), 0.02),
        "hy_w2": nrm((DEPTH, HY_HID, HY_HID), HY_HID ** -0.5),
        "hy_b2": nrm((DEPTH, HY_HID), 0.02),
        "hy_w3": nrm((DEPTH, HY_HID, HY_FILT_OUT), 0.03 * HY_HID ** -0.5),
        "hy_freq": 1.0 + nrm((DEPTH, 2, HY_HID), 0.05),
        "hy_skip": nrm((DEPTH, HY_ORDER, BRANCH_W), 0.5),
        "q_norm": 1.0 + nrm((DEPTH, HEAD_DIM), 0.02),
        "k_norm": 1.0 + nrm((DEPTH, HEAD_DIM), 0.02),
        "w_gate": nrm((DEPTH, N_BRANCH, D, D), D ** -0.5),
        "b_gate": nrm((DEPTH, N_BRANCH, D), 0.02),
        "w_branch": nrm((DEPTH, N_BRANCH, BRANCH_W, D), BRANCH_W ** -0.5 * BETA),
        "w_o": nrm((DEPTH, D, D), D ** -0.5 * BETA),
        "ln_g": 1.0 + nrm((DEPTH, 2, D), 0.02),
        "ln_b": nrm((DEPTH, 2, D), 0.02),
        "ffn_w1": nrm((n_dense, D, FF_DENSE), D ** -0.5),
        "ffn_w3": nrm((n_dense, D, FF_DENSE), D ** -0.5),
        "ffn_w2": nrm((n_dense, FF_DENSE, D), FF_DENSE ** -0.5 * BETA),
        "router": nrm((n_moe, D, N_EXPERTS), D ** -0.5),
        "moe_w1": nrm((n_moe, N_EXPERTS, D, FF_EXPERT), D ** -0.5),
        "moe_w3": nrm((n_moe, N_EXPERTS, D, FF_EXPERT), D ** -0.5),
        "moe_w2": nrm((n_moe, N_EXPERTS, FF_EXPERT, D), FF_EXPERT ** -0.5 * BETA),
    }


def reference(x, c, ctx, c_ctx, w_mod, b_mod, w_in, conv_a_w, conv_a_b, ret_decay,
              hy_conv_w, hy_conv_b, hy_w1, hy_b1, hy_w2, hy_b2, hy_w3, hy_freq, hy_skip,
              q_norm, k_norm, w_gate, b_gate, w_branch, w_o, ln_g, ln_b,
              ffn_w1, ffn_w3, ffn_w2, router, moe_w1, moe_w3, moe_w2):
    n_lat, n_ctx = x.shape[1], ctx.shape[1]
    rows = n_lat // GRID_W
    cos, sin = axial_rope_tables(rows)
    c_act = jax.nn.silu(c)[:, None, :]
    cc_act = jax.nn.silu(c_ctx)
    h = ctx
    for l in range(DEPTH):
        need_ctx = l < DEPTH - 1
        mod = jnp.split(c_act @ w_mod[l] + b_mod[l], N_MOD, axis=-1)
        mod_c = jnp.split(cc_act @ w_mod[l] + b_mod[l], N_MOD, axis=-1)

        u = x * (1.0 + mod[1]) + mod[0]
        uc = h * (1.0 + mod_c[1]) + mod_c[0]
        zA, zR, zH, zD = jnp.split(u @ w_in[l], IN_SPLITS, axis=-1)
        cA, cR, cH, cD = jnp.split(uc @ w_in[l], IN_SPLITS, axis=-1)
        log_gamma = -jnp.exp(ret_decay[l].astype(jnp.float32))
        yR, yRc = retention_mixer(zR, cR, cos, sin, log_gamma, need_ctx)
        yD, yDc = attention_mixer(zD, cD, cos, sin, q_norm[l], k_norm[l], need_ctx)
        yA = short_conv_mixer(zA, conv_a_w[l], conv_a_b[l])
        filt = hyena_filters(n_lat, hy_w1[l], hy_b1[l], hy_w2[l], hy_b2[l], hy_w3[l], hy_freq[l])
        yH = hyena_mixer(zH, hy_conv_w[l], hy_conv_b[l], filt, hy_skip[l])
        mix = merge_branches(u, (yA, yR, yH, yD), w_gate[l], b_gate[l], w_branch[l], w_o[l])
        x = layer_norm(ALPHA * x + mod[2] * mix, ln_g[l, 0], ln_b[l, 0])
        if need_ctx:
            yAc = short_conv_mixer(cA, conv_a_w[l], conv_a_b[l])
            filt_c = hyena_filters(n_ctx, hy_w1[l], hy_b1[l], hy_w2[l], hy_b2[l], hy_w3[l], hy_freq[l])
            yHc = hyena_mixer(cH, hy_conv_w[l], hy_conv_b[l], filt_c, hy_skip[l])
            mix_c = merge_branches(uc, (yAc, yRc, yHc, yDc), w_gate[l], b_gate[l], w_branch[l], w_o[l])
            h = layer_norm(ALPHA * h + mod_c[2] * mix_c, ln_g[l, 0], ln_b[l, 0])

        u = x * (1.0 + mod[4]) + mod[3]
        i = l // 2
        if l % 2 == 0:
            f = swiglu(u, ffn_w1[i], ffn_w3[i], ffn_w2[i])
            if need_ctx:
                uc = h * (1.0 + mod_c[4]) + mod_c[3]
                fc = swiglu(uc, ffn_w1[i], ffn_w3[i], ffn_w2[i])
        else:
            flat = u.reshape(-1, D_MODEL)
            if need_ctx:
                uc = h * (1.0 + mod_c[4]) + mod_c[3]
                flat = jnp.concatenate([flat, uc.reshape(-1, D_MODEL)], axis=0)
            fo = moe_ffn(flat, router[i], moe_w1[i], moe_w3[i], moe_w2[i])
            n_u = u.shape[0] * n_lat
            f = fo[:n_u].reshape(u.shape)
            if need_ctx:
                fc = fo[n_u:].reshape(h.shape)
        x = layer_norm(ALPHA * x + mod[5] * f, ln_g[l, 1], ln_b[l, 1])
        if need_ctx:
            h = layer_norm(ALPHA * h + mod_c[5] * fc, ln_g[l, 1], ln_b[l, 1])
    return x
```

```python
import contextlib
import numpy as np
import concourse.bass as bass
import concourse.mybir as mybir

F32 = mybir.dt.float32
BF16 = mybir.dt.bfloat16
AF = mybir.ActivationFunctionType
ALU = mybir.AluOpType
AX = mybir.AxisListType

NDMA = 24
ENGS = ['pe', 'act', 'dve', 'pool', 'sp']


class Buf:
    __slots__ = ('w', 'r', 'name', 'excl')

    def __init__(self, name=''):
        self.w = None
        self.r = {}
        self.name = name
        self.excl = False


PSUM_KEYS = {'pt', 'ptb', 'pa', 'pb', 'pc', 'pg', 'pp', 'pmx', 'pg1', 'pg3', 'pf', 'pw', 'pm'}


class Bufs:
    def __init__(self, name=''):
        self.d = {}
        self.name = name

    def __getitem__(self, k):
        if k == 'ptb2':
            k = 'ptb'
        b = self.d.get(k)
        if b is None:
            b = Buf(f"{self.name}{k}")
            k0 = k[0] if isinstance(k, tuple) else k
            b.excl = k0 in PSUM_KEYS
            self.d[k] = b
        return b


class Prog:
    def __init__(self, nc, stack, same_engine_sync=True):
        self.nc = nc
        self.stack = stack
        self.q = {e: [] for e in ENGS}
        self.sem = {e: stack.enter_context(nc.semaphore(f"s_{e}")) for e in ENGS}
        self.cnt = {e: 0 for e in ENGS}
        self.seen = {e: {} for e in ENGS}
        self.dsem = [stack.enter_context(nc.semaphore(f"dq{i}")) for i in range(NDMA)]
        self.dval = [0] * NDMA
        self.dnext = 0
        self.ses = same_engine_sync
        self.out_tokens = []

    def sb(self, name, shape, dt):
        self._uid = getattr(self, '_uid', 0) + 1
        return self.stack.enter_context(self.nc.sbuf_tensor(f"{name}_u{self._uid}", list(shape), dt))

    def ps(self, name, shape, dt=F32):
        self._uid = getattr(self, '_uid', 0) + 1
        return self.stack.enter_context(self.nc.psum_tensor(f"{name}_u{self._uid}", list(shape), dt))

    def _semh(self, k):
        return self.sem[k] if isinstance(k, str) else self.dsem[k[1]]

    def _deps(self, e, reads, writes):
        need = {}
        xr = [b for b in reads if b.excl]
        if xr:
            writes = list(writes) + xr

        def add(tok):
            if tok is None:
                return
            k, v = tok
            if need.get(k, 0) < v:
                need[k] = v

        for b in reads:
            add(b.w)
        for b in writes:
            add(b.w)
            for k, v in b.r.items():
                add((k, v))
        waits = []
        for k, v in need.items():
            if k == e and (e == 'pe' or not self.ses):
                continue
            if self.seen[e].get(k, 0) >= v:
                continue
            self.seen[e][k] = v
            waits.append((k, v))
        return waits

    def _mark(self, tok, reads, writes):
        k, v = tok
        xr = [b for b in reads if b.excl]
        if xr:
            writes = list(writes) + xr
        for b in reads:
            if b.r.get(k, 0) < v:
                b.r[k] = v
        for b in writes:
            b.w = tok
            b.r = {}

    mute = False

    def op(self, e, fn, reads=(), writes=()):
        if self.mute:
            return
        waits = self._deps(e, reads, writes)
        self.cnt[e] += 1
        tok = (e, self.cnt[e])
        self._mark(tok, reads, writes)
        self.q[e].append((waits, fn, (self.sem[e], 1)))

    def dma(self, e, fn, reads=(), writes=(), is_output=False):
        if self.mute:
            return
        if e == 'pool':
            i = self._dpool = (getattr(self, '_dpool', -1) + 1) % 8
        else:
            i = 8 + self.dnext
            self.dnext = (self.dnext + 1) % (NDMA - 8)
        waits = self._deps(e, reads, writes)
        k = ('d', i)
        if self.dval[i] > 0 and self.seen[e].get(k, 0) < self.dval[i]:
            waits.append((k, self.dval[i]))
            self.seen[e][k] = self.dval[i]
        self.dval[i] += 16
        tok = (k, self.dval[i])
        self._mark(tok, reads, writes)
        self.q[e].append((waits, fn, (self.dsem[i], 16)))
        if is_output:
            self.out_tokens.append(tok)

    def flush(self):
        nc = self.nc
        q = self.q
        semh = self._semh
        ex = []
        for e in ['pe', 'act', 'dve', 'pool']:
            if self.cnt[e] > 0:
                ex.append((e, self.cnt[e]))
        for i in range(NDMA):
            if self.dval[i] > 0:
                ex.append((('d', i), self.dval[i]))

        def emit(eng, ename):
            for waits, fn, (sem, n) in q[ename]:
                for (k, v) in waits:
                    eng.wait_ge(semh(k), v)
                fn(eng).then_inc(sem, n)
            for (k, v) in ex:
                if self.seen[ename].get(k, 0) < v:
                    eng.wait_ge(semh(k), v)
                    self.seen[ename][k] = v

        with nc.Block() as block:
            @block.tensor
            def _(t):
                emit(t, 'pe')

            @block.scalar
            def _(t):
                emit(t, 'act')

            @block.vector
            def _(t):
                emit(t, 'dve')

            @block.gpsimd
            def _(t):
                emit(t, 'pool')

            @block.sync
            def _(t):
                emit(t, 'sp')
        self.q = {e: [] for e in ENGS}

from concourse.bass_utils import run_bass_kernel_spmd

D = 1024
ALPHA = (2 * 4) ** 0.25
NTILE = 17
NTOK = NTILE * 128
BLOCKS = [(0, 4), (4, 4), (8, 4), (12, 4), (16, 1)]


def _din(nc, name, shape, dt=F32):
    return nc.dram_tensor(name, list(shape), dt, kind="ExternalInput").ap()


def _dout(nc, name, shape, dt=F32):
    return nc.dram_tensor(name, list(shape), dt, kind="ExternalOutput").ap()


def build_M():
    nc = bass.Bass("TRN2", target_bir_lowering=False)
    cT = _din(nc, "cT", [128, 8, 5])
    w = _din(nc, "w", [128, 8, 3072])
    b = _din(nc, "b", [5, 3072])
    o = _dout(nc, "o", [5, 3072])
    with contextlib.ExitStack() as st:
        P = Prog(nc, st)
        B = Bufs()
        cs = P.sb("cs", [128, 8, 5], F32)
        sc = P.sb("sc", [128, 8, 5], F32)
        ws = P.sb("ws", [128, 8, 3072], F32)
        bs = P.sb("bs", [5, 3072], F32)
        os_ = P.sb("os", [5, 3072], F32)
        pm = P.ps("pm", [128, 2, 512], F32)
        P.dma('sp', lambda e: e.dma_start(out=cs[:], in_=cT), writes=[B['cs']])
        P.dma('sp', lambda e: e.dma_start(out=bs[:], in_=b), writes=[B['bs']])
        for kt in range(8):
            P.dma('sp', lambda e, kt=kt: e.dma_start(out=ws[:, kt, :], in_=w[:, kt, :]), writes=[B['ws', kt]])
        P.op('act', lambda e: e.activation(out=sc[:], in_=cs[:], func=AF.Silu), reads=[B['cs']], writes=[B['sc']])
        for nb in range(6):
            pb = B['pm', nb % 2]
            for kt in range(8):
                P.op('pe', lambda e, nb=nb, kt=kt: e.matmul(pm[0:5, nb % 2, :], lhsT=sc[:, kt, :], rhs=ws[:, kt, nb * 512:(nb + 1) * 512],
                                                          start=(kt == 0), stop=(kt == 7)),
                     reads=[B['sc'], B['ws', kt]], writes=[pb])
            P.op('dve', lambda e, nb=nb: e.tensor_tensor(out=os_[:, nb * 512:(nb + 1) * 512], in0=pm[0:5, nb % 2, :],
                                                        in1=bs[:, nb * 512:(nb + 1) * 512], op=ALU.add),
                 reads=[pb, B['bs']], writes=[B['os', nb]])
        P.dma('sp', lambda e: e.dma_start(out=o, in_=os_[:]), reads=[B['os', nb] for nb in range(6)], is_output=True)
        P.flush()
    return nc


def emit_B(nc, P, Dm, moe, gt=None):
    if gt is None:
        gt = lambda t: t
    KC = 4 if moe else 2
    NCH = 56 if moe else 11
    CPE = 7 if moe else 11
    x = Dm['x']
    yT = Dm['yT']
    modcols = Dm['modcols']
    modrows = Dm['modrows']
    lnrows = Dm['lnrows']
    bgc = Dm['bgc']
    ident = Dm['ident']
    wg = Dm['wg']
    wb = Dm['wb']
    wo = Dm['wo']
    w1 = Dm['w1']
    w3 = Dm['w3']
    w2 = Dm['w2']
    x1o = Dm['x1o']
    xo = Dm['xo']
    if moe:
        rt = Dm['rt']
        sel = Dm['sel']
    with contextlib.ExitStack() as st:
        P.stack = st
        B = Bufs()
        ids = P.sb("ids", [128, 128], F32)
        mcol = P.sb("mcol", [128, 2, 4, 8], F32)
        bgs = P.sb("bgs", [128, 4, 8], F32)
        u2T = P.sb("u2T", [128, 8, NTOK], BF16)
        P.dma('sp', lambda e: e.dma_start(out=ids[:], in_=ident), writes=[B['ids']])
        if isinstance(modcols, tuple):
            P.dma('sp', lambda e: e.dma_start(out=mcol[:, :, 0:2, :], in_=modcols[0]), writes=[B['mcol']])
            P.dma('sp', lambda e: e.dma_start(out=mcol[:, :, 2:4, :], in_=modcols[1]), writes=[B['mcol']])
        else:
            P.dma('sp', lambda e: e.dma_start(out=mcol[:], in_=modcols), writes=[B['mcol']])
        P.dma('sp', lambda e: e.dma_start(out=bgs[:], in_=bgc), writes=[B['bgs']])
        P.op('dve', lambda e: e.tensor_scalar_add(out=mcol[:, :, 1, :], in0=mcol[:, :, 1, :], scalar1=1.0), reads=[B['mcol']], writes=[B['mcol']])
        P.op('dve', lambda e: e.tensor_scalar_add(out=mcol[:, :, 3, :], in0=mcol[:, :, 3, :], scalar1=1.0), reads=[B['mcol']], writes=[B['mcol']])
        if moe:
            rts = P.sb("rts", [128, 8, 8], F32)
            sels = P.sb("sels", [8, 8, 128], F32)
            wall = P.sb("wall", [128, NTILE, 8], F32)
            WT = P.sb("WT", [8, NTOK], F32)
            P.dma('sp', lambda e: e.dma_start(out=rts[:], in_=rt), writes=[B['rts']])
            P.dma('sp', lambda e: e.dma_start(out=sels[:], in_=sel), writes=[B['sels']])

        def ln_tile(r, stt, mv, rstd, gi, key):
            rb = B[key]
            for hf in range(2):
                P.op('dve', lambda e, hf=hf: e.bn_stats(out=stt[:, hf, :], in_=r[:, hf * 512:(hf + 1) * 512]), reads=[rb], writes=[B[key, 'st', hf]])
            P.op('dve', lambda e: e.bn_aggr(out=mv[:], in_=stt[:].rearrange("p a b -> p (a b)")), reads=[B[key, 'st', 0], B[key, 'st', 1]], writes=[B[key, 'mv']])
            P.op('dve', lambda e: e.tensor_scalar_add(out=rstd[:], in0=mv[:, 1:2], scalar1=1e-6), reads=[B[key, 'mv']], writes=[B[key, 'rstd']])
            P.op('act', lambda e: e.sqrt(out=rstd[:], in_=rstd[:]), reads=[B[key, 'rstd']], writes=[B[key, 'rstd']])
            P.op('dve', lambda e: e.reciprocal(out=rstd[:], in_=rstd[:]), reads=[B[key, 'rstd']], writes=[B[key, 'rstd']])
            P.op('dve', lambda e: e.tensor_scalar(out=r[:], in0=r[:], scalar1=mv[:, 0:1], scalar2=rstd[:, 0:1], op0=ALU.subtract, op1=ALU.mult),
                 reads=[rb, B[key, 'mv'], B[key, 'rstd']], writes=[rb])
            P.op('pool', lambda e: e.tensor_tensor(out=r[:], in0=r[:], in1=lnr[:, gi, :], op=ALU.mult), reads=[rb, B['lnr']], writes=[rb])
            P.op('pool', lambda e: e.tensor_tensor(out=r[:], in0=r[:], in1=lnr[:, gi + 1, :], op=ALU.add), reads=[rb, B['lnr']], writes=[rb])

        with contextlib.ExitStack() as s1:
            P.stack = s1
            mrow = P.sb("mrow", [128, 2, 2, D], F32)
            lnr = P.sb("lnr", [128, 4, D], F32)
            P.dma('sp', lambda e: e.dma_start(out=mrow[:], in_=modrows), writes=[B['mrow']])
            P.dma('sp', lambda e: e.dma_start(out=lnr[:], in_=lnrows), writes=[B['lnr']])
            xs = P.sb("xs", [128, 4, D], F32)
            uT = P.sb("uT", [128, 8, 512], BF16)
            yTb = P.sb("yTb", [128, 16, 512], BF16)
            mT = P.sb("mT", [128, 8, 512], BF16)
            wgs = [P.sb(f"wgs{i}", [128, 4, 8, 128], BF16) for i in range(2)]
            wbs = [P.sb(f"wbs{i}", [128, 4, 4, 128], BF16) for i in range(2)]
            wos = P.sb("wos", [128, 8, D], BF16)
            sg = [P.sb(f"sg{i}", [128, 512], F32) for i in range(2)]
            macc = P.sb("macc", [128, 512], F32)
            mtmp = P.sb("mtmp", [128, 512], F32)
            rr = [P.sb(f"rr{i}", [128, D], F32) for i in range(2)]
            t1 = P.sb("t1", [128, D], F32)
            stt = P.sb("stt", [128, 2, 6], F32)
            mv = P.sb("mv", [128, 2], F32)
            rstd = P.sb("rstd", [128, 1], F32)
            u2f = P.sb("u2f", [128, 8, 128], F32)
            pt = P.ps("pt", [128, 2, 512], F32)
            pg = P.ps("pg", [128, 2, 512], F32)
            pp = P.ps("pp", [128, 2, 512], F32)
            pmx = P.ps("pmx", [128, 2, 512], F32)
            if moe:
                lg = P.sb("lg", [128, 8], F32)
                m8 = P.sb("m8", [128, 8], F32)
                msk = P.sb("msk", [128, 8], F32)
                ex = P.sb("ex", [128, 8], F32)
                sm = P.sb("sm", [128, 4], F32)

            P.dma('pool', lambda e: e.dma_start(out=wos[:], in_=wo), writes=[B['wos']])
            wcnt = 0
            tcnt = 0
            for bi, (t0, nt) in enumerate(BLOCKS):
                ms = 1 if bi == 4 else 0
                ntok = nt * 128
                tok0 = gt(t0) * 128
                P.dma('sp', lambda e, t0=t0, nt=nt: e.dma_start(out=xs[:, 0:nt, :], in_=x[gt(t0):gt(t0) + nt].rearrange("t p f -> p t f")),
                      writes=[B['xs', j] for j in range(nt)])
                P.dma('pool', lambda e, tok0=tok0, ntok=ntok: e.dma_start(out=yTb[:, :, 0:ntok], in_=yT[:, :, tok0:tok0 + ntok].rearrange("c p t -> p c t")),
                      writes=[B['yTb']])
                for j in range(nt):
                    for f in range(8):
                        pb = B['pt', tcnt % 2]
                        P.op('pe', lambda e, j=j, f=f, s=tcnt % 2: e.transpose(pt[:, s, 0:128], xs[:, j, f * 128:(f + 1) * 128], ids[:]),
                             reads=[B['xs', j], B['ids']], writes=[pb])
                        P.op('act', lambda e, j=j, f=f, s=tcnt % 2, ms=ms: e.activation(out=uT[:, f, j * 128:(j + 1) * 128], in_=pt[:, s, 0:128], func=AF.Identity,
                                                                                      bias=mcol[:, ms, 0, f:f + 1], scale=mcol[:, ms, 1, f:f + 1]),
                             reads=[pb, B['mcol']], writes=[B['uT', f]])
                        tcnt += 1
                for fo in range(8):
                    ws_ = wcnt % 2
                    P.dma('pool', lambda e, fo=fo, ws_=ws_: e.dma_start(out=wgs[ws_][:], in_=wg[fo]), writes=[B['wgs', ws_]])
                    P.dma('pool', lambda e, fo=fo, ws_=ws_: e.dma_start(out=wbs[ws_][:], in_=wb[fo]), writes=[B['wbs', ws_]])
                    wcnt += 1
                    for br in range(4):
                        s = br % 2
                        for kt in range(8):
                            P.op('pe', lambda e, br=br, kt=kt, s=s, ws_=ws_, ntok=ntok: e.matmul(pg[:, s, 0:ntok], lhsT=wgs[ws_][:, br, kt, :], rhs=uT[:, kt, 0:ntok],
                                                                                               start=(kt == 0), stop=(kt == 7)),
                                 reads=[B['wgs', ws_], B['uT', kt]], writes=[B['pg', s]])
                        for kt in range(4):
                            P.op('pe', lambda e, br=br, kt=kt, s=s, ws_=ws_, ntok=ntok: e.matmul(pp[:, s, 0:ntok], lhsT=wbs[ws_][:, br, kt, :], rhs=yTb[:, br * 4 + kt, 0:ntok],
                                                                                               start=(kt == 0), stop=(kt == 3)),
                                 reads=[B['wbs', ws_], B['yTb']], writes=[B['pp', s]])
                        P.op('act', lambda e, br=br, fo=fo, s=s, ntok=ntok: e.activation(out=sg[s][:, 0:ntok], in_=pg[:, s, 0:ntok], func=AF.Sigmoid,
                                                                                       bias=bgs[:, br, fo:fo + 1], scale=1.0),
                             reads=[B['pg', s], B['bgs']], writes=[B['sg', s]])
                        if br == 0:
                            P.op('dve', lambda e, s=s, ntok=ntok: e.tensor_tensor(out=macc[:, 0:ntok], in0=sg[s][:, 0:ntok], in1=pp[:, s, 0:ntok], op=ALU.mult),
                                 reads=[B['sg', s], B['pp', s]], writes=[B['macc']])
                        else:
                            P.op('dve', lambda e, s=s, ntok=ntok: e.tensor_tensor(out=mtmp[:, 0:ntok], in0=sg[s][:, 0:ntok], in1=pp[:, s, 0:ntok], op=ALU.mult),
                                 reads=[B['sg', s], B['pp', s]], writes=[B['mtmp']])
                            if br < 3:
                                P.op('pool', lambda e, ntok=ntok: e.tensor_tensor(out=macc[:, 0:ntok], in0=macc[:, 0:ntok], in1=mtmp[:, 0:ntok], op=ALU.add),
                                     reads=[B['macc'], B['mtmp']], writes=[B['macc']])
                            else:
                                P.op('pool', lambda e, fo=fo, ntok=ntok: e.tensor_tensor(out=mT[:, fo, 0:ntok], in0=macc[:, 0:ntok], in1=mtmp[:, 0:ntok], op=ALU.add),
                                     reads=[B['macc'], B['mtmp']], writes=[B['mT', fo]])
                for j in range(nt):
                    tile = t0 + j
                    r = rr[tile % 2]
                    rk = ('rr', tile % 2)
                    for hf in range(2):
                        for kt in range(8):
                            P.op('pe', lambda e, j=j, hf=hf, kt=kt: e.matmul(pmx[:, hf, :], lhsT=mT[:, kt, j * 128:(j + 1) * 128], rhs=wos[:, kt, hf * 512:(hf + 1) * 512],
                                                                           start=(kt == 0), stop=(kt == 7)),
                                 reads=[B['mT', kt], B['wos']], writes=[B['pmx', hf]])
                        P.op('dve', lambda e, hf=hf, ms=ms: e.tensor_tensor(out=t1[:, hf * 512:(hf + 1) * 512], in0=pmx[:, hf, :], in1=mrow[:, ms, 0, hf * 512:(hf + 1) * 512], op=ALU.mult),
                             reads=[B['pmx', hf], B['mrow']], writes=[B['t1', hf]])
                    P.op('dve', lambda e, j=j, r=r: e.scalar_tensor_tensor(out=r[:], in0=xs[:, j, :], scalar=ALPHA, in1=t1[:], op0=ALU.mult, op1=ALU.add),
                         reads=[B['xs', j], B['t1', 0], B['t1', 1]], writes=[B[rk]])
                    ln_tile(r, stt, mv, rstd, 0, rk)
                    P.dma('sp', lambda e, tile=tile, r=r: e.dma_start(out=x1o[gt(tile)], in_=r[:]), reads=[B[rk]], writes=[B['x1o', tile]])
                    for f in range(8):
                        pb = B['pt', tcnt % 2]
                        P.op('pe', lambda e, r=r, f=f, s=tcnt % 2: e.transpose(pt[:, s, 0:128], r[:, f * 128:(f + 1) * 128], ids[:]),
                             reads=[B[rk], B['ids']], writes=[pb])
                        if moe:
                            P.op('act', lambda e, f=f, s=tcnt % 2, ms=ms: e.activation(out=u2f[:, f, :], in_=pt[:, s, 0:128], func=AF.Identity,
                                                                                     bias=mcol[:, ms, 2, f:f + 1], scale=mcol[:, ms, 3, f:f + 1]),
                                 reads=[pb, B['mcol']], writes=[B['u2f', f]])
                            P.op('dve', lambda e, f=f, tile=tile: e.tensor_copy(out=u2T[:, f, tile * 128:(tile + 1) * 128], in_=u2f[:, f, :]),
                                 reads=[B['u2f', f]], writes=[B['u2T', f, tile]])
                        else:
                            P.op('act', lambda e, f=f, s=tcnt % 2, ms=ms, tile=tile: e.activation(out=u2T[:, f, tile * 128:(tile + 1) * 128], in_=pt[:, s, 0:128], func=AF.Identity,
                                                                                                bias=mcol[:, ms, 2, f:f + 1], scale=mcol[:, ms, 3, f:f + 1]),
                                 reads=[pb, B['mcol']], writes=[B['u2T', f, tile]])
                        tcnt += 1
                    if moe:
                        for kt in range(8):
                            P.op('pe', lambda e, kt=kt: e.matmul(pg[:, 0, 0:8], lhsT=u2f[:, kt, :], rhs=rts[:, kt, :], start=(kt == 0), stop=(kt == 7)),
                                 reads=[B['u2f', kt], B['rts']], writes=[B['pg', 0]])
                        P.op('dve', lambda e: e.tensor_copy(out=lg[:], in_=pg[:, 0, 0:8]), reads=[B['pg', 0]], writes=[B['lg']])
                        P.op('dve', lambda e: e.max(out=m8[:], in_=lg[:]), reads=[B['lg']], writes=[B['m8']])
                        P.op('dve', lambda e: e.tensor_scalar(out=msk[:], in0=lg[:], scalar1=m8[:, 1:2], scalar2=None, op0=ALU.is_ge), reads=[B['lg'], B['m8']], writes=[B['msk']])
                        P.op('dve', lambda e: e.tensor_scalar(out=sm[:, 0:1], in0=m8[:, 0:1], scalar1=-1.0, scalar2=None, op0=ALU.mult), reads=[B['m8']], writes=[B['sm', 0]])
                        P.op('dve', lambda e: e.tensor_tensor(out=sm[:, 1:2], in0=m8[:, 1:2], in1=m8[:, 0:1], op=ALU.subtract), reads=[B['m8']], writes=[B['sm', 1]])
                        P.op('act', lambda e: e.activation(out=ex[:], in_=lg[:], func=AF.Exp, bias=sm[:, 0:1], scale=1.0), reads=[B['lg'], B['sm', 0]], writes=[B['ex']])
                        P.op('act', lambda e: e.activation(out=sm[:, 2:3], in_=sm[:, 1:2], func=AF.Exp), reads=[B['sm', 1]], writes=[B['sm', 2]])
                        P.op('dve', lambda e: e.tensor_scalar_add(out=sm[:, 2:3], in0=sm[:, 2:3], scalar1=1.0), reads=[B['sm', 2]], writes=[B['sm', 2]])
                        P.op('dve', lambda e: e.reciprocal(out=sm[:, 3:4], in_=sm[:, 2:3]), reads=[B['sm', 2]], writes=[B['sm', 3]])
                        P.op('dve', lambda e: e.tensor_tensor(out=ex[:], in0=ex[:], in1=msk[:], op=ALU.mult), reads=[B['ex'], B['msk']], writes=[B['ex']])
                        P.op('dve', lambda e, tile=tile: e.tensor_scalar(out=wall[:, tile, :], in0=ex[:], scalar1=sm[:, 3:4], scalar2=None, op0=ALU.mult),
                             reads=[B['ex'], B['sm', 3]], writes=[B['wall', tile]])
                        P.op('pe', lambda e, tile=tile: e.transpose(pp[0:8, 0, 0:128], wall[:, tile, :], ids[:]), reads=[B['wall', tile], B['ids']], writes=[B['pp', 0]])
                        P.op('act', lambda e, tile=tile: e.copy(out=WT[:, tile * 128:(tile + 1) * 128], in_=pp[0:8, 0, 0:128]), reads=[B['pp', 0]], writes=[B['WT']])
            P.flush()
        s2o = st.enter_context(contextlib.ExitStack())
        P.stack = s2o
        facc = P.sb("facc", [128, NTILE, D], F32)
        pg1 = P.ps("pg1", [128, 2, 512], F32)
        pg3 = P.ps("pg3", [128, 2, 512], F32)
        pf = P.ps("pf", [128, 2, 512], F32)
        pw = P.ps("pw", [128, 512], F32)
        with contextlib.ExitStack() as s2:
            P.stack = s2
            w1s = [P.sb(f"w1s{i}", [128, 8, KC * 128], BF16) for i in range(2)]
            w3s = [P.sb(f"w3s{i}", [128, 8, KC * 128], BF16) for i in range(2)]
            w2s = [P.sb(f"w2s{i}", [128, KC, D], BF16) for i in range(2)]
            s1b = [P.sb(f"s1b{i}", [128, 512], F32) for i in range(2)]
            hT = [P.sb(f"hT{i}", [128, KC, 512], BF16) for i in range(2)]
            gcnt = 0
            fcnt = 0
            hcnt = 0
            for ci in range(NCH):
                wsl = ci % 2
                ex_ = ci // CPE
                P.dma('pool', lambda e, ci=ci, wsl=wsl: e.dma_start(out=w1s[wsl][:], in_=w1[ci]), writes=[B['w1s', wsl]])
                P.dma('pool', lambda e, ci=ci, wsl=wsl: e.dma_start(out=w3s[wsl][:], in_=w3[ci]), writes=[B['w3s', wsl]])
                P.dma('pool', lambda e, ci=ci, wsl=wsl: e.dma_start(out=w2s[wsl][:], in_=w2[ci]), writes=[B['w2s', wsl]])
                for bi, (t0, nt) in enumerate(BLOCKS):
                    ntok = nt * 128
                    tok0 = t0 * 128
                    hs = hcnt % 2
                    hcnt += 1
                    if moe:
                        P.op('pe', lambda e, ex_=ex_, tok0=tok0, ntok=ntok: e.matmul(pw[:, 0:ntok], lhsT=sels[:, ex_, :], rhs=WT[:, tok0:tok0 + ntok], start=True, stop=True),
                             reads=[B['sels'], B['WT']], writes=[B['pw']])
                    for kk in range(KC):
                        g = gcnt % 2
                        gcnt += 1
                        for kt in range(8):
                            P.op('pe', lambda e, kk=kk, kt=kt, g=g, wsl=wsl, tok0=tok0, ntok=ntok: e.matmul(pg1[:, g, 0:ntok], lhsT=w1s[wsl][:, kt, kk * 128:(kk + 1) * 128],
                                                                                                       rhs=u2T[:, kt, tok0:tok0 + ntok], start=(kt == 0), stop=(kt == 7)),
                                 reads=[B['w1s', wsl], B['u2T']], writes=[B['pg1', g]])
                        for kt in range(8):
                            P.op('pe', lambda e, kk=kk, kt=kt, g=g, wsl=wsl, tok0=tok0, ntok=ntok: e.matmul(pg3[:, g, 0:ntok], lhsT=w3s[wsl][:, kt, kk * 128:(kk + 1) * 128],
                                                                                                       rhs=u2T[:, kt, tok0:tok0 + ntok], start=(kt == 0), stop=(kt == 7)),
                                 reads=[B['w3s', wsl], B['u2T']], writes=[B['pg3', g]])
                        P.op('act', lambda e, g=g, ntok=ntok: e.activation(out=s1b[g][:, 0:ntok], in_=pg1[:, g, 0:ntok], func=AF.Silu), reads=[B['pg1', g]], writes=[B['s1b', g]])
                        if moe:
                            P.op('dve', lambda e, g=g, ntok=ntok: e.tensor_tensor(out=s1b[g][:, 0:ntok], in0=s1b[g][:, 0:ntok], in1=pg3[:, g, 0:ntok], op=ALU.mult),
                                 reads=[B['s1b', g], B['pg3', g]], writes=[B['s1b', g]])
                            P.op('dve', lambda e, g=g, kk=kk, hs=hs, ntok=ntok: e.tensor_tensor(out=hT[hs][:, kk, 0:ntok], in0=s1b[g][:, 0:ntok], in1=pw[:, 0:ntok], op=ALU.mult),
                                 reads=[B['s1b', g], B['pw']], writes=[B['hT', hs, kk]])
                        else:
                            P.op('dve', lambda e, g=g, kk=kk, hs=hs, ntok=ntok: e.tensor_tensor(out=hT[hs][:, kk, 0:ntok], in0=s1b[g][:, 0:ntok], in1=pg3[:, g, 0:ntok], op=ALU.mult),
                                 reads=[B['s1b', g], B['pg3', g]], writes=[B['hT', hs, kk]])
                    for j in range(nt):
                        tile = t0 + j
                        for hf in range(2):
                            fs = fcnt % 2
                            fcnt += 1
                            for kk in range(KC):
                                P.op('pe', lambda e, j=j, hf=hf, kk=kk, fs=fs, hs=hs, wsl=wsl: e.matmul(pf[:, fs, :], lhsT=hT[hs][:, kk, j * 128:(j + 1) * 128],
                                                                                                    rhs=w2s[wsl][:, kk, hf * 512:(hf + 1) * 512], start=(kk == 0), stop=(kk == KC - 1)),
                                     reads=[B['hT', hs, kk], B['w2s', wsl]], writes=[B['pf', fs]])
                            fb = B['facc', tile, hf]
                            if ci == 0:
                                P.op('act', lambda e, tile=tile, hf=hf, fs=fs: e.copy(out=facc[:, tile, hf * 512:(hf + 1) * 512], in_=pf[:, fs, :]), reads=[B['pf', fs]], writes=[fb])
                            else:
                                P.op('dve', lambda e, tile=tile, hf=hf, fs=fs: e.tensor_tensor(out=facc[:, tile, hf * 512:(hf + 1) * 512], in0=facc[:, tile, hf * 512:(hf + 1) * 512],
                                                                                              in1=pf[:, fs, :], op=ALU.add), reads=[B['pf', fs], fb], writes=[fb])
            P.flush()
        with contextlib.ExitStack() as s3:
            P.stack = s3
            mrow = P.sb("mrow3", [128, 2, 2, D], F32)
            lnr = P.sb("lnr3", [128, 4, D], F32)
            P.dma('sp', lambda e: e.dma_start(out=mrow[:], in_=modrows), writes=[B['mrow']])
            P.dma('sp', lambda e: e.dma_start(out=lnr[:], in_=lnrows), writes=[B['lnr']])
            xr = [P.sb(f"xr{i}", [128, D], F32) for i in range(2)]
            t1 = P.sb("t1b", [128, D], F32)
            stt = P.sb("stt2", [128, 2, 6], F32)
            mv = P.sb("mv2", [128, 2], F32)
            rstd = P.sb("rstd2", [128, 1], F32)
            for tile in range(NTILE):
                ms = 1 if tile == 16 else 0
                r = xr[tile % 2]
                rk = ('xr', tile % 2)
                P.dma('sp', lambda e, tile=tile, r=r: e.dma_start(out=r[:], in_=x1o[gt(tile)]), reads=[B['x1o', tile]], writes=[B[rk]])
                P.op('dve', lambda e, tile=tile, ms=ms: e.tensor_tensor(out=t1[:], in0=facc[:, tile, :], in1=mrow[:, ms, 1, :], op=ALU.mult),
                     reads=[B['facc', tile, 0], B['facc', tile, 1], B['mrow']], writes=[B['t1b']])
                P.op('dve', lambda e, r=r: e.scalar_tensor_tensor(out=r[:], in0=r[:], scalar=ALPHA, in1=t1[:], op0=ALU.mult, op1=ALU.add),
                     reads=[B[rk], B['t1b']], writes=[B[rk]])
                ln_tile(r, stt, mv, rstd, 2, rk)
                P.dma('sp', lambda e, tile=tile, r=r: e.dma_start(out=xo[gt(tile)], in_=r[:]), reads=[B[rk]], is_output=True)
            P.flush()


def build_B(moe):
    KC = 4 if moe else 2
    NCH = 56 if moe else 11
    CPE = 7 if moe else 11
    nc = bass.Bass("TRN2", target_bir_lowering=False)
    x = _din(nc, "x", [NTILE, 128, D])
    yT = _din(nc, "yT", [16, 128, NTOK])
    modcols = _din(nc, "modcols", [128, 2, 4, 8])
    modrows = _din(nc, "modrows", [128, 2, 2, D])
    lnrows = _din(nc, "lnrows", [128, 4, D])
    bgc = _din(nc, "bgc", [128, 4, 8])
    ident = _din(nc, "ident", [128, 128])
    wg = _din(nc, "wg", [8, 128, 4, 8, 128])
    wb = _din(nc, "wb", [8, 128, 4, 4, 128])
    wo = _din(nc, "wo", [128, 8, D])
    w1 = _din(nc, "w1", [NCH, 128, 8, KC * 128])
    w3 = _din(nc, "w3", [NCH, 128, 8, KC * 128])
    w2 = _din(nc, "w2", [NCH, 128, KC, D])
    if moe:
        rt = _din(nc, "rt", [128, 8, 8])
        sel = _din(nc, "sel", [8, 8, 128])
    x1o = _dout(nc, "x1o", [NTILE, 128, D])
    xo = _dout(nc, "xo", [NTILE, 128, D])

    Dm = dict(x=x, yT=yT, modcols=modcols, modrows=modrows, lnrows=lnrows, bgc=bgc, ident=ident, wg=wg, wb=wb, wo=wo, w1=w1, w3=w3, w2=w2, x1o=x1o, xo=xo)
    if moe:
        Dm['rt'] = rt
        Dm['sel'] = sel
    with contextlib.ExitStack() as st0:
        P = Prog(nc, st0)
        emit_B(nc, P, Dm, moe)
    return nc


_CACHE = {}


def _prog(name, fn, *a):
    k = (name,) + a
    if k not in _CACHE:
        _CACHE[k] = fn(*a)
    return _CACHE[k]


def _tile_rows(w):
    K, N = w.shape
    return np.ascontiguousarray(w.reshape(K // 128, 128, N).transpose(1, 0, 2))


def _cols(v):
    return np.ascontiguousarray(v.reshape(-1, 128).T)


def _rep(v):
    return np.ascontiguousarray(np.broadcast_to(v[None], (128,) + v.shape))


_IDENT = np.eye(128, dtype=np.float32)


def run_M(c, c_ctx, w_mod, b_mod):
    cc = np.concatenate([c, c_ctx[None]], 0)
    cT = np.ascontiguousarray(cc.T.reshape(8, 128, 5).transpose(1, 0, 2))
    Wm = w_mod.transpose(1, 0, 2).reshape(1024, 4 * 6144)
    bm = b_mod.reshape(4 * 6144)
    ins = []
    for i in range(8):
        sl = slice(i * 3072, (i + 1) * 3072)
        ins.append(dict(cT=cT, w=_tile_rows(Wm[:, sl]), b=np.ascontiguousarray(np.broadcast_to(bm[None, sl], (5, 3072)))))
    res = run_bass_kernel_spmd(_prog('M', build_M), ins, core_ids=list(range(8)))
    mod = np.concatenate([res.results[i]['o'] for i in range(8)], axis=1)
    return mod.reshape(5, 4, 6144)


def prep_B_weights(l, inp):
    moe = (l % 2 == 1)
    i = l // 2
    d = {}
    d['wg'] = np.ascontiguousarray(inp['w_gate'][l].reshape(4, 8, 128, 8, 128).transpose(3, 2, 0, 1, 4))
    d['wb'] = np.ascontiguousarray(inp['w_branch'][l].reshape(4, 4, 128, 8, 128).transpose(3, 2, 0, 1, 4))
    d['wo'] = _tile_rows(inp['w_o'][l])
    d['lnrows'] = _rep(np.stack([inp['ln_g'][l, 0], inp['ln_b'][l, 0], inp['ln_g'][l, 1], inp['ln_b'][l, 1]], 0))
    d['bgc'] = np.ascontiguousarray(inp['b_gate'][l].reshape(4, 8, 128).transpose(2, 0, 1))
    d['ident'] = _IDENT
    if not moe:
        KC, NCH = 2, 11
        d['w1'] = np.ascontiguousarray(inp['ffn_w1'][i].reshape(8, 128, NCH, KC * 128).transpose(2, 1, 0, 3))
        d['w3'] = np.ascontiguousarray(inp['ffn_w3'][i].reshape(8, 128, NCH, KC * 128).transpose(2, 1, 0, 3))
        d['w2'] = np.ascontiguousarray(inp['ffn_w2'][i].reshape(NCH, KC, 128, 1024).transpose(0, 2, 1, 3))
    else:
        KC, CPE = 4, 7
        d['w1'] = np.ascontiguousarray(inp['moe_w1'][i].reshape(8, 8, 128, CPE, KC * 128).transpose(0, 3, 2, 1, 4)).reshape(56, 128, 8, KC * 128)
        d['w3'] = np.ascontiguousarray(inp['moe_w3'][i].reshape(8, 8, 128, CPE, KC * 128).transpose(0, 3, 2, 1, 4)).reshape(56, 128, 8, KC * 128)
        d['w2'] = np.ascontiguousarray(inp['moe_w2'][i].reshape(8, CPE, KC, 128, 1024).transpose(0, 1, 3, 2, 4)).reshape(56, 128, KC, 1024)
        d['rt'] = _tile_rows(inp['router'][i])
        sel = np.zeros((8, 8, 128), np.float32)
        for e in range(8):
            sel[e, e, :] = 1.0
        d['sel'] = sel
    return d


def run_B(l, x_cur, h_cur, YT, mod, inp):
    moe = (l % 2 == 1)
    wd = prep_B_weights(l, inp)
    ins = []
    for b in range(4):
        for half in range(2):
            d = dict(wd)
            xt = np.concatenate([x_cur[b, half * 2048:(half + 1) * 2048], h_cur[b, half * 128:(half + 1) * 128]], 0)
            d['x'] = np.ascontiguousarray(xt.reshape(17, 128, 1024))
            toks = np.concatenate([np.arange(half * 2048, (half + 1) * 2048), 4096 + np.arange(half * 128, (half + 1) * 128)])
            d['yT'] = np.ascontiguousarray(YT[b][:, toks].reshape(16, 128, NTOK))
            ms = [mod[b], mod[4]]
            d['modcols'] = np.ascontiguousarray(np.stack([np.stack([_cols(m[k * 1024:(k + 1) * 1024]) for k in (0, 1, 3, 4)], 1) for m in ms], 1))
            d['modrows'] = _rep(np.stack([np.stack([m[2048:3072], m[5120:6144]], 0) for m in ms], 0))
            ins.append(d)
    res = run_bass_kernel_spmd(_prog('B', build_B, moe), ins, core_ids=list(range(8)))
    x_new = np.empty_like(x_cur)
    h_new = np.empty_like(h_cur)
    x1 = np.empty_like(x_cur)
    for b in range(4):
        for half in range(2):
            o = res.results[b * 2 + half]['xo'].reshape(NTOK, 1024)
            x_new[b, half * 2048:(half + 1) * 2048] = o[:2048]
            h_new[b, half * 128:(half + 1) * 128] = o[2048:]
            x1[b, half * 2048:(half + 1) * 2048] = res.results[b * 2 + half]['x1o'].reshape(NTOK, 1024)[:2048]
    return x_new, h_new, x1


NT_A = 34
NTOK_A = NT_A * 128
TBLK_A = [(i * 512, 512) for i in range(8)] + [(4096, 256)]
PI = float(np.pi)


def emit_A(nc, P, Dm):
    x = Dm['x']
    modcols = Dm['modcols']
    ident = Dm['ident']
    wA = Dm['wA']
    wR = Dm['wR']
    wH = Dm['wH']
    wD = Dm['wD']
    caw = Dm['caw']
    hcw = Dm['hcw']
    hsk = Dm['hsk']
    ropec = Dm['ropec']
    ropes = Dm['ropes']
    qkn = Dm['qkn']
    rdec = Dm['rdec']
    rtab = Dm['rtab']
    rcol = Dm['rcol']
    featsT = Dm['featsT']
    featsTc = Dm['featsTc']
    hw1 = Dm['hw1']
    hw2 = Dm['hw2']
    hcols = Dm['hcols']
    hw3 = Dm['hw3']
    negt = Dm['negt']
    adel = Dm['adel']
    Fm = Dm['Fm']
    Gm = Dm['Gm']
    Fc = Dm['Fc']
    Gc = Dm['Gc']
    YT = Dm['YT']
    with contextlib.ExitStack() as st:
        P.stack = st
        B = Bufs()
        ids = P.sb("ids", [128, 128], F32)
        idb = P.sb("idb", [128, 128], BF16)
        mcol = P.sb("mcol", [128, 2, 2, 8], F32)
        P.dma('sp', lambda e: e.dma_start(out=ids[:], in_=ident), writes=[B['ids']])
        P.dma('sp', lambda e: e.dma_start(out=mcol[:], in_=modcols), writes=[B['mcol']])
        P.op('dve', lambda e: e.tensor_copy(out=idb[:], in_=ids[:]), reads=[B['ids']], writes=[B['idb']])
        P.op('dve', lambda e: e.tensor_scalar_add(out=mcol[:, :, 1, :], in0=mcol[:, :, 1, :], scalar1=1.0), reads=[B['mcol']], writes=[B['mcol']])
        pt = P.ps("pt", [128, 2, 512], F32)
        ptb = P.ps("ptb", [128, 1024], BF16)
        pa = P.ps("pa", [128, 2, 512], F32)
        pb_ = P.ps("pb", [128, 2, 512], F32)
        pc = P.ps("pc", [128, 512], F32)
        cnt = {'t': 0}

        def build_uT(uT, stk):
            P.stack = stk
            xb = [P.sb(f"xb{i}", [128, D], F32) for i in range(2)]
            for t in range(NT_A):
                ms = 1 if t >= 32 else 0
                xs = xb[t % 2]
                P.dma('sp', lambda e, t=t, xs=xs: e.dma_start(out=xs[:], in_=x[t]), writes=[B['xb', t % 2]])
                for f in range(8):
                    s = cnt['t'] % 2
                    cnt['t'] += 1
                    P.op('pe', lambda e, xs=xs, f=f, s=s: e.transpose(pt[:, s, 0:128], xs[:, f * 128:(f + 1) * 128], ids[:]),
                         reads=[B['xb', t % 2], B['ids']], writes=[B['pt', s]])
                    P.op('act', lambda e, t=t, f=f, s=s, ms=ms: e.activation(out=uT[:, f, t * 128:(t + 1) * 128], in_=pt[:, s, 0:128], func=AF.Identity,
                                                                           bias=mcol[:, ms, 0, f:f + 1], scale=mcol[:, ms, 1, f:f + 1]),
                         reads=[B['pt', s], B['mcol']], writes=[B['uT']])

        def inproj_fm(uT, wsb, wkey, col0, dst, dkey):
            for bi, (tok0, ntok) in enumerate(TBLK_A):
                s = bi % 2
                for kt in range(8):
                    P.op('pe', lambda e, kt=kt, s=s, tok0=tok0, ntok=ntok: e.matmul(pa[:, s, 0:ntok], lhsT=wsb[:, kt, col0:col0 + 128], rhs=uT[:, kt, tok0:tok0 + ntok],
                                                                                  start=(kt == 0), stop=(kt == 7)),
                         reads=[B[wkey], B['uT']], writes=[B['pa', s]])
                eng = 'act' if bi % 2 == 0 else 'dve'
                if eng == 'act':
                    P.op('act', lambda e, s=s, tok0=tok0, ntok=ntok: e.copy(out=dst[:, tok0:tok0 + ntok], in_=pa[:, s, 0:ntok]), reads=[B['pa', s]], writes=[B[dkey]])
                else:
                    P.op('dve', lambda e, s=s, tok0=tok0, ntok=ntok: e.tensor_copy(out=dst[:, tok0:tok0 + ntok], in_=pa[:, s, 0:ntok]), reads=[B['pa', s]], writes=[B[dkey]])

        def dwconv(z, zkey, wc, wkey, acc, akey, out, okey):
            P.op('dve', lambda e: e.tensor_scalar(out=acc[:], in0=z, scalar1=wc[:, 1:2], scalar2=wc[:, 3:4], op0=ALU.mult, op1=ALU.add),
                 reads=[B[zkey], B[wkey]], writes=[B[akey]])
            for (a, n) in ((0, 4096), (4096, 256)):
                P.op('dve', lambda e, a=a, n=n: e.scalar_tensor_tensor(out=acc[:, a + 1:a + n], in0=z[:, a:a + n - 1], scalar=wc[:, 0:1], in1=acc[:, a + 1:a + n],
                                                                      op0=ALU.mult, op1=ALU.add), reads=[B[zkey], B[wkey], B[akey]], writes=[B[akey]])
                P.op('dve', lambda e, a=a, n=n: e.scalar_tensor_tensor(out=acc[:, a:a + n - 1], in0=z[:, a + 1:a + n], scalar=wc[:, 2:3], in1=acc[:, a:a + n - 1],
                                                                      op0=ALU.mult, op1=ALU.add), reads=[B[zkey], B[wkey], B[akey]], writes=[B[akey]])
            if out is not None:
                P.op('pool', lambda e: e.tensor_copy(out=out, in_=acc[:]), reads=[B[akey]], writes=[B[okey]])

        import os as _os
        _dbg = _os.environ.get('A_DBG', '').split(',')
        P.mute = ('noH' in _dbg)
        sH = st.enter_context(contextlib.ExitStack())
        P.stack = sH
        zH = P.sb("zH", [128, 6, NTOK_A], BF16)
        with contextlib.ExitStack() as s0:
            P.stack = s0
            uT = P.sb("uT", [128, 8, NTOK_A], BF16)
            wHs = P.sb("wHs", [128, 8, 768], BF16)
            P.dma('pool', lambda e: e.dma_start(out=wHs[:], in_=wH), writes=[B['wHs']])
            build_uT(uT, s0)
            for c in range(6):
                inproj_fm(uT, wHs, 'wHs', c * 128, zH[:, c, :], ('zH', c))
            P.flush()
        P.stack = sH
        hsks = P.sb("hsks", [128, 2, 2], F32)
        P.dma('sp', lambda e: e.dma_start(out=hsks[:], in_=hsk), writes=[B['hsks']])
        with contextlib.ExitStack() as s1:
            P.stack = s1
            hcws = P.sb("hcws", [128, 6, 4], F32)
            acc = P.sb("acc", [128, NTOK_A], F32)
            P.dma('sp', lambda e: e.dma_start(out=hcws[:], in_=hcw), writes=[B['hcws']])
            for c in range(6):
                dwconv(zH[:, c, :], ('zH', c), hcws[:, c, :], 'hcws', acc, 'acc', zH[:, c, :], ('zH', c))
            P.flush()
        P.stack = sH
        HTc = P.sb("HTc", [128, 2, 512], BF16)
        Fcs = P.sb("Fcs", [128, 2, 512], BF16)
        Gcs = P.sb("Gcs", [128, 4, 256], BF16)
        P.dma('sp', lambda e: e.dma_start(out=Fcs[:], in_=Fc), writes=[B['Fcs']])
        P.dma('sp', lambda e: e.dma_start(out=Gcs[:], in_=Gc), writes=[B['Gcs']])
        Ft = [P.sb(f"Ft{i}", [128, 1024], BF16) for i in range(3)]
        fcnt = {'n': 0}
        h2T = P.sb("h2T", [64, 4352], F32)
        hcs = P.sb("hcs", [64, 6], F32)
        w3s = P.sb("w3s", [64, 2, 3, 256], F32)
        negts = P.sb("negts", [128, 34], F32)
        adels = P.sb("adels", [128, 2, 3, 256], F32)
        npi = P.sb("npi", [64, 1], F32)
        P.dma('sp', lambda e: e.dma_start(out=hcs[:, 0:4], in_=hcols), writes=[B['hcs']])
        P.dma('sp', lambda e: e.dma_start(out=w3s[:, :, 0:2, :], in_=hw3), writes=[B['w3s']])
        P.dma('sp', lambda e: e.dma_start(out=negts[:], in_=negt), writes=[B['negts']])
        P.dma('sp', lambda e: e.dma_start(out=adels[:, :, 0:2, :], in_=adel), writes=[B['adels']])
        P.op('dve', lambda e: e.tensor_scalar(out=w3s[:, :, 2, :], in0=w3s[:, :, 1, :], scalar1=-1.0, scalar2=None, op0=ALU.mult), reads=[B['w3s']], writes=[B['w3s']])
        P.op('dve', lambda e: e.tensor_copy(out=adels[:, :, 2, :], in_=adels[:, :, 1, :]), reads=[B['adels']], writes=[B['adels']])
        P.op('dve', lambda e: e.tensor_tensor(out=hcs[:, 4:6], in0=hcs[:, 0:2], in1=hcs[:, 2:4], op=ALU.mult), reads=[B['hcs']], writes=[B['hcs']])
        P.op('dve', lambda e: e.memset(npi[:], -PI), writes=[B['npi']])

        def sin_layer(src, skey, wmat, wkey2, kdim, li, dst, dkey2, argb):
            for bi, (tok0, ntok) in enumerate(TBLK_A):
                s = bi % 2
                P.op('pe', lambda e, s=s, tok0=tok0, ntok=ntok: e.matmul(pa[0:64, s, 0:ntok], lhsT=wmat[0:kdim, :], rhs=src[0:kdim, tok0:tok0 + ntok], start=True, stop=True),
                     reads=[B[skey], B[wkey2]], writes=[B['pa', s]])
                ab = argb[s]
                P.op('dve', lambda e, s=s, ntok=ntok, ab=ab: e.tensor_scalar(out=ab[:, 0:ntok], in0=pa[0:64, s, 0:ntok], scalar1=hcs[:, 2 + li:3 + li], scalar2=hcs[:, 4 + li:5 + li],
                                                                            op0=ALU.mult, op1=ALU.add), reads=[B['pa', s], B['hcs']], writes=[B['argb', s]])
                m1 = argb[2 + s]
                P.op('dve', lambda e, ntok=ntok, ab=ab, m1=m1: e.tensor_scalar(out=m1[:, 0:ntok], in0=ab[:, 0:ntok], scalar1=PI, scalar2=None, op0=ALU.is_gt),
                     reads=[B['argb', s]], writes=[B['argm', s]])
                P.op('dve', lambda e, ntok=ntok, ab=ab, m1=m1: e.scalar_tensor_tensor(out=ab[:, 0:ntok], in0=m1[:, 0:ntok], scalar=-2.0 * PI, in1=ab[:, 0:ntok], op0=ALU.mult, op1=ALU.add),
                     reads=[B['argb', s], B['argm', s]], writes=[B['argb', s]])
                P.op('dve', lambda e, ntok=ntok, ab=ab, m1=m1: e.tensor_scalar(out=m1[:, 0:ntok], in0=ab[:, 0:ntok], scalar1=-PI, scalar2=None, op0=ALU.is_lt),
                     reads=[B['argb', s], B['argm', s]], writes=[B['argm', s]])
                P.op('dve', lambda e, ntok=ntok, ab=ab, m1=m1: e.scalar_tensor_tensor(out=ab[:, 0:ntok], in0=m1[:, 0:ntok], scalar=2.0 * PI, in1=ab[:, 0:ntok], op0=ALU.mult, op1=ALU.add),
                     reads=[B['argb', s], B['argm', s]], writes=[B['argb', s]])
                P.op('act', lambda e, tok0=tok0, ntok=ntok, ab=ab: e.activation(out=dst[:, tok0:tok0 + ntok], in_=ab[:, 0:ntok], func=AF.Sin),
                     reads=[B['argb', s]], writes=[B[dkey2]])

        with contextlib.ExitStack() as sm:
            P.stack = sm
            h1T = P.sb("h1T", [64, 4352], F32)
            w2s = P.sb("w2s", [64, 64], F32)
            argb = [P.sb(f"argb{i}", [64, 512], F32) for i in range(4)]
            P.dma('sp', lambda e: e.dma_start(out=w2s[:], in_=hw2), writes=[B['w2s']])
            with contextlib.ExitStack() as sm2:
                P.stack = sm2
                fT = P.sb("fT", [33, 4352], F32)
                w1s = P.sb("w1s", [33, 64], F32)
                P.dma('sp', lambda e: e.dma_start(out=fT[:, 0:4096], in_=featsT), writes=[B['fT']])
                P.dma('sp', lambda e: e.dma_start(out=fT[:, 4096:4352], in_=featsTc), writes=[B['fT']])
                P.dma('sp', lambda e: e.dma_start(out=w1s[:], in_=hw1), writes=[B['w1s']])
                sin_layer(fT, 'fT', w1s, 'w1s', 33, 0, h1T, 'h1T', argb)
                P.flush()
            sin_layer(h1T, 'h1T', w2s, 'w2s', 64, 1, h2T, 'h2T', argb)
            P.flush()

        gcnt = {'n': 0}
        for o in range(2):
          with contextlib.ExitStack() as so:
            P.stack = so
            HT = P.sb(f"HT{o}", [128, 2, 8192], BF16)
            with contextlib.ExitStack() as sf:
                P.stack = sf
                env = [P.sb(f"env{i}", [128, 768], F32) for i in range(2)]
                filt = P.sb("filt", [128, 34, 768], BF16)
                for t in range(NT_A):
                    s = t % 2
                    ev = env[s]
                    P.op('act', lambda e, t=t, o=o, ev=ev: e.activation(out=ev[:], in_=adels[:, o, :, :].rearrange("p a b -> p (a b)"), func=AF.Exp, scale=negts[:, t:t + 1]),
                         reads=[B['adels'], B['negts']], writes=[B['env', s]])
                    P.op('pe', lambda e, t=t, o=o, s=s: e.matmul(pb_[:, s, 0:512], lhsT=h2T[:, t * 128:(t + 1) * 128], rhs=w3s[:, o, 0:2, :].rearrange("p a b -> p (a b)"), start=True, stop=True),
                         reads=[B['h2T'], B['w3s']], writes=[B['pb', s, 0]])
                    P.op('pe', lambda e, t=t, o=o, s=s: e.matmul(pa[:, s, 0:256], lhsT=h2T[:, t * 128:(t + 1) * 128], rhs=w3s[:, o, 2, :], start=True, stop=True),
                         reads=[B['h2T'], B['w3s']], writes=[B['pa', s]])
                    P.op('dve', lambda e, t=t, s=s, ev=ev: e.tensor_tensor(out=filt[:, t, 0:512], in0=pb_[:, s, 0:512], in1=ev[:, 0:512], op=ALU.mult),
                         reads=[B['pb', s, 0], B['env', s]], writes=[B['filt', t]])
                    P.op('dve', lambda e, t=t, s=s, ev=ev: e.tensor_tensor(out=filt[:, t, 512:768], in0=pa[:, s, 0:256], in1=ev[:, 512:768], op=ALU.mult),
                         reads=[B['pa', s], B['env', s]], writes=[B['filt', t]])
                P.op('dve', lambda e: e.memset(filt[0:1, 0, 256:768], 0.0), reads=[B['filt', 0]], writes=[B['filt', 0]])
                P.op('dve', lambda e: e.memset(filt[0:1, 32, 256:768], 0.0), reads=[B['filt', 32]], writes=[B['filt', 32]])
                for j in range(8):
                    for t in range(32):
                        fs = fcnt['n'] % 3
                        fcnt['n'] += 1
                        P.dma('sp', lambda e, t=t, j=j, fs=fs: e.dma_start(out=Ft[fs][:], in_=Fm[t, :, j, :]), writes=[B['Ft', fs]])
                        for c in range(2):
                            P.op('pe', lambda e, t=t, c=c, fs=fs: e.matmul(pa[:, c, :], lhsT=filt[:, t, c * 128:(c + 1) * 128], rhs=Ft[fs][:, 0:512], start=(t == 0), stop=False),
                                 reads=[B['filt', t], B['Ft', fs]], writes=[B['pa', c]])
                            P.op('pe', lambda e, t=t, c=c, fs=fs: e.matmul(pa[:, c, :], lhsT=filt[:, t, 256 + c * 128:256 + (c + 1) * 128], rhs=Ft[fs][:, 0:512], start=False, stop=(t == 31)),
                                 reads=[B['filt', t], B['Ft', fs]], writes=[B['pa', c]])
                            P.op('pe', lambda e, t=t, c=c, fs=fs: e.matmul(pb_[:, c, :], lhsT=filt[:, t, c * 128:(c + 1) * 128], rhs=Ft[fs][:, 512:1024], start=(t == 0), stop=False),
                                 reads=[B['filt', t], B['Ft', fs]], writes=[B['pb', c, 0]])
                            P.op('pe', lambda e, t=t, c=c, fs=fs: e.matmul(pb_[:, c, :], lhsT=filt[:, t, 512 + c * 128:512 + (c + 1) * 128], rhs=Ft[fs][:, 512:1024], start=False, stop=(t == 31)),
                                 reads=[B['filt', t], B['Ft', fs]], writes=[B['pb', c, 0]])
                    for c in range(2):
                        P.op('act', lambda e, c=c, j=j: e.copy(out=HT[:, c, j * 1024:j * 1024 + 512], in_=pa[:, c, :]), reads=[B['pa', c]], writes=[B['HT', c]])
                        P.op('dve', lambda e, c=c, j=j: e.tensor_copy(out=HT[:, c, j * 1024 + 512:(j + 1) * 1024], in_=pb_[:, c, :]), reads=[B['pb', c, 0]], writes=[B['HT', c]])
                for c in range(2):
                    for t in range(2):
                        tt = 32 + t
                        P.op('pe', lambda e, t=t, tt=tt, c=c: e.matmul(pa[:, c, 0:256], lhsT=filt[:, tt, c * 128:(c + 1) * 128], rhs=Fcs[:, t, 0:256], start=(t == 0), stop=False),
                             reads=[B['filt', tt], B['Fcs']], writes=[B['pa', c]])
                        P.op('pe', lambda e, t=t, tt=tt, c=c: e.matmul(pa[:, c, 0:256], lhsT=filt[:, tt, 256 + c * 128:256 + (c + 1) * 128], rhs=Fcs[:, t, 0:256], start=False, stop=(t == 1)),
                             reads=[B['filt', tt], B['Fcs']], writes=[B['pa', c]])
                        P.op('pe', lambda e, t=t, tt=tt, c=c: e.matmul(pb_[:, c, 0:256], lhsT=filt[:, tt, c * 128:(c + 1) * 128], rhs=Fcs[:, t, 256:512], start=(t == 0), stop=False),
                             reads=[B['filt', tt], B['Fcs']], writes=[B['pb', c, 0]])
                        P.op('pe', lambda e, t=t, tt=tt, c=c: e.matmul(pb_[:, c, 0:256], lhsT=filt[:, tt, 512 + c * 128:512 + (c + 1) * 128], rhs=Fcs[:, t, 256:512], start=False, stop=(t == 1)),
                             reads=[B['filt', tt], B['Fcs']], writes=[B['pb', c, 0]])
                    P.op('act', lambda e, c=c: e.copy(out=HTc[:, c, 0:256], in_=pa[:, c, 0:256]), reads=[B['pa', c]], writes=[B['HTc', c]])
                    P.op('dve', lambda e, c=c: e.tensor_copy(out=HTc[:, c, 256:512], in_=pb_[:, c, 0:256]), reads=[B['pb', c, 0]], writes=[B['HTc', c]])
                P.flush()
            with contextlib.ExitStack() as s3:
                P.stack = s3
                stm = P.sb("stm", [128, NT_A, 256], BF16)
                YhT = P.sb("YhT", [128, 2, 1024], BF16)
                Ytm = P.sb("Ytm", [128, 68, 256], BF16)
                Gt = [P.sb(f"Gt{i}", [128, 1024], BF16) for i in range(3)]
                pr1 = P.sb("pr1", [128, 512], F32)
                pr2 = P.sb("pr2", [128, 512], F32)
                yst = P.sb("yst", [128, 512], F32)
                ysk = P.sb("ysk", [128, 512], F32)
                for t in range(NT_A):
                    for c in range(2):
                        P.op('pe', lambda e, t=t, c=c: e.transpose(ptb[:, c * 128:(c + 1) * 128], zH[:, c, t * 128:(t + 1) * 128], idb[:]),
                             reads=[B['zH', c], B['idb']], writes=[B['ptb']])
                    if t % 2 == 0:
                        P.op('act', lambda e, t=t: e.copy(out=stm[:, t, :], in_=ptb[:, 0:256]), reads=[B['ptb']], writes=[B['stm', t]])
                    else:
                        P.op('dve', lambda e, t=t: e.tensor_copy(out=stm[:, t, :], in_=ptb[:, 0:256]), reads=[B['ptb']], writes=[B['stm', t]])

                def product(c, ucs, uss, hc, hs, n, keyc, keys_):
                    P.op('dve', lambda e: e.tensor_tensor(out=pr1[:, 0:n], in0=ucs, in1=hc, op=ALU.mult), reads=[B[keyc]], writes=[B['pr1']])
                    P.op('dve', lambda e: e.tensor_tensor(out=pr2[:, 0:n], in0=uss, in1=hs, op=ALU.mult), reads=[B[keys_]], writes=[B['pr2']])
                    P.op('pool', lambda e: e.tensor_tensor(out=YhT[:, c, 0:n], in0=pr1[:, 0:n], in1=pr2[:, 0:n], op=ALU.subtract), reads=[B['pr1'], B['pr2']], writes=[B['YhT', c]])
                    P.op('dve', lambda e: e.tensor_tensor(out=pr1[:, 0:n], in0=ucs, in1=hs, op=ALU.mult), reads=[B[keyc], B['pr1']], writes=[B['pr1']])
                    P.op('dve', lambda e: e.tensor_tensor(out=pr2[:, 0:n], in0=uss, in1=hc, op=ALU.mult), reads=[B[keys_], B['pr2']], writes=[B['pr2']])
                    P.op('pool', lambda e: e.tensor_tensor(out=YhT[:, c, 512:512 + n], in0=pr1[:, 0:n], in1=pr2[:, 0:n], op=ALU.add), reads=[B['pr1'], B['pr2']], writes=[B['YhT', c]])

                def ytrans(kts, cols):
                    for kt, c0 in zip(kts, cols):
                        for c in range(2):
                            P.op('pe', lambda e, c=c, c0=c0: e.transpose(ptb[:, c * 128:(c + 1) * 128], YhT[:, c, c0:c0 + 128], idb[:]),
                                 reads=[B['YhT', c], B['idb']], writes=[B['ptb']])
                        if kt % 2 == 0:
                            P.op('act', lambda e, kt=kt: e.copy(out=Ytm[:, kt, :], in_=ptb[:, 0:256]), reads=[B['ptb']], writes=[B['Ytm', kt]])
                        else:
                            P.op('dve', lambda e, kt=kt: e.tensor_copy(out=Ytm[:, kt, :], in_=ptb[:, 0:256]), reads=[B['ptb']], writes=[B['Ytm', kt]])

                for j in range(8):
                    for t in range(32):
                        fs = fcnt['n'] % 3
                        fcnt['n'] += 1
                        P.dma('sp', lambda e, t=t, j=j, fs=fs: e.dma_start(out=Ft[fs][:], in_=Fm[t, :, j, :]), writes=[B['Ft', fs]])
                        for c in range(2):
                            P.op('pe', lambda e, t=t, c=c, fs=fs: e.matmul(pa[:, c, :], lhsT=stm[:, t, c * 128:(c + 1) * 128], rhs=Ft[fs][:, 0:512], start=(t == 0), stop=(t == 31)),
                                 reads=[B['stm', t], B['Ft', fs]], writes=[B['pa', c]])
                            P.op('pe', lambda e, t=t, c=c, fs=fs: e.matmul(pb_[:, c, :], lhsT=stm[:, t, c * 128:(c + 1) * 128], rhs=Ft[fs][:, 512:1024], start=(t == 0), stop=(t == 31)),
                                 reads=[B['stm', t], B['Ft', fs]], writes=[B['pb', c, 0]])
                    for c in range(2):
                        product(c, pa[:, c, :], pb_[:, c, :], HT[:, c, j * 1024:j * 1024 + 512], HT[:, c, j * 1024 + 512:(j + 1) * 1024], 512, ('pa', c), ('pb', c, 0))
                    ytrans([j * 4 + i for i in range(4)] + [32 + j * 4 + i for i in range(4)], [i * 128 for i in range(4)] + [512 + i * 128 for i in range(4)])
                for c in range(2):
                    for t in range(2):
                        P.op('pe', lambda e, t=t, c=c: e.matmul(pa[:, c, 0:256], lhsT=stm[:, 32 + t, c * 128:(c + 1) * 128], rhs=Fcs[:, t, 0:256], start=(t == 0), stop=(t == 1)),
                             reads=[B['stm', 32 + t], B['Fcs']], writes=[B['pa', c]])
                        P.op('pe', lambda e, t=t, c=c: e.matmul(pb_[:, c, 0:256], lhsT=stm[:, 32 + t, c * 128:(c + 1) * 128], rhs=Fcs[:, t, 256:512], start=(t == 0), stop=(t == 1)),
                             reads=[B['stm', 32 + t], B['Fcs']], writes=[B['pb', c, 0]])
                    product(c, pa[:, c, 0:256], pb_[:, c, 0:256], HTc[:, c, 0:256], HTc[:, c, 256:512], 256, ('pa', c), ('pb', c, 0))
                ytrans([64, 65, 66, 67], [0, 128, 512, 640])

                def finish_blk(c, ps_ap, pkey, tok0, n, scale):
                    P.op('pool', lambda e: e.tensor_scalar(out=ysk[:, 0:n], in0=zH[:, c, tok0:tok0 + n], scalar1=hsks[:, o, c:c + 1], scalar2=None, op0=ALU.mult),
                         reads=[B['zH', c], B['hsks']], writes=[B['ysk']])
                    P.op('dve', lambda e: e.scalar_tensor_tensor(out=yst[:, 0:n], in0=ps_ap, scalar=scale, in1=ysk[:, 0:n], op0=ALU.mult, op1=ALU.add),
                         reads=[B[pkey], B['ysk']], writes=[B['yst']])
                    if o == 0:
                        P.op('pool', lambda e: e.tensor_tensor(out=zH[:, c, tok0:tok0 + n], in0=yst[:, 0:n], in1=zH[:, 2 + c, tok0:tok0 + n], op=ALU.mult),
                             reads=[B['yst'], B['zH', 2 + c]], writes=[B['zH', c]])
                    else:
                        P.op('pool', lambda e: e.tensor_tensor(out=yst[:, 0:n], in0=yst[:, 0:n], in1=zH[:, 4 + c, tok0:tok0 + n], op=ALU.mult),
                             reads=[B['yst'], B['zH', 4 + c]], writes=[B['yst']])
                        P.dma('sp', lambda e: e.dma_start(out=YT[4 + c, :, tok0:tok0 + n], in_=yst[:, 0:n]), reads=[B['yst']], is_output=True)

                for ps_ in range(4):
                    for kt in range(64):
                        gs = gcnt['n'] % 3
                        gcnt['n'] += 1
                        P.dma('sp', lambda e, ps_=ps_, kt=kt, gs=gs: e.dma_start(out=Gt[gs][:], in_=Gm[ps_, kt]), writes=[B['Gt', gs]])
                        for c in range(2):
                            for nb in range(2):
                                acc_ap = (pa if c == 0 else pb_)[:, nb, :]
                                P.op('pe', lambda e, kt=kt, c=c, nb=nb, gs=gs, acc_ap=acc_ap: e.matmul(acc_ap, lhsT=Ytm[:, kt, c * 128:(c + 1) * 128], rhs=Gt[gs][:, nb * 512:(nb + 1) * 512],
                                                                                                  start=(kt == 0), stop=(kt == 63)),
                                     reads=[B['Ytm', kt], B['Gt', gs]], writes=[B['pa', nb] if c == 0 else B['pb', nb, 0]])
                    for c in range(2):
                        for nb in range(2):
                            finish_blk(c, (pa if c == 0 else pb_)[:, nb, :], ('pa', nb) if c == 0 else ('pb', nb, 0), ps_ * 1024 + nb * 512, 512, 2.0 / 8192.0)
                for c in range(2):
                    for kt in range(4):
                        P.op('pe', lambda e, kt=kt, c=c: e.matmul(pc[:, 0:256], lhsT=Ytm[:, 64 + kt, c * 128:(c + 1) * 128], rhs=Gcs[:, kt, :], start=(kt == 0), stop=(kt == 3)),
                             reads=[B['Ytm', 64 + kt], B['Gcs']], writes=[B['pc']])
                    finish_blk(c, pc[:, 0:256], 'pc', 4096, 256, 2.0 / 512.0)
                P.flush()
        sH.close()
        P.stack = st
        P.mute = ('noR' in _dbg)
        build_A_rest(nc, P, B, st, x, wA, wR, wD, caw, ropec, ropes, qkn, rdec, rtab, rcol, YT, ids, idb, mcol, pt, ptb, pa, pb_, pc, build_uT, inproj_fm, dwconv)


def build_A():
    nc = bass.Bass("TRN2", target_bir_lowering=False)
    x = _din(nc, "x", [NT_A, 128, D])
    modcols = _din(nc, "modcols", [128, 2, 2, 8])
    ident = _din(nc, "ident", [128, 128])
    wA = _din(nc, "wA", [128, 8, 768])
    wR = _din(nc, "wR", [128, 8, 1024])
    wH = _din(nc, "wH", [128, 8, 768])
    wD = _din(nc, "wD", [128, 8, 512])
    caw = _din(nc, "caw", [128, 2, 4])
    hcw = _din(nc, "hcw", [128, 6, 4])
    hsk = _din(nc, "hsk", [128, 2, 2])
    ropec = _din(nc, "ropec", [128, 32, 64])
    ropes = _din(nc, "ropes", [128, 32, 64])
    qkn = _din(nc, "qkn", [128, 2, 128])
    rdec = _din(nc, "rdec", [128, 4])
    rtab = _din(nc, "rtab", [128, 6, 128])
    rcol = _din(nc, "rcol", [128, 4])
    featsT = _din(nc, "featsT", [33, 4096])
    featsTc = _din(nc, "featsTc", [33, 256])
    hw1 = _din(nc, "hw1", [33, 64])
    hw2 = _din(nc, "hw2", [64, 64])
    hcols = _din(nc, "hcols", [64, 4])
    hw3 = _din(nc, "hw3", [64, 2, 2, 256])
    negt = _din(nc, "negt", [128, 34])
    adel = _din(nc, "adel", [128, 2, 2, 256])
    Fm = _din(nc, "Fm", [32, 128, 8, 1024], BF16)
    Gm = _din(nc, "Gm", [4, 64, 128, 1024], BF16)
    Fc = _din(nc, "Fc", [128, 2, 512], BF16)
    Gc = _din(nc, "Gc", [128, 4, 256], BF16)
    YT = _dout(nc, "YT", [8, 128, NTOK_A])

    Dm = dict(x=x, modcols=modcols, ident=ident, wA=wA, wR=wR, wH=wH, wD=wD, caw=caw, hcw=hcw, hsk=hsk, ropec=ropec, ropes=ropes, qkn=qkn, rdec=rdec, rtab=rtab, rcol=rcol, featsT=featsT, featsTc=featsTc, hw1=hw1, hw2=hw2, hcols=hcols, hw3=hw3, negt=negt, adel=adel, Fm=Fm, Gm=Gm, Fc=Fc, Gc=Gc, YT=YT)
    with contextlib.ExitStack() as st0:
        P = Prog(nc, st0)
        emit_A(nc, P, Dm)
    return nc


def build_A_rest(nc, P, B, st, x, wA, wR, wD, caw, ropec, ropes, qkn, rdec, rtab, rcol, YT, ids, idb, mcol, pt, ptb, pa, pb_, pc, build_uT, inproj_fm, dwconv):
    SC = 128.0 ** -0.5
    sG = st.enter_context(contextlib.ExitStack())
    P.stack = sG
    uT = P.sb("uT2", [128, 8, NTOK_A], BF16)
    rc = P.sb("rc", [128, 32, 64], F32)
    rs = P.sb("rs", [128, 32, 64], F32)
    P.dma('sp', lambda e: e.dma_start(out=rc[:], in_=ropec), writes=[B['rope']])
    P.dma('sp', lambda e: e.dma_start(out=rs[:], in_=ropes), writes=[B['rope']])
    with contextlib.ExitStack() as s0:
        build_uT(uT, s0)
        P.flush()

    def rope(src, skey, t, tmp, tkey):
        for ax in range(2):
            x1 = src[:, ax * 64:ax * 64 + 32]
            x2 = src[:, ax * 64 + 32:ax * 64 + 64]
            c_ = rc[:, t, ax * 32:(ax + 1) * 32]
            s_ = rs[:, t, ax * 32:(ax + 1) * 32]
            P.op('dve', lambda e, x1=x1, c_=c_: e.tensor_tensor(out=tmp[:, 0, :], in0=x1, in1=c_, op=ALU.mult), reads=[B[skey], B['rope']], writes=[B[tkey, 0]])
            P.op('dve', lambda e, x2=x2, s_=s_: e.tensor_tensor(out=tmp[:, 1, :], in0=x2, in1=s_, op=ALU.mult), reads=[B[skey], B['rope']], writes=[B[tkey, 1]])
            P.op('dve', lambda e, x2=x2, c_=c_: e.tensor_tensor(out=tmp[:, 2, :], in0=x2, in1=c_, op=ALU.mult), reads=[B[skey], B['rope']], writes=[B[tkey, 2]])
            P.op('dve', lambda e, x1=x1, s_=s_: e.tensor_tensor(out=tmp[:, 3, :], in0=x1, in1=s_, op=ALU.mult), reads=[B[skey], B['rope']], writes=[B[tkey, 3]])
            P.op('dve', lambda e, x1=x1: e.tensor_tensor(out=x1, in0=tmp[:, 0, :], in1=tmp[:, 1, :], op=ALU.subtract), reads=[B[tkey, 0], B[tkey, 1], B[skey]], writes=[B[skey]])
            P.op('dve', lambda e, x2=x2: e.tensor_tensor(out=x2, in0=tmp[:, 2, :], in1=tmp[:, 3, :], op=ALU.add), reads=[B[tkey, 2], B[tkey, 3], B[skey]], writes=[B[skey]])

    import os as _os
    _dbg = _os.environ.get('A_DBG', '').split(',')
    _base = P.mute
    P.mute = _base or ('noS' in _dbg)
    with contextlib.ExitStack() as s1:
        P.stack = s1
        wAs = P.sb("wAs", [128, 8, 768], BF16)
        caws = P.sb("caws", [128, 2, 4], F32)
        P.dma('pool', lambda e: e.dma_start(out=wAs[:], in_=wA), writes=[B['wAs']])
        P.dma('sp', lambda e: e.dma_start(out=caws[:], in_=caw), writes=[B['caws']])
        zA = P.sb("zA", [128, 3, NTOK_A], BF16)
        pp_ = P.sb("pp_", [128, NTOK_A], BF16)
        acc = P.sb("accA", [128, NTOK_A], F32)
        for c in range(2):
            for g in range(3):
                inproj_fm(uT, wAs, 'wAs', g * 256 + c * 128, zA[:, g, :], ('zA', g))
            P.op('pool', lambda e: e.tensor_tensor(out=pp_[:], in0=zA[:, 1, :], in1=zA[:, 2, :], op=ALU.mult), reads=[B['zA', 1], B['zA', 2]], writes=[B['pp_']])
            dwconv(pp_[:], 'pp_', caws[:, c, :], 'caws', acc, 'accA', None, None)
            P.op('dve', lambda e: e.tensor_tensor(out=acc[:], in0=acc[:], in1=zA[:, 0, :], op=ALU.mult), reads=[B['accA'], B['zA', 0]], writes=[B['accA']])
            P.dma('sp', lambda e, c=c: e.dma_start(out=YT[0 + c], in_=acc[:]), reads=[B['accA']], is_output=True)
        P.flush()

    P.mute = _base or ('noD' in _dbg)
    with contextlib.ExitStack() as s2:
        P.stack = s2
        wDs = P.sb("wDs", [128, 8, 512], BF16)
        qkns = P.sb("qkns", [128, 2, 128], F32)
        P.dma('pool', lambda e: e.dma_start(out=wDs[:], in_=wD), writes=[B['wDs']])
        P.dma('sp', lambda e: e.dma_start(out=qkns[:], in_=qkn), writes=[B['qkns']])
        qT = P.sb("qT", [128, 2, NTOK_A], BF16)
        kT = P.sb("kT", [128, NTOK_A], BF16)
        vtm = P.sb("vtm", [128, NT_A, 128], BF16)
        onesb = P.sb("onesb", [128, 128], BF16)
        P.op('dve', lambda e: e.memset(onesb[:], 1.0), writes=[B['onesb']])
        qn = [P.sb(f"qn{i}", [128, 3, 128], F32) for i in range(2)]
        qb = [P.sb(f"qb{i}", [128, 3, 128], BF16) for i in range(2)]
        sq3 = P.sb("sq3", [128, 384], F32)
        ss = [P.sb(f"ss{i}", [128, 4], F32) for i in range(2)]
        tmp = P.sb("tmpr", [128, 4, 32], F32)
        for t in range(NT_A):
            s = t % 2
            for kt in range(8):
                P.op('pe', lambda e, t=t, kt=kt, s=s: e.matmul(pa[:, s, :], lhsT=uT[:, kt, t * 128:(t + 1) * 128], rhs=wDs[:, kt, :], start=(kt == 0), stop=(kt == 7)),
                     reads=[B['uT'], B['wDs']], writes=[B['pa', s]])
            P.op('act', lambda e, s=s: e.activation(out=sq3[:], in_=pa[:, s, 0:384], func=AF.Square), reads=[B['pa', s]], writes=[B['sq3']])
            P.op('dve', lambda e, s=s: e.reduce_sum(out=ss[s][:, 0:3], in_=sq3[:].rearrange("p (h d) -> p h d", h=3), axis=AX.X),
                 reads=[B['sq3']], writes=[B['ss', s, 0], B['ss', s, 1], B['ss', s, 2]])
            P.op('dve', lambda e, s=s: e.tensor_scalar(out=ss[s][:, 0:3], in0=ss[s][:, 0:3], scalar1=1.0 / 128.0, scalar2=1e-6, op0=ALU.mult, op1=ALU.add),
                 reads=[B['ss', s, h] for h in range(3)], writes=[B['ss', s, 'a']])
            P.op('act', lambda e, s=s: e.sqrt(out=ss[s][:, 0:3], in_=ss[s][:, 0:3]), reads=[B['ss', s, 'a']], writes=[B['ss', s, 'a']])
            P.op('dve', lambda e, s=s: e.reciprocal(out=ss[s][:, 0:3], in_=ss[s][:, 0:3]), reads=[B['ss', s, 'a']], writes=[B['ss', s, 'a']])
            for h in range(3):
                P.op('dve', lambda e, h=h, s=s: e.scalar_tensor_tensor(out=qn[s][:, h, :], in0=pa[:, s, h * 128:(h + 1) * 128], scalar=ss[s][:, h:h + 1], in1=qkns[:, 0 if h < 2 else 1, :],
                                                                     op0=ALU.mult, op1=ALU.mult), reads=[B['pa', s], B['ss', s, 'a'], B['qkns']], writes=[B['qn', s, h]])
                if t < 32:
                    rope(qn[s][:, h, :], ('qn', s, h), t, tmp, 'tmpr')
                P.op('pool', lambda e, h=h, s=s: e.tensor_copy(out=qb[s][:, h, :], in_=qn[s][:, h, :]), reads=[B['qn', s, h]], writes=[B['qb', s, h]])
            P.op('act', lambda e, t=t, s=s: e.copy(out=vtm[:, t, :], in_=pa[:, s, 384:512]), reads=[B['pa', s]], writes=[B['vtm', t]])
            for h in range(3):
                P.op('pe', lambda e, h=h, s=s: e.transpose(ptb[:, h * 128:(h + 1) * 128], qb[s][:, h, :], idb[:]), reads=[B['qb', s, h], B['idb']], writes=[B['ptb']])
            P.op('dve', lambda e, t=t: e.tensor_copy(out=qT[:, :, t * 128:(t + 1) * 128], in_=ptb[:, 0:256].rearrange("p (h d) -> p h d", h=2)), reads=[B['ptb']], writes=[B['qT']])
            P.op('act', lambda e, t=t: e.copy(out=kT[:, t * 128:(t + 1) * 128], in_=ptb[:, 256:384]), reads=[B['ptb']], writes=[B['kT']])
        if 'dumpQK' in _dbg:
            dq = P.sb("dq", [128, NTOK_A], F32)
            for i_, src_ in enumerate((qT[:, 0, :], qT[:, 1, :], kT[:])):
                P.op('dve', lambda e, src_=src_: e.tensor_copy(out=dq[:], in_=src_), reads=[B['qT'], B['kT']], writes=[B['dq']])
                P.dma('sp', lambda e, i_=i_: e.dma_start(out=YT[5 + i_], in_=dq[:]), reads=[B['dq']], writes=[B['dqo', i_]], is_output=True)
            P.op('dve', lambda e: e.tensor_copy(out=dq[:].rearrange("p (t d) -> p t d", d=128), in_=vtm[:]), reads=[B['vtm', t_] for t_ in range(NT_A)], writes=[B['dq']])
            P.dma('sp', lambda e: e.dma_start(out=YT[4], in_=dq[:]), reads=[B['dq']], is_output=True)
        P.mute = P.mute or ('noD2' in _dbg)
        pT = [P.sb(f"pT{i}", [128, 512], BF16) for i in range(3)]
        rd = P.sb("rd", [128, 512], F32)
        yo = [P.sb(f"yo{i}", [128, 512], F32) for i in range(2)]
        pcnt = 0
        ocnt = 0
        for h in range(2):
            for bi, (tok0, ntok) in enumerate(TBLK_A):
                if 'cOne' in _dbg and (h, bi) != (0, 0):
                    continue
                ktiles = list(range(34)) if bi < 8 else [32, 33]
                for ki, kt in enumerate(ktiles):
                    s = pcnt % 2
                    p3 = pcnt % 3
                    pcnt += 1
                    P.op('pe', lambda e, kt=kt, h=h, s=s, tok0=tok0, ntok=ntok: e.matmul(pa[:, s, 0:ntok], lhsT=kT[:, kt * 128:(kt + 1) * 128], rhs=qT[:, h, tok0:tok0 + ntok], start=True, stop=True),
                         reads=[B['kT'], B['qT']], writes=[B['pa', s]])
                    P.op('act', lambda e, s=s, p3=p3, ntok=ntok: e.activation(out=pT[p3][:, 0:ntok], in_=pa[:, s, 0:ntok], func=AF.Exp, scale=SC), reads=[B['pa', s]], writes=[B['pT', p3]])
                    if 'cQK' in _dbg:
                        continue
                    P.op('pe', lambda e, kt=kt, p3=p3, ntok=ntok, ki=ki, n=len(ktiles): e.matmul(pb_[:, 0, 0:ntok], lhsT=vtm[:, kt, :], rhs=pT[p3][:, 0:ntok], start=(ki == 0), stop=(ki == n - 1)),
                         reads=[B['vtm', kt], B['pT', p3]], writes=[B['pb', 0, 0]])
                    P.op('pe', lambda e, p3=p3, ntok=ntok, ki=ki, n=len(ktiles): e.matmul(pb_[:, 1, 0:ntok], lhsT=onesb[:], rhs=pT[p3][:, 0:ntok], start=(ki == 0), stop=(ki == n - 1)),
                         reads=[B['onesb'], B['pT', p3]], writes=[B['pb', 1, 0]])
                if 'cQK' in _dbg:
                    continue
                P.op('dve', lambda e, ntok=ntok: e.reciprocal(out=rd[:, 0:ntok], in_=pb_[:, 1, 0:ntok]), reads=[B['pb', 1, 0]], writes=[B['rd']])
                y = yo[ocnt % 2]
                yk = ('yo', ocnt % 2)
                ocnt += 1
                P.op('dve', lambda e, ntok=ntok, y=y: e.tensor_tensor(out=y[:, 0:ntok], in0=pb_[:, 0, 0:ntok], in1=rd[:, 0:ntok], op=ALU.mult), reads=[B['pb', 0, 0], B['rd']], writes=[B[yk]])
                P.dma('sp', lambda e, h=h, tok0=tok0, ntok=ntok, y=y: e.dma_start(out=YT[6 + h, :, tok0:tok0 + ntok], in_=y[:, 0:ntok]), reads=[B[yk]], is_output=True)
        P.flush()

    P.mute = _base or ('noT' in _dbg)
    with contextlib.ExitStack() as s3:
        P.stack = s3
        wRs = P.sb("wRs", [128, 8, 1024], BF16)
        P.dma('pool', lambda e: e.dma_start(out=wRs[:], in_=wR), writes=[B['wRs']])
        rds = P.sb("rds", [128, 4], F32)
        lgam = P.sb("lgam", [128, 4], F32)
        rtabs = P.sb("rtabs", [128, 6, 128], F32)
        rcols = P.sb("rcols", [128, 4], F32)
        P.dma('sp', lambda e: e.dma_start(out=rds[:], in_=rdec), writes=[B['rds']])
        P.dma('sp', lambda e: e.dma_start(out=rtabs[:], in_=rtab), writes=[B['rtabs']])
        P.dma('sp', lambda e: e.dma_start(out=rcols[:], in_=rcol), writes=[B['rcols']])
        P.op('act', lambda e: e.activation(out=lgam[:], in_=rds[:], func=AF.Exp), reads=[B['rds']], writes=[B['lgam']])
        P.op('dve', lambda e: e.tensor_scalar(out=lgam[:], in0=lgam[:], scalar1=-1.0, scalar2=None, op0=ALU.mult), reads=[B['lgam']], writes=[B['lgam']])
        mask = P.sb("mask", [128, 128], F32)
        mtmp = P.sb("mtmpR", [128, 128], F32)
        qdf = P.sb("qdf", [128, 128], F32)
        qdb = P.sb("qdb", [128, 128], F32)
        kdc = P.sb("kdc", [128, 4], F32)
        qT = P.sb("rqT", [128, NTOK_A], BF16)
        kT = P.sb("rkT", [128, NTOK_A], BF16)
        qdfT = P.sb("qdfT", [128, NTOK_A], BF16)
        qdbT = P.sb("qdbT", [128, NTOK_A], BF16)
        kdf = P.sb("kdf", [128, NT_A, 128], BF16)
        kdb = P.sb("kdb", [128, NT_A, 128], BF16)
        vtm = P.sb("rvtm", [128, NT_A, 128], BF16)
        sgt = P.sb("sgt", [128, NT_A, 128], BF16)
        Sf = P.sb("Sf", [128, NT_A, 128], BF16)
        Sb = P.sb("Sb", [128, NT_A, 128], BF16)
        stf = [P.sb(f"stf{i}", [128, 128], F32) for i in range(2)]
        qk = [P.sb(f"rqk{i}", [128, 2, 128], F32) for i in range(2)]
        qkb = [P.sb(f"rqkb{i}", [128, 2, 128], BF16) for i in range(2)]
        tmp = P.sb("tmpr2", [128, 4, 32], F32)
        smT = [P.sb(f"smT{i}", [128, 128], BF16) for i in range(2)]
        stt = P.sb("rstt", [128, 6], F32)
        mv = P.sb("rmv", [128, 2], F32)
        rstd = P.sb("rrstd", [128, 1], F32)
        on = [P.sb(f"on{i}", [128, 128], F32) for i in range(2)]
        ob = [P.sb(f"ob{i}", [128, 128], BF16) for i in range(2)]
        ysg = [P.sb(f"rysg{i}", [128, 512], F32) for i in range(2)]
        for h in range(2):
            lf = lgam[:, h:h + 1]
            lb = lgam[:, 2 + h:3 + h]
            P.op('act', lambda e, lf=lf: e.activation(out=mask[:], in_=rtabs[:, 0, :], func=AF.Exp, scale=lf), reads=[B['rtabs'], B['lgam']], writes=[B['mask']])
            P.op('dve', lambda e: e.tensor_tensor(out=mask[:], in0=mask[:], in1=rtabs[:, 1, :], op=ALU.mult), reads=[B['mask'], B['rtabs']], writes=[B['mask']])
            P.op('act', lambda e, lb=lb: e.activation(out=mtmp[:], in_=rtabs[:, 2, :], func=AF.Exp, scale=lb), reads=[B['rtabs'], B['lgam']], writes=[B['mtmpR']])
            P.op('dve', lambda e: e.tensor_tensor(out=mtmp[:], in0=mtmp[:], in1=rtabs[:, 3, :], op=ALU.mult), reads=[B['mtmpR'], B['rtabs']], writes=[B['mtmpR']])
            P.op('dve', lambda e: e.tensor_tensor(out=mask[:], in0=mask[:], in1=mtmp[:], op=ALU.add), reads=[B['mask'], B['mtmpR']], writes=[B['mask']])
            P.op('act', lambda e, lf=lf: e.activation(out=qdf[:], in_=rtabs[:, 4, :], func=AF.Exp, scale=lf), reads=[B['rtabs'], B['lgam']], writes=[B['qdf']])
            P.op('act', lambda e, lb=lb: e.activation(out=qdb[:], in_=rtabs[:, 5, :], func=AF.Exp, scale=lb), reads=[B['rtabs'], B['lgam']], writes=[B['qdb']])
            P.op('act', lambda e, lf=lf: e.activation(out=kdc[:, 0:1], in_=rcols[:, 0:1], func=AF.Exp, scale=lf), reads=[B['rcols'], B['lgam']], writes=[B['kdc', 0]])
            P.op('act', lambda e, lb=lb: e.activation(out=kdc[:, 1:2], in_=rcols[:, 1:2], func=AF.Exp, scale=lb), reads=[B['rcols'], B['lgam']], writes=[B['kdc', 1]])
            P.op('act', lambda e, lf=lf: e.activation(out=kdc[:, 2:3], in_=rcols[:, 2:3], func=AF.Exp, scale=lf), reads=[B['rcols'], B['lgam']], writes=[B['kdc', 2]])
            P.op('act', lambda e, lb=lb: e.activation(out=kdc[:, 3:4], in_=rcols[:, 2:3], func=AF.Exp, scale=lb), reads=[B['rcols'], B['lgam']], writes=[B['kdc', 3]])
            kd = [B['kdc', i] for i in range(4)]
            for t in range(NT_A):
                s = t % 2
                for g in range(4):
                    for kt in range(8):
                        P.op('pe', lambda e, t=t, kt=kt, s=s, g=g, h=h: e.matmul(pa[:, s, g * 128:(g + 1) * 128], lhsT=uT[:, kt, t * 128:(t + 1) * 128], rhs=wRs[:, kt, g * 256 + h * 128:g * 256 + (h + 1) * 128],
                                                                        start=(kt == 0), stop=(kt == 7)), reads=[B['uT'], B['wRs']], writes=[B['pa', s]])
                P.op('act', lambda e, s=s: e.copy(out=qk[s][:, 0, :], in_=pa[:, s, 0:128]), reads=[B['pa', s]], writes=[B['rqk', s, 0]])
                P.op('act', lambda e, s=s: e.activation(out=qk[s][:, 1, :], in_=pa[:, s, 128:256], func=AF.Copy, scale=SC), reads=[B['pa', s]], writes=[B['rqk', s, 1]])
                P.op('act', lambda e, s=s, t=t: e.copy(out=vtm[:, t, :], in_=pa[:, s, 256:384]), reads=[B['pa', s]], writes=[B['rvtm', t]])
                P.op('act', lambda e, s=s, t=t: e.activation(out=sgt[:, t, :], in_=pa[:, s, 384:512], func=AF.Silu), reads=[B['pa', s]], writes=[B['sgt', t]])
                for j in range(2):
                    if t < 32:
                        rope(qk[s][:, j, :], ('rqk', s, j), t, tmp, 'tmpr2')
                    P.op('pool', lambda e, s=s, j=j: e.tensor_copy(out=qkb[s][:, j, :], in_=qk[s][:, j, :]), reads=[B['rqk', s, j]], writes=[B['rqkb', s, j]])
                P.op('dve', lambda e, s=s, t=t: e.tensor_scalar(out=kdf[:, t, :], in0=qk[s][:, 1, :], scalar1=kdc[:, 0:1], scalar2=None, op0=ALU.mult), reads=[B['rqk', s, 1], kd[0]], writes=[B['kdf', t]])
                P.op('dve', lambda e, s=s, t=t: e.tensor_scalar(out=kdb[:, t, :], in0=qk[s][:, 1, :], scalar1=kdc[:, 1:2], scalar2=None, op0=ALU.mult), reads=[B['rqk', s, 1], kd[1]], writes=[B['kdb', t]])
                for j in range(2):
                    P.op('pe', lambda e, s=s, j=j: e.transpose(ptb[:, j * 128:(j + 1) * 128], qkb[s][:, j, :], idb[:]), reads=[B['rqkb', s, j], B['idb']], writes=[B['ptb']])
                sl = slice(t * 128, (t + 1) * 128)
                P.op('act', lambda e, sl=sl: e.copy(out=qT[:, sl], in_=ptb[:, 0:128]), reads=[B['ptb']], writes=[B['rqT', t]])
                P.op('act', lambda e, sl=sl: e.copy(out=kT[:, sl], in_=ptb[:, 128:256]), reads=[B['ptb']], writes=[B['rkT', t]])
                P.op('dve', lambda e, sl=sl: e.tensor_tensor(out=qdfT[:, sl], in0=ptb[:, 0:128], in1=qdf[:], op=ALU.mult), reads=[B['ptb'], B['qdf']], writes=[B['qdfT', t]])
                P.op('dve', lambda e, sl=sl: e.tensor_tensor(out=qdbT[:, sl], in0=ptb[:, 0:128], in1=qdb[:], op=ALU.mult), reads=[B['ptb'], B['qdb']], writes=[B['qdbT', t]])

            def chain(order, kdx, Sx, cd, ckey, name):
                cur = None
                for n_, t in enumerate(order):
                    if cur is None:
                        P.op('pool', lambda e, t=t: e.memset(Sx[:, t, :], 0.0), writes=[B[name, t]])
                    else:
                        P.op('pool', lambda e, t=t, cur=cur: e.tensor_copy(out=Sx[:, t, :], in_=stf[cur][:]), reads=[B['stf', cur]], writes=[B[name, t]])
                    if n_ == len(order) - 1:
                        break
                    P.op('pe', lambda e, t=t: e.matmul(pc[:, 0:128], lhsT=kdx[:, t, :], rhs=vtm[:, t, :], start=True, stop=True), reads=[B[name + 'k', t], B['rvtm', t]], writes=[B['pc']])
                    nxt = 0 if cur is None else 1 - cur
                    if cur is None:
                        P.op('dve', lambda e, nxt=nxt: e.tensor_copy(out=stf[nxt][:], in_=pc[:, 0:128]), reads=[B['pc']], writes=[B['stf', nxt]])
                    else:
                        P.op('dve', lambda e, nxt=nxt, cur=cur: e.scalar_tensor_tensor(out=stf[nxt][:], in0=stf[cur][:], scalar=cd, in1=pc[:, 0:128], op0=ALU.mult, op1=ALU.add),
                             reads=[B['stf', cur], B['pc'], ckey], writes=[B['stf', nxt]])
                    cur = nxt
            for t in range(NT_A):
                B.d[('Sfk', t)] = B['kdf', t]
                B.d[('Sbk', t)] = B['kdb', t]
            chain([32, 33] + list(range(32)), kdf, Sf, kdc[:, 2:3], kd[2], 'Sf')
            chain([33, 32] + list(range(31, -1, -1)), kdb, Sb, kdc[:, 3:4], kd[3], 'Sb')
            for t in range(NT_A):
                s = t % 2
                sl = slice(t * 128, (t + 1) * 128)
                P.op('pe', lambda e, sl=sl, s=s: e.matmul(pa[:, s, 0:128], lhsT=kT[:, sl], rhs=qT[:, sl], start=True, stop=True), reads=[B['rkT', t], B['rqT', t]], writes=[B['pa', s]])
                P.op('dve', lambda e, s=s: e.tensor_tensor(out=smT[s][:], in0=pa[:, s, 0:128], in1=mask[:], op=ALU.mult), reads=[B['pa', s], B['mask']], writes=[B['smT', s]])
                P.op('pe', lambda e, t=t, s=s: e.matmul(pb_[:, s, 0:128], lhsT=smT[s][:], rhs=vtm[:, t, :], start=True, stop=False), reads=[B['smT', s], B['rvtm', t]], writes=[B['pb', s, 0]])
                P.op('pe', lambda e, t=t, s=s, sl=sl: e.matmul(pb_[:, s, 0:128], lhsT=qdfT[:, sl], rhs=Sf[:, t, :], start=False, stop=False), reads=[B['qdfT', t], B['Sf', t]], writes=[B['pb', s, 0]])
                P.op('pe', lambda e, t=t, s=s, sl=sl: e.matmul(pb_[:, s, 0:128], lhsT=qdbT[:, sl], rhs=Sb[:, t, :], start=False, stop=True), reads=[B['qdbT', t], B['Sb', t]], writes=[B['pb', s, 0]])
                P.op('act', lambda e, s=s: e.copy(out=on[s][:], in_=pb_[:, s, 0:128]), reads=[B['pb', s, 0]], writes=[B['on', s]])
                P.op('dve', lambda e, s=s: e.bn_stats(out=stt[:], in_=on[s][:]), reads=[B['on', s]], writes=[B['rstt']])
                P.op('dve', lambda e: e.bn_aggr(out=mv[:], in_=stt[:]), reads=[B['rstt']], writes=[B['rmv']])
                P.op('dve', lambda e: e.tensor_scalar_add(out=rstd[:], in0=mv[:, 1:2], scalar1=1e-6), reads=[B['rmv']], writes=[B['rrstd']])
                P.op('act', lambda e: e.sqrt(out=rstd[:], in_=rstd[:]), reads=[B['rrstd']], writes=[B['rrstd']])
                P.op('dve', lambda e: e.reciprocal(out=rstd[:], in_=rstd[:]), reads=[B['rrstd']], writes=[B['rrstd']])
                P.op('dve', lambda e, s=s: e.tensor_scalar(out=on[s][:], in0=on[s][:], scalar1=mv[:, 0:1], scalar2=rstd[:, 0:1], op0=ALU.subtract, op1=ALU.mult),
                     reads=[B['on', s], B['rmv'], B['rrstd']], writes=[B['on', s]])
                P.op('pool', lambda e, s=s, t=t: e.tensor_tensor(out=ob[s][:], in0=on[s][:], in1=sgt[:, t, :], op=ALU.mult), reads=[B['on', s], B['sgt', t]], writes=[B['ob', s]])
                P.op('pe', lambda e, s=s: e.transpose(ptb[:, 512:640], ob[s][:], idb[:]), reads=[B['ob', s], B['idb']], writes=[B['ptb2']])
                g4 = t // 4
                yg = ysg[g4 % 2]
                P.op('act', lambda e, t=t, yg=yg: e.copy(out=yg[:, (t % 4) * 128:(t % 4 + 1) * 128], in_=ptb[:, 512:640]), reads=[B['ptb2']], writes=[B['rysg', g4 % 2]])
                if t % 4 == 3 or t == NT_A - 1:
                    n_ = (t % 4 + 1) * 128
                    P.dma('sp', lambda e, h=h, g4=g4, yg=yg, n_=n_: e.dma_start(out=YT[2 + h, :, g4 * 512:g4 * 512 + n_], in_=yg[:, 0:n_]), reads=[B['rysg', g4 % 2]], is_output=True)
        P.flush()


import ml_dtypes
_BF = ml_dtypes.bfloat16
_CONST = {}


def _consts():
    if _CONST:
        return _CONST
    C = _CONST
    rows = 64
    row = np.repeat(np.arange(rows, dtype=np.float32), 64)
    col = np.tile(np.arange(64, dtype=np.float32), rows)
    inv = (10000.0 ** (-np.arange(32, dtype=np.float32) / 32)).astype(np.float32)
    ang = np.stack([row[:, None] * inv, col[:, None] * inv], axis=1).astype(np.float32)
    C['ropec'] = np.ascontiguousarray(np.cos(ang).astype(np.float32).reshape(32, 128, 64).transpose(1, 0, 2))
    C['ropes'] = np.ascontiguousarray(np.sin(ang).astype(np.float32).reshape(32, 128, 64).transpose(1, 0, 2))
    j = np.arange(128, dtype=np.float32)[:, None]
    i = np.arange(128, dtype=np.float32)[None, :]
    rtab = np.stack([np.maximum(i - j, 0), (i >= j).astype(np.float32), np.maximum(j - i, 0), (j >= i).astype(np.float32),
                     np.broadcast_to(i + 1, (128, 128)), np.broadcast_to(128 - i, (128, 128))], axis=1).astype(np.float32)
    C['rtab'] = np.ascontiguousarray(rtab)
    jj = np.arange(128, dtype=np.float32)
    C['rcol'] = np.ascontiguousarray(np.stack([127 - jj, jj, np.full(128, 128.0), np.zeros(128)], 1).astype(np.float32))

    def feats(l):
        t = np.linspace(0.0, 1.0, l, dtype=np.float32)[:, None]
        w = (2.0 * np.pi * np.arange(l, dtype=np.float32) / l).astype(np.float32)
        f = np.linspace(1e-4, 15, 16, dtype=np.float32)
        a = (w[:, None] * f[None, :]).astype(np.float32)
        return np.concatenate([t, np.cos(a), -np.sin(a)], axis=-1).astype(np.float32), t[:, 0]
    f4, t4 = feats(4096)
    fc, tc = feats(256)
    C['featsT'] = np.ascontiguousarray(f4.T)
    C['featsTc'] = np.ascontiguousarray(fc.T)
    negt = np.concatenate([-t4.reshape(32, 128), -tc.reshape(2, 128)], 0).T
    C['negt'] = np.ascontiguousarray(negt.astype(np.float32))
    mx = np.log(1e-2) / 0.3
    mn = np.log(1e-2) / 1.5
    C['absdelta'] = np.abs(np.linspace(mn, mx, 2048, dtype=np.float32)).reshape(2, 2, 512)
    n = np.arange(4096, dtype=np.float64)
    k = np.arange(4096, dtype=np.float64) + 0.5
    Fm = np.empty((32, 128, 8, 1024), dtype=_BF)
    for nt in range(32):
        th = 2 * np.pi * np.outer(n[nt * 128:(nt + 1) * 128], k) / 8192.0
        Fm[nt, :, :, 0:512] = np.cos(th).reshape(128, 8, 512).astype(_BF)
        Fm[nt, :, :, 512:1024] = np.sin(th).reshape(128, 8, 512).astype(_BF)
    C['Fm'] = Fm
    Gm = np.empty((4, 64, 128, 1024), dtype=_BF)
    for kt in range(32):
        th = 2 * np.pi * np.outer(k[kt * 128:(kt + 1) * 128], n) / 8192.0
        Gm[:, kt] = np.cos(th).reshape(128, 4, 1024).transpose(1, 0, 2).astype(_BF)
        Gm[:, 32 + kt] = np.sin(th).reshape(128, 4, 1024).transpose(1, 0, 2).astype(_BF)
    C['Gm'] = Gm
    nc_ = np.arange(256, dtype=np.float64)
    kc = np.arange(256, dtype=np.float64) + 0.5
    th = 2 * np.pi * np.outer(nc_, kc) / 512.0
    Fc = np.concatenate([np.cos(th), np.sin(th)], 1).reshape(2, 128, 512).transpose(1, 0, 2)
    C['Fc'] = np.ascontiguousarray(Fc).astype(_BF)
    Gc = np.concatenate([np.cos(th.T), np.sin(th.T)], 0).reshape(4, 128, 256).transpose(1, 0, 2)
    C['Gc'] = np.ascontiguousarray(Gc).astype(_BF)
    return C


def run_A(l, x_cur, h_cur, mod, inp):
    C = _consts()
    w_in = inp['w_in'][l]
    ins = []
    for b in range(4):
        xt = np.ascontiguousarray(np.concatenate([x_cur[b], h_cur[b]], 0).reshape(NT_A, 128, 1024))
        ms = [mod[b], mod[4]]
        modcols = np.ascontiguousarray(np.stack([np.stack([_cols(m[k_ * 1024:(k_ + 1) * 1024]) for k_ in (0, 1)], 1) for m in ms], 1))
        for half in range(2):
            r256 = half * 256 + np.arange(256)
            colsA = np.concatenate([g * 512 + r256 for g in range(3)])
            colsR = np.concatenate([1536 + g * 512 + r256 for g in range(4)])
            colsH = np.concatenate([3584 + g * 512 + r256 for g in range(3)])
            colsD = np.concatenate([5120 + r256, 5120 + 512 + half * 128 + np.arange(128), 5120 + 768 + half * 128 + np.arange(128)])
            ch = half * 256 + np.arange(256)
            caw = np.stack([inp['conv_a_w'][l][0, ch], inp['conv_a_w'][l][1, ch], inp['conv_a_w'][l][2, ch], inp['conv_a_b'][l][ch]], -1)
            hch = np.concatenate([g * 512 + ch for g in range(3)])
            hcw = np.stack([inp['hy_conv_w'][l][0, hch], inp['hy_conv_w'][l][1, hch], inp['hy_conv_w'][l][2, hch], inp['hy_conv_b'][l][hch]], -1)
            d = dict(
                x=xt, modcols=modcols, ident=_IDENT,
                wA=_tile_rows(w_in[:, colsA]), wR=_tile_rows(w_in[:, colsR]), wH=_tile_rows(w_in[:, colsH]), wD=_tile_rows(w_in[:, colsD]),
                caw=np.ascontiguousarray(caw.reshape(2, 128, 4).transpose(1, 0, 2)),
                hcw=np.ascontiguousarray(hcw.reshape(6, 128, 4).transpose(1, 0, 2)),
                hsk=np.ascontiguousarray(inp['hy_skip'][l][:, ch].reshape(2, 2, 128).transpose(2, 0, 1)),
                ropec=C['ropec'], ropes=C['ropes'],
                qkn=_rep(np.stack([inp['q_norm'][l], inp['k_norm'][l]], 0)),
                rdec=_rep(inp['ret_decay'][l][:, half * 2:half * 2 + 2].reshape(4)),
                rtab=C['rtab'], rcol=C['rcol'], featsT=C['featsT'], featsTc=C['featsTc'],
                hw1=np.ascontiguousarray(inp['hy_w1'][l]), hw2=np.ascontiguousarray(inp['hy_w2'][l]),
                hcols=np.ascontiguousarray(np.stack([inp['hy_b1'][l], inp['hy_b2'][l], inp['hy_freq'][l][0], inp['hy_freq'][l][1]], 1)),
                hw3=np.ascontiguousarray(inp['hy_w3'][l].reshape(64, 2, 2, 512)[:, :, :, half * 256:(half + 1) * 256]),
                negt=C['negt'], adel=_rep(np.ascontiguousarray(C['absdelta'][:, :, half * 256:(half + 1) * 256])),
                Fm=C['Fm'], Gm=C['Gm'], Fc=C['Fc'], Gc=C['Gc'],
            )
            ins.append(d)
    res = run_bass_kernel_spmd(_prog('A', build_A), ins, core_ids=list(range(8)))
    YT = []
    for b in range(4):
        y = np.empty((4, 2, 2, 128, NTOK_A), np.float32)
        for half in range(2):
            o = res.results[b * 2 + half]['YT'].reshape(4, 2, 128, NTOK_A)
            y[:, half] = o
        YT.append(y.reshape(2048, NTOK_A))
    return YT


class _YTView:
    def __init__(self, base, half):
        self.base = base
        self.half = half

    def _m(self, i8):
        return (i8 // 2) * 4 + self.half * 2 + (i8 % 2)

    def __getitem__(self, idx):
        if isinstance(idx, tuple):
            return self.base[(self._m(idx[0]),) + tuple(idx[1:])]
        return self.base[self._m(idx)]


def emit_M2(nc, P, Dm):
    cT, wm, bcol, brow, modc, modr = Dm['cT'], Dm['wm'], Dm['bcol'], Dm['brow'], Dm['modc'], Dm['modr']
    with contextlib.ExitStack() as st:
        P.stack = st
        B = Bufs()
        cs = P.sb("cs", [128, 8, 2], F32)
        sc = P.sb("sc", [128, 8, 2], F32)
        ones = P.sb("ones", [128, 128], F32)
        scb = P.sb("scb", [128, 8, 2, 128], F32)
        wch = [P.sb(f"wch{i}", [128, 8, 1024], F32) for i in range(2)]
        bc = P.sb("bc", [128, 48], F32)
        br = P.sb("br", [128, 2, 1024], F32)
        mc = P.sb("mc", [128, 2, 48], F32)
        mr = P.sb("mr", [128, 2, 2, 1024], F32)
        pm = P.ps("pm", [128, 2, 512], F32)
        pcl = P.ps("pc", [128, 512], F32)
        P.dma('sp', lambda e: e.dma_start(out=cs[:], in_=cT), writes=[B['cs']])
        P.op('act', lambda e: e.activation(out=sc[:], in_=cs[:], func=AF.Silu), reads=[B['cs']], writes=[B['sc']])
        P.op('dve', lambda e: e.memset(ones[:], 1.0), writes=[B['ones']])
        for kt in range(8):
            for ms in range(2):
                P.op('dve', lambda e, kt=kt, ms=ms: e.tensor_scalar(out=scb[:, kt, ms, :], in0=ones[:], scalar1=sc[:, kt, ms:ms + 1], scalar2=None, op0=ALU.mult),
                     reads=[B['ones'], B['sc']], writes=[B['scb']])
        wc = 0
        for l in range(4):
            P.dma('sp', lambda e, l=l: e.dma_start(out=bc[:], in_=bcol[l]), writes=[B['bc']])
            P.dma('sp', lambda e, l=l: e.dma_start(out=br[:], in_=brow[l]), writes=[B['br']])
            for k in range(6):
                ws_ = wc % 2
                wc += 1
                P.dma('sp', lambda e, l=l, k=k, ws_=ws_: e.dma_start(out=wch[ws_][:], in_=wm[l, k]), writes=[B['wch', ws_]])
                for f in range(8):
                    for kt in range(8):
                        P.op('pe', lambda e, f=f, kt=kt, ws_=ws_: e.matmul(pcl[:, f * 2:f * 2 + 2], lhsT=wch[ws_][:, kt, f * 128:(f + 1) * 128], rhs=sc[:, kt, :], start=(kt == 0), stop=(kt == 7)),
                             reads=[B['wch', ws_], B['sc']], writes=[B['pc']])
                for ms in range(2):
                    P.op('dve', lambda e, k=k, ms=ms: e.tensor_tensor(out=mc[:, ms, k * 8:(k + 1) * 8], in0=pcl[:, 0:16].rearrange("p (f m) -> p m f", m=2)[:, ms, :], in1=bc[:, k * 8:(k + 1) * 8], op=ALU.add),
                         reads=[B['pc'], B['bc']], writes=[B['mc']])
                if k in (2, 5):
                    j = 0 if k == 2 else 1
                    for ms in range(2):
                        for nb in range(2):
                            for kt in range(8):
                                P.op('pe', lambda e, ms=ms, nb=nb, kt=kt, ws_=ws_: e.matmul(pm[:, nb, :], lhsT=scb[:, kt, ms, :], rhs=wch[ws_][:, kt, nb * 512:(nb + 1) * 512], start=(kt == 0), stop=(kt == 7)),
                                     reads=[B['scb'], B['wch', ws_]], writes=[B['pm', nb]])
                            P.op('dve', lambda e, ms=ms, nb=nb, j=j: e.tensor_tensor(out=mr[:, ms, j, nb * 512:(nb + 1) * 512], in0=pm[:, nb, :], in1=br[:, j, nb * 512:(nb + 1) * 512], op=ALU.add),
                                 reads=[B['pm', nb], B['br']], writes=[B['mr']])
            P.dma('sp', lambda e, l=l: e.dma_start(out=modc[l], in_=mc[:]), reads=[B['mc']], writes=[B['modc', l]])
            P.dma('sp', lambda e, l=l: e.dma_start(out=modr[l], in_=mr[:]), reads=[B['mr']], writes=[B['modr', l]])
        P.flush()


def build_F():
    nc = bass.Bass("TRN2", target_bir_lowering=False)
    I = lambda name, shape, dt=F32: _din(nc, name, shape, dt)
    T = lambda name, shape, dt=F32: nc.dram_tensor(name, list(shape), dt, kind="Internal").ap()
    x0 = I("x0", [NT_A, 128, D])
    cT = I("cT", [128, 8, 2])
    wm = I("wm", [4, 6, 128, 8, 1024])
    bcol = I("bcol", [4, 128, 48])
    brow = I("brow", [4, 128, 2, 1024])
    ident = I("ident", [128, 128])
    wA = I("wA", [4, 2, 128, 8, 768])
    wR = I("wR", [4, 2, 128, 8, 1024])
    wH = I("wH", [4, 2, 128, 8, 768])
    wD = I("wD", [4, 2, 128, 8, 512])
    caw = I("caw", [4, 2, 128, 2, 4])
    hcw = I("hcw", [4, 2, 128, 6, 4])
    hsk = I("hsk", [4, 2, 128, 2, 2])
    ropec = I("ropec", [128, 32, 64])
    ropes = I("ropes", [128, 32, 64])
    qkn = I("qkn", [4, 128, 2, 128])
    rdec = I("rdec", [4, 2, 128, 4])
    rtab = I("rtab", [128, 6, 128])
    rcol = I("rcol", [128, 4])
    featsT = I("featsT", [33, 4096])
    featsTc = I("featsTc", [33, 256])
    hw1 = I("hw1", [4, 33, 64])
    hw2 = I("hw2", [4, 64, 64])
    hcols = I("hcols", [4, 64, 4])
    hw3 = I("hw3", [4, 2, 64, 2, 2, 256])
    negt = I("negt", [128, 34])
    adel = I("adel", [2, 128, 2, 2, 256])
    Fm = I("Fm", [32, 128, 8, 1024], BF16)
    Gm = I("Gm", [4, 64, 128, 1024], BF16)
    Fc = I("Fc", [128, 2, 512], BF16)
    Gc = I("Gc", [128, 4, 256], BF16)
    wg = I("wg", [4, 8, 128, 4, 8, 128])
    wb = I("wb", [4, 8, 128, 4, 4, 128])
    wo = I("wo", [4, 128, 8, D])
    lnrows = I("lnrows", [4, 128, 4, D])
    bgc = I("bgc", [4, 128, 4, 8])
    w1d = I("w1d", [2, 11, 128, 8, 256])
    w3d = I("w3d", [2, 11, 128, 8, 256])
    w2d = I("w2d", [2, 11, 128, 2, D])
    w1m = I("w1m", [2, 56, 128, 8, 512])
    w3m = I("w3m", [2, 56, 128, 8, 512])
    w2m = I("w2m", [2, 56, 128, 4, D])
    rt = I("rt", [2, 128, 8, 8])
    sel = I("sel", [8, 8, 128])
    out = _dout(nc, "out", [NT_A, 128, D])
    xb = [T("xbuf0", [NT_A, 128, D]), T("xbuf1", [NT_A, 128, D])]
    x1s = T("x1s", [NT_A, 128, D])
    YTs = T("YTs", [16, 128, NTOK_A])
    modc = T("modc", [4, 128, 2, 48])
    modr = T("modr", [4, 128, 2, 2, D])
    with contextlib.ExitStack() as st0:
        P = Prog(nc, st0)
        emit_M2(nc, P, dict(cT=cT, wm=wm, bcol=bcol, brow=brow, modc=modc, modr=modr))
        import os as _os
        NL = int(_os.environ.get('F_LAYERS', '4'))
        for l in range(NL):
            xin = x0 if l == 0 else xb[l % 2]
            xout = out if l == NL - 1 else xb[(l + 1) % 2]
            mcA = modc[l][:, :, 0:16].rearrange("p m (k f) -> p m k f", k=2)
            mcB = (mcA, modc[l][:, :, 24:40].rearrange("p m (k f) -> p m k f", k=2))
            for half in range(2):
                emit_A(nc, P, dict(x=xin, modcols=mcA, ident=ident, wA=wA[l, half], wR=wR[l, half], wH=wH[l, half], wD=wD[l, half],
                                   caw=caw[l, half], hcw=hcw[l, half], hsk=hsk[l, half], ropec=ropec, ropes=ropes, qkn=qkn[l], rdec=rdec[l, half],
                                   rtab=rtab, rcol=rcol, featsT=featsT, featsTc=featsTc, hw1=hw1[l], hw2=hw2[l], hcols=hcols[l], hw3=hw3[l, half],
                                   negt=negt, adel=adel[half], Fm=Fm, Gm=Gm, Fc=Fc, Gc=Gc, YT=_YTView(YTs, half)))
            moe = (l % 2 == 1)
            i = l // 2
            for hb in range(2):
                Dm = dict(x=xin, yT=YTs, modcols=mcB, modrows=modr[l], lnrows=lnrows[l], bgc=bgc[l], ident=ident, wg=wg[l], wb=wb[l], wo=wo[l],
                          x1o=x1s, xo=xout)
                if moe:
                    Dm.update(w1=w1m[i], w3=w3m[i], w2=w2m[i], rt=rt[i], sel=sel)
                else:
                    Dm.update(w1=w1d[i], w3=w3d[i], w2=w2d[i])
                emit_B(nc, P, Dm, moe, gt=(lambda t, hb=hb: hb * 16 + t if t < 16 else 32 + hb))
    return nc


def _pack_F(inp):
    C = _consts()
    sh = dict(ident=_IDENT, ropec=C['ropec'], ropes=C['ropes'], rtab=C['rtab'], rcol=C['rcol'], featsT=C['featsT'], featsTc=C['featsTc'],
              negt=C['negt'], Fm=C['Fm'], Gm=C['Gm'], Fc=C['Fc'], Gc=C['Gc'])
    sh['adel'] = np.stack([_rep(np.ascontiguousarray(C['absdelta'][:, :, h * 256:(h + 1) * 256])) for h in range(2)], 0)
    w_mod = inp['w_mod']
    sh['wm'] = np.ascontiguousarray(w_mod.reshape(4, 8, 128, 6, 1024).transpose(0, 3, 2, 1, 4))
    sh['bcol'] = np.ascontiguousarray(inp['b_mod'].reshape(4, 48, 128).transpose(0, 2, 1))
    sh['brow'] = np.stack([_rep(np.stack([inp['b_mod'][l, 2048:3072], inp['b_mod'][l, 5120:6144]], 0)) for l in range(4)], 0)
    wA, wR, wH, wD, caw, hcw, hsk, rdec, hw3 = [], [], [], [], [], [], [], [], []
    for l in range(4):
        w_in = inp['w_in'][l]
        rows = [[] for _ in range(9)]
        for half in range(2):
            r256 = half * 256 + np.arange(256)
            colsA = np.concatenate([g * 512 + r256 for g in range(3)])
            colsR = np.concatenate([1536 + g * 512 + r256 for g in range(4)])
            colsH = np.concatenate([3584 + g * 512 + r256 for g in range(3)])
            colsD = np.concatenate([5120 + r256, 5120 + 512 + half * 128 + np.arange(128), 5120 + 768 + half * 128 + np.arange(128)])
            ch = r256
            cawv = np.stack([inp['conv_a_w'][l][0, ch], inp['conv_a_w'][l][1, ch], inp['conv_a_w'][l][2, ch], inp['conv_a_b'][l][ch]], -1)
            hch = np.concatenate([g * 512 + ch for g in range(3)])
            hcwv = np.stack([inp['hy_conv_w'][l][0, hch], inp['hy_conv_w'][l][1, hch], inp['hy_conv_w'][l][2, hch], inp['hy_conv_b'][l][hch]], -1)
            vals = [_tile_rows(w_in[:, colsA]), _tile_rows(w_in[:, colsR]), _tile_rows(w_in[:, colsH]), _tile_rows(w_in[:, colsD]),
                    cawv.reshape(2, 128, 4).transpose(1, 0, 2), hcwv.reshape(6, 128, 4).transpose(1, 0, 2),
                    inp['hy_skip'][l][:, ch].reshape(2, 2, 128).transpose(2, 0, 1),
                    _rep(inp['ret_decay'][l][:, half * 2:half * 2 + 2].reshape(4)),
                    inp['hy_w3'][l].reshape(64, 2, 2, 512)[:, :, :, half * 256:(half + 1) * 256]]
            for r_, v_ in zip(rows, vals):
                r_.append(v_)
        for lst, r_ in zip((wA, wR, wH, wD, caw, hcw, hsk, rdec, hw3), rows):
            lst.append(np.stack(r_, 0))
    for n_, lst in zip(('wA', 'wR', 'wH', 'wD', 'caw', 'hcw', 'hsk', 'rdec', 'hw3'), (wA, wR, wH, wD, caw, hcw, hsk, rdec, hw3)):
        sh[n_] = np.ascontiguousarray(np.stack(lst, 0))
    sh['qkn'] = np.stack([_rep(np.stack([inp['q_norm'][l], inp['k_norm'][l]], 0)) for l in range(4)], 0)
    sh['hw1'] = np.ascontiguousarray(inp['hy_w1'])
    sh['hw2'] = np.ascontiguousarray(inp['hy_w2'])
    sh['hcols'] = np.ascontiguousarray(np.stack([inp['hy_b1'], inp['hy_b2'], inp['hy_freq'][:, 0], inp['hy_freq'][:, 1]], -1))
    pw = [prep_B_weights(l, inp) for l in range(4)]
    for n_ in ('wg', 'wb', 'wo', 'lnrows', 'bgc'):
        sh[n_] = np.stack([pw[l][n_] for l in range(4)], 0)
    for n_ in ('w1', 'w3', 'w2'):
        sh[n_ + 'd'] = np.stack([pw[0][n_], pw[2][n_]], 0)
        sh[n_ + 'm'] = np.stack([pw[1][n_], pw[3][n_]], 0)
    sh['rt'] = np.stack([pw[1]['rt'], pw[3]['rt']], 0)
    sh['sel'] = pw[1]['sel']
    per = []
    for b in range(4):
        cc = np.stack([inp['c'][b], inp['c_ctx']], 0)
        per.append(dict(x0=np.ascontiguousarray(np.concatenate([inp['x'][b], inp['ctx'][b]], 0).reshape(NT_A, 128, 1024)),
                        cT=np.ascontiguousarray(cc.T.reshape(8, 128, 2).transpose(1, 0, 2))))
    return sh, per


def kernel_fused(**inp):
    inp = {k_: np.asarray(v) for k_, v in inp.items()}
    sh, per = _pack_F(inp)
    ins = [dict(sh, **per[b]) for b in range(4)]
    res = run_bass_kernel_spmd(_prog('F', build_F), ins, core_ids=list(range(4)))
    out = np.stack([res.results[b]['out'].reshape(NTOK_A, 1024)[:4096] for b in range(4)], 0)
    return np.ascontiguousarray(out, dtype=np.float32)


def kernel_unfused(**inp):
    inp = {k_: np.asarray(v) for k_, v in inp.items()}
    mod = run_M(inp['c'], inp['c_ctx'], inp['w_mod'], inp['b_mod'])
    x_cur = np.ascontiguousarray(inp['x'], dtype=np.float32)
    h_cur = np.ascontiguousarray(inp['ctx'], dtype=np.float32)
    for l in range(4):
        YT = run_A(l, x_cur, h_cur, mod[:, l], inp)
        x_cur, h_cur, _ = run_B(l, x_cur, h_cur, YT, mod[:, l], inp)
    return x_cur


def kernel(**inp):
    return kernel_fused(**inp)
```

```python
import contextlib
import numpy as np
import concourse.bass as bass
import concourse.mybir as mybir

F32 = mybir.dt.float32
BF16 = mybir.dt.bfloat16
AF = mybir.ActivationFunctionType
ALU = mybir.AluOpType
AX = mybir.AxisListType

NDMA = 24
ENGS = ['pe', 'act', 'dve', 'pool', 'sp']


class Buf:
    __slots__ = ('w', 'r', 'name', 'excl')

    def __init__(self, name=''):
        self.w = None
        self.r = {}
        self.name = name
        self.excl = False


PSUM_KEYS = {'pt', 'ptb', 'pa', 'pb', 'pc', 'pg', 'pp', 'pmx', 'pg1', 'pg3', 'pf', 'pw', 'pm'}


class Bufs:
    def __init__(self, name=''):
        self.d = {}
        self.name = name

    def __getitem__(self, k):
        if k == 'ptb2':
            k = 'ptb'
        b = self.d.get(k)
        if b is None:
            b = Buf(f"{self.name}{k}")
            k0 = k[0] if isinstance(k, tuple) else k
            b.excl = k0 in PSUM_KEYS
            self.d[k] = b
        return b


class Prog:
    def __init__(self, nc, stack, same_engine_sync=True):
        self.nc = nc
        self.stack = stack
        self.q = {e: [] for e in ENGS}
        self.sem = {e: stack.enter_context(nc.semaphore(f"s_{e}")) for e in ENGS}
        self.cnt = {e: 0 for e in ENGS}
        self.seen = {e: {} for e in ENGS}
        self.dsem = [stack.enter_context(nc.semaphore(f"dq{i}")) for i in range(NDMA)]
        self.dval = [0] * NDMA
        self.dnext = 0
        self.ses = same_engine_sync
        self.out_tokens = []

    def sb(self, name, shape, dt):
        self._uid = getattr(self, '_uid', 0) + 1
        return self.stack.enter_context(self.nc.sbuf_tensor(f"{name}_u{self._uid}", list(shape), dt))

    def ps(self, name, shape, dt=F32):
        self._uid = getattr(self, '_uid', 0) + 1
        return self.stack.enter_context(self.nc.psum_tensor(f"{name}_u{self._uid}", list(shape), dt))

    def _semh(self, k):
        return self.sem[k] if isinstance(k, str) else self.dsem[k[1]]

    def _deps(self, e, reads, writes):
        need = {}
        xr = [b for b in reads if b.excl]
        if xr:
            writes = list(writes) + xr

        def add(tok):
            if tok is None:
                return
            k, v = tok
            if need.get(k, 0) < v:
                need[k] = v

        for b in reads:
            add(b.w)
        for b in writes:
            add(b.w)
            for k, v in b.r.items():
                add((k, v))
        waits = []
        for k, v in need.items():
            if k == e and (e == 'pe' or not self.ses):
                continue
            if self.seen[e].get(k, 0) >= v:
                continue
            self.seen[e][k] = v
            waits.append((k, v))
        return waits

    def _mark(self, tok, reads, writes):
        k, v = tok
        xr = [b for b in reads if b.excl]
        if xr:
            writes = list(writes) + xr
        for b in reads:
            if b.r.get(k, 0) < v:
                b.r[k] = v
        for b in writes:
            b.w = tok
            b.r = {}

    mute = False

    def op(self, e, fn, reads=(), writes=()):
        if self.mute:
            return
        waits = self._deps(e, reads, writes)
        self.cnt[e] += 1
        tok = (e, self.cnt[e])
        self._mark(tok, reads, writes)
        self.q[e].append((waits, fn, (self.sem[e], 1)))

    def dma(self, e, fn, reads=(), writes=(), is_output=False):
        if self.mute:
            return
        if e == 'pool':
            i = self._dpool = (getattr(self, '_dpool', -1) + 1) % 8
        else:
            i = 8 + self.dnext
            self.dnext = (self.dnext + 1) % (NDMA - 8)
        waits = self._deps(e, reads, writes)
        k = ('d', i)
        if self.dval[i] > 0 and self.seen[e].get(k, 0) < self.dval[i]:
            waits.append((k, self.dval[i]))
            self.seen[e][k] = self.dval[i]
        self.dval[i] += 16
        tok = (k, self.dval[i])
        self._mark(tok, reads, writes)
        self.q[e].append((waits, fn, (self.dsem[i], 16)))
        if is_output:
            self.out_tokens.append(tok)

    def flush(self):
        nc = self.nc
        q = self.q
        semh = self._semh
        ex = []
        for e in ['pe', 'act', 'dve', 'pool']:
            if self.cnt[e] > 0:
                ex.append((e, self.cnt[e]))
        for i in range(NDMA):
            if self.dval[i] > 0:
                ex.append((('d', i), self.dval[i]))

        def emit(eng, ename):
            for waits, fn, (sem, n) in q[ename]:
                for (k, v) in waits:
                    eng.wait_ge(semh(k), v)
                fn(eng).then_inc(sem, n)
            for (k, v) in ex:
                if self.seen[ename].get(k, 0) < v:
                    eng.wait_ge(semh(k), v)
                    self.seen[ename][k] = v

        with nc.Block() as block:
            @block.tensor
            def _(t):
                emit(t, 'pe')

            @block.scalar
            def _(t):
                emit(t, 'act')

            @block.vector
            def _(t):
                emit(t, 'dve')

            @block.gpsimd
            def _(t):
                emit(t, 'pool')

            @block.sync
            def _(t):
                emit(t, 'sp')
        self.q = {e: [] for e in ENGS}

from concourse.bass_utils import run_bass_kernel_spmd

D = 1024
ALPHA = (2 * 4) ** 0.25
NTILE = 17
NTOK = NTILE * 128
BLOCKS = [(0, 4), (4, 4), (8, 4), (12, 4), (16, 1)]


def _din(nc, name, shape, dt=F32):
    return nc.dram_tensor(name, list(shape), dt, kind="ExternalInput").ap()


def _dout(nc, name, shape, dt=F32):
    return nc.dram_tensor(name, list(shape), dt, kind="ExternalOutput").ap()


def build_M():
    nc = bass.Bass("TRN2", target_bir_lowering=False)
    cT = _din(nc, "cT", [128, 8, 5])
    w = _din(nc, "w", [128, 8, 3072])
    b = _din(nc, "b", [5, 3072])
    o = _dout(nc, "o", [5, 3072])
    with contextlib.ExitStack() as st:
        P = Prog(nc, st)
        B = Bufs()
        cs = P.sb("cs", [128, 8, 5], F32)
        sc = P.sb("sc", [128, 8, 5], F32)
        ws = P.sb("ws", [128, 8, 3072], F32)
        bs = P.sb("bs", [5, 3072], F32)
        os_ = P.sb("os", [5, 3072], F32)
        pm = P.ps("pm", [128, 2, 512], F32)
        P.dma('sp', lambda e: e.dma_start(out=cs[:], in_=cT), writes=[B['cs']])
        P.dma('sp', lambda e: e.dma_start(out=bs[:], in_=b), writes=[B['bs']])
        for kt in range(8):
            P.dma('sp', lambda e, kt=kt: e.dma_start(out=ws[:, kt, :], in_=w[:, kt, :]), writes=[B['ws', kt]])
        P.op('act', lambda e: e.activation(out=sc[:], in_=cs[:], func=AF.Silu), reads=[B['cs']], writes=[B['sc']])
        for nb in range(6):
            pb = B['pm', nb % 2]
            for kt in range(8):
                P.op('pe', lambda e, nb=nb, kt=kt: e.matmul(pm[0:5, nb % 2, :], lhsT=sc[:, kt, :], rhs=ws[:, kt, nb * 512:(nb + 1) * 512],
                                                          start=(kt == 0), stop=(kt == 7)),
                     reads=[B['sc'], B['ws', kt]], writes=[pb])
            P.op('dve', lambda e, nb=nb: e.tensor_tensor(out=os_[:, nb * 512:(nb + 1) * 512], in0=pm[0:5, nb % 2, :],
                                                        in1=bs[:, nb * 512:(nb + 1) * 512], op=ALU.add),
                 reads=[pb, B['bs']], writes=[B['os', nb]])
        P.dma('sp', lambda e: e.dma_start(out=o, in_=os_[:]), reads=[B['os', nb] for nb in range(6)], is_output=True)
        P.flush()
    return nc


def emit_B(nc, P, Dm, moe, gt=None):
    if gt is None:
        gt = lambda t: t
    KC = 4 if moe else 2
    NCH = 56 if moe else 11
    CPE = 7 if moe else 11
    x = Dm['x']
    yT = Dm['yT']
    modcols = Dm['modcols']
    modrows = Dm['modrows']
    lnrows = Dm['lnrows']
    bgc = Dm['bgc']
    ident = Dm['ident']
    wg = Dm['wg']
    wb = Dm['wb']
    wo = Dm['wo']
    w1 = Dm['w1']
    w3 = Dm['w3']
    w2 = Dm['w2']
    x1o = Dm['x1o']
    xo = Dm['xo']
    if moe:
        rt = Dm['rt']
        sel = Dm['sel']
    with contextlib.ExitStack() as st:
        P.stack = st
        B = Bufs()
        ids = P.sb("ids", [128, 128], F32)
        mcol = P.sb("mcol", [128, 2, 4, 8], F32)
        bgs = P.sb("bgs", [128, 4, 8], F32)
        u2T = P.sb("u2T", [128, 8, NTOK], BF16)
        P.dma('sp', lambda e: e.dma_start(out=ids[:], in_=ident), writes=[B['ids']])
        if isinstance(modcols, tuple):
            P.dma('sp', lambda e: e.dma_start(out=mcol[:, :, 0:2, :], in_=modcols[0]), writes=[B['mcol']])
            P.dma('sp', lambda e: e.dma_start(out=mcol[:, :, 2:4, :], in_=modcols[1]), writes=[B['mcol']])
        else:
            P.dma('sp', lambda e: e.dma_start(out=mcol[:], in_=modcols), writes=[B['mcol']])
        P.dma('sp', lambda e: e.dma_start(out=bgs[:], in_=bgc), writes=[B['bgs']])
        P.op('dve', lambda e: e.tensor_scalar_add(out=mcol[:, :, 1, :], in0=mcol[:, :, 1, :], scalar1=1.0), reads=[B['mcol']], writes=[B['mcol']])
        P.op('dve', lambda e: e.tensor_scalar_add(out=mcol[:, :, 3, :], in0=mcol[:, :, 3, :], scalar1=1.0), reads=[B['mcol']], writes=[B['mcol']])
        if moe:
            rts = P.sb("rts", [128, 8, 8], F32)
            sels = P.sb("sels", [8, 8, 128], F32)
            wall = P.sb("wall", [128, NTILE, 8], F32)
            WT = P.sb("WT", [8, NTOK], F32)
            P.dma('sp', lambda e: e.dma_start(out=rts[:], in_=rt), writes=[B['rts']])
            P.dma('sp', lambda e: e.dma_start(out=sels[:], in_=sel), writes=[B['sels']])

        def ln_tile(r, stt, mv, rstd, gi, key):
            rb = B[key]
            for hf in range(2):
                P.op('dve', lambda e, hf=hf: e.bn_stats(out=stt[:, hf, :], in_=r[:, hf * 512:(hf + 1) * 512]), reads=[rb], writes=[B[key, 'st', hf]])
            P.op('dve', lambda e: e.bn_aggr(out=mv[:], in_=stt[:].rearrange("p a b -> p (a b)")), reads=[B[key, 'st', 0], B[key, 'st', 1]], writes=[B[key, 'mv']])
            P.op('dve', lambda e: e.tensor_scalar_add(out=rstd[:], in0=mv[:, 1:2], scalar1=1e-6), reads=[B[key, 'mv']], writes=[B[key, 'rstd']])
            P.op('act', lambda e: e.sqrt(out=rstd[:], in_=rstd[:]), reads=[B[key, 'rstd']], writes=[B[key, 'rstd']])
            P.op('dve', lambda e: e.reciprocal(out=rstd[:], in_=rstd[:]), reads=[B[key, 'rstd']], writes=[B[key, 'rstd']])
            P.op('dve', lambda e: e.tensor_scalar(out=r[:], in0=r[:], scalar1=mv[:, 0:1], scalar2=rstd[:, 0:1], op0=ALU.subtract, op1=ALU.mult),
                 reads=[rb, B[key, 'mv'], B[key, 'rstd']], writes=[rb])
            P.op('pool', lambda e: e.tensor_tensor(out=r[:], in0=r[:], in1=lnr[:, gi, :], op=ALU.mult), reads=[rb, B['lnr']], writes=[rb])
            P.op('pool', lambda e: e.tensor_tensor(out=r[:], in0=r[:], in1=lnr[:, gi + 1, :], op=ALU.add), reads=[rb, B['lnr']], writes=[rb])

        with contextlib.ExitStack() as s1:
            P.stack = s1
            mrow = P.sb("mrow", [128, 2, 2, D], F32)
            lnr = P.sb("lnr", [128, 4, D], F32)
            P.dma('sp', lambda e: e.dma_start(out=mrow[:], in_=modrows), writes=[B['mrow']])
            P.dma('sp', lambda e: e.dma_start(out=lnr[:], in_=lnrows), writes=[B['lnr']])
            xs = P.sb("xs", [128, 4, D], F32)
            uT = P.sb("uT", [128, 8, 512], BF16)
            yTb = P.sb("yTb", [128, 16, 512], BF16)
            mT = P.sb("mT", [128, 8, 512], BF16)
            wgs = [P.sb(f"wgs{i}", [128, 4, 8, 128], BF16) for i in range(2)]
            wbs = [P.sb(f"wbs{i}", [128, 4, 4, 128], BF16) for i in range(2)]
            wos = P.sb("wos", [128, 8, D], BF16)
            sg = [P.sb(f"sg{i}", [128, 512], F32) for i in range(2)]
            macc = P.sb("macc", [128, 512], F32)
            mtmp = P.sb("mtmp", [128, 512], F32)
            rr = [P.sb(f"rr{i}", [128, D], F32) for i in range(2)]
            t1 = P.sb("t1", [128, D], F32)
            stt = P.sb("stt", [128, 2, 6], F32)
            mv = P.sb("mv", [128, 2], F32)
            rstd = P.sb("rstd", [128, 1], F32)
            u2f = P.sb("u2f", [128, 8, 128], F32)
            pt = P.ps("pt", [128, 2, 512], F32)
            pg = P.ps("pg", [128, 2, 512], F32)
            pp = P.ps("pp", [128, 2, 512], F32)
            pmx = P.ps("pmx", [128, 2, 512], F32)
            if moe:
                lg = P.sb("lg", [128, 8], F32)
                m8 = P.sb("m8", [128, 8], F32)
                msk = P.sb("msk", [128, 8], F32)
                ex = P.sb("ex", [128, 8], F32)
                sm = P.sb("sm", [128, 4], F32)

            P.dma('pool', lambda e: e.dma_start(out=wos[:], in_=wo), writes=[B['wos']])
            wcnt = 0
            tcnt = 0
            for bi, (t0, nt) in enumerate(BLOCKS):
                ms = 1 if bi == 4 else 0
                ntok = nt * 128
                tok0 = gt(t0) * 128
                P.dma('sp', lambda e, t0=t0, nt=nt: e.dma_start(out=xs[:, 0:nt, :], in_=x[gt(t0):gt(t0) + nt].rearrange("t p f -> p t f")),
                      writes=[B['xs', j] for j in range(nt)])
                P.dma('pool', lambda e, tok0=tok0, ntok=ntok: e.dma_start(out=yTb[:, :, 0:ntok], in_=yT[:, :, tok0:tok0 + ntok].rearrange("c p t -> p c t")),
                      writes=[B['yTb']])
                for j in range(nt):
                    for f in range(8):
                        pb = B['pt', tcnt % 2]
                        P.op('pe', lambda e, j=j, f=f, s=tcnt % 2: e.transpose(pt[:, s, 0:128], xs[:, j, f * 128:(f + 1) * 128], ids[:]),
                             reads=[B['xs', j], B['ids']], writes=[pb])
                        P.op('act', lambda e, j=j, f=f, s=tcnt % 2, ms=ms: e.activation(out=uT[:, f, j * 128:(j + 1) * 128], in_=pt[:, s, 0:128], func=AF.Identity,
                                                                                      bias=mcol[:, ms, 0, f:f + 1], scale=mcol[:, ms, 1, f:f + 1]),
                             reads=[pb, B['mcol']], writes=[B['uT', f]])
                        tcnt += 1
                for fo in range(8):
                    ws_ = wcnt % 2
                    P.dma('pool', lambda e, fo=fo, ws_=ws_: e.dma_start(out=wgs[ws_][:], in_=wg[fo]), writes=[B['wgs', ws_]])
                    P.dma('pool', lambda e, fo=fo, ws_=ws_: e.dma_start(out=wbs[ws_][:], in_=wb[fo]), writes=[B['wbs', ws_]])
                    wcnt += 1
                    for br in range(4):
                        s = br % 2
                        for kt in range(8):
                            P.op('pe', lambda e, br=br, kt=kt, s=s, ws_=ws_, ntok=ntok: e.matmul(pg[:, s, 0:ntok], lhsT=wgs[ws_][:, br, kt, :], rhs=uT[:, kt, 0:ntok],
                                                                                               start=(kt == 0), stop=(kt == 7)),
                                 reads=[B['wgs', ws_], B['uT', kt]], writes=[B['pg', s]])
                        for kt in range(4):
                            P.op('pe', lambda e, br=br, kt=kt, s=s, ws_=ws_, ntok=ntok: e.matmul(pp[:, s, 0:ntok], lhsT=wbs[ws_][:, br, kt, :], rhs=yTb[:, br * 4 + kt, 0:ntok],
                                                                                               start=(kt == 0), stop=(kt == 3)),
                                 reads=[B['wbs', ws_], B['yTb']], writes=[B['pp', s]])
                        P.op('act', lambda e, br=br, fo=fo, s=s, ntok=ntok: e.activation(out=sg[s][:, 0:ntok], in_=pg[:, s, 0:ntok], func=AF.Sigmoid,
                                                                                       bias=bgs[:, br, fo:fo + 1], scale=1.0),
                             reads=[B['pg', s], B['bgs']], writes=[B['sg', s]])
                        if br == 0:
                            P.op('dve', lambda e, s=s, ntok=ntok: e.tensor_tensor(out=macc[:, 0:ntok], in0=sg[s][:, 0:ntok], in1=pp[:, s, 0:ntok], op=ALU.mult),
                                 reads=[B['sg', s], B['pp', s]], writes=[B['macc']])
                        else:
                            P.op('dve', lambda e, s=s, ntok=ntok: e.tensor_tensor(out=mtmp[:, 0:ntok], in0=sg[s][:, 0:ntok], in1=pp[:, s, 0:ntok], op=ALU.mult),
                                 reads=[B['sg', s], B['pp', s]], writes=[B['mtmp']])
                            if br < 3:
                                P.op('pool', lambda e, ntok=ntok: e.tensor_tensor(out=macc[:, 0:ntok], in0=macc[:, 0:ntok], in1=mtmp[:, 0:ntok], op=ALU.add),
                                     reads=[B['macc'], B['mtmp']], writes=[B['macc']])
                            else:
                                P.op('pool', lambda e, fo=fo, ntok=ntok: e.tensor_tensor(out=mT[:, fo, 0:ntok], in0=macc[:, 0:ntok], in1=mtmp[:, 0:ntok], op=ALU.add),
                                     reads=[B['macc'], B['mtmp']], writes=[B['mT', fo]])
                for j in range(nt):
                    tile = t0 + j
                    r = rr[tile % 2]
                    rk = ('rr', tile % 2)
                    for hf in range(2):
                        for kt in range(8):
                            P.op('pe', lambda e, j=j, hf=hf, kt=kt: e.matmul(pmx[:, hf, :], lhsT=mT[:, kt, j * 128:(j + 1) * 128], rhs=wos[:, kt, hf * 512:(hf + 1) * 512],
                                                                           start=(kt == 0), stop=(kt == 7)),
                                 reads=[B['mT', kt], B['wos']], writes=[B['pmx', hf]])
                        P.op('dve', lambda e, hf=hf, ms=ms: e.tensor_tensor(out=t1[:, hf * 512:(hf + 1) * 512], in0=pmx[:, hf, :], in1=mrow[:, ms, 0, hf * 512:(hf + 1) * 512], op=ALU.mult),
                             reads=[B['pmx', hf], B['mrow']], writes=[B['t1', hf]])
                    P.op('dve', lambda e, j=j, r=r: e.scalar_tensor_tensor(out=r[:], in0=xs[:, j, :], scalar=ALPHA, in1=t1[:], op0=ALU.mult, op1=ALU.add),
                         reads=[B['xs', j], B['t1', 0], B['t1', 1]], writes=[B[rk]])
                    ln_tile(r, stt, mv, rstd, 0, rk)
                    P.dma('sp', lambda e, tile=tile, r=r: e.dma_start(out=x1o[gt(tile)], in_=r[:]), reads=[B[rk]], writes=[B['x1o', tile]])
                    for f in range(8):
                        pb = B['pt', tcnt % 2]
                        P.op('pe', lambda e, r=r, f=f, s=tcnt % 2: e.transpose(pt[:, s, 0:128], r[:, f * 128:(f + 1) * 128], ids[:]),
                             reads=[B[rk], B['ids']], writes=[pb])
                        if moe:
                            P.op('act', lambda e, f=f, s=tcnt % 2, ms=ms: e.activation(out=u2f[:, f, :], in_=pt[:, s, 0:128], func=AF.Identity,
                                                                                     bias=mcol[:, ms, 2, f:f + 1], scale=mcol[:, ms, 3, f:f + 1]),
                                 reads=[pb, B['mcol']], writes=[B['u2f', f]])
                            P.op('dve', lambda e, f=f, tile=tile: e.tensor_copy(out=u2T[:, f, tile * 128:(tile + 1) * 128], in_=u2f[:, f, :]),
                                 reads=[B['u2f', f]], writes=[B['u2T', f, tile]])
                        else:
                            P.op('act', lambda e, f=f, s=tcnt % 2, ms=ms, tile=tile: e.activation(out=u2T[:, f, tile * 128:(tile + 1) * 128], in_=pt[:, s, 0:128], func=AF.Identity,
                                                                                                bias=mcol[:, ms, 2, f:f + 1], scale=mcol[:, ms, 3, f:f + 1]),
                                 reads=[pb, B['mcol']], writes=[B['u2T', f, tile]])
                        tcnt += 1
                    if moe:
                        for kt in range(8):
                            P.op('pe', lambda e, kt=kt: e.matmul(pg[:, 0, 0:8], lhsT=u2f[:, kt, :], rhs=rts[:, kt, :], start=(kt == 0), stop=(kt == 7)),
                                 reads=[B['u2f', kt], B['rts']], writes=[B['pg', 0]])
                        P.op('dve', lambda e: e.tensor_copy(out=lg[:], in_=pg[:, 0, 0:8]), reads=[B['pg', 0]], writes=[B['lg']])
                        P.op('dve', lambda e: e.max(out=m8[:], in_=lg[:]), reads=[B['lg']], writes=[B['m8']])
                        P.op('dve', lambda e: e.tensor_scalar(out=msk[:], in0=lg[:], scalar1=m8[:, 1:2], scalar2=None, op0=ALU.is_ge), reads=[B['lg'], B['m8']], writes=[B['msk']])
                        P.op('dve', lambda e: e.tensor_scalar(out=sm[:, 0:1], in0=m8[:, 0:1], scalar1=-1.0, scalar2=None, op0=ALU.mult), reads=[B['m8']], writes=[B['sm', 0]])
                        P.op('dve', lambda e: e.tensor_tensor(out=sm[:, 1:2], in0=m8[:, 1:2], in1=m8[:, 0:1], op=ALU.subtract), reads=[B['m8']], writes=[B['sm', 1]])
                        P.op('act', lambda e: e.activation(out=ex[:], in_=lg[:], func=AF.Exp, bias=sm[:, 0:1], scale=1.0), reads=[B['lg'], B['sm', 0]], writes=[B['ex']])
                        P.op('act', lambda e: e.activation(out=sm[:, 2:3], in_=sm[:, 1:2], func=AF.Exp), reads=[B['sm', 1]], writes=[B['sm', 2]])
                        P.op('dve', lambda e: e.tensor_scalar_add(out=sm[:, 2:3], in0=sm[:, 2:3], scalar1=1.0), reads=[B['sm', 2]], writes=[B['sm', 2]])
                        P.op('dve', lambda e: e.reciprocal(out=sm[:, 3:4], in_=sm[:, 2:3]), reads=[B['sm', 2]], writes=[B['sm', 3]])
                        P.op('dve', lambda e: e.tensor_tensor(out=ex[:], in0=ex[:], in1=msk[:], op=ALU.mult), reads=[B['ex'], B['msk']], writes=[B['ex']])
                        P.op('dve', lambda e, tile=tile: e.tensor_scalar(out=wall[:, tile, :], in0=ex[:], scalar1=sm[:, 3:4], scalar2=None, op0=ALU.mult),
                             reads=[B['ex'], B['sm', 3]], writes=[B['wall', tile]])
                        P.op('pe', lambda e, tile=tile: e.transpose(pp[0:8, 0, 0:128], wall[:, tile, :], ids[:]), reads=[B['wall', tile], B['ids']], writes=[B['pp', 0]])
                        P.op('act', lambda e, tile=tile: e.copy(out=WT[:, tile * 128:(tile + 1) * 128], in_=pp[0:8, 0, 0:128]), reads=[B['pp', 0]], writes=[B['WT']])
            P.flush()
        s2o = st.enter_context(contextlib.ExitStack())
        P.stack = s2o
        facc = P.sb("facc", [128, NTILE, D], F32)
        pg1 = P.ps("pg1", [128, 2, 512], F32)
        pg3 = P.ps("pg3", [128, 2, 512], F32)
        pf = P.ps("pf", [128, 2, 512], F32)
        pw = P.ps("pw", [128, 512], F32)
        with contextlib.ExitStack() as s2:
            P.stack = s2
            w1s = [P.sb(f"w1s{i}", [128, 8, KC * 128], BF16) for i in range(2)]
            w3s = [P.sb(f"w3s{i}", [128, 8, KC * 128], BF16) for i in range(2)]
            w2s = [P.sb(f"w2s{i}", [128, KC, D], BF16) for i in range(2)]
            s1b = [P.sb(f"s1b{i}", [128, 512], F32) for i in range(2)]
            hT = [P.sb(f"hT{i}", [128, KC, 512], BF16) for i in range(2)]
            gcnt = 0
            fcnt = 0
            hcnt = 0
            for ci in range(NCH):
                wsl = ci % 2
                ex_ = ci // CPE
                P.dma('pool', lambda e, ci=ci, wsl=wsl: e.dma_start(out=w1s[wsl][:], in_=w1[ci]), writes=[B['w1s', wsl]])
                P.dma('pool', lambda e, ci=ci, wsl=wsl: e.dma_start(out=w3s[wsl][:], in_=w3[ci]), writes=[B['w3s', wsl]])
                P.dma('pool', lambda e, ci=ci, wsl=wsl: e.dma_start(out=w2s[wsl][:], in_=w2[ci]), writes=[B['w2s', wsl]])
                for bi, (t0, nt) in enumerate(BLOCKS):
                    ntok = nt * 128
                    tok0 = t0 * 128
                    hs = hcnt % 2
                    hcnt += 1
                    if moe:
                        P.op('pe', lambda e, ex_=ex_, tok0=tok0, ntok=ntok: e.matmul(pw[:, 0:ntok], lhsT=sels[:, ex_, :], rhs=WT[:, tok0:tok0 + ntok], start=True, stop=True),
                             reads=[B['sels'], B['WT']], writes=[B['pw']])
                    for kk in range(KC):
                        g = gcnt % 2
                        gcnt += 1
                        for kt in range(8):
                            P.op('pe', lambda e, kk=kk, kt=kt, g=g, wsl=wsl, tok0=tok0, ntok=ntok: e.matmul(pg1[:, g, 0:ntok], lhsT=w1s[wsl][:, kt, kk * 128:(kk + 1) * 128],
                                                                                                       rhs=u2T[:, kt, tok0:tok0 + ntok], start=(kt == 0), stop=(kt == 7)),
                                 reads=[B['w1s', wsl], B['u2T']], writes=[B['pg1', g]])
                        for kt in range(8):
                            P.op('pe', lambda e, kk=kk, kt=kt, g=g, wsl=wsl, tok0=tok0, ntok=ntok: e.matmul(pg3[:, g, 0:ntok], lhsT=w3s[wsl][:, kt, kk * 128:(kk + 1) * 128],
                                                                                                       rhs=u2T[:, kt, tok0:tok0 + ntok], start=(kt == 0), stop=(kt == 7)),
                                 reads=[B['w3s', wsl], B['u2T']], writes=[B['pg3', g]])
                        P.op('act', lambda e, g=g, ntok=ntok: e.activation(out=s1b[g][:, 0:ntok], in_=pg1[:, g, 0:ntok], func=AF.Silu), reads=[B['pg1', g]], writes=[B['s1b', g]])
                        if moe:
                            P.op('dve', lambda e, g=g, ntok=ntok: e.tensor_tensor(out=s1b[g][:, 0:ntok], in0=s1b[g][:, 0:ntok], in1=pg3[:, g, 0:ntok], op=ALU.mult),
                                 reads=[B['s1b', g], B['pg3', g]], writes=[B['s1b', g]])
                            P.op('dve', lambda e, g=g, kk=kk, hs=hs, ntok=ntok: e.tensor_tensor(out=hT[hs][:, kk, 0:ntok], in0=s1b[g][:, 0:ntok], in1=pw[:, 0:ntok], op=ALU.mult),
                                 reads=[B['s1b', g], B['pw']], writes=[B['hT', hs, kk]])
                        else:
                            P.op('dve', lambda e, g=g, kk=kk, hs=hs, ntok=ntok: e.tensor_tensor(out=hT[hs][:, kk, 0:ntok], in0=s1b[g][:, 0:ntok], in1=pg3[:, g, 0:ntok], op=ALU.mult),
                                 reads=[B['s1b', g], B['pg3', g]], writes=[B['hT', hs, kk]])
                    for j in range(nt):
                        tile = t0 + j
                        for hf in range(2):
                            fs = fcnt % 2
                            fcnt += 1
                            for kk in range(KC):
                                P.op('pe', lambda e, j=j, hf=hf, kk=kk, fs=fs, hs=hs, wsl=wsl: e.matmul(pf[:, fs, :], lhsT=hT[hs][:, kk, j * 128:(j + 1) * 128],
                                                                                                    rhs=w2s[wsl][:, kk, hf * 512:(hf + 1) * 512], start=(kk == 0), stop=(kk == KC - 1)),
                                     reads=[B['hT', hs, kk], B['w2s', wsl]], writes=[B['pf', fs]])
                            fb = B['facc', tile, hf]
                            if ci == 0:
                                P.op('act', lambda e, tile=tile, hf=hf, fs=fs: e.copy(out=facc[:, tile, hf * 512:(hf + 1) * 512], in_=pf[:, fs, :]), reads=[B['pf', fs]], writes=[fb])
                            else:
                                P.op('dve', lambda e, tile=tile, hf=hf, fs=fs: e.tensor_tensor(out=facc[:, tile, hf * 512:(hf + 1) * 512], in0=facc[:, tile, hf * 512:(hf + 1) * 512],
                                                                                              in1=pf[:, fs, :], op=ALU.add), reads=[B['pf', fs], fb], writes=[fb])
            P.flush()
        with contextlib.ExitStack() as s3:
            P.stack = s3
            mrow = P.sb("mrow3", [128, 2, 2, D], F32)
            lnr = P.sb("lnr3", [128, 4, D], F32)
            P.dma('sp', lambda e: e.dma_start(out=mrow[:], in_=modrows), writes=[B['mrow']])
            P.dma('sp', lambda e: e.dma_start(out=lnr[:], in_=lnrows), writes=[B['lnr']])
            xr = [P.sb(f"xr{i}", [128, D], F32) for i in range(2)]
            t1 = P.sb("t1b", [128, D], F32)
            stt = P.sb("stt2", [128, 2, 6], F32)
            mv = P.sb("mv2", [128, 2], F32)
            rstd = P.sb("rstd2", [128, 1], F32)
            for tile in range(NTILE):
                ms = 1 if tile == 16 else 0
                r = xr[tile % 2]
                rk = ('xr', tile % 2)
                P.dma('sp', lambda e, tile=tile, r=r: e.dma_start(out=r[:], in_=x1o[gt(tile)]), reads=[B['x1o', tile]], writes=[B[rk]])
                P.op('dve', lambda e, tile=tile, ms=ms: e.tensor_tensor(out=t1[:], in0=facc[:, tile, :], in1=mrow[:, ms, 1, :], op=ALU.mult),
                     reads=[B['facc', tile, 0], B['facc', tile, 1], B['mrow']], writes=[B['t1b']])
                P.op('dve', lambda e, r=r: e.scalar_tensor_tensor(out=r[:], in0=r[:], scalar=ALPHA, in1=t1[:], op0=ALU.mult, op1=ALU.add),
                     reads=[B[rk], B['t1b']], writes=[B[rk]])
                ln_tile(r, stt, mv, rstd, 2, rk)
                P.dma('sp', lambda e, tile=tile, r=r: e.dma_start(out=xo[gt(tile)], in_=r[:]), reads=[B[rk]], is_output=True)
            P.flush()


def build_B(moe):
    KC = 4 if moe else 2
    NCH = 56 if moe else 11
    CPE = 7 if moe else 11
    nc = bass.Bass("TRN2", target_bir_lowering=False)
    x = _din(nc, "x", [NTILE, 128, D])
    yT = _din(nc, "yT", [16, 128, NTOK])
    modcols = _din(nc, "modcols", [128, 2, 4, 8])
    modrows = _din(nc, "modrows", [128, 2, 2, D])
    lnrows = _din(nc, "lnrows", [128, 4, D])
    bgc = _din(nc, "bgc", [128, 4, 8])
    ident = _din(nc, "ident", [128, 128])
    wg = _din(nc, "wg", [8, 128, 4, 8, 128])
    wb = _din(nc, "wb", [8, 128, 4, 4, 128])
    wo = _din(nc, "wo", [128, 8, D])
    w1 = _din(nc, "w1", [NCH, 128, 8, KC * 128])
    w3 = _din(nc, "w3", [NCH, 128, 8, KC * 128])
    w2 = _din(nc, "w2", [NCH, 128, KC, D])
    if moe:
        rt = _din(nc, "rt", [128, 8, 8])
        sel = _din(nc, "sel", [8, 8, 128])
    x1o = _dout(nc, "x1o", [NTILE, 128, D])
    xo = _dout(nc, "xo", [NTILE, 128, D])

    Dm = dict(x=x, yT=yT, modcols=modcols, modrows=modrows, lnrows=lnrows, bgc=bgc, ident=ident, wg=wg, wb=wb, wo=wo, w1=w1, w3=w3, w2=w2, x1o=x1o, xo=xo)
    if moe:
        Dm['rt'] = rt
        Dm['sel'] = sel
    with contextlib.ExitStack() as st0:
        P = Prog(nc, st0)
        emit_B(nc, P, Dm, moe)
    return nc


_CACHE = {}


def _prog(name, fn, *a):
    k = (name,) + a
    if k not in _CACHE:
        _CACHE[k] = fn(*a)
    return _CACHE[k]


def _tile_rows(w):
    K, N = w.shape
    return np.ascontiguousarray(w.reshape(K // 128, 128, N).transpose(1, 0, 2))


def _cols(v):
    return np.ascontiguousarray(v.reshape(-1, 128).T)


def _rep(v):
    return np.ascontiguousarray(np.broadcast_to(v[None], (128,) + v.shape))


_IDENT = np.eye(128, dtype=np.float32)


def run_M(c, c_ctx, w_mod, b_mod):
    cc = np.concatenate([c, c_ctx[None]], 0)
    cT = np.ascontiguousarray(cc.T.reshape(8, 128, 5).transpose(1, 0, 2))
    Wm = w_mod.transpose(1, 0, 2).reshape(1024, 4 * 6144)
    bm = b_mod.reshape(4 * 6144)
    ins = []
    for i in range(8):
        sl = slice(i * 3072, (i + 1) * 3072)
        ins.append(dict(cT=cT, w=_tile_rows(Wm[:, sl]), b=np.ascontiguousarray(np.broadcast_to(bm[None, sl], (5, 3072)))))
    res = run_bass_kernel_spmd(_prog('M', build_M), ins, core_ids=list(range(8)))
    mod = np.concatenate([res.results[i]['o'] for i in range(8)], axis=1)
    return mod.reshape(5, 4, 6144)


def prep_B_weights(l, inp):
    moe = (l % 2 == 1)
    i = l // 2
    d = {}
    d['wg'] = np.ascontiguousarray(inp['w_gate'][l].reshape(4, 8, 128, 8, 128).transpose(3, 2, 0, 1, 4))
    d['wb'] = np.ascontiguousarray(inp['w_branch'][l].reshape(4, 4, 128, 8, 128).transpose(3, 2, 0, 1, 4))
    d['wo'] = _tile_rows(inp['w_o'][l])
    d['lnrows'] = _rep(np.stack([inp['ln_g'][l, 0], inp['ln_b'][l, 0], inp['ln_g'][l, 1], inp['ln_b'][l, 1]], 0))
    d['bgc'] = np.ascontiguousarray(inp['b_gate'][l].reshape(4, 8, 128).transpose(2, 0, 1))
    d['ident'] = _IDENT
    if not moe:
        KC, NCH = 2, 11
        d['w1'] = np.ascontiguousarray(inp['ffn_w1'][i].reshape(8, 128, NCH, KC * 128).transpose(2, 1, 0, 3))
        d['w3'] = np.ascontiguousarray(inp['ffn_w3'][i].reshape(8, 128, NCH, KC * 128).transpose(2, 1, 0, 3))
        d['w2'] = np.ascontiguousarray(inp['ffn_w2'][i].reshape(NCH, KC, 128, 1024).transpose(0, 2, 1, 3))
    else:
        KC, CPE = 4, 7
        d['w1'] = np.ascontiguousarray(inp['moe_w1'][i].reshape(8, 8, 128, CPE, KC * 128).transpose(0, 3, 2, 1, 4)).reshape(56, 128, 8, KC * 128)
        d['w3'] = np.ascontiguousarray(inp['moe_w3'][i].reshape(8, 8, 128, CPE, KC * 128).transpose(0, 3, 2, 1, 4)).reshape(56, 128, 8, KC * 128)
        d['w2'] = np.ascontiguousarray(inp['moe_w2'][i].reshape(8, CPE, KC, 128, 1024).transpose(0, 1, 3, 2, 4)).reshape(56, 128, KC, 1024)
        d['rt'] = _tile_rows(inp['router'][i])
        sel = np.zeros((8, 8, 128), np.float32)
        for e in range(8):
            sel[e, e, :] = 1.0
        d['sel'] = sel
    return d


def run_B(l, x_cur, h_cur, YT, mod, inp):
    moe = (l % 2 == 1)
    wd = prep_B_weights(l, inp)
    ins = []
    for b in range(4):
        for half in range(2):
            d = dict(wd)
            xt = np.concatenate([x_cur[b, half * 2048:(half + 1) * 2048], h_cur[b, half * 128:(half + 1) * 128]], 0)
            d['x'] = np.ascontiguousarray(xt.reshape(17, 128, 1024))
            toks = np.concatenate([np.arange(half * 2048, (half + 1) * 2048), 4096 + np.arange(half * 128, (half + 1) * 128)])
            d['yT'] = np.ascontiguousarray(YT[b][:, toks].reshape(16, 128, NTOK))
            ms = [mod[b], mod[4]]
            d['modcols'] = np.ascontiguousarray(np.stack([np.stack([_cols(m[k * 1024:(k + 1) * 1024]) for k in (0, 1, 3, 4)], 1) for m in ms], 1))
            d['modrows'] = _rep(np.stack([np.stack([m[2048:3072], m[5120:6144]], 0) for m in ms], 0))
            ins.append(d)
    res = run_bass_kernel_spmd(_prog('B', build_B, moe), ins, core_ids=list(range(8)))
    x_new = np.empty_like(x_cur)
    h_new = np.empty_like(h_cur)
    x1 = np.empty_like(x_cur)
    for b in range(4):
        for half in range(2):
            o = res.results[b * 2 + half]['xo'].reshape(NTOK, 1024)
            x_new[b, half * 2048:(half + 1) * 2048] = o[:2048]
            h_new[b, half * 128:(half + 1) * 128] = o[2048:]
            x1[b, half * 2048:(half + 1) * 2048] = res.results[b * 2 + half]['x1o'].reshape(NTOK, 1024)[:2048]
    return x_new, h_new, x1


NT_A = 34
NFB = 6
NTOK_A = NT_A * 128
TBLK_A = [(i * 512, 512) for i in range(8)] + [(4096, 256)]
PI = float(np.pi)


def emit_A(nc, P, Dm):
    x = Dm['x']
    modcols = Dm['modcols']
    ident = Dm['ident']
    wA = Dm['wA']
    wR = Dm['wR']
    wH = Dm['wH']
    wD = Dm['wD']
    caw = Dm['caw']
    hcw = Dm['hcw']
    hsk = Dm['hsk']
    ropec = Dm['ropec']
    ropes = Dm['ropes']
    qkn = Dm['qkn']
    rdec = Dm['rdec']
    rtab = Dm['rtab']
    rcol = Dm['rcol']
    featsT = Dm['featsT']
    featsTc = Dm['featsTc']
    hw1 = Dm['hw1']
    hw2 = Dm['hw2']
    hcols = Dm['hcols']
    hw3 = Dm['hw3']
    negt = Dm['negt']
    adel = Dm['adel']
    Fm = Dm['Fm']
    Gm = Dm['Gm']
    Fc = Dm['Fc']
    Gc = Dm['Gc']
    YT = Dm['YT']
    with contextlib.ExitStack() as st:
        P.stack = st
        B = Bufs()
        ids = P.sb("ids", [128, 128], F32)
        idb = P.sb("idb", [128, 128], BF16)
        mcol = P.sb("mcol", [128, 2, 2, 8], F32)
        P.dma('sp', lambda e: e.dma_start(out=ids[:], in_=ident), writes=[B['ids']])
        P.dma('sp', lambda e: e.dma_start(out=mcol[:], in_=modcols), writes=[B['mcol']])
        P.op('dve', lambda e: e.tensor_copy(out=idb[:], in_=ids[:]), reads=[B['ids']], writes=[B['idb']])
        P.op('dve', lambda e: e.tensor_scalar_add(out=mcol[:, :, 1, :], in0=mcol[:, :, 1, :], scalar1=1.0), reads=[B['mcol']], writes=[B['mcol']])
        pt = P.ps("pt", [128, 2, 512], F32)
        ptb = P.ps("ptb", [128, 1024], BF16)
        pa = P.ps("pa", [128, 2, 512], F32)
        pb_ = P.ps("pb", [128, 2, 512], F32)
        pc = P.ps("pc", [128, 512], F32)
        cnt = {'t': 0}

        def build_uT(uT, stk):
            P.stack = stk
            xb = [P.sb(f"xb{i}", [128, D], F32) for i in range(2)]
            for t in range(NT_A):
                ms = 1 if t >= 32 else 0
                xs = xb[t % 2]
                P.dma('sp', lambda e, t=t, xs=xs: e.dma_start(out=xs[:], in_=x[t]), writes=[B['xb', t % 2]])
                for f in range(8):
                    s = cnt['t'] % 2
                    cnt['t'] += 1
                    P.op('pe', lambda e, xs=xs, f=f, s=s: e.transpose(pt[:, s, 0:128], xs[:, f * 128:(f + 1) * 128], ids[:]),
                         reads=[B['xb', t % 2], B['ids']], writes=[B['pt', s]])
                    P.op('act', lambda e, t=t, f=f, s=s, ms=ms: e.activation(out=uT[:, f, t * 128:(t + 1) * 128], in_=pt[:, s, 0:128], func=AF.Identity,
                                                                           bias=mcol[:, ms, 0, f:f + 1], scale=mcol[:, ms, 1, f:f + 1]),
                         reads=[B['pt', s], B['mcol']], writes=[B['uT']])

        def inproj_fm(uT, wsb, wkey, col0, dst, dkey):
            for bi, (tok0, ntok) in enumerate(TBLK_A):
                s = bi % 2
                for kt in range(8):
                    P.op('pe', lambda e, kt=kt, s=s, tok0=tok0, ntok=ntok: e.matmul(pa[:, s, 0:ntok], lhsT=wsb[:, kt, col0:col0 + 128], rhs=uT[:, kt, tok0:tok0 + ntok],
                                                                                  start=(kt == 0), stop=(kt == 7)),
                         reads=[B[wkey], B['uT']], writes=[B['pa', s]])
                eng = 'act' if bi % 2 == 0 else 'dve'
                if eng == 'act':
                    P.op('act', lambda e, s=s, tok0=tok0, ntok=ntok: e.copy(out=dst[:, tok0:tok0 + ntok], in_=pa[:, s, 0:ntok]), reads=[B['pa', s]], writes=[B[dkey]])
                else:
                    P.op('dve', lambda e, s=s, tok0=tok0, ntok=ntok: e.tensor_copy(out=dst[:, tok0:tok0 + ntok], in_=pa[:, s, 0:ntok]), reads=[B['pa', s]], writes=[B[dkey]])

        def dwconv(z, zkey, wc, wkey, acc, akey, out, okey):
            P.op('dve', lambda e: e.tensor_scalar(out=acc[:], in0=z, scalar1=wc[:, 1:2], scalar2=wc[:, 3:4], op0=ALU.mult, op1=ALU.add),
                 reads=[B[zkey], B[wkey]], writes=[B[akey]])
            for (a, n) in ((0, 4096), (4096, 256)):
                P.op('dve', lambda e, a=a, n=n: e.scalar_tensor_tensor(out=acc[:, a + 1:a + n], in0=z[:, a:a + n - 1], scalar=wc[:, 0:1], in1=acc[:, a + 1:a + n],
                                                                      op0=ALU.mult, op1=ALU.add), reads=[B[zkey], B[wkey], B[akey]], writes=[B[akey]])
                P.op('dve', lambda e, a=a, n=n: e.scalar_tensor_tensor(out=acc[:, a:a + n - 1], in0=z[:, a + 1:a + n], scalar=wc[:, 2:3], in1=acc[:, a:a + n - 1],
                                                                      op0=ALU.mult, op1=ALU.add), reads=[B[zkey], B[wkey], B[akey]], writes=[B[akey]])
            if out is not None:
                P.op('pool', lambda e: e.tensor_copy(out=out, in_=acc[:]), reads=[B[akey]], writes=[B[okey]])

        import os as _os
        _dbg = _os.environ.get('A_DBG', '').split(',')
        P.mute = ('noH' in _dbg)
        sH = st.enter_context(contextlib.ExitStack())
        P.stack = sH
        zH = P.sb("zH", [128, 6, NTOK_A], BF16)
        with contextlib.ExitStack() as s0:
            P.stack = s0
            uT = P.sb("uT", [128, 8, NTOK_A], BF16)
            wHs = P.sb("wHs", [128, 8, 768], BF16)
            P.dma('pool', lambda e: e.dma_start(out=wHs[:], in_=wH), writes=[B['wHs']])
            build_uT(uT, s0)
            for c in range(6):
                inproj_fm(uT, wHs, 'wHs', c * 128, zH[:, c, :], ('zH', c))
            P.flush()
        P.stack = sH
        hsks = P.sb("hsks", [128, 2, 2], F32)
        P.dma('sp', lambda e: e.dma_start(out=hsks[:], in_=hsk), writes=[B['hsks']])
        with contextlib.ExitStack() as s1:
            P.stack = s1
            hcws = P.sb("hcws", [128, 6, 4], F32)
            acc = P.sb("acc", [128, NTOK_A], F32)
            P.dma('sp', lambda e: e.dma_start(out=hcws[:], in_=hcw), writes=[B['hcws']])
            for c in range(6):
                dwconv(zH[:, c, :], ('zH', c), hcws[:, c, :], 'hcws', acc, 'acc', zH[:, c, :], ('zH', c))
            P.flush()
        P.stack = sH
        HTc = P.sb("HTc", [128, 2, 512], BF16)
        Fcs = P.sb("Fcs", [128, 2, 512], BF16)
        Gcs = P.sb("Gcs", [128, 4, 256], BF16)
        P.dma('sp', lambda e: e.dma_start(out=Fcs[:], in_=Fc), writes=[B['Fcs']])
        P.dma('sp', lambda e: e.dma_start(out=Gcs[:], in_=Gc), writes=[B['Gcs']])
        Ft = [P.sb(f"Ft{i}", [128, 1024], BF16) for i in range(NFB)]
        fcnt = {'n': 0}
        h2T = P.sb("h2T", [64, 4352], F32)
        hcs = P.sb("hcs", [64, 6], F32)
        w3s = P.sb("w3s", [64, 2, 3, 256], F32)
        negts = P.sb("negts", [128, 34], F32)
        adels = P.sb("adels", [128, 2, 3, 256], F32)
        npi = P.sb("npi", [64, 1], F32)
        P.dma('sp', lambda e: e.dma_start(out=hcs[:, 0:4], in_=hcols), writes=[B['hcs']])
        P.dma('sp', lambda e: e.dma_start(out=w3s[:, :, 0:2, :], in_=hw3), writes=[B['w3s']])
        P.dma('sp', lambda e: e.dma_start(out=negts[:], in_=negt), writes=[B['negts']])
        P.dma('sp', lambda e: e.dma_start(out=adels[:, :, 0:2, :], in_=adel), writes=[B['adels']])
        P.op('dve', lambda e: e.tensor_scalar(out=w3s[:, :, 2, :], in0=w3s[:, :, 1, :], scalar1=-1.0, scalar2=None, op0=ALU.mult), reads=[B['w3s']], writes=[B['w3s']])
        P.op('dve', lambda e: e.tensor_copy(out=adels[:, :, 2, :], in_=adels[:, :, 1, :]), reads=[B['adels']], writes=[B['adels']])
        P.op('dve', lambda e: e.tensor_tensor(out=hcs[:, 4:6], in0=hcs[:, 0:2], in1=hcs[:, 2:4], op=ALU.mult), reads=[B['hcs']], writes=[B['hcs']])
        P.op('dve', lambda e: e.memset(npi[:], -PI), writes=[B['npi']])

        def sin_layer(src, skey, wmat, wkey2, kdim, li, dst, dkey2, argb):
            for bi, (tok0, ntok) in enumerate(TBLK_A):
                s = bi % 2
                P.op('pe', lambda e, s=s, tok0=tok0, ntok=ntok: e.matmul(pa[0:64, s, 0:ntok], lhsT=wmat[0:kdim, :], rhs=src[0:kdim, tok0:tok0 + ntok], start=True, stop=True),
                     reads=[B[skey], B[wkey2]], writes=[B['pa', s]])
                ab = argb[s]
                P.op('dve', lambda e, s=s, ntok=ntok, ab=ab: e.tensor_scalar(out=ab[:, 0:ntok], in0=pa[0:64, s, 0:ntok], scalar1=hcs[:, 2 + li:3 + li], scalar2=hcs[:, 4 + li:5 + li],
                                                                            op0=ALU.mult, op1=ALU.add), reads=[B['pa', s], B['hcs']], writes=[B['argb', s]])
                m1 = argb[2 + s]
                P.op('dve', lambda e, ntok=ntok, ab=ab, m1=m1: e.tensor_scalar(out=m1[:, 0:ntok], in0=ab[:, 0:ntok], scalar1=PI, scalar2=None, op0=ALU.is_gt),
                     reads=[B['argb', s]], writes=[B['argm', s]])
                P.op('dve', lambda e, ntok=ntok, ab=ab, m1=m1: e.scalar_tensor_tensor(out=ab[:, 0:ntok], in0=m1[:, 0:ntok], scalar=-2.0 * PI, in1=ab[:, 0:ntok], op0=ALU.mult, op1=ALU.add),
                     reads=[B['argb', s], B['argm', s]], writes=[B['argb', s]])
                P.op('dve', lambda e, ntok=ntok, ab=ab, m1=m1: e.tensor_scalar(out=m1[:, 0:ntok], in0=ab[:, 0:ntok], scalar1=-PI, scalar2=None, op0=ALU.is_lt),
                     reads=[B['argb', s], B['argm', s]], writes=[B['argm', s]])
                P.op('dve', lambda e, ntok=ntok, ab=ab, m1=m1: e.scalar_tensor_tensor(out=ab[:, 0:ntok], in0=m1[:, 0:ntok], scalar=2.0 * PI, in1=ab[:, 0:ntok], op0=ALU.mult, op1=ALU.add),
                     reads=[B['argb', s], B['argm', s]], writes=[B['argb', s]])
                P.op('act', lambda e, tok0=tok0, ntok=ntok, ab=ab: e.activation(out=dst[:, tok0:tok0 + ntok], in_=ab[:, 0:ntok], func=AF.Sin),
                     reads=[B['argb', s]], writes=[B[dkey2]])

        with contextlib.ExitStack() as sm:
            P.stack = sm
            h1T = P.sb("h1T", [64, 4352], F32)
            w2s = P.sb("w2s", [64, 64], F32)
            argb = [P.sb(f"argb{i}", [64, 512], F32) for i in range(4)]
            P.dma('sp', lambda e: e.dma_start(out=w2s[:], in_=hw2), writes=[B['w2s']])
            with contextlib.ExitStack() as sm2:
                P.stack = sm2
                fT = P.sb("fT", [33, 4352], F32)
                w1s = P.sb("w1s", [33, 64], F32)
                P.dma('sp', lambda e: e.dma_start(out=fT[:, 0:4096], in_=featsT), writes=[B['fT']])
                P.dma('sp', lambda e: e.dma_start(out=fT[:, 4096:4352], in_=featsTc), writes=[B['fT']])
                P.dma('sp', lambda e: e.dma_start(out=w1s[:], in_=hw1), writes=[B['w1s']])
                sin_layer(fT, 'fT', w1s, 'w1s', 33, 0, h1T, 'h1T', argb)
                P.flush()
            sin_layer(h1T, 'h1T', w2s, 'w2s', 64, 1, h2T, 'h2T', argb)
            P.flush()

        gcnt = {'n': 0}
        for o in range(2):
          with contextlib.ExitStack() as so:
            P.stack = so
            HT = P.sb(f"HT{o}", [128, 2, 8192], BF16)
            with contextlib.ExitStack() as sf:
                P.stack = sf
                env = [P.sb(f"env{i}", [128, 768], F32) for i in range(2)]
                filt = P.sb("filt", [128, 34, 768], BF16)
                for t in range(NT_A):
                    s = t % 2
                    ev = env[s]
                    P.op('act', lambda e, t=t, o=o, ev=ev: e.activation(out=ev[:], in_=adels[:, o, :, :].rearrange("p a b -> p (a b)"), func=AF.Exp, scale=negts[:, t:t + 1]),
                         reads=[B['adels'], B['negts']], writes=[B['env', s]])
                    P.op('pe', lambda e, t=t, o=o, s=s: e.matmul(pb_[:, s, 0:512], lhsT=h2T[:, t * 128:(t + 1) * 128], rhs=w3s[:, o, 0:2, :].rearrange("p a b -> p (a b)"), start=True, stop=True),
                         reads=[B['h2T'], B['w3s']], writes=[B['pb', s, 0]])
                    P.op('pe', lambda e, t=t, o=o, s=s: e.matmul(pa[:, s, 0:256], lhsT=h2T[:, t * 128:(t + 1) * 128], rhs=w3s[:, o, 2, :], start=True, stop=True),
                         reads=[B['h2T'], B['w3s']], writes=[B['pa', s]])
                    P.op('dve', lambda e, t=t, s=s, ev=ev: e.tensor_tensor(out=filt[:, t, 0:512], in0=pb_[:, s, 0:512], in1=ev[:, 0:512], op=ALU.mult),
                         reads=[B['pb', s, 0], B['env', s]], writes=[B['filt', t]])
                    P.op('dve', lambda e, t=t, s=s, ev=ev: e.tensor_tensor(out=filt[:, t, 512:768], in0=pa[:, s, 0:256], in1=ev[:, 512:768], op=ALU.mult),
                         reads=[B['pa', s], B['env', s]], writes=[B['filt', t]])
                P.op('dve', lambda e: e.memset(filt[0:1, 0, 256:768], 0.0), reads=[B['filt', 0]], writes=[B['filt', 0]])
                P.op('dve', lambda e: e.memset(filt[0:1, 32, 256:768], 0.0), reads=[B['filt', 32]], writes=[B['filt', 32]])
                for j in range(8):
                    for t in range(32):
                        fs = fcnt['n'] % NFB
                        fcnt['n'] += 1
                        P.dma('sp', lambda e, t=t, j=j, fs=fs: e.dma_start(out=Ft[fs][:], in_=Fm[t, :, j, :]), writes=[B['Ft', fs]])
                        for c in range(2):
                            P.op('pe', lambda e, t=t, c=c, fs=fs: e.matmul(pa[:, c, :], lhsT=filt[:, t, c * 128:(c + 1) * 128], rhs=Ft[fs][:, 0:512], start=(t == 0), stop=False),
                                 reads=[B['filt', t], B['Ft', fs]], writes=[B['pa', c]])
                            P.op('pe', lambda e, t=t, c=c, fs=fs: e.matmul(pa[:, c, :], lhsT=filt[:, t, 256 + c * 128:256 + (c + 1) * 128], rhs=Ft[fs][:, 0:512], start=False, stop=(t == 31)),
                                 reads=[B['filt', t], B['Ft', fs]], writes=[B['pa', c]])
                            P.op('pe', lambda e, t=t, c=c, fs=fs: e.matmul(pb_[:, c, :], lhsT=filt[:, t, c * 128:(c + 1) * 128], rhs=Ft[fs][:, 512:1024], start=(t == 0), stop=False),
                                 reads=[B['filt', t], B['Ft', fs]], writes=[B['pb', c, 0]])
                            P.op('pe', lambda e, t=t, c=c, fs=fs: e.matmul(pb_[:, c, :], lhsT=filt[:, t, 512 + c * 128:512 + (c + 1) * 128], rhs=Ft[fs][:, 512:1024], start=False, stop=(t == 31)),
                                 reads=[B['filt', t], B['Ft', fs]], writes=[B['pb', c, 0]])
                    for c in range(2):
                        P.op('act', lambda e, c=c, j=j: e.copy(out=HT[:, c, j * 1024:j * 1024 + 512], in_=pa[:, c, :]), reads=[B['pa', c]], writes=[B['HT', c]])
                        P.op('dve', lambda e, c=c, j=j: e.tensor_copy(out=HT[:, c, j * 1024 + 512:(j + 1) * 1024], in_=pb_[:, c, :]), reads=[B['pb', c, 0]], writes=[B['HT', c]])
                for c in range(2):
                    for t in range(2):
                        tt = 32 + t
                        P.op('pe', lambda e, t=t, tt=tt, c=c: e.matmul(pa[:, c, 0:256], lhsT=filt[:, tt, c * 128:(c + 1) * 128], rhs=Fcs[:, t, 0:256], start=(t == 0), stop=False),
                             reads=[B['filt', tt], B['Fcs']], writes=[B['pa', c]])
                        P.op('pe', lambda e, t=t, tt=tt, c=c: e.matmul(pa[:, c, 0:256], lhsT=filt[:, tt, 256 + c * 128:256 + (c + 1) * 128], rhs=Fcs[:, t, 0:256], start=False, stop=(t == 1)),
                             reads=[B['filt', tt], B['Fcs']], writes=[B['pa', c]])
                        P.op('pe', lambda e, t=t, tt=tt, c=c: e.matmul(pb_[:, c, 0:256], lhsT=filt[:, tt, c * 128:(c + 1) * 128], rhs=Fcs[:, t, 256:512], start=(t == 0), stop=False),
                             reads=[B['filt', tt], B['Fcs']], writes=[B['pb', c, 0]])
                        P.op('pe', lambda e, t=t, tt=tt, c=c: e.matmul(pb_[:, c, 0:256], lhsT=filt[:, tt, 512 + c * 128:512 + (c + 1) * 128], rhs=Fcs[:, t, 256:512], start=False, stop=(t == 1)),
                             reads=[B['filt', tt], B['Fcs']], writes=[B['pb', c, 0]])
                    P.op('act', lambda e, c=c: e.copy(out=HTc[:, c, 0:256], in_=pa[:, c, 0:256]), reads=[B['pa', c]], writes=[B['HTc', c]])
                    P.op('dve', lambda e, c=c: e.tensor_copy(out=HTc[:, c, 256:512], in_=pb_[:, c, 0:256]), reads=[B['pb', c, 0]], writes=[B['HTc', c]])
                P.flush()
            with contextlib.ExitStack() as s3:
                P.stack = s3
                stm = P.sb("stm", [128, NT_A, 256], BF16)
                YhT = P.sb("YhT", [128, 2, 1024], BF16)
                Ytm = P.sb("Ytm", [128, 68, 256], BF16)
                Gt = [P.sb(f"Gt{i}", [128, 1024], BF16) for i in range(NFB)]
                pr1 = P.sb("pr1", [128, 512], F32)
                pr2 = P.sb("pr2", [128, 512], F32)
                yst = P.sb("yst", [128, 512], F32)
                ysk = P.sb("ysk", [128, 512], F32)
                for t in range(NT_A):
                    for c in range(2):
                        P.op('pe', lambda e, t=t, c=c: e.transpose(ptb[:, c * 128:(c + 1) * 128], zH[:, c, t * 128:(t + 1) * 128], idb[:]),
                             reads=[B['zH', c], B['idb']], writes=[B['ptb']])
                    if t % 2 == 0:
                        P.op('act', lambda e, t=t: e.copy(out=stm[:, t, :], in_=ptb[:, 0:256]), reads=[B['ptb']], writes=[B['stm', t]])
                    else:
                        P.op('dve', lambda e, t=t: e.tensor_copy(out=stm[:, t, :], in_=ptb[:, 0:256]), reads=[B['ptb']], writes=[B['stm', t]])

                def product(c, ucs, uss, hc, hs, n, keyc, keys_):
                    P.op('dve', lambda e: e.tensor_tensor(out=pr1[:, 0:n], in0=ucs, in1=hc, op=ALU.mult), reads=[B[keyc]], writes=[B['pr1']])
                    P.op('dve', lambda e: e.tensor_tensor(out=pr2[:, 0:n], in0=uss, in1=hs, op=ALU.mult), reads=[B[keys_]], writes=[B['pr2']])
                    P.op('pool', lambda e: e.tensor_tensor(out=YhT[:, c, 0:n], in0=pr1[:, 0:n], in1=pr2[:, 0:n], op=ALU.subtract), reads=[B['pr1'], B['pr2']], writes=[B['YhT', c]])
                    P.op('dve', lambda e: e.tensor_tensor(out=pr1[:, 0:n], in0=ucs, in1=hs, op=ALU.mult), reads=[B[keyc], B['pr1']], writes=[B['pr1']])
                    P.op('dve', lambda e: e.tensor_tensor(out=pr2[:, 0:n], in0=uss, in1=hc, op=ALU.mult), reads=[B[keys_], B['pr2']], writes=[B['pr2']])
                    P.op('pool', lambda e: e.tensor_tensor(out=YhT[:, c, 512:512 + n], in0=pr1[:, 0:n], in1=pr2[:, 0:n], op=ALU.add), reads=[B['pr1'], B['pr2']], writes=[B['YhT', c]])

                def ytrans(kts, cols):
                    for kt, c0 in zip(kts, cols):
                        for c in range(2):
                            P.op('pe', lambda e, c=c, c0=c0: e.transpose(ptb[:, c * 128:(c + 1) * 128], YhT[:, c, c0:c0 + 128], idb[:]),
                                 reads=[B['YhT', c], B['idb']], writes=[B['ptb']])
                        if kt % 2 == 0:
                            P.op('act', lambda e, kt=kt: e.copy(out=Ytm[:, kt, :], in_=ptb[:, 0:256]), reads=[B['ptb']], writes=[B['Ytm', kt]])
                        else:
                            P.op('dve', lambda e, kt=kt: e.tensor_copy(out=Ytm[:, kt, :], in_=ptb[:, 0:256]), reads=[B['ptb']], writes=[B['Ytm', kt]])

                for j in range(8):
                    for t in range(32):
                        fs = fcnt['n'] % NFB
                        fcnt['n'] += 1
                        P.dma('sp', lambda e, t=t, j=j, fs=fs: e.dma_start(out=Ft[fs][:], in_=Fm[t, :, j, :]), writes=[B['Ft', fs]])
                        for c in range(2):
                            P.op('pe', lambda e, t=t, c=c, fs=fs: e.matmul(pa[:, c, :], lhsT=stm[:, t, c * 128:(c + 1) * 128], rhs=Ft[fs][:, 0:512], start=(t == 0), stop=(t == 31)),
                                 reads=[B['stm', t], B['Ft', fs]], writes=[B['pa', c]])
                            P.op('pe', lambda e, t=t, c=c, fs=fs: e.matmul(pb_[:, c, :], lhsT=stm[:, t, c * 128:(c + 1) * 128], rhs=Ft[fs][:, 512:1024], start=(t == 0), stop=(t == 31)),
                                 reads=[B['stm', t], B['Ft', fs]], writes=[B['pb', c, 0]])
                    for c in range(2):
                        product(c, pa[:, c, :], pb_[:, c, :], HT[:, c, j * 1024:j * 1024 + 512], HT[:, c, j * 1024 + 512:(j + 1) * 1024], 512, ('pa', c), ('pb', c, 0))
                    ytrans([j * 4 + i for i in range(4)] + [32 + j * 4 + i for i in range(4)], [i * 128 for i in range(4)] + [512 + i * 128 for i in range(4)])
                for c in range(2):
                    for t in range(2):
                        P.op('pe', lambda e, t=t, c=c: e.matmul(pa[:, c, 0:256], lhsT=stm[:, 32 + t, c * 128:(c + 1) * 128], rhs=Fcs[:, t, 0:256], start=(t == 0), stop=(t == 1)),
                             reads=[B['stm', 32 + t], B['Fcs']], writes=[B['pa', c]])
                        P.op('pe', lambda e, t=t, c=c: e.matmul(pb_[:, c, 0:256], lhsT=stm[:, 32 + t, c * 128:(c + 1) * 128], rhs=Fcs[:, t, 256:512], start=(t == 0), stop=(t == 1)),
                             reads=[B['stm', 32 + t], B['Fcs']], writes=[B['pb', c, 0]])
                    product(c, pa[:, c, 0:256], pb_[:, c, 0:256], HTc[:, c, 0:256], HTc[:, c, 256:512], 256, ('pa', c), ('pb', c, 0))
                ytrans([64, 65, 66, 67], [0, 128, 512, 640])

                def finish_blk(c, ps_ap, pkey, tok0, n, scale):
                    P.op('pool', lambda e: e.tensor_scalar(out=ysk[:, 0:n], in0=zH[:, c, tok0:tok0 + n], scalar1=hsks[:, o, c:c + 1], scalar2=None, op0=ALU.mult),
                         reads=[B['zH', c], B['hsks']], writes=[B['ysk']])
                    P.op('dve', lambda e: e.scalar_tensor_tensor(out=yst[:, 0:n], in0=ps_ap, scalar=scale, in1=ysk[:, 0:n], op0=ALU.mult, op1=ALU.add),
                         reads=[B[pkey], B['ysk']], writes=[B['yst']])
                    if o == 0:
                        P.op('pool', lambda e: e.tensor_tensor(out=zH[:, c, tok0:tok0 + n], in0=yst[:, 0:n], in1=zH[:, 2 + c, tok0:tok0 + n], op=ALU.mult),
                             reads=[B['yst'], B['zH', 2 + c]], writes=[B['zH', c]])
                    else:
                        P.op('pool', lambda e: e.tensor_tensor(out=yst[:, 0:n], in0=yst[:, 0:n], in1=zH[:, 4 + c, tok0:tok0 + n], op=ALU.mult),
                             reads=[B['yst'], B['zH', 4 + c]], writes=[B['yst']])
                        P.dma('sp', lambda e: e.dma_start(out=YT[4 + c, :, tok0:tok0 + n], in_=yst[:, 0:n]), reads=[B['yst']], is_output=True)

                for ps_ in range(4):
                    for kt in range(64):
                        gs = gcnt['n'] % NFB
                        gcnt['n'] += 1
                        P.dma('sp', lambda e, ps_=ps_, kt=kt, gs=gs: e.dma_start(out=Gt[gs][:], in_=Gm[ps_, kt]), writes=[B['Gt', gs]])
                        for c in range(2):
                            for nb in range(2):
                                acc_ap = (pa if c == 0 else pb_)[:, nb, :]
                                P.op('pe', lambda e, kt=kt, c=c, nb=nb, gs=gs, acc_ap=acc_ap: e.matmul(acc_ap, lhsT=Ytm[:, kt, c * 128:(c + 1) * 128], rhs=Gt[gs][:, nb * 512:(nb + 1) * 512],
                                                                                                  start=(kt == 0), stop=(kt == 63)),
                                     reads=[B['Ytm', kt], B['Gt', gs]], writes=[B['pa', nb] if c == 0 else B['pb', nb, 0]])
                    for c in range(2):
                        for nb in range(2):
                            finish_blk(c, (pa if c == 0 else pb_)[:, nb, :], ('pa', nb) if c == 0 else ('pb', nb, 0), ps_ * 1024 + nb * 512, 512, 2.0 / 8192.0)
                for c in range(2):
                    for kt in range(4):
                        P.op('pe', lambda e, kt=kt, c=c: e.matmul(pc[:, 0:256], lhsT=Ytm[:, 64 + kt, c * 128:(c + 1) * 128], rhs=Gcs[:, kt, :], start=(kt == 0), stop=(kt == 3)),
                             reads=[B['Ytm', 64 + kt], B['Gcs']], writes=[B['pc']])
                    finish_blk(c, pc[:, 0:256], 'pc', 4096, 256, 2.0 / 512.0)
                P.flush()
        sH.close()
        P.stack = st
        P.mute = ('noR' in _dbg)
        build_A_rest(nc, P, B, st, x, wA, wR, wD, caw, ropec, ropes, qkn, rdec, rtab, rcol, YT, ids, idb, mcol, pt, ptb, pa, pb_, pc, build_uT, inproj_fm, dwconv)


def build_A():
    nc = bass.Bass("TRN2", target_bir_lowering=False)
    x = _din(nc, "x", [NT_A, 128, D])
    modcols = _din(nc, "modcols", [128, 2, 2, 8])
    ident = _din(nc, "ident", [128, 128])
    wA = _din(nc, "wA", [128, 8, 768])
    wR = _din(nc, "wR", [128, 8, 1024])
    wH = _din(nc, "wH", [128, 8, 768])
    wD = _din(nc, "wD", [128, 8, 512])
    caw = _din(nc, "caw", [128, 2, 4])
    hcw = _din(nc, "hcw", [128, 6, 4])
    hsk = _din(nc, "hsk", [128, 2, 2])
    ropec = _din(nc, "ropec", [128, 32, 64])
    ropes = _din(nc, "ropes", [128, 32, 64])
    qkn = _din(nc, "qkn", [128, 2, 128])
    rdec = _din(nc, "rdec", [128, 4])
    rtab = _din(nc, "rtab", [128, 6, 128])
    rcol = _din(nc, "rcol", [128, 4])
    featsT = _din(nc, "featsT", [33, 4096])
    featsTc = _din(nc, "featsTc", [33, 256])
    hw1 = _din(nc, "hw1", [33, 64])
    hw2 = _din(nc, "hw2", [64, 64])
    hcols = _din(nc, "hcols", [64, 4])
    hw3 = _din(nc, "hw3", [64, 2, 2, 256])
    negt = _din(nc, "negt", [128, 34])
    adel = _din(nc, "adel", [128, 2, 2, 256])
    Fm = _din(nc, "Fm", [32, 128, 8, 1024], BF16)
    Gm = _din(nc, "Gm", [4, 64, 128, 1024], BF16)
    Fc = _din(nc, "Fc", [128, 2, 512], BF16)
    Gc = _din(nc, "Gc", [128, 4, 256], BF16)
    YT = _dout(nc, "YT", [8, 128, NTOK_A])

    Dm = dict(x=x, modcols=modcols, ident=ident, wA=wA, wR=wR, wH=wH, wD=wD, caw=caw, hcw=hcw, hsk=hsk, ropec=ropec, ropes=ropes, qkn=qkn, rdec=rdec, rtab=rtab, rcol=rcol, featsT=featsT, featsTc=featsTc, hw1=hw1, hw2=hw2, hcols=hcols, hw3=hw3, negt=negt, adel=adel, Fm=Fm, Gm=Gm, Fc=Fc, Gc=Gc, YT=YT)
    with contextlib.ExitStack() as st0:
        P = Prog(nc, st0)
        emit_A(nc, P, Dm)
    return nc


def build_A_rest(nc, P, B, st, x, wA, wR, wD, caw, ropec, ropes, qkn, rdec, rtab, rcol, YT, ids, idb, mcol, pt, ptb, pa, pb_, pc, build_uT, inproj_fm, dwconv):
    SC = 128.0 ** -0.5
    sG = st.enter_context(contextlib.ExitStack())
    P.stack = sG
    uT = P.sb("uT2", [128, 8, NTOK_A], BF16)
    rc = P.sb("rc", [128, 32, 64], F32)
    rs = P.sb("rs", [128, 32, 64], F32)
    P.dma('sp', lambda e: e.dma_start(out=rc[:], in_=ropec), writes=[B['rope']])
    P.dma('sp', lambda e: e.dma_start(out=rs[:], in_=ropes), writes=[B['rope']])
    with contextlib.ExitStack() as s0:
        build_uT(uT, s0)
        P.flush()

    def rope(src, skey, t, tmp, tkey):
        for ax in range(2):
            x1 = src[:, ax * 64:ax * 64 + 32]
            x2 = src[:, ax * 64 + 32:ax * 64 + 64]
            c_ = rc[:, t, ax * 32:(ax + 1) * 32]
            s_ = rs[:, t, ax * 32:(ax + 1) * 32]
            P.op('dve', lambda e, x1=x1, c_=c_: e.tensor_tensor(out=tmp[:, 0, :], in0=x1, in1=c_, op=ALU.mult), reads=[B[skey], B['rope']], writes=[B[tkey, 0]])
            P.op('dve', lambda e, x2=x2, s_=s_: e.tensor_tensor(out=tmp[:, 1, :], in0=x2, in1=s_, op=ALU.mult), reads=[B[skey], B['rope']], writes=[B[tkey, 1]])
            P.op('dve', lambda e, x2=x2, c_=c_: e.tensor_tensor(out=tmp[:, 2, :], in0=x2, in1=c_, op=ALU.mult), reads=[B[skey], B['rope']], writes=[B[tkey, 2]])
            P.op('dve', lambda e, x1=x1, s_=s_: e.tensor_tensor(out=tmp[:, 3, :], in0=x1, in1=s_, op=ALU.mult), reads=[B[skey], B['rope']], writes=[B[tkey, 3]])
            P.op('dve', lambda e, x1=x1: e.tensor_tensor(out=x1, in0=tmp[:, 0, :], in1=tmp[:, 1, :], op=ALU.subtract), reads=[B[tkey, 0], B[tkey, 1], B[skey]], writes=[B[skey]])
            P.op('dve', lambda e, x2=x2: e.tensor_tensor(out=x2, in0=tmp[:, 2, :], in1=tmp[:, 3, :], op=ALU.add), reads=[B[tkey, 2], B[tkey, 3], B[skey]], writes=[B[skey]])

    import os as _os
    _dbg = _os.environ.get('A_DBG', '').split(',')
    _base = P.mute
    P.mute = _base or ('noS' in _dbg)
    with contextlib.ExitStack() as s1:
        P.stack = s1
        wAs = P.sb("wAs", [128, 8, 768], BF16)
        caws = P.sb("caws", [128, 2, 4], F32)
        P.dma('pool', lambda e: e.dma_start(out=wAs[:], in_=wA), writes=[B['wAs']])
        P.dma('sp', lambda e: e.dma_start(out=caws[:], in_=caw), writes=[B['caws']])
        zA = P.sb("zA", [128, 3, NTOK_A], BF16)
        pp_ = P.sb("pp_", [128, NTOK_A], BF16)
        acc = P.sb("accA", [128, NTOK_A], F32)
        for c in range(2):
            for g in range(3):
                inproj_fm(uT, wAs, 'wAs', g * 256 + c * 128, zA[:, g, :], ('zA', g))
            P.op('pool', lambda e: e.tensor_tensor(out=pp_[:], in0=zA[:, 1, :], in1=zA[:, 2, :], op=ALU.mult), reads=[B['zA', 1], B['zA', 2]], writes=[B['pp_']])
            dwconv(pp_[:], 'pp_', caws[:, c, :], 'caws', acc, 'accA', None, None)
            P.op('dve', lambda e: e.tensor_tensor(out=acc[:], in0=acc[:], in1=zA[:, 0, :], op=ALU.mult), reads=[B['accA'], B['zA', 0]], writes=[B['accA']])
            P.dma('sp', lambda e, c=c: e.dma_start(out=YT[0 + c], in_=acc[:]), reads=[B['accA']], is_output=True)
        P.flush()

    P.mute = _base or ('noD' in _dbg)
    with contextlib.ExitStack() as s2:
        P.stack = s2
        wDs = P.sb("wDs", [128, 8, 512], BF16)
        qkns = P.sb("qkns", [128, 2, 128], F32)
        P.dma('pool', lambda e: e.dma_start(out=wDs[:], in_=wD), writes=[B['wDs']])
        P.dma('sp', lambda e: e.dma_start(out=qkns[:], in_=qkn), writes=[B['qkns']])
        qT = P.sb("qT", [128, 2, NTOK_A], BF16)
        kT = P.sb("kT", [128, NTOK_A], BF16)
        vtm = P.sb("vtm", [128, NT_A, 128], BF16)
        onesb = P.sb("onesb", [128, 128], BF16)
        P.op('dve', lambda e: e.memset(onesb[:], 1.0), writes=[B['onesb']])
        qn = [P.sb(f"qn{i}", [128, 3, 128], F32) for i in range(2)]
        qb = [P.sb(f"qb{i}", [128, 3, 128], BF16) for i in range(2)]
        sq3 = P.sb("sq3", [128, 384], F32)
        ss = [P.sb(f"ss{i}", [128, 4], F32) for i in range(2)]
        tmp = P.sb("tmpr", [128, 4, 32], F32)
        for t in range(NT_A):
            s = t % 2
            for kt in range(8):
                P.op('pe', lambda e, t=t, kt=kt, s=s: e.matmul(pa[:, s, :], lhsT=uT[:, kt, t * 128:(t + 1) * 128], rhs=wDs[:, kt, :], start=(kt == 0), stop=(kt == 7)),
                     reads=[B['uT'], B['wDs']], writes=[B['pa', s]])
            P.op('act', lambda e, s=s: e.activation(out=sq3[:], in_=pa[:, s, 0:384], func=AF.Square), reads=[B['pa', s]], writes=[B['sq3']])
            P.op('dve', lambda e, s=s: e.reduce_sum(out=ss[s][:, 0:3], in_=sq3[:].rearrange("p (h d) -> p h d", h=3), axis=AX.X),
                 reads=[B['sq3']], writes=[B['ss', s, 0], B['ss', s, 1], B['ss', s, 2]])
            P.op('dve', lambda e, s=s: e.tensor_scalar(out=ss[s][:, 0:3], in0=ss[s][:, 0:3], scalar1=1.0 / 128.0, scalar2=1e-6, op0=ALU.mult, op1=ALU.add),
                 reads=[B['ss', s, h] for h in range(3)], writes=[B['ss', s, 'a']])
            P.op('act', lambda e, s=s: e.sqrt(out=ss[s][:, 0:3], in_=ss[s][:, 0:3]), reads=[B['ss', s, 'a']], writes=[B['ss', s, 'a']])
            P.op('dve', lambda e, s=s: e.reciprocal(out=ss[s][:, 0:3], in_=ss[s][:, 0:3]), reads=[B['ss', s, 'a']], writes=[B['ss', s, 'a']])
            for h in range(3):
                P.op('dve', lambda e, h=h, s=s: e.scalar_tensor_tensor(out=qn[s][:, h, :], in0=pa[:, s, h * 128:(h + 1) * 128], scalar=ss[s][:, h:h + 1], in1=qkns[:, 0 if h < 2 else 1, :],
                                                                     op0=ALU.mult, op1=ALU.mult), reads=[B['pa', s], B['ss', s, 'a'], B['qkns']], writes=[B['qn', s, h]])
                if t < 32:
                    rope(qn[s][:, h, :], ('qn', s, h), t, tmp, 'tmpr')
                P.op('pool', lambda e, h=h, s=s: e.tensor_copy(out=qb[s][:, h, :], in_=qn[s][:, h, :]), reads=[B['qn', s, h]], writes=[B['qb', s, h]])
            P.op('act', lambda e, t=t, s=s: e.copy(out=vtm[:, t, :], in_=pa[:, s, 384:512]), reads=[B['pa', s]], writes=[B['vtm', t]])
            for h in range(3):
                P.op('pe', lambda e, h=h, s=s: e.transpose(ptb[:, h * 128:(h + 1) * 128], qb[s][:, h, :], idb[:]), reads=[B['qb', s, h], B['idb']], writes=[B['ptb']])
            P.op('dve', lambda e, t=t: e.tensor_copy(out=qT[:, :, t * 128:(t + 1) * 128], in_=ptb[:, 0:256].rearrange("p (h d) -> p h d", h=2)), reads=[B['ptb']], writes=[B['qT']])
            P.op('act', lambda e, t=t: e.copy(out=kT[:, t * 128:(t + 1) * 128], in_=ptb[:, 256:384]), reads=[B['ptb']], writes=[B['kT']])
        if 'dumpQK' in _dbg:
            dq = P.sb("dq", [128, NTOK_A], F32)
            for i_, src_ in enumerate((qT[:, 0, :], qT[:, 1, :], kT[:])):
                P.op('dve', lambda e, src_=src_: e.tensor_copy(out=dq[:], in_=src_), reads=[B['qT'], B['kT']], writes=[B['dq']])
                P.dma('sp', lambda e, i_=i_: e.dma_start(out=YT[5 + i_], in_=dq[:]), reads=[B['dq']], writes=[B['dqo', i_]], is_output=True)
            P.op('dve', lambda e: e.tensor_copy(out=dq[:].rearrange("p (t d) -> p t d", d=128), in_=vtm[:]), reads=[B['vtm', t_] for t_ in range(NT_A)], writes=[B['dq']])
            P.dma('sp', lambda e: e.dma_start(out=YT[4], in_=dq[:]), reads=[B['dq']], is_output=True)
        P.mute = P.mute or ('noD2' in _dbg)
        pT = [P.sb(f"pT{i}", [128, 512], BF16) for i in range(3)]
        rd = P.sb("rd", [128, 512], F32)
        yo = [P.sb(f"yo{i}", [128, 512], F32) for i in range(2)]
        pcnt = 0
        ocnt = 0
        for h in range(2):
            for bi, (tok0, ntok) in enumerate(TBLK_A):
                if 'cOne' in _dbg and (h, bi) != (0, 0):
                    continue
                ktiles = list(range(34)) if bi < 8 else [32, 33]
                nkt = len(ktiles)
                slots = []
                for ki, kt in enumerate(ktiles):
                    slots.append((pcnt % 2, pcnt % 3))
                    pcnt += 1

                def qk_exp(ki):
                    kt = ktiles[ki]
                    s, p3 = slots[ki]
                    P.op('pe', lambda e, kt=kt, h=h, s=s, tok0=tok0, ntok=ntok: e.matmul(pa[:, s, 0:ntok], lhsT=kT[:, kt * 128:(kt + 1) * 128], rhs=qT[:, h, tok0:tok0 + ntok], start=True, stop=True),
                         reads=[B['kT'], B['qT']], writes=[B['pa', s]])
                    P.op('act', lambda e, s=s, p3=p3, ntok=ntok: e.activation(out=pT[p3][:, 0:ntok], in_=pa[:, s, 0:ntok], func=AF.Exp, scale=SC), reads=[B['pa', s]], writes=[B['pT', p3]])

                def pv(ki):
                    kt = ktiles[ki]
                    s, p3 = slots[ki]
                    P.op('pe', lambda e, kt=kt, p3=p3, ntok=ntok, ki=ki, nkt=nkt: e.matmul(pb_[:, 0, 0:ntok], lhsT=vtm[:, kt, :], rhs=pT[p3][:, 0:ntok], start=(ki == 0), stop=(ki == nkt - 1)),
                         reads=[B['vtm', kt], B['pT', p3]], writes=[B['pb', 0, 0]])
                    P.op('pe', lambda e, p3=p3, ntok=ntok, ki=ki, nkt=nkt: e.matmul(pb_[:, 1, 0:ntok], lhsT=onesb[:], rhs=pT[p3][:, 0:ntok], start=(ki == 0), stop=(ki == nkt - 1)),
                         reads=[B['onesb'], B['pT', p3]], writes=[B['pb', 1, 0]])

                qk_exp(0)
                for ki in range(1, nkt):
                    qk_exp(ki)
                    pv(ki - 1)
                pv(nkt - 1)
                if 'cQK' in _dbg:
                    continue
                P.op('dve', lambda e, ntok=ntok: e.reciprocal(out=rd[:, 0:ntok], in_=pb_[:, 1, 0:ntok]), reads=[B['pb', 1, 0]], writes=[B['rd']])
                y = yo[ocnt % 2]
                yk = ('yo', ocnt % 2)
                ocnt += 1
                P.op('dve', lambda e, ntok=ntok, y=y: e.tensor_tensor(out=y[:, 0:ntok], in0=pb_[:, 0, 0:ntok], in1=rd[:, 0:ntok], op=ALU.mult), reads=[B['pb', 0, 0], B['rd']], writes=[B[yk]])
                P.dma('sp', lambda e, h=h, tok0=tok0, ntok=ntok, y=y: e.dma_start(out=YT[6 + h, :, tok0:tok0 + ntok], in_=y[:, 0:ntok]), reads=[B[yk]], is_output=True)
        P.flush()

    P.mute = _base or ('noT' in _dbg)
    with contextlib.ExitStack() as s3:
        P.stack = s3
        wRs = P.sb("wRs", [128, 8, 1024], BF16)
        P.dma('pool', lambda e: e.dma_start(out=wRs[:], in_=wR), writes=[B['wRs']])
        rds = P.sb("rds", [128, 4], F32)
        lgam = P.sb("lgam", [128, 4], F32)
        rtabs = P.sb("rtabs", [128, 6, 128], F32)
        rcols = P.sb("rcols", [128, 4], F32)
        P.dma('sp', lambda e: e.dma_start(out=rds[:], in_=rdec), writes=[B['rds']])
        P.dma('sp', lambda e: e.dma_start(out=rtabs[:], in_=rtab), writes=[B['rtabs']])
        P.dma('sp', lambda e: e.dma_start(out=rcols[:], in_=rcol), writes=[B['rcols']])
        P.op('act', lambda e: e.activation(out=lgam[:], in_=rds[:], func=AF.Exp), reads=[B['rds']], writes=[B['lgam']])
        P.op('dve', lambda e: e.tensor_scalar(out=lgam[:], in0=lgam[:], scalar1=-1.0, scalar2=None, op0=ALU.mult), reads=[B['lgam']], writes=[B['lgam']])
        mask = P.sb("mask", [128, 128], F32)
        mtmp = P.sb("mtmpR", [128, 128], F32)
        qdf = P.sb("qdf", [128, 128], F32)
        qdb = P.sb("qdb", [128, 128], F32)
        kdc = P.sb("kdc", [128, 4], F32)
        qT = P.sb("rqT", [128, NTOK_A], BF16)
        kT = P.sb("rkT", [128, NTOK_A], BF16)
        qdfT = P.sb("qdfT", [128, NTOK_A], BF16)
        qdbT = P.sb("qdbT", [128, NTOK_A], BF16)
        kdf = P.sb("kdf", [128, NT_A, 128], BF16)
        kdb = P.sb("kdb", [128, NT_A, 128], BF16)
        vtm = P.sb("rvtm", [128, NT_A, 128], BF16)
        sgt = P.sb("sgt", [128, NT_A, 128], BF16)
        Sf = P.sb("Sf", [128, NT_A, 128], BF16)
        Sb = P.sb("Sb", [128, NT_A, 128], BF16)
        stf = [P.sb(f"stf{i}", [128, 128], F32) for i in range(2)]
        qk = [P.sb(f"rqk{i}", [128, 2, 128], F32) for i in range(2)]
        qkb = [P.sb(f"rqkb{i}", [128, 2, 128], BF16) for i in range(2)]
        tmp = P.sb("tmpr2", [128, 4, 32], F32)
        smT = [P.sb(f"smT{i}", [128, 128], BF16) for i in range(2)]
        stt = P.sb("rstt", [128, 6], F32)
        mv = P.sb("rmv", [128, 2], F32)
        rstd = P.sb("rrstd", [128, 1], F32)
        on = [P.sb(f"on{i}", [128, 128], F32) for i in range(2)]
        ob = [P.sb(f"ob{i}", [128, 128], BF16) for i in range(2)]
        ysg = [P.sb(f"rysg{i}", [128, 512], F32) for i in range(2)]
        for h in range(2):
            lf = lgam[:, h:h + 1]
            lb = lgam[:, 2 + h:3 + h]
            P.op('act', lambda e, lf=lf: e.activation(out=mask[:], in_=rtabs[:, 0, :], func=AF.Exp, scale=lf), reads=[B['rtabs'], B['lgam']], writes=[B['mask']])
            P.op('dve', lambda e: e.tensor_tensor(out=mask[:], in0=mask[:], in1=rtabs[:, 1, :], op=ALU.mult), reads=[B['mask'], B['rtabs']], writes=[B['mask']])
            P.op('act', lambda e, lb=lb: e.activation(out=mtmp[:], in_=rtabs[:, 2, :], func=AF.Exp, scale=lb), reads=[B['rtabs'], B['lgam']], writes=[B['mtmpR']])
            P.op('dve', lambda e: e.tensor_tensor(out=mtmp[:], in0=mtmp[:], in1=rtabs[:, 3, :], op=ALU.mult), reads=[B['mtmpR'], B['rtabs']], writes=[B['mtmpR']])
            P.op('dve', lambda e: e.tensor_tensor(out=mask[:], in0=mask[:], in1=mtmp[:], op=ALU.add), reads=[B['mask'], B['mtmpR']], writes=[B['mask']])
            P.op('act', lambda e, lf=lf: e.activation(out=qdf[:], in_=rtabs[:, 4, :], func=AF.Exp, scale=lf), reads=[B['rtabs'], B['lgam']], writes=[B['qdf']])
            P.op('act', lambda e, lb=lb: e.activation(out=qdb[:], in_=rtabs[:, 5, :], func=AF.Exp, scale=lb), reads=[B['rtabs'], B['lgam']], writes=[B['qdb']])
            P.op('act', lambda e, lf=lf: e.activation(out=kdc[:, 0:1], in_=rcols[:, 0:1], func=AF.Exp, scale=lf), reads=[B['rcols'], B['lgam']], writes=[B['kdc', 0]])
            P.op('act', lambda e, lb=lb: e.activation(out=kdc[:, 1:2], in_=rcols[:, 1:2], func=AF.Exp, scale=lb), reads=[B['rcols'], B['lgam']], writes=[B['kdc', 1]])
            P.op('act', lambda e, lf=lf: e.activation(out=kdc[:, 2:3], in_=rcols[:, 2:3], func=AF.Exp, scale=lf), reads=[B['rcols'], B['lgam']], writes=[B['kdc', 2]])
            P.op('act', lambda e, lb=lb: e.activation(out=kdc[:, 3:4], in_=rcols[:, 2:3], func=AF.Exp, scale=lb), reads=[B['rcols'], B['lgam']], writes=[B['kdc', 3]])
            kd = [B['kdc', i] for i in range(4)]
            for t in range(NT_A):
                s = t % 2
                for g in range(4):
                    for kt in range(8):
                        P.op('pe', lambda e, t=t, kt=kt, s=s, g=g, h=h: e.matmul(pa[:, s, g * 128:(g + 1) * 128], lhsT=uT[:, kt, t * 128:(t + 1) * 128], rhs=wRs[:, kt, g * 256 + h * 128:g * 256 + (h + 1) * 128],
                                                                        start=(kt == 0), stop=(kt == 7)), reads=[B['uT'], B['wRs']], writes=[B['pa', s]])
                P.op('act', lambda e, s=s: e.copy(out=qk[s][:, 0, :], in_=pa[:, s, 0:128]), reads=[B['pa', s]], writes=[B['rqk', s, 0]])
                P.op('act', lambda e, s=s: e.activation(out=qk[s][:, 1, :], in_=pa[:, s, 128:256], func=AF.Copy, scale=SC), reads=[B['pa', s]], writes=[B['rqk', s, 1]])
                P.op('act', lambda e, s=s, t=t: e.copy(out=vtm[:, t, :], in_=pa[:, s, 256:384]), reads=[B['pa', s]], writes=[B['rvtm', t]])
                P.op('act', lambda e, s=s, t=t: e.activation(out=sgt[:, t, :], in_=pa[:, s, 384:512], func=AF.Silu), reads=[B['pa', s]], writes=[B['sgt', t]])
                for j in range(2):
                    if t < 32:
                        rope(qk[s][:, j, :], ('rqk', s, j), t, tmp, 'tmpr2')
                    P.op('pool', lambda e, s=s, j=j: e.tensor_copy(out=qkb[s][:, j, :], in_=qk[s][:, j, :]), reads=[B['rqk', s, j]], writes=[B['rqkb', s, j]])
                P.op('dve', lambda e, s=s, t=t: e.tensor_scalar(out=kdf[:, t, :], in0=qk[s][:, 1, :], scalar1=kdc[:, 0:1], scalar2=None, op0=ALU.mult), reads=[B['rqk', s, 1], kd[0]], writes=[B['kdf', t]])
                P.op('dve', lambda e, s=s, t=t: e.tensor_scalar(out=kdb[:, t, :], in0=qk[s][:, 1, :], scalar1=kdc[:, 1:2], scalar2=None, op0=ALU.mult), reads=[B['rqk', s, 1], kd[1]], writes=[B['kdb', t]])
                for j in range(2):
                    P.op('pe', lambda e, s=s, j=j: e.transpose(ptb[:, j * 128:(j + 1) * 128], qkb[s][:, j, :], idb[:]), reads=[B['rqkb', s, j], B['idb']], writes=[B['ptb']])
                sl = slice(t * 128, (t + 1) * 128)
                P.op('act', lambda e, sl=sl: e.copy(out=qT[:, sl], in_=ptb[:, 0:128]), reads=[B['ptb']], writes=[B['rqT', t]])
                P.op('act', lambda e, sl=sl: e.copy(out=kT[:, sl], in_=ptb[:, 128:256]), reads=[B['ptb']], writes=[B['rkT', t]])
                P.op('dve', lambda e, sl=sl: e.tensor_tensor(out=qdfT[:, sl], in0=ptb[:, 0:128], in1=qdf[:], op=ALU.mult), reads=[B['ptb'], B['qdf']], writes=[B['qdfT', t]])
                P.op('dve', lambda e, sl=sl: e.tensor_tensor(out=qdbT[:, sl], in0=ptb[:, 0:128], in1=qdb[:], op=ALU.mult), reads=[B['ptb'], B['qdb']], writes=[B['qdbT', t]])

            def chain(order, kdx, Sx, cd, ckey, name):
                cur = None
                for n_, t in enumerate(order):
                    if cur is None:
                        P.op('pool', lambda e, t=t: e.memset(Sx[:, t, :], 0.0), writes=[B[name, t]])
                    else:
                        P.op('pool', lambda e, t=t, cur=cur: e.tensor_copy(out=Sx[:, t, :], in_=stf[cur][:]), reads=[B['stf', cur]], writes=[B[name, t]])
                    if n_ == len(order) - 1:
                        break
                    P.op('pe', lambda e, t=t: e.matmul(pc[:, 0:128], lhsT=kdx[:, t, :], rhs=vtm[:, t, :], start=True, stop=True), reads=[B[name + 'k', t], B['rvtm', t]], writes=[B['pc']])
                    nxt = 0 if cur is None else 1 - cur
                    if cur is None:
                        P.op('dve', lambda e, nxt=nxt: e.tensor_copy(out=stf[nxt][:], in_=pc[:, 0:128]), reads=[B['pc']], writes=[B['stf', nxt]])
                    else:
                        P.op('dve', lambda e, nxt=nxt, cur=cur: e.scalar_tensor_tensor(out=stf[nxt][:], in0=stf[cur][:], scalar=cd, in1=pc[:, 0:128], op0=ALU.mult, op1=ALU.add),
                             reads=[B['stf', cur], B['pc'], ckey], writes=[B['stf', nxt]])
                    cur = nxt
            for t in range(NT_A):
                B.d[('Sfk', t)] = B['kdf', t]
                B.d[('Sbk', t)] = B['kdb', t]
            chain([32, 33] + list(range(32)), kdf, Sf, kdc[:, 2:3], kd[2], 'Sf')
            chain([33, 32] + list(range(31, -1, -1)), kdb, Sb, kdc[:, 3:4], kd[3], 'Sb')
            for t in range(NT_A):
                s = t % 2
                sl = slice(t * 128, (t + 1) * 128)
                P.op('pe', lambda e, sl=sl, s=s: e.matmul(pa[:, s, 0:128], lhsT=kT[:, sl], rhs=qT[:, sl], start=True, stop=True), reads=[B['rkT', t], B['rqT', t]], writes=[B['pa', s]])
                P.op('dve', lambda e, s=s: e.tensor_tensor(out=smT[s][:], in0=pa[:, s, 0:128], in1=mask[:], op=ALU.mult), reads=[B['pa', s], B['mask']], writes=[B['smT', s]])
                P.op('pe', lambda e, t=t, s=s: e.matmul(pb_[:, s, 0:128], lhsT=smT[s][:], rhs=vtm[:, t, :], start=True, stop=False), reads=[B['smT', s], B['rvtm', t]], writes=[B['pb', s, 0]])
                P.op('pe', lambda e, t=t, s=s, sl=sl: e.matmul(pb_[:, s, 0:128], lhsT=qdfT[:, sl], rhs=Sf[:, t, :], start=False, stop=False), reads=[B['qdfT', t], B['Sf', t]], writes=[B['pb', s, 0]])
                P.op('pe', lambda e, t=t, s=s, sl=sl: e.matmul(pb_[:, s, 0:128], lhsT=qdbT[:, sl], rhs=Sb[:, t, :], start=False, stop=True), reads=[B['qdbT', t], B['Sb', t]], writes=[B['pb', s, 0]])
                P.op('act', lambda e, s=s: e.copy(out=on[s][:], in_=pb_[:, s, 0:128]), reads=[B['pb', s, 0]], writes=[B['on', s]])
                P.op('dve', lambda e, s=s: e.bn_stats(out=stt[:], in_=on[s][:]), reads=[B['on', s]], writes=[B['rstt']])
                P.op('dve', lambda e: e.bn_aggr(out=mv[:], in_=stt[:]), reads=[B['rstt']], writes=[B['rmv']])
                P.op('dve', lambda e: e.tensor_scalar_add(out=rstd[:], in0=mv[:, 1:2], scalar1=1e-6), reads=[B['rmv']], writes=[B['rrstd']])
                P.op('act', lambda e: e.sqrt(out=rstd[:], in_=rstd[:]), reads=[B['rrstd']], writes=[B['rrstd']])
                P.op('dve', lambda e: e.reciprocal(out=rstd[:], in_=rstd[:]), reads=[B['rrstd']], writes=[B['rrstd']])
                P.op('dve', lambda e, s=s: e.tensor_scalar(out=on[s][:], in0=on[s][:], scalar1=mv[:, 0:1], scalar2=rstd[:, 0:1], op0=ALU.subtract, op1=ALU.mult),
                     reads=[B['on', s], B['rmv'], B['rrstd']], writes=[B['on', s]])
                P.op('pool', lambda e, s=s, t=t: e.tensor_tensor(out=ob[s][:], in0=on[s][:], in1=sgt[:, t, :], op=ALU.mult), reads=[B['on', s], B['sgt', t]], writes=[B['ob', s]])
                P.op('pe', lambda e, s=s: e.transpose(ptb[:, 512:640], ob[s][:], idb[:]), reads=[B['ob', s], B['idb']], writes=[B['ptb2']])
                g4 = t // 4
                yg = ysg[g4 % 2]
                P.op('act', lambda e, t=t, yg=yg: e.copy(out=yg[:, (t % 4) * 128:(t % 4 + 1) * 128], in_=ptb[:, 512:640]), reads=[B['ptb2']], writes=[B['rysg', g4 % 2]])
                if t % 4 == 3 or t == NT_A - 1:
                    n_ = (t % 4 + 1) * 128
                    P.dma('sp', lambda e, h=h, g4=g4, yg=yg, n_=n_: e.dma_start(out=YT[2 + h, :, g4 * 512:g4 * 512 + n_], in_=yg[:, 0:n_]), reads=[B['rysg', g4 % 2]], is_output=True)
        P.flush()


import ml_dtypes
_BF = ml_dtypes.bfloat16
_CONST = {}


def _consts():
    if _CONST:
        return _CONST
    C = _CONST
    rows = 64
    row = np.repeat(np.arange(rows, dtype=np.float32), 64)
    col = np.tile(np.arange(64, dtype=np.float32), rows)
    inv = (10000.0 ** (-np.arange(32, dtype=np.float32) / 32)).astype(np.float32)
    ang = np.stack([row[:, None] * inv, col[:, None] * inv], axis=1).astype(np.float32)
    C['ropec'] = np.ascontiguousarray(np.cos(ang).astype(np.float32).reshape(32, 128, 64).transpose(1, 0, 2))
    C['ropes'] = np.ascontiguousarray(np.sin(ang).astype(np.float32).reshape(32, 128, 64).transpose(1, 0, 2))
    j = np.arange(128, dtype=np.float32)[:, None]
    i = np.arange(128, dtype=np.float32)[None, :]
    rtab = np.stack([np.maximum(i - j, 0), (i >= j).astype(np.float32), np.maximum(j - i, 0), (j >= i).astype(np.float32),
                     np.broadcast_to(i + 1, (128, 128)), np.broadcast_to(128 - i, (128, 128))], axis=1).astype(np.float32)
    C['rtab'] = np.ascontiguousarray(rtab)
    jj = np.arange(128, dtype=np.float32)
    C['rcol'] = np.ascontiguousarray(np.stack([127 - jj, jj, np.full(128, 128.0), np.zeros(128)], 1).astype(np.float32))

    def feats(l):
        t = np.linspace(0.0, 1.0, l, dtype=np.float32)[:, None]
        w = (2.0 * np.pi * np.arange(l, dtype=np.float32) / l).astype(np.float32)
        f = np.linspace(1e-4, 15, 16, dtype=np.float32)
        a = (w[:, None] * f[None, :]).astype(np.float32)
        return np.concatenate([t, np.cos(a), -np.sin(a)], axis=-1).astype(np.float32), t[:, 0]
    f4, t4 = feats(4096)
    fc, tc = feats(256)
    C['featsT'] = np.ascontiguousarray(f4.T)
    C['featsTc'] = np.ascontiguousarray(fc.T)
    negt = np.concatenate([-t4.reshape(32, 128), -tc.reshape(2, 128)], 0).T
    C['negt'] = np.ascontiguousarray(negt.astype(np.float32))
    mx = np.log(1e-2) / 0.3
    mn = np.log(1e-2) / 1.5
    C['absdelta'] = np.abs(np.linspace(mn, mx, 2048, dtype=np.float32)).reshape(2, 2, 512)
    n = np.arange(4096, dtype=np.float64)
    k = np.arange(4096, dtype=np.float64) + 0.5
    Fm = np.empty((32, 128, 8, 1024), dtype=_BF)
    for nt in range(32):
        th = 2 * np.pi * np.outer(n[nt * 128:(nt + 1) * 128], k) / 8192.0
        Fm[nt, :, :, 0:512] = np.cos(th).reshape(128, 8, 512).astype(_BF)
        Fm[nt, :, :, 512:1024] = np.sin(th).reshape(128, 8, 512).astype(_BF)
    C['Fm'] = Fm
    Gm = np.empty((4, 64, 128, 1024), dtype=_BF)
    for kt in range(32):
        th = 2 * np.pi * np.outer(k[kt * 128:(kt + 1) * 128], n) / 8192.0
        Gm[:, kt] = np.cos(th).reshape(128, 4, 1024).transpose(1, 0, 2).astype(_BF)
        Gm[:, 32 + kt] = np.sin(th).reshape(128, 4, 1024).transpose(1, 0, 2).astype(_BF)
    C['Gm'] = Gm
    nc_ = np.arange(256, dtype=np.float64)
    kc = np.arange(256, dtype=np.float64) + 0.5
    th = 2 * np.pi * np.outer(nc_, kc) / 512.0
    Fc = np.concatenate([np.cos(th), np.sin(th)], 1).reshape(2, 128, 512).transpose(1, 0, 2)
    C['Fc'] = np.ascontiguousarray(Fc).astype(_BF)
    Gc = np.concatenate([np.cos(th.T), np.sin(th.T)], 0).reshape(4, 128, 256).transpose(1, 0, 2)
    C['Gc'] = np.ascontiguousarray(Gc).astype(_BF)
    return C


def run_A(l, x_cur, h_cur, mod, inp):
    C = _consts()
    w_in = inp['w_in'][l]
    ins = []
    for b in range(4):
        xt = np.ascontiguousarray(np.concatenate([x_cur[b], h_cur[b]], 0).reshape(NT_A, 128, 1024))
        ms = [mod[b], mod[4]]
        modcols = np.ascontiguousarray(np.stack([np.stack([_cols(m[k_ * 1024:(k_ + 1) * 1024]) for k_ in (0, 1)], 1) for m in ms], 1))
        for half in range(2):
            r256 = half * 256 + np.arange(256)
            colsA = np.concatenate([g * 512 + r256 for g in range(3)])
            colsR = np.concatenate([1536 + g * 512 + r256 for g in range(4)])
            colsH = np.concatenate([3584 + g * 512 + r256 for g in range(3)])
            colsD = np.concatenate([5120 + r256, 5120 + 512 + half * 128 + np.arange(128), 5120 + 768 + half * 128 + np.arange(128)])
            ch = half * 256 + np.arange(256)
            caw = np.stack([inp['conv_a_w'][l][0, ch], inp['conv_a_w'][l][1, ch], inp['conv_a_w'][l][2, ch], inp['conv_a_b'][l][ch]], -1)
            hch = np.concatenate([g * 512 + ch for g in range(3)])
            hcw = np.stack([inp['hy_conv_w'][l][0, hch], inp['hy_conv_w'][l][1, hch], inp['hy_conv_w'][l][2, hch], inp['hy_conv_b'][l][hch]], -1)
            d = dict(
                x=xt, modcols=modcols, ident=_IDENT,
                wA=_tile_rows(w_in[:, colsA]), wR=_tile_rows(w_in[:, colsR]), wH=_tile_rows(w_in[:, colsH]), wD=_tile_rows(w_in[:, colsD]),
                caw=np.ascontiguousarray(caw.reshape(2, 128, 4).transpose(1, 0, 2)),
                hcw=np.ascontiguousarray(hcw.reshape(6, 128, 4).transpose(1, 0, 2)),
                hsk=np.ascontiguousarray(inp['hy_skip'][l][:, ch].reshape(2, 2, 128).transpose(2, 0, 1)),
                ropec=C['ropec'], ropes=C['ropes'],
                qkn=_rep(np.stack([inp['q_norm'][l], inp['k_norm'][l]], 0)),
                rdec=_rep(inp['ret_decay'][l][:, half * 2:half * 2 + 2].reshape(4)),
                rtab=C['rtab'], rcol=C['rcol'], featsT=C['featsT'], featsTc=C['featsTc'],
                hw1=np.ascontiguousarray(inp['hy_w1'][l]), hw2=np.ascontiguousarray(inp['hy_w2'][l]),
                hcols=np.ascontiguousarray(np.stack([inp['hy_b1'][l], inp['hy_b2'][l], inp['hy_freq'][l][0], inp['hy_freq'][l][1]], 1)),
                hw3=np.ascontiguousarray(inp['hy_w3'][l].reshape(64, 2, 2, 512)[:, :, :, half * 256:(half + 1) * 256]),
                negt=C['negt'], adel=_rep(np.ascontiguousarray(C['absdelta'][:, :, half * 256:(half + 1) * 256])),
                Fm=C['Fm'], Gm=C['Gm'], Fc=C['Fc'], Gc=C['Gc'],
            )
            ins.append(d)
    res = run_bass_kernel_spmd(_prog('A', build_A), ins, core_ids=list(range(8)))
    YT = []
    for b in range(4):
        y = np.empty((4, 2, 2, 128, NTOK_A), np.float32)
        for half in range(2):
            o = res.results[b * 2 + half]['YT'].reshape(4, 2, 128, NTOK_A)
            y[:, half] = o
        YT.append(y.reshape(2048, NTOK_A))
    return YT


class _YTView:
    def __init__(self, base, half):
        self.base = base
        self.half = half

    def _m(self, i8):
        return (i8 // 2) * 4 + self.half * 2 + (i8 % 2)

    def __getitem__(self, idx):
        if isinstance(idx, tuple):
            return self.base[(self._m(idx[0]),) + tuple(idx[1:])]
        return self.base[self._m(idx)]


def emit_M2(nc, P, Dm):
    cT, wm, bcol, brow, modc, modr = Dm['cT'], Dm['wm'], Dm['bcol'], Dm['brow'], Dm['modc'], Dm['modr']
    with contextlib.ExitStack() as st:
        P.stack = st
        B = Bufs()
        cs = P.sb("cs", [128, 8, 2], F32)
        sc = P.sb("sc", [128, 8, 2], F32)
        ones = P.sb("ones", [128, 128], F32)
        scb = P.sb("scb", [128, 8, 2, 128], F32)
        wch = [P.sb(f"wch{i}", [128, 8, 1024], F32) for i in range(2)]
        bc = P.sb("bc", [128, 48], F32)
        br = P.sb("br", [128, 2, 1024], F32)
        mc = P.sb("mc", [128, 2, 48], F32)
        mr = P.sb("mr", [128, 2, 2, 1024], F32)
        pm = P.ps("pm", [128, 2, 512], F32)
        pcl = P.ps("pc", [128, 512], F32)
        P.dma('sp', lambda e: e.dma_start(out=cs[:], in_=cT), writes=[B['cs']])
        P.op('act', lambda e: e.activation(out=sc[:], in_=cs[:], func=AF.Silu), reads=[B['cs']], writes=[B['sc']])
        P.op('dve', lambda e: e.memset(ones[:], 1.0), writes=[B['ones']])
        for kt in range(8):
            for ms in range(2):
                P.op('dve', lambda e, kt=kt, ms=ms: e.tensor_scalar(out=scb[:, kt, ms, :], in0=ones[:], scalar1=sc[:, kt, ms:ms + 1], scalar2=None, op0=ALU.mult),
                     reads=[B['ones'], B['sc']], writes=[B['scb']])
        wc = 0
        for l in range(4):
            P.dma('sp', lambda e, l=l: e.dma_start(out=bc[:], in_=bcol[l]), writes=[B['bc']])
            P.dma('sp', lambda e, l=l: e.dma_start(out=br[:], in_=brow[l]), writes=[B['br']])
            for k in range(6):
                ws_ = wc % 2
                wc += 1
                P.dma('sp', lambda e, l=l, k=k, ws_=ws_: e.dma_start(out=wch[ws_][:], in_=wm[l, k]), writes=[B['wch', ws_]])
                for f in range(8):
                    for kt in range(8):
                        P.op('pe', lambda e, f=f, kt=kt, ws_=ws_: e.matmul(pcl[:, f * 2:f * 2 + 2], lhsT=wch[ws_][:, kt, f * 128:(f + 1) * 128], rhs=sc[:, kt, :], start=(kt == 0), stop=(kt == 7)),
                             reads=[B['wch', ws_], B['sc']], writes=[B['pc']])
                for ms in range(2):
                    P.op('dve', lambda e, k=k, ms=ms: e.tensor_tensor(out=mc[:, ms, k * 8:(k + 1) * 8], in0=pcl[:, 0:16].rearrange("p (f m) -> p m f", m=2)[:, ms, :], in1=bc[:, k * 8:(k + 1) * 8], op=ALU.add),
                         reads=[B['pc'], B['bc']], writes=[B['mc']])
                if k in (2, 5):
                    j = 0 if k == 2 else 1
                    for ms in range(2):
                        for nb in range(2):
                            for kt in range(8):
                                P.op('pe', lambda e, ms=ms, nb=nb, kt=kt, ws_=ws_: e.matmul(pm[:, nb, :], lhsT=scb[:, kt, ms, :], rhs=wch[ws_][:, kt, nb * 512:(nb + 1) * 512], start=(kt == 0), stop=(kt == 7)),
                                     reads=[B['scb'], B['wch', ws_]], writes=[B['pm', nb]])
                            P.op('dve', lambda e, ms=ms, nb=nb, j=j: e.tensor_tensor(out=mr[:, ms, j, nb * 512:(nb + 1) * 512], in0=pm[:, nb, :], in1=br[:, j, nb * 512:(nb + 1) * 512], op=ALU.add),
                                 reads=[B['pm', nb], B['br']], writes=[B['mr']])
            P.dma('sp', lambda e, l=l: e.dma_start(out=modc[l], in_=mc[:]), reads=[B['mc']], writes=[B['modc', l]])
            P.dma('sp', lambda e, l=l: e.dma_start(out=modr[l], in_=mr[:]), reads=[B['mr']], writes=[B['modr', l]])
        P.flush()


def build_F():
    nc = bass.Bass("TRN2", target_bir_lowering=False)
    I = lambda name, shape, dt=F32: _din(nc, name, shape, dt)
    T = lambda name, shape, dt=F32: nc.dram_tensor(name, list(shape), dt, kind="Internal").ap()
    x0 = I("x0", [NT_A, 128, D])
    cT = I("cT", [128, 8, 2])
    wm = I("wm", [4, 6, 128, 8, 1024])
    bcol = I("bcol", [4, 128, 48])
    brow = I("brow", [4, 128, 2, 1024])
    ident = I("ident", [128, 128])
    wA = I("wA", [4, 2, 128, 8, 768])
    wR = I("wR", [4, 2, 128, 8, 1024])
    wH = I("wH", [4, 2, 128, 8, 768])
    wD = I("wD", [4, 2, 128, 8, 512])
    caw = I("caw", [4, 2, 128, 2, 4])
    hcw = I("hcw", [4, 2, 128, 6, 4])
    hsk = I("hsk", [4, 2, 128, 2, 2])
    ropec = I("ropec", [128, 32, 64])
    ropes = I("ropes", [128, 32, 64])
    qkn = I("qkn", [4, 128, 2, 128])
    rdec = I("rdec", [4, 2, 128, 4])
    rtab = I("rtab", [128, 6, 128])
    rcol = I("rcol", [128, 4])
    featsT = I("featsT", [33, 4096])
    featsTc = I("featsTc", [33, 256])
    hw1 = I("hw1", [4, 33, 64])
    hw2 = I("hw2", [4, 64, 64])
    hcols = I("hcols", [4, 64, 4])
    hw3 = I("hw3", [4, 2, 64, 2, 2, 256])
    negt = I("negt", [128, 34])
    adel = I("adel", [2, 128, 2, 2, 256])
    Fm = I("Fm", [32, 128, 8, 1024], BF16)
    Gm = I("Gm", [4, 64, 128, 1024], BF16)
    Fc = I("Fc", [128, 2, 512], BF16)
    Gc = I("Gc", [128, 4, 256], BF16)
    wg = I("wg", [4, 8, 128, 4, 8, 128])
    wb = I("wb", [4, 8, 128, 4, 4, 128])
    wo = I("wo", [4, 128, 8, D])
    lnrows = I("lnrows", [4, 128, 4, D])
    bgc = I("bgc", [4, 128, 4, 8])
    w1d = I("w1d", [2, 11, 128, 8, 256])
    w3d = I("w3d", [2, 11, 128, 8, 256])
    w2d = I("w2d", [2, 11, 128, 2, D])
    w1m = I("w1m", [2, 56, 128, 8, 512])
    w3m = I("w3m", [2, 56, 128, 8, 512])
    w2m = I("w2m", [2, 56, 128, 4, D])
    rt = I("rt", [2, 128, 8, 8])
    sel = I("sel", [8, 8, 128])
    out = _dout(nc, "out", [NT_A, 128, D])
    xb = [T("xbuf0", [NT_A, 128, D]), T("xbuf1", [NT_A, 128, D])]
    x1s = T("x1s", [NT_A, 128, D])
    YTs = T("YTs", [16, 128, NTOK_A])
    modc = T("modc", [4, 128, 2, 48])
    modr = T("modr", [4, 128, 2, 2, D])
    with contextlib.ExitStack() as st0:
        P = Prog(nc, st0)
        emit_M2(nc, P, dict(cT=cT, wm=wm, bcol=bcol, brow=brow, modc=modc, modr=modr))
        import os as _os
        NL = int(_os.environ.get('F_LAYERS', '4'))
        for l in range(NL):
            xin = x0 if l == 0 else xb[l % 2]
            xout = out if l == NL - 1 else xb[(l + 1) % 2]
            mcA = modc[l][:, :, 0:16].rearrange("p m (k f) -> p m k f", k=2)
            mcB = (mcA, modc[l][:, :, 24:40].rearrange("p m (k f) -> p m k f", k=2))
            for half in range(2):
                emit_A(nc, P, dict(x=xin, modcols=mcA, ident=ident, wA=wA[l, half], wR=wR[l, half], wH=wH[l, half], wD=wD[l, half],
                                   caw=caw[l, half], hcw=hcw[l, half], hsk=hsk[l, half], ropec=ropec, ropes=ropes, qkn=qkn[l], rdec=rdec[l, half],
                                   rtab=rtab, rcol=rcol, featsT=featsT, featsTc=featsTc, hw1=hw1[l], hw2=hw2[l], hcols=hcols[l], hw3=hw3[l, half],
                                   negt=negt, adel=adel[half], Fm=Fm, Gm=Gm, Fc=Fc, Gc=Gc, YT=_YTView(YTs, half)))
            moe = (l % 2 == 1)
            i = l // 2
            for hb in range(2):
                Dm = dict(x=xin, yT=YTs, modcols=mcB, modrows=modr[l], lnrows=lnrows[l], bgc=bgc[l], ident=ident, wg=wg[l], wb=wb[l], wo=wo[l],
                          x1o=x1s, xo=xout)
                if moe:
                    Dm.update(w1=w1m[i], w3=w3m[i], w2=w2m[i], rt=rt[i], sel=sel)
                else:
                    Dm.update(w1=w1d[i], w3=w3d[i], w2=w2d[i])
                emit_B(nc, P, Dm, moe, gt=(lambda t, hb=hb: hb * 16 + t if t < 16 else 32 + hb))
    return nc


def _pack_F(inp):
    C = _consts()
    sh = dict(ident=_IDENT, ropec=C['ropec'], ropes=C['ropes'], rtab=C['rtab'], rcol=C['rcol'], featsT=C['featsT'], featsTc=C['featsTc'],
              negt=C['negt'], Fm=C['Fm'], Gm=C['Gm'], Fc=C['Fc'], Gc=C['Gc'])
    sh['adel'] = np.stack([_rep(np.ascontiguousarray(C['absdelta'][:, :, h * 256:(h + 1) * 256])) for h in range(2)], 0)
    w_mod = inp['w_mod']
    sh['wm'] = np.ascontiguousarray(w_mod.reshape(4, 8, 128, 6, 1024).transpose(0, 3, 2, 1, 4))
    sh['bcol'] = np.ascontiguousarray(inp['b_mod'].reshape(4, 48, 128).transpose(0, 2, 1))
    sh['brow'] = np.stack([_rep(np.stack([inp['b_mod'][l, 2048:3072], inp['b_mod'][l, 5120:6144]], 0)) for l in range(4)], 0)
    wA, wR, wH, wD, caw, hcw, hsk, rdec, hw3 = [], [], [], [], [], [], [], [], []
    for l in range(4):
        w_in = inp['w_in'][l]
        rows = [[] for _ in range(9)]
        for half in range(2):
            r256 = half * 256 + np.arange(256)
            colsA = np.concatenate([g * 512 + r256 for g in range(3)])
            colsR = np.concatenate([1536 + g * 512 + r256 for g in range(4)])
            colsH = np.concatenate([3584 + g * 512 + r256 for g in range(3)])
            colsD = np.concatenate([5120 + r256, 5120 + 512 + half * 128 + np.arange(128), 5120 + 768 + half * 128 + np.arange(128)])
            ch = r256
            cawv = np.stack([inp['conv_a_w'][l][0, ch], inp['conv_a_w'][l][1, ch], inp['conv_a_w'][l][2, ch], inp['conv_a_b'][l][ch]], -1)
            hch = np.concatenate([g * 512 + ch for g in range(3)])
            hcwv = np.stack([inp['hy_conv_w'][l][0, hch], inp['hy_conv_w'][l][1, hch], inp['hy_conv_w'][l][2, hch], inp['hy_conv_b'][l][hch]], -1)
            vals = [_tile_rows(w_in[:, colsA]), _tile_rows(w_in[:, colsR]), _tile_rows(w_in[:, colsH]), _tile_rows(w_in[:, colsD]),
                    cawv.reshape(2, 128, 4).transpose(1, 0, 2), hcwv.reshape(6, 128, 4).transpose(1, 0, 2),
                    inp['hy_skip'][l][:, ch].reshape(2, 2, 128).transpose(2, 0, 1),
                    _rep(inp['ret_decay'][l][:, half * 2:half * 2 + 2].reshape(4)),
                    inp['hy_w3'][l].reshape(64, 2, 2, 512)[:, :, :, half * 256:(half + 1) * 256]]
            for r_, v_ in zip(rows, vals):
                r_.append(v_)
        for lst, r_ in zip((wA, wR, wH, wD, caw, hcw, hsk, rdec, hw3), rows):
            lst.append(np.stack(r_, 0))
    for n_, lst in zip(('wA', 'wR', 'wH', 'wD', 'caw', 'hcw', 'hsk', 'rdec', 'hw3'), (wA, wR, wH, wD, caw, hcw, hsk, rdec, hw3)):
        sh[n_] = np.ascontiguousarray(np.stack(lst, 0))
    sh['qkn'] = np.stack([_rep(np.stack([inp['q_norm'][l], inp['k_norm'][l]], 0)) for l in range(4)], 0)
    sh['hw1'] = np.ascontiguousarray(inp['hy_w1'])
    sh['hw2'] = np.ascontiguousarray(inp['hy_w2'])
    sh['hcols'] = np.ascontiguousarray(np.stack([inp['hy_b1'], inp['hy_b2'], inp['hy_freq'][:, 0], inp['hy_freq'][:, 1]], -1))
    pw = [prep_B_weights(l, inp) for l in range(4)]
    for n_ in ('wg', 'wb', 'wo', 'lnrows', 'bgc'):
        sh[n_] = np.stack([pw[l][n_] for l in range(4)], 0)
    for n_ in ('w1', 'w3', 'w2'):
        sh[n_ + 'd'] = np.stack([pw[0][n_], pw[2][n_]], 0)
        sh[n_ + 'm'] = np.stack([pw[1][n_], pw[3][n_]], 0)
    sh['rt'] = np.stack([pw[1]['rt'], pw[3]['rt']], 0)
    sh['sel'] = pw[1]['sel']
    per = []
    for b in range(4):
        cc = np.stack([inp['c'][b], inp['c_ctx']], 0)
        per.append(dict(x0=np.ascontiguousarray(np.concatenate([inp['x'][b], inp['ctx'][b]], 0).reshape(NT_A, 128, 1024)),
                        cT=np.ascontiguousarray(cc.T.reshape(8, 128, 2).transpose(1, 0, 2))))
    return sh, per


def kernel_fused(**inp):
    inp = {k_: np.asarray(v) for k_, v in inp.items()}
    sh, per = _pack_F(inp)
    ins = [dict(sh, **per[b]) for b in range(4)]
    res = run_bass_kernel_spmd(_prog('F', build_F), ins, core_ids=list(range(4)))
    out = np.stack([res.results[b]['out'].reshape(NTOK_A, 1024)[:4096] for b in range(4)], 0)
    return np.ascontiguousarray(out, dtype=np.float32)


def kernel_unfused(**inp):
    inp = {k_: np.asarray(v) for k_, v in inp.items()}
    mod = run_M(inp['c'], inp['c_ctx'], inp['w_mod'], inp['b_mod'])
    x_cur = np.ascontiguousarray(inp['x'], dtype=np.float32)
    h_cur = np.ascontiguousarray(inp['ctx'], dtype=np.float32)
    for l in range(4):
        YT = run_A(l, x_cur, h_cur, mod[:, l], inp)
        x_cur, h_cur, _ = run_B(l, x_cur, h_cur, YT, mod[:, l], inp)
    return x_cur


def kernel(**inp):
    return kernel_fused(**inp)
```

```python
import contextlib
import numpy as np
import concourse.bass as bass
import concourse.mybir as mybir

F32 = mybir.dt.float32
BF16 = mybir.dt.bfloat16
AF = mybir.ActivationFunctionType
ALU = mybir.AluOpType
AX = mybir.AxisListType

NDMA = 24
ENGS = ['pe', 'act', 'dve', 'pool', 'sp']


class Buf:
    __slots__ = ('w', 'r', 'name', 'excl')

    def __init__(self, name=''):
        self.w = None
        self.r = {}
        self.name = name
        self.excl = False


PSUM_KEYS = {'pt', 'ptb', 'pa', 'pb', 'pc', 'pg', 'pp', 'pmx', 'pg1', 'pg3', 'pf', 'pw', 'pm'}


class Bufs:
    def __init__(self, name=''):
        self.d = {}
        self.name = name

    def __getitem__(self, k):
        if k == 'ptb2':
            k = 'ptb'
        b = self.d.get(k)
        if b is None:
            b = Buf(f"{self.name}{k}")
            k0 = k[0] if isinstance(k, tuple) else k
            b.excl = k0 in PSUM_KEYS
            self.d[k] = b
        return b


class Prog:
    def __init__(self, nc, stack, same_engine_sync=True):
        self.nc = nc
        self.stack = stack
        self.q = {e: [] for e in ENGS}
        self.sem = {e: stack.enter_context(nc.semaphore(f"s_{e}")) for e in ENGS}
        self.cnt = {e: 0 for e in ENGS}
        self.seen = {e: {} for e in ENGS}
        self.dsem = [stack.enter_context(nc.semaphore(f"dq{i}")) for i in range(NDMA)]
        self.dval = [0] * NDMA
        self.dnext = 0
        self.ses = same_engine_sync
        self.out_tokens = []

    def sb(self, name, shape, dt):
        self._uid = getattr(self, '_uid', 0) + 1
        return self.stack.enter_context(self.nc.sbuf_tensor(f"{name}_u{self._uid}", list(shape), dt))

    def ps(self, name, shape, dt=F32):
        self._uid = getattr(self, '_uid', 0) + 1
        return self.stack.enter_context(self.nc.psum_tensor(f"{name}_u{self._uid}", list(shape), dt))

    def _semh(self, k):
        return self.sem[k] if isinstance(k, str) else self.dsem[k[1]]

    def _deps(self, e, reads, writes):
        need = {}
        xr = [b for b in reads if b.excl]
        if xr:
            writes = list(writes) + xr

        def add(tok):
            if tok is None:
                return
            k, v = tok
            if need.get(k, 0) < v:
                need[k] = v

        for b in reads:
            add(b.w)
        for b in writes:
            add(b.w)
            for k, v in b.r.items():
                add((k, v))
        waits = []
        for k, v in need.items():
            if k == e and (e == 'pe' or not self.ses):
                continue
            if self.seen[e].get(k, 0) >= v:
                continue
            self.seen[e][k] = v
            waits.append((k, v))
        return waits

    def _mark(self, tok, reads, writes):
        k, v = tok
        xr = [b for b in reads if b.excl]
        if xr:
            writes = list(writes) + xr
        for b in reads:
            if b.r.get(k, 0) < v:
                b.r[k] = v
        for b in writes:
            b.w = tok
            b.r = {}

    mute = False

    def op(self, e, fn, reads=(), writes=()):
        if self.mute:
            return
        waits = self._deps(e, reads, writes)
        self.cnt[e] += 1
        tok = (e, self.cnt[e])
        self._mark(tok, reads, writes)
        self.q[e].append((waits, fn, (self.sem[e], 1)))

    def dma(self, e, fn, reads=(), writes=(), is_output=False):
        if self.mute:
            return
        if e == 'pool':
            i = self._dpool = (getattr(self, '_dpool', -1) + 1) % 8
        else:
            i = 8 + self.dnext
            self.dnext = (self.dnext + 1) % (NDMA - 8)
        waits = self._deps(e, reads, writes)
        k = ('d', i)
        if self.dval[i] > 0 and self.seen[e].get(k, 0) < self.dval[i]:
            waits.append((k, self.dval[i]))
            self.seen[e][k] = self.dval[i]
        self.dval[i] += 16
        tok = (k, self.dval[i])
        self._mark(tok, reads, writes)
        self.q[e].append((waits, fn, (self.dsem[i], 16)))
        if is_output:
            self.out_tokens.append(tok)

    def flush(self):
        nc = self.nc
        q = self.q
        semh = self._semh
        ex = []
        for e in ['pe', 'act', 'dve', 'pool']:
            if self.cnt[e] > 0:
                ex.append((e, self.cnt[e]))
        for i in range(NDMA):
            if self.dval[i] > 0:
                ex.append((('d', i), self.dval[i]))

        def emit(eng, ename):
            for waits, fn, (sem, n) in q[ename]:
                for (k, v) in waits:
                    eng.wait_ge(semh(k), v)
                fn(eng).then_inc(sem, n)
            for (k, v) in ex:
                if self.seen[ename].get(k, 0) < v:
                    eng.wait_ge(semh(k), v)
                    self.seen[ename][k] = v

        with nc.Block() as block:
            @block.tensor
            def _(t):
                emit(t, 'pe')

            @block.scalar
            def _(t):
                emit(t, 'act')

            @block.vector
            def _(t):
                emit(t, 'dve')

            @block.gpsimd
            def _(t):
                emit(t, 'pool')

            @block.sync
            def _(t):
                emit(t, 'sp')
        self.q = {e: [] for e in ENGS}

from concourse.bass_utils import run_bass_kernel_spmd

D = 1024
ALPHA = (2 * 4) ** 0.25
NTILE = 17
NTOK = NTILE * 128
BLOCKS = [(0, 4), (4, 4), (8, 4), (12, 4), (16, 1)]


def _din(nc, name, shape, dt=F32):
    return nc.dram_tensor(name, list(shape), dt, kind="ExternalInput").ap()


def _dout(nc, name, shape, dt=F32):
    return nc.dram_tensor(name, list(shape), dt, kind="ExternalOutput").ap()


def build_M():
    nc = bass.Bass("TRN2", target_bir_lowering=False)
    cT = _din(nc, "cT", [128, 8, 5])
    w = _din(nc, "w", [128, 8, 3072])
    b = _din(nc, "b", [5, 3072])
    o = _dout(nc, "o", [5, 3072])
    with contextlib.ExitStack() as st:
        P = Prog(nc, st)
        B = Bufs()
        cs = P.sb("cs", [128, 8, 5], F32)
        sc = P.sb("sc", [128, 8, 5], F32)
        ws = P.sb("ws", [128, 8, 3072], F32)
        bs = P.sb("bs", [5, 3072], F32)
        os_ = P.sb("os", [5, 3072], F32)
        pm = P.ps("pm", [128, 2, 512], F32)
        P.dma('sp', lambda e: e.dma_start(out=cs[:], in_=cT), writes=[B['cs']])
        P.dma('sp', lambda e: e.dma_start(out=bs[:], in_=b), writes=[B['bs']])
        for kt in range(8):
            P.dma('sp', lambda e, kt=kt: e.dma_start(out=ws[:, kt, :], in_=w[:, kt, :]), writes=[B['ws', kt]])
        P.op('act', lambda e: e.activation(out=sc[:], in_=cs[:], func=AF.Silu), reads=[B['cs']], writes=[B['sc']])
        for nb in range(6):
            pb = B['pm', nb % 2]
            for kt in range(8):
                P.op('pe', lambda e, nb=nb, kt=kt: e.matmul(pm[0:5, nb % 2, :], lhsT=sc[:, kt, :], rhs=ws[:, kt, nb * 512:(nb + 1) * 512],
                                                          start=(kt == 0), stop=(kt == 7)),
                     reads=[B['sc'], B['ws', kt]], writes=[pb])
            P.op('dve', lambda e, nb=nb: e.tensor_tensor(out=os_[:, nb * 512:(nb + 1) * 512], in0=pm[0:5, nb % 2, :],
                                                        in1=bs[:, nb * 512:(nb + 1) * 512], op=ALU.add),
                 reads=[pb, B['bs']], writes=[B['os', nb]])
        P.dma('sp', lambda e: e.dma_start(out=o, in_=os_[:]), reads=[B['os', nb] for nb in range(6)], is_output=True)
        P.flush()
    return nc


def emit_B(nc, P, Dm, moe, gt=None):
    if gt is None:
        gt = lambda t: t
    KC = 4 if moe else 2
    NCH = 56 if moe else 11
    CPE = 7 if moe else 11
    x = Dm['x']
    yT = Dm['yT']
    modcols = Dm['modcols']
    modrows = Dm['modrows']
    lnrows = Dm['lnrows']
    bgc = Dm['bgc']
    ident = Dm['ident']
    wg = Dm['wg']
    wb = Dm['wb']
    wo = Dm['wo']
    w1 = Dm['w1']
    w3 = Dm['w3']
    w2 = Dm['w2']
    x1o = Dm['x1o']
    xo = Dm['xo']
    if moe:
        rt = Dm['rt']
        sel = Dm['sel']
    with contextlib.ExitStack() as st:
        P.stack = st
        B = Bufs()
        ids = P.sb("ids", [128, 128], F32)
        mcol = P.sb("mcol", [128, 2, 4, 8], F32)
        bgs = P.sb("bgs", [128, 4, 8], F32)
        u2T = P.sb("u2T", [128, 8, NTOK], BF16)
        P.dma('sp', lambda e: e.dma_start(out=ids[:], in_=ident), writes=[B['ids']])
        if isinstance(modcols, tuple):
            P.dma('sp', lambda e: e.dma_start(out=mcol[:, :, 0:2, :], in_=modcols[0]), writes=[B['mcol']])
            P.dma('sp', lambda e: e.dma_start(out=mcol[:, :, 2:4, :], in_=modcols[1]), writes=[B['mcol']])
        else:
            P.dma('sp', lambda e: e.dma_start(out=mcol[:], in_=modcols), writes=[B['mcol']])
        P.dma('sp', lambda e: e.dma_start(out=bgs[:], in_=bgc), writes=[B['bgs']])
        P.op('dve', lambda e: e.tensor_scalar_add(out=mcol[:, :, 1, :], in0=mcol[:, :, 1, :], scalar1=1.0), reads=[B['mcol']], writes=[B['mcol']])
        P.op('dve', lambda e: e.tensor_scalar_add(out=mcol[:, :, 3, :], in0=mcol[:, :, 3, :], scalar1=1.0), reads=[B['mcol']], writes=[B['mcol']])
        if moe:
            rts = P.sb("rts", [128, 8, 8], F32)
            sels = P.sb("sels", [8, 8, 128], F32)
            wall = P.sb("wall", [128, NTILE, 8], F32)
            WT = P.sb("WT", [8, NTOK], F32)
            P.dma('sp', lambda e: e.dma_start(out=rts[:], in_=rt), writes=[B['rts']])
            P.dma('sp', lambda e: e.dma_start(out=sels[:], in_=sel), writes=[B['sels']])

        def ln_tile(r, stt, mv, rstd, gi, key):
            rb = B[key]
            for hf in range(2):
                P.op('dve', lambda e, hf=hf: e.bn_stats(out=stt[:, hf, :], in_=r[:, hf * 512:(hf + 1) * 512]), reads=[rb], writes=[B[key, 'st', hf]])
            P.op('dve', lambda e: e.bn_aggr(out=mv[:], in_=stt[:].rearrange("p a b -> p (a b)")), reads=[B[key, 'st', 0], B[key, 'st', 1]], writes=[B[key, 'mv']])
            P.op('dve', lambda e: e.tensor_scalar_add(out=rstd[:], in0=mv[:, 1:2], scalar1=1e-6), reads=[B[key, 'mv']], writes=[B[key, 'rstd']])
            P.op('act', lambda e: e.sqrt(out=rstd[:], in_=rstd[:]), reads=[B[key, 'rstd']], writes=[B[key, 'rstd']])
            P.op('dve', lambda e: e.reciprocal(out=rstd[:], in_=rstd[:]), reads=[B[key, 'rstd']], writes=[B[key, 'rstd']])
            P.op('dve', lambda e: e.tensor_scalar(out=r[:], in0=r[:], scalar1=mv[:, 0:1], scalar2=rstd[:, 0:1], op0=ALU.subtract, op1=ALU.mult),
                 reads=[rb, B[key, 'mv'], B[key, 'rstd']], writes=[rb])
            P.op('pool', lambda e: e.tensor_tensor(out=r[:], in0=r[:], in1=lnr[:, gi, :], op=ALU.mult), reads=[rb, B['lnr']], writes=[rb])
            P.op('pool', lambda e: e.tensor_tensor(out=r[:], in0=r[:], in1=lnr[:, gi + 1, :], op=ALU.add), reads=[rb, B['lnr']], writes=[rb])

        with contextlib.ExitStack() as s1:
            P.stack = s1
            mrow = P.sb("mrow", [128, 2, 2, D], F32)
            lnr = P.sb("lnr", [128, 4, D], F32)
            P.dma('sp', lambda e: e.dma_start(out=mrow[:], in_=modrows), writes=[B['mrow']])
            P.dma('sp', lambda e: e.dma_start(out=lnr[:], in_=lnrows), writes=[B['lnr']])
            xs = P.sb("xs", [128, 4, D], F32)
            uT = P.sb("uT", [128, 8, 512], BF16)
            yTb = P.sb("yTb", [128, 16, 512], BF16)
            mT = P.sb("mT", [128, 8, 512], BF16)
            wgs = [P.sb(f"wgs{i}", [128, 4, 8, 128], BF16) for i in range(2)]
            wbs = [P.sb(f"wbs{i}", [128, 4, 4, 128], BF16) for i in range(2)]
            wos = P.sb("wos", [128, 8, D], BF16)
            sg = [P.sb(f"sg{i}", [128, 512], F32) for i in range(2)]
            macc = P.sb("macc", [128, 512], F32)
            mtmp = P.sb("mtmp", [128, 512], F32)
            rr = [P.sb(f"rr{i}", [128, D], F32) for i in range(2)]
            t1 = P.sb("t1", [128, D], F32)
            stt = P.sb("stt", [128, 2, 6], F32)
            mv = P.sb("mv", [128, 2], F32)
            rstd = P.sb("rstd", [128, 1], F32)
            u2f = P.sb("u2f", [128, 8, 128], F32)
            pt = P.ps("pt", [128, 2, 512], F32)
            pg = P.ps("pg", [128, 2, 512], F32)
            pp = P.ps("pp", [128, 2, 512], F32)
            pmx = P.ps("pmx", [128, 2, 512], F32)
            if moe:
                lg = P.sb("lg", [128, 8], F32)
                m8 = P.sb("m8", [128, 8], F32)
                msk = P.sb("msk", [128, 8], F32)
                ex = P.sb("ex", [128, 8], F32)
                sm = P.sb("sm", [128, 4], F32)

            P.dma('sp' if Dm.get('wbf16') else 'pool', lambda e: e.dma_start(out=wos[:], in_=wo), writes=[B['wos']])
            wcnt = 0
            tcnt = 0
            for bi, (t0, nt) in enumerate(BLOCKS):
                ms = 1 if bi == 4 else 0
                ntok = nt * 128
                tok0 = gt(t0) * 128
                P.dma('sp', lambda e, t0=t0, nt=nt: e.dma_start(out=xs[:, 0:nt, :], in_=x[gt(t0):gt(t0) + nt].rearrange("t p f -> p t f")),
                      writes=[B['xs', j] for j in range(nt)])
                P.dma('pool', lambda e, tok0=tok0, ntok=ntok: e.dma_start(out=yTb[:, :, 0:ntok], in_=yT[:, :, tok0:tok0 + ntok].rearrange("c p t -> p c t")),
                      writes=[B['yTb']])
                for j in range(nt):
                    for f in range(8):
                        pb = B['pt', tcnt % 2]
                        P.op('pe', lambda e, j=j, f=f, s=tcnt % 2: e.transpose(pt[:, s, 0:128], xs[:, j, f * 128:(f + 1) * 128], ids[:]),
                             reads=[B['xs', j], B['ids']], writes=[pb])
                        P.op('act', lambda e, j=j, f=f, s=tcnt % 2, ms=ms: e.activation(out=uT[:, f, j * 128:(j + 1) * 128], in_=pt[:, s, 0:128], func=AF.Identity,
                                                                                      bias=mcol[:, ms, 0, f:f + 1], scale=mcol[:, ms, 1, f:f + 1]),
                             reads=[pb, B['mcol']], writes=[B['uT', f]])
                        tcnt += 1
                for fo in range(8):
                    ws_ = wcnt % 2
                    wq = 'sp' if Dm.get('wbf16') else 'pool'
                    P.dma(wq, lambda e, fo=fo, ws_=ws_: e.dma_start(out=wgs[ws_][:], in_=wg[fo]), writes=[B['wgs', ws_]])
                    P.dma(wq, lambda e, fo=fo, ws_=ws_: e.dma_start(out=wbs[ws_][:], in_=wb[fo]), writes=[B['wbs', ws_]])
                    wcnt += 1
                    for br in range(4):
                        s = br % 2
                        for kt in range(8):
                            P.op('pe', lambda e, br=br, kt=kt, s=s, ws_=ws_, ntok=ntok: e.matmul(pg[:, s, 0:ntok], lhsT=wgs[ws_][:, br, kt, :], rhs=uT[:, kt, 0:ntok],
                                                                                               start=(kt == 0), stop=(kt == 7)),
                                 reads=[B['wgs', ws_], B['uT', kt]], writes=[B['pg', s]])
                        for kt in range(4):
                            P.op('pe', lambda e, br=br, kt=kt, s=s, ws_=ws_, ntok=ntok: e.matmul(pp[:, s, 0:ntok], lhsT=wbs[ws_][:, br, kt, :], rhs=yTb[:, br * 4 + kt, 0:ntok],
                                                                                               start=(kt == 0), stop=(kt == 3)),
                                 reads=[B['wbs', ws_], B['yTb']], writes=[B['pp', s]])
                        P.op('act', lambda e, br=br, fo=fo, s=s, ntok=ntok: e.activation(out=sg[s][:, 0:ntok], in_=pg[:, s, 0:ntok], func=AF.Sigmoid,
                                                                                       bias=bgs[:, br, fo:fo + 1], scale=1.0),
                             reads=[B['pg', s], B['bgs']], writes=[B['sg', s]])
                        if br == 0:
                            P.op('dve', lambda e, s=s, ntok=ntok: e.tensor_tensor(out=macc[:, 0:ntok], in0=sg[s][:, 0:ntok], in1=pp[:, s, 0:ntok], op=ALU.mult),
                                 reads=[B['sg', s], B['pp', s]], writes=[B['macc']])
                        else:
                            P.op('dve', lambda e, s=s, ntok=ntok: e.tensor_tensor(out=mtmp[:, 0:ntok], in0=sg[s][:, 0:ntok], in1=pp[:, s, 0:ntok], op=ALU.mult),
                                 reads=[B['sg', s], B['pp', s]], writes=[B['mtmp']])
                            if br < 3:
                                P.op('pool', lambda e, ntok=ntok: e.tensor_tensor(out=macc[:, 0:ntok], in0=macc[:, 0:ntok], in1=mtmp[:, 0:ntok], op=ALU.add),
                                     reads=[B['macc'], B['mtmp']], writes=[B['macc']])
                            else:
                                P.op('pool', lambda e, fo=fo, ntok=ntok: e.tensor_tensor(out=mT[:, fo, 0:ntok], in0=macc[:, 0:ntok], in1=mtmp[:, 0:ntok], op=ALU.add),
                                     reads=[B['macc'], B['mtmp']], writes=[B['mT', fo]])
                for j in range(nt):
                    tile = t0 + j
                    r = rr[tile % 2]
                    rk = ('rr', tile % 2)
                    for hf in range(2):
                        for kt in range(8):
                            P.op('pe', lambda e, j=j, hf=hf, kt=kt: e.matmul(pmx[:, hf, :], lhsT=mT[:, kt, j * 128:(j + 1) * 128], rhs=wos[:, kt, hf * 512:(hf + 1) * 512],
                                                                           start=(kt == 0), stop=(kt == 7)),
                                 reads=[B['mT', kt], B['wos']], writes=[B['pmx', hf]])
                        P.op('dve', lambda e, hf=hf, ms=ms: e.tensor_tensor(out=t1[:, hf * 512:(hf + 1) * 512], in0=pmx[:, hf, :], in1=mrow[:, ms, 0, hf * 512:(hf + 1) * 512], op=ALU.mult),
                             reads=[B['pmx', hf], B['mrow']], writes=[B['t1', hf]])
                    P.op('dve', lambda e, j=j, r=r: e.scalar_tensor_tensor(out=r[:], in0=xs[:, j, :], scalar=ALPHA, in1=t1[:], op0=ALU.mult, op1=ALU.add),
                         reads=[B['xs', j], B['t1', 0], B['t1', 1]], writes=[B[rk]])
                    ln_tile(r, stt, mv, rstd, 0, rk)
                    P.dma('sp', lambda e, tile=tile, r=r: e.dma_start(out=x1o[gt(tile)], in_=r[:]), reads=[B[rk]], writes=[B['x1o', tile]])
                    for f in range(8):
                        pb = B['pt', tcnt % 2]
                        P.op('pe', lambda e, r=r, f=f, s=tcnt % 2: e.transpose(pt[:, s, 0:128], r[:, f * 128:(f + 1) * 128], ids[:]),
                             reads=[B[rk], B['ids']], writes=[pb])
                        if moe:
                            P.op('act', lambda e, f=f, s=tcnt % 2, ms=ms: e.activation(out=u2f[:, f, :], in_=pt[:, s, 0:128], func=AF.Identity,
                                                                                     bias=mcol[:, ms, 2, f:f + 1], scale=mcol[:, ms, 3, f:f + 1]),
                                 reads=[pb, B['mcol']], writes=[B['u2f', f]])
                            P.op('dve', lambda e, f=f, tile=tile: e.tensor_copy(out=u2T[:, f, tile * 128:(tile + 1) * 128], in_=u2f[:, f, :]),
                                 reads=[B['u2f', f]], writes=[B['u2T', f, tile]])
                        else:
                            P.op('act', lambda e, f=f, s=tcnt % 2, ms=ms, tile=tile: e.activation(out=u2T[:, f, tile * 128:(tile + 1) * 128], in_=pt[:, s, 0:128], func=AF.Identity,
                                                                                                bias=mcol[:, ms, 2, f:f + 1], scale=mcol[:, ms, 3, f:f + 1]),
                                 reads=[pb, B['mcol']], writes=[B['u2T', f, tile]])
                        tcnt += 1
                    if moe:
                        for kt in range(8):
                            P.op('pe', lambda e, kt=kt: e.matmul(pg[:, 0, 0:8], lhsT=u2f[:, kt, :], rhs=rts[:, kt, :], start=(kt == 0), stop=(kt == 7)),
                                 reads=[B['u2f', kt], B['rts']], writes=[B['pg', 0]])
                        P.op('dve', lambda e: e.tensor_copy(out=lg[:], in_=pg[:, 0, 0:8]), reads=[B['pg', 0]], writes=[B['lg']])
                        P.op('dve', lambda e: e.max(out=m8[:], in_=lg[:]), reads=[B['lg']], writes=[B['m8']])
                        P.op('dve', lambda e: e.tensor_scalar(out=msk[:], in0=lg[:], scalar1=m8[:, 1:2], scalar2=None, op0=ALU.is_ge), reads=[B['lg'], B['m8']], writes=[B['msk']])
                        P.op('dve', lambda e: e.tensor_scalar(out=sm[:, 0:1], in0=m8[:, 0:1], scalar1=-1.0, scalar2=None, op0=ALU.mult), reads=[B['m8']], writes=[B['sm', 0]])
                        P.op('dve', lambda e: e.tensor_tensor(out=sm[:, 1:2], in0=m8[:, 1:2], in1=m8[:, 0:1], op=ALU.subtract), reads=[B['m8']], writes=[B['sm', 1]])
                        P.op('act', lambda e: e.activation(out=ex[:], in_=lg[:], func=AF.Exp, bias=sm[:, 0:1], scale=1.0), reads=[B['lg'], B['sm', 0]], writes=[B['ex']])
                        P.op('act', lambda e: e.activation(out=sm[:, 2:3], in_=sm[:, 1:2], func=AF.Exp), reads=[B['sm', 1]], writes=[B['sm', 2]])
                        P.op('dve', lambda e: e.tensor_scalar_add(out=sm[:, 2:3], in0=sm[:, 2:3], scalar1=1.0), reads=[B['sm', 2]], writes=[B['sm', 2]])
                        P.op('dve', lambda e: e.reciprocal(out=sm[:, 3:4], in_=sm[:, 2:3]), reads=[B['sm', 2]], writes=[B['sm', 3]])
                        P.op('dve', lambda e: e.tensor_tensor(out=ex[:], in0=ex[:], in1=msk[:], op=ALU.mult), reads=[B['ex'], B['msk']], writes=[B['ex']])
                        P.op('dve', lambda e, tile=tile: e.tensor_scalar(out=wall[:, tile, :], in0=ex[:], scalar1=sm[:, 3:4], scalar2=None, op0=ALU.mult),
                             reads=[B['ex'], B['sm', 3]], writes=[B['wall', tile]])
                        P.op('pe', lambda e, tile=tile: e.transpose(pp[0:8, 0, 0:128], wall[:, tile, :], ids[:]), reads=[B['wall', tile], B['ids']], writes=[B['pp', 0]])
                        P.op('act', lambda e, tile=tile: e.copy(out=WT[:, tile * 128:(tile + 1) * 128], in_=pp[0:8, 0, 0:128]), reads=[B['pp', 0]], writes=[B['WT']])
            P.flush()
        s2o = st.enter_context(contextlib.ExitStack())
        P.stack = s2o
        facc = P.sb("facc", [128, NTILE, D], F32)
        pg1 = P.ps("pg1", [128, 2, 512], F32)
        pg3 = P.ps("pg3", [128, 2, 512], F32)
        pf = P.ps("pf", [128, 2, 512], F32)
        pw = P.ps("pw", [128, 512], F32)
        with contextlib.ExitStack() as s2:
            P.stack = s2
            w1s = [P.sb(f"w1s{i}", [128, 8, KC * 128], BF16) for i in range(2)]
            w3s = [P.sb(f"w3s{i}", [128, 8, KC * 128], BF16) for i in range(2)]
            w2s = [P.sb(f"w2s{i}", [128, KC, D], BF16) for i in range(2)]
            s1b = [P.sb(f"s1b{i}", [128, 512], F32) for i in range(2)]
            hT = [P.sb(f"hT{i}", [128, KC, 512], BF16) for i in range(2)]
            gcnt = 0
            fcnt = 0
            hcnt = 0
            for ci in range(NCH):
                wsl = ci % 2
                ex_ = ci // CPE
                P.dma('pool', lambda e, ci=ci, wsl=wsl: e.dma_start(out=w1s[wsl][:], in_=w1[ci]), writes=[B['w1s', wsl]])
                P.dma('pool', lambda e, ci=ci, wsl=wsl: e.dma_start(out=w3s[wsl][:], in_=w3[ci]), writes=[B['w3s', wsl]])
                P.dma('pool', lambda e, ci=ci, wsl=wsl: e.dma_start(out=w2s[wsl][:], in_=w2[ci]), writes=[B['w2s', wsl]])
                for bi, (t0, nt) in enumerate(BLOCKS):
                    ntok = nt * 128
                    tok0 = t0 * 128
                    hs = hcnt % 2
                    hcnt += 1
                    if moe:
                        P.op('pe', lambda e, ex_=ex_, tok0=tok0, ntok=ntok: e.matmul(pw[:, 0:ntok], lhsT=sels[:, ex_, :], rhs=WT[:, tok0:tok0 + ntok], start=True, stop=True),
                             reads=[B['sels'], B['WT']], writes=[B['pw']])
                    for kk in range(KC):
                        g = gcnt % 2
                        gcnt += 1
                        for kt in range(8):
                            P.op('pe', lambda e, kk=kk, kt=kt, g=g, wsl=wsl, tok0=tok0, ntok=ntok: e.matmul(pg1[:, g, 0:ntok], lhsT=w1s[wsl][:, kt, kk * 128:(kk + 1) * 128],
                                                                                                       rhs=u2T[:, kt, tok0:tok0 + ntok], start=(kt == 0), stop=(kt == 7)),
                                 reads=[B['w1s', wsl], B['u2T']], writes=[B['pg1', g]])
                        for kt in range(8):
                            P.op('pe', lambda e, kk=kk, kt=kt, g=g, wsl=wsl, tok0=tok0, ntok=ntok: e.matmul(pg3[:, g, 0:ntok], lhsT=w3s[wsl][:, kt, kk * 128:(kk + 1) * 128],
                                                                                                       rhs=u2T[:, kt, tok0:tok0 + ntok], start=(kt == 0), stop=(kt == 7)),
                                 reads=[B['w3s', wsl], B['u2T']], writes=[B['pg3', g]])
                        P.op('act', lambda e, g=g, ntok=ntok: e.activation(out=s1b[g][:, 0:ntok], in_=pg1[:, g, 0:ntok], func=AF.Silu), reads=[B['pg1', g]], writes=[B['s1b', g]])
                        if moe:
                            P.op('dve', lambda e, g=g, ntok=ntok: e.tensor_tensor(out=s1b[g][:, 0:ntok], in0=s1b[g][:, 0:ntok], in1=pg3[:, g, 0:ntok], op=ALU.mult),
                                 reads=[B['s1b', g], B['pg3', g]], writes=[B['s1b', g]])
                            P.op('dve', lambda e, g=g, kk=kk, hs=hs, ntok=ntok: e.tensor_tensor(out=hT[hs][:, kk, 0:ntok], in0=s1b[g][:, 0:ntok], in1=pw[:, 0:ntok], op=ALU.mult),
                                 reads=[B['s1b', g], B['pw']], writes=[B['hT', hs, kk]])
                        else:
                            P.op('dve', lambda e, g=g, kk=kk, hs=hs, ntok=ntok: e.tensor_tensor(out=hT[hs][:, kk, 0:ntok], in0=s1b[g][:, 0:ntok], in1=pg3[:, g, 0:ntok], op=ALU.mult),
                                 reads=[B['s1b', g], B['pg3', g]], writes=[B['hT', hs, kk]])
                    for j in range(nt):
                        tile = t0 + j
                        for hf in range(2):
                            fs = fcnt % 2
                            fcnt += 1
                            for kk in range(KC):
                                P.op('pe', lambda e, j=j, hf=hf, kk=kk, fs=fs, hs=hs, wsl=wsl: e.matmul(pf[:, fs, :], lhsT=hT[hs][:, kk, j * 128:(j + 1) * 128],
                                                                                                    rhs=w2s[wsl][:, kk, hf * 512:(hf + 1) * 512], start=(kk == 0), stop=(kk == KC - 1)),
                                     reads=[B['hT', hs, kk], B['w2s', wsl]], writes=[B['pf', fs]])
                            fb = B['facc', tile, hf]
                            if ci == 0:
                                P.op('act', lambda e, tile=tile, hf=hf, fs=fs: e.copy(out=facc[:, tile, hf * 512:(hf + 1) * 512], in_=pf[:, fs, :]), reads=[B['pf', fs]], writes=[fb])
                            else:
                                P.op('dve', lambda e, tile=tile, hf=hf, fs=fs: e.tensor_tensor(out=facc[:, tile, hf * 512:(hf + 1) * 512], in0=facc[:, tile, hf * 512:(hf + 1) * 512],
                                                                                              in1=pf[:, fs, :], op=ALU.add), reads=[B['pf', fs], fb], writes=[fb])
            P.flush()
        with contextlib.ExitStack() as s3:
            P.stack = s3
            mrow = P.sb("mrow3", [128, 2, 2, D], F32)
            lnr = P.sb("lnr3", [128, 4, D], F32)
            P.dma('sp', lambda e: e.dma_start(out=mrow[:], in_=modrows), writes=[B['mrow']])
            P.dma('sp', lambda e: e.dma_start(out=lnr[:], in_=lnrows), writes=[B['lnr']])
            xr = [P.sb(f"xr{i}", [128, D], F32) for i in range(2)]
            t1 = P.sb("t1b", [128, D], F32)
            stt = P.sb("stt2", [128, 2, 6], F32)
            mv = P.sb("mv2", [128, 2], F32)
            rstd = P.sb("rstd2", [128, 1], F32)
            for tile in range(NTILE):
                ms = 1 if tile == 16 else 0
                r = xr[tile % 2]
                rk = ('xr', tile % 2)
                P.dma('sp', lambda e, tile=tile, r=r: e.dma_start(out=r[:], in_=x1o[gt(tile)]), reads=[B['x1o', tile]], writes=[B[rk]])
                P.op('dve', lambda e, tile=tile, ms=ms: e.tensor_tensor(out=t1[:], in0=facc[:, tile, :], in1=mrow[:, ms, 1, :], op=ALU.mult),
                     reads=[B['facc', tile, 0], B['facc', tile, 1], B['mrow']], writes=[B['t1b']])
                P.op('dve', lambda e, r=r: e.scalar_tensor_tensor(out=r[:], in0=r[:], scalar=ALPHA, in1=t1[:], op0=ALU.mult, op1=ALU.add),
                     reads=[B[rk], B['t1b']], writes=[B[rk]])
                ln_tile(r, stt, mv, rstd, 2, rk)
                P.dma('sp', lambda e, tile=tile, r=r: e.dma_start(out=xo[gt(tile)], in_=r[:]), reads=[B[rk]], is_output=True)
            P.flush()


def build_B(moe):
    KC = 4 if moe else 2
    NCH = 56 if moe else 11
    CPE = 7 if moe else 11
    nc = bass.Bass("TRN2", target_bir_lowering=False)
    x = _din(nc, "x", [NTILE, 128, D])
    yT = _din(nc, "yT", [16, 128, NTOK])
    modcols = _din(nc, "modcols", [128, 2, 4, 8])
    modrows = _din(nc, "modrows", [128, 2, 2, D])
    lnrows = _din(nc, "lnrows", [128, 4, D])
    bgc = _din(nc, "bgc", [128, 4, 8])
    ident = _din(nc, "ident", [128, 128])
    wg = _din(nc, "wg", [8, 128, 4, 8, 128])
    wb = _din(nc, "wb", [8, 128, 4, 4, 128])
    wo = _din(nc, "wo", [128, 8, D])
    w1 = _din(nc, "w1", [NCH, 128, 8, KC * 128])
    w3 = _din(nc, "w3", [NCH, 128, 8, KC * 128])
    w2 = _din(nc, "w2", [NCH, 128, KC, D])
    if moe:
        rt = _din(nc, "rt", [128, 8, 8])
        sel = _din(nc, "sel", [8, 8, 128])
    x1o = _dout(nc, "x1o", [NTILE, 128, D])
    xo = _dout(nc, "xo", [NTILE, 128, D])

    Dm = dict(x=x, yT=yT, modcols=modcols, modrows=modrows, lnrows=lnrows, bgc=bgc, ident=ident, wg=wg, wb=wb, wo=wo, w1=w1, w3=w3, w2=w2, x1o=x1o, xo=xo)
    if moe:
        Dm['rt'] = rt
        Dm['sel'] = sel
    with contextlib.ExitStack() as st0:
        P = Prog(nc, st0)
        emit_B(nc, P, Dm, moe)
    return nc


_CACHE = {}


def _prog(name, fn, *a):
    k = (name,) + a
    if k not in _CACHE:
        _CACHE[k] = fn(*a)
    return _CACHE[k]


def _tile_rows(w):
    K, N = w.shape
    return np.ascontiguousarray(w.reshape(K // 128, 128, N).transpose(1, 0, 2))


def _cols(v):
    return np.ascontiguousarray(v.reshape(-1, 128).T)


def _rep(v):
    return np.ascontiguousarray(np.broadcast_to(v[None], (128,) + v.shape))


_IDENT = np.eye(128, dtype=np.float32)


def run_M(c, c_ctx, w_mod, b_mod):
    cc = np.concatenate([c, c_ctx[None]], 0)
    cT = np.ascontiguousarray(cc.T.reshape(8, 128, 5).transpose(1, 0, 2))
    Wm = w_mod.transpose(1, 0, 2).reshape(1024, 4 * 6144)
    bm = b_mod.reshape(4 * 6144)
    ins = []
    for i in range(8):
        sl = slice(i * 3072, (i + 1) * 3072)
        ins.append(dict(cT=cT, w=_tile_rows(Wm[:, sl]), b=np.ascontiguousarray(np.broadcast_to(bm[None, sl], (5, 3072)))))
    res = run_bass_kernel_spmd(_prog('M', build_M), ins, core_ids=list(range(8)))
    mod = np.concatenate([res.results[i]['o'] for i in range(8)], axis=1)
    return mod.reshape(5, 4, 6144)


def prep_B_weights(l, inp):
    moe = (l % 2 == 1)
    i = l // 2
    d = {}
    d['wg'] = np.ascontiguousarray(inp['w_gate'][l].reshape(4, 8, 128, 8, 128).transpose(3, 2, 0, 1, 4))
    d['wb'] = np.ascontiguousarray(inp['w_branch'][l].reshape(4, 4, 128, 8, 128).transpose(3, 2, 0, 1, 4))
    d['wo'] = _tile_rows(inp['w_o'][l])
    d['lnrows'] = _rep(np.stack([inp['ln_g'][l, 0], inp['ln_b'][l, 0], inp['ln_g'][l, 1], inp['ln_b'][l, 1]], 0))
    d['bgc'] = np.ascontiguousarray(inp['b_gate'][l].reshape(4, 8, 128).transpose(2, 0, 1))
    d['ident'] = _IDENT
    if not moe:
        KC, NCH = 2, 11
        d['w1'] = np.ascontiguousarray(inp['ffn_w1'][i].reshape(8, 128, NCH, KC * 128).transpose(2, 1, 0, 3))
        d['w3'] = np.ascontiguousarray(inp['ffn_w3'][i].reshape(8, 128, NCH, KC * 128).transpose(2, 1, 0, 3))
        d['w2'] = np.ascontiguousarray(inp['ffn_w2'][i].reshape(NCH, KC, 128, 1024).transpose(0, 2, 1, 3))
    else:
        KC, CPE = 4, 7
        d['w1'] = np.ascontiguousarray(inp['moe_w1'][i].reshape(8, 8, 128, CPE, KC * 128).transpose(0, 3, 2, 1, 4)).reshape(56, 128, 8, KC * 128)
        d['w3'] = np.ascontiguousarray(inp['moe_w3'][i].reshape(8, 8, 128, CPE, KC * 128).transpose(0, 3, 2, 1, 4)).reshape(56, 128, 8, KC * 128)
        d['w2'] = np.ascontiguousarray(inp['moe_w2'][i].reshape(8, CPE, KC, 128, 1024).transpose(0, 1, 3, 2, 4)).reshape(56, 128, KC, 1024)
        d['rt'] = _tile_rows(inp['router'][i])
        sel = np.zeros((8, 8, 128), np.float32)
        for e in range(8):
            sel[e, e, :] = 1.0
        d['sel'] = sel
    return d


def run_B(l, x_cur, h_cur, YT, mod, inp):
    moe = (l % 2 == 1)
    wd = prep_B_weights(l, inp)
    ins = []
    for b in range(4):
        for half in range(2):
            d = dict(wd)
            xt = np.concatenate([x_cur[b, half * 2048:(half + 1) * 2048], h_cur[b, half * 128:(half + 1) * 128]], 0)
            d['x'] = np.ascontiguousarray(xt.reshape(17, 128, 1024))
            toks = np.concatenate([np.arange(half * 2048, (half + 1) * 2048), 4096 + np.arange(half * 128, (half + 1) * 128)])
            d['yT'] = np.ascontiguousarray(YT[b][:, toks].reshape(16, 128, NTOK))
            ms = [mod[b], mod[4]]
            d['modcols'] = np.ascontiguousarray(np.stack([np.stack([_cols(m[k * 1024:(k + 1) * 1024]) for k in (0, 1, 3, 4)], 1) for m in ms], 1))
            d['modrows'] = _rep(np.stack([np.stack([m[2048:3072], m[5120:6144]], 0) for m in ms], 0))
            ins.append(d)
    res = run_bass_kernel_spmd(_prog('B', build_B, moe), ins, core_ids=list(range(8)))
    x_new = np.empty_like(x_cur)
    h_new = np.empty_like(h_cur)
    x1 = np.empty_like(x_cur)
    for b in range(4):
        for half in range(2):
            o = res.results[b * 2 + half]['xo'].reshape(NTOK, 1024)
            x_new[b, half * 2048:(half + 1) * 2048] = o[:2048]
            h_new[b, half * 128:(half + 1) * 128] = o[2048:]
            x1[b, half * 2048:(half + 1) * 2048] = res.results[b * 2 + half]['x1o'].reshape(NTOK, 1024)[:2048]
    return x_new, h_new, x1


NT_A = 34
NFB = 6
NTOK_A = NT_A * 128
TBLK_A = [(i * 512, 512) for i in range(8)] + [(4096, 256)]
PI = float(np.pi)


def emit_A(nc, P, Dm):
    x = Dm['x']
    modcols = Dm['modcols']
    ident = Dm['ident']
    wA = Dm['wA']
    wR = Dm['wR']
    wH = Dm['wH']
    wD = Dm['wD']
    caw = Dm['caw']
    hcw = Dm['hcw']
    hsk = Dm['hsk']
    ropec = Dm['ropec']
    ropes = Dm['ropes']
    qkn = Dm['qkn']
    rdec = Dm['rdec']
    rtab = Dm['rtab']
    rcol = Dm['rcol']
    featsT = Dm['featsT']
    featsTc = Dm['featsTc']
    hw1 = Dm['hw1']
    hw2 = Dm['hw2']
    hcols = Dm['hcols']
    hw3 = Dm['hw3']
    negt = Dm['negt']
    adel = Dm['adel']
    Fm = Dm['Fm']
    Gm = Dm['Gm']
    Fc = Dm['Fc']
    Gc = Dm['Gc']
    YT = Dm['YT']
    with contextlib.ExitStack() as st:
        P.stack = st
        B = Bufs()
        ids = P.sb("ids", [128, 128], F32)
        idb = P.sb("idb", [128, 128], BF16)
        mcol = P.sb("mcol", [128, 2, 2, 8], F32)
        P.dma('sp', lambda e: e.dma_start(out=ids[:], in_=ident), writes=[B['ids']])
        P.dma('sp', lambda e: e.dma_start(out=mcol[:], in_=modcols), writes=[B['mcol']])
        P.op('dve', lambda e: e.tensor_copy(out=idb[:], in_=ids[:]), reads=[B['ids']], writes=[B['idb']])
        P.op('dve', lambda e: e.tensor_scalar_add(out=mcol[:, :, 1, :], in0=mcol[:, :, 1, :], scalar1=1.0), reads=[B['mcol']], writes=[B['mcol']])
        pt = P.ps("pt", [128, 2, 512], F32)
        ptb = P.ps("ptb", [128, 1024], BF16)
        pa = P.ps("pa", [128, 2, 512], F32)
        pb_ = P.ps("pb", [128, 2, 512], F32)
        pc = P.ps("pc", [128, 512], F32)
        cnt = {'t': 0}

        uTs = Dm.get('uTs')
        ustate = Dm.get('ustate', {'have': False})

        def build_uT(uT, stk):
            P.stack = stk
            if uTs is not None and ustate['have']:
                for f in range(8):
                    P.dma('sp', lambda e, f=f: e.dma_start(out=uT[:, f, :], in_=uTs[:, f, :]), writes=[B['uT']])
                return
            xb = [P.sb(f"xb{i}", [128, D], F32) for i in range(2)]
            for t in range(NT_A):
                ms = 1 if t >= 32 else 0
                xs = xb[t % 2]
                P.dma('sp', lambda e, t=t, xs=xs: e.dma_start(out=xs[:], in_=x[t]), writes=[B['xb', t % 2]])
                for f in range(8):
                    s = cnt['t'] % 2
                    cnt['t'] += 1
                    P.op('pe', lambda e, xs=xs, f=f, s=s: e.transpose(pt[:, s, 0:128], xs[:, f * 128:(f + 1) * 128], ids[:]),
                         reads=[B['xb', t % 2], B['ids']], writes=[B['pt', s]])
                    P.op('act', lambda e, t=t, f=f, s=s, ms=ms: e.activation(out=uT[:, f, t * 128:(t + 1) * 128], in_=pt[:, s, 0:128], func=AF.Identity,
                                                                           bias=mcol[:, ms, 0, f:f + 1], scale=mcol[:, ms, 1, f:f + 1]),
                         reads=[B['pt', s], B['mcol']], writes=[B['uT']])
            if uTs is not None:
                for f in range(8):
                    P.dma('sp', lambda e, f=f: e.dma_start(out=uTs[:, f, :], in_=uT[:, f, :]), reads=[B['uT']], writes=[B['uTs']])
                ustate['have'] = True

        def inproj_fm(uT, wsb, wkey, col0, dst, dkey):
            for bi, (tok0, ntok) in enumerate(TBLK_A):
                s = bi % 2
                for kt in range(8):
                    P.op('pe', lambda e, kt=kt, s=s, tok0=tok0, ntok=ntok: e.matmul(pa[:, s, 0:ntok], lhsT=wsb[:, kt, col0:col0 + 128], rhs=uT[:, kt, tok0:tok0 + ntok],
                                                                                  start=(kt == 0), stop=(kt == 7)),
                         reads=[B[wkey], B['uT']], writes=[B['pa', s]])
                eng = 'act' if bi % 2 == 0 else 'dve'
                if eng == 'act':
                    P.op('act', lambda e, s=s, tok0=tok0, ntok=ntok: e.copy(out=dst[:, tok0:tok0 + ntok], in_=pa[:, s, 0:ntok]), reads=[B['pa', s]], writes=[B[dkey]])
                else:
                    P.op('dve', lambda e, s=s, tok0=tok0, ntok=ntok: e.tensor_copy(out=dst[:, tok0:tok0 + ntok], in_=pa[:, s, 0:ntok]), reads=[B['pa', s]], writes=[B[dkey]])

        def dwconv(z, zkey, wc, wkey, acc, akey, out, okey):
            P.op('dve', lambda e: e.tensor_scalar(out=acc[:], in0=z, scalar1=wc[:, 1:2], scalar2=wc[:, 3:4], op0=ALU.mult, op1=ALU.add),
                 reads=[B[zkey], B[wkey]], writes=[B[akey]])
            for (a, n) in ((0, 4096), (4096, 256)):
                P.op('dve', lambda e, a=a, n=n: e.scalar_tensor_tensor(out=acc[:, a + 1:a + n], in0=z[:, a:a + n - 1], scalar=wc[:, 0:1], in1=acc[:, a + 1:a + n],
                                                                      op0=ALU.mult, op1=ALU.add), reads=[B[zkey], B[wkey], B[akey]], writes=[B[akey]])
                P.op('dve', lambda e, a=a, n=n: e.scalar_tensor_tensor(out=acc[:, a:a + n - 1], in0=z[:, a + 1:a + n], scalar=wc[:, 2:3], in1=acc[:, a:a + n - 1],
                                                                      op0=ALU.mult, op1=ALU.add), reads=[B[zkey], B[wkey], B[akey]], writes=[B[akey]])
            if out is not None:
                P.op('pool', lambda e: e.tensor_copy(out=out, in_=acc[:]), reads=[B[akey]], writes=[B[okey]])

        import os as _os
        _dbg = _os.environ.get('A_DBG', '').split(',')
        P.mute = ('noH' in _dbg)
        sH = st.enter_context(contextlib.ExitStack())
        P.stack = sH
        zH = P.sb("zH", [128, 6, NTOK_A], BF16)
        with contextlib.ExitStack() as s0:
            P.stack = s0
            uT = P.sb("uT", [128, 8, NTOK_A], BF16)
            wHs = P.sb("wHs", [128, 8, 768], BF16)
            P.dma('pool', lambda e: e.dma_start(out=wHs[:], in_=wH), writes=[B['wHs']])
            build_uT(uT, s0)
            for c in range(6):
                inproj_fm(uT, wHs, 'wHs', c * 128, zH[:, c, :], ('zH', c))
            P.flush()
        P.stack = sH
        hsks = P.sb("hsks", [128, 2, 2], F32)
        P.dma('sp', lambda e: e.dma_start(out=hsks[:], in_=hsk), writes=[B['hsks']])
        with contextlib.ExitStack() as s1:
            P.stack = s1
            hcws = P.sb("hcws", [128, 6, 4], F32)
            acc = P.sb("acc", [128, NTOK_A], F32)
            P.dma('sp', lambda e: e.dma_start(out=hcws[:], in_=hcw), writes=[B['hcws']])
            for c in range(6):
                dwconv(zH[:, c, :], ('zH', c), hcws[:, c, :], 'hcws', acc, 'acc', zH[:, c, :], ('zH', c))
            P.flush()
        P.stack = sH
        HTc = P.sb("HTc", [128, 2, 512], BF16)
        Fcs = P.sb("Fcs", [128, 2, 512], BF16)
        Gcs = P.sb("Gcs", [128, 4, 256], BF16)
        P.dma('sp', lambda e: e.dma_start(out=Fcs[:], in_=Fc), writes=[B['Fcs']])
        P.dma('sp', lambda e: e.dma_start(out=Gcs[:], in_=Gc), writes=[B['Gcs']])
        Ft = [P.sb(f"Ft{i}", [128, 1024], BF16) for i in range(NFB)]
        fcnt = {'n': 0}
        h2T = P.sb("h2T", [64, 4352], F32)
        hcs = P.sb("hcs", [64, 6], F32)
        w3s = P.sb("w3s", [64, 2, 3, 256], F32)
        negts = P.sb("negts", [128, 34], F32)
        adels = P.sb("adels", [128, 2, 3, 256], F32)
        npi = P.sb("npi", [64, 1], F32)
        P.dma('sp', lambda e: e.dma_start(out=hcs[:, 0:4], in_=hcols), writes=[B['hcs']])
        P.dma('sp', lambda e: e.dma_start(out=w3s[:, :, 0:2, :], in_=hw3), writes=[B['w3s']])
        P.dma('sp', lambda e: e.dma_start(out=negts[:], in_=negt), writes=[B['negts']])
        P.dma('sp', lambda e: e.dma_start(out=adels[:, :, 0:2, :], in_=adel), writes=[B['adels']])
        P.op('dve', lambda e: e.tensor_scalar(out=w3s[:, :, 2, :], in0=w3s[:, :, 1, :], scalar1=-1.0, scalar2=None, op0=ALU.mult), reads=[B['w3s']], writes=[B['w3s']])
        P.op('dve', lambda e: e.tensor_copy(out=adels[:, :, 2, :], in_=adels[:, :, 1, :]), reads=[B['adels']], writes=[B['adels']])
        P.op('dve', lambda e: e.tensor_tensor(out=hcs[:, 4:6], in0=hcs[:, 0:2], in1=hcs[:, 2:4], op=ALU.mult), reads=[B['hcs']], writes=[B['hcs']])
        P.op('dve', lambda e: e.memset(npi[:], -PI), writes=[B['npi']])

        def sin_layer(src, skey, wmat, wkey2, kdim, li, dst, dkey2, argb):
            for bi, (tok0, ntok) in enumerate(TBLK_A):
                s = bi % 2
                P.op('pe', lambda e, s=s, tok0=tok0, ntok=ntok: e.matmul(pa[0:64, s, 0:ntok], lhsT=wmat[0:kdim, :], rhs=src[0:kdim, tok0:tok0 + ntok], start=True, stop=True),
                     reads=[B[skey], B[wkey2]], writes=[B['pa', s]])
                ab = argb[s]
                P.op('dve', lambda e, s=s, ntok=ntok, ab=ab: e.tensor_scalar(out=ab[:, 0:ntok], in0=pa[0:64, s, 0:ntok], scalar1=hcs[:, 2 + li:3 + li], scalar2=hcs[:, 4 + li:5 + li],
                                                                            op0=ALU.mult, op1=ALU.add), reads=[B['pa', s], B['hcs']], writes=[B['argb', s]])
                m1 = argb[2 + s]
                P.op('dve', lambda e, ntok=ntok, ab=ab, m1=m1: e.tensor_scalar(out=m1[:, 0:ntok], in0=ab[:, 0:ntok], scalar1=PI, scalar2=None, op0=ALU.is_gt),
                     reads=[B['argb', s]], writes=[B['argm', s]])
                P.op('dve', lambda e, ntok=ntok, ab=ab, m1=m1: e.scalar_tensor_tensor(out=ab[:, 0:ntok], in0=m1[:, 0:ntok], scalar=-2.0 * PI, in1=ab[:, 0:ntok], op0=ALU.mult, op1=ALU.add),
                     reads=[B['argb', s], B['argm', s]], writes=[B['argb', s]])
                P.op('dve', lambda e, ntok=ntok, ab=ab, m1=m1: e.tensor_scalar(out=m1[:, 0:ntok], in0=ab[:, 0:ntok], scalar1=-PI, scalar2=None, op0=ALU.is_lt),
                     reads=[B['argb', s], B['argm', s]], writes=[B['argm', s]])
                P.op('dve', lambda e, ntok=ntok, ab=ab, m1=m1: e.scalar_tensor_tensor(out=ab[:, 0:ntok], in0=m1[:, 0:ntok], scalar=2.0 * PI, in1=ab[:, 0:ntok], op0=ALU.mult, op1=ALU.add),
                     reads=[B['argb', s], B['argm', s]], writes=[B['argb', s]])
                P.op('act', lambda e, tok0=tok0, ntok=ntok, ab=ab: e.activation(out=dst[:, tok0:tok0 + ntok], in_=ab[:, 0:ntok], func=AF.Sin),
                     reads=[B['argb', s]], writes=[B[dkey2]])

        with contextlib.ExitStack() as sm:
            P.stack = sm
            h1T = P.sb("h1T", [64, 4352], F32)
            w2s = P.sb("w2s", [64, 64], F32)
            argb = [P.sb(f"argb{i}", [64, 512], F32) for i in range(4)]
            P.dma('sp', lambda e: e.dma_start(out=w2s[:], in_=hw2), writes=[B['w2s']])
            with contextlib.ExitStack() as sm2:
                P.stack = sm2
                fT = P.sb("fT", [33, 4352], F32)
                w1s = P.sb("w1s", [33, 64], F32)
                P.dma('sp', lambda e: e.dma_start(out=fT[:, 0:4096], in_=featsT), writes=[B['fT']])
                P.dma('sp', lambda e: e.dma_start(out=fT[:, 4096:4352], in_=featsTc), writes=[B['fT']])
                P.dma('sp', lambda e: e.dma_start(out=w1s[:], in_=hw1), writes=[B['w1s']])
                sin_layer(fT, 'fT', w1s, 'w1s', 33, 0, h1T, 'h1T', argb)
                P.flush()
            sin_layer(h1T, 'h1T', w2s, 'w2s', 64, 1, h2T, 'h2T', argb)
            P.flush()

        gcnt = {'n': 0}
        for o in range(2):
          with contextlib.ExitStack() as so:
            P.stack = so
            HT = P.sb(f"HT{o}", [128, 2, 8192], BF16)
            with contextlib.ExitStack() as sf:
                P.stack = sf
                env = [P.sb(f"env{i}", [128, 768], F32) for i in range(2)]
                filt = P.sb("filt", [128, 34, 768], BF16)
                for t in range(NT_A):
                    s = t % 2
                    ev = env[s]
                    P.op('act', lambda e, t=t, o=o, ev=ev: e.activation(out=ev[:], in_=adels[:, o, :, :].rearrange("p a b -> p (a b)"), func=AF.Exp, scale=negts[:, t:t + 1]),
                         reads=[B['adels'], B['negts']], writes=[B['env', s]])
                    P.op('pe', lambda e, t=t, o=o, s=s: e.matmul(pb_[:, s, 0:512], lhsT=h2T[:, t * 128:(t + 1) * 128], rhs=w3s[:, o, 0:2, :].rearrange("p a b -> p (a b)"), start=True, stop=True),
                         reads=[B['h2T'], B['w3s']], writes=[B['pb', s, 0]])
                    P.op('dve', lambda e, t=t, s=s, ev=ev: e.tensor_tensor(out=filt[:, t, 0:512], in0=pb_[:, s, 0:512], in1=ev[:, 0:512], op=ALU.mult),
                         reads=[B['pb', s, 0], B['env', s]], writes=[B['filt', t]])
                P.op('dve', lambda e: e.memset(filt[0:1, 0, 256:512], 0.0), reads=[B['filt', 0]], writes=[B['filt', 0]])
                P.op('dve', lambda e: e.memset(filt[0:1, 32, 256:512], 0.0), reads=[B['filt', 32]], writes=[B['filt', 32]])
                for t in range(NT_A):
                    P.op('pool', lambda e, t=t: e.tensor_tensor(out=filt[:, t, 512:768], in0=filt[:, t, 0:256], in1=filt[:, t, 256:512], op=ALU.add),
                         reads=[B['filt', t]], writes=[B['filt', t]])
                    P.op('pool', lambda e, t=t: e.tensor_tensor(out=filt[:, t, 0:256], in0=filt[:, t, 0:256], in1=filt[:, t, 256:512], op=ALU.subtract),
                         reads=[B['filt', t]], writes=[B['filt', t]])
                for j in range(8):
                    for t in range(32):
                        fs = fcnt['n'] % NFB
                        fcnt['n'] += 1
                        P.dma('sp' if fcnt['n'] % 2 else 'act', lambda e, t=t, j=j, fs=fs: e.dma_start(out=Ft[fs][:], in_=Fm[t, :, j, :]), writes=[B['Ft', fs]])
                        for c in range(2):
                            P.op('pe', lambda e, t=t, c=c, fs=fs: e.matmul(pa[:, c, :], lhsT=filt[:, t, 512 + c * 128:512 + (c + 1) * 128], rhs=Ft[fs][:, 0:512], start=(t == 0), stop=(t == 31)),
                                 reads=[B['filt', t], B['Ft', fs]], writes=[B['pa', c]])
                            P.op('pe', lambda e, t=t, c=c, fs=fs: e.matmul(pb_[:, c, :], lhsT=filt[:, t, c * 128:(c + 1) * 128], rhs=Ft[fs][:, 512:1024], start=(t == 0), stop=(t == 31)),
                                 reads=[B['filt', t], B['Ft', fs]], writes=[B['pb', c, 0]])
                    for c in range(2):
                        P.op('act', lambda e, c=c, j=j: e.copy(out=HT[:, c, j * 1024:j * 1024 + 512], in_=pa[:, c, :]), reads=[B['pa', c]], writes=[B['HT', c]])
                        P.op('dve', lambda e, c=c, j=j: e.tensor_copy(out=HT[:, c, j * 1024 + 512:(j + 1) * 1024], in_=pb_[:, c, :]), reads=[B['pb', c, 0]], writes=[B['HT', c]])
                for c in range(2):
                    for t in range(2):
                        tt = 32 + t
                        P.op('pe', lambda e, t=t, tt=tt, c=c: e.matmul(pa[:, c, 0:256], lhsT=filt[:, tt, 512 + c * 128:512 + (c + 1) * 128], rhs=Fcs[:, t, 0:256], start=(t == 0), stop=(t == 1)),
                             reads=[B['filt', tt], B['Fcs']], writes=[B['pa', c]])
                        P.op('pe', lambda e, t=t, tt=tt, c=c: e.matmul(pb_[:, c, 0:256], lhsT=filt[:, tt, c * 128:(c + 1) * 128], rhs=Fcs[:, t, 256:512], start=(t == 0), stop=(t == 1)),
                             reads=[B['filt', tt], B['Fcs']], writes=[B['pb', c, 0]])
                    P.op('act', lambda e, c=c: e.copy(out=HTc[:, c, 0:256], in_=pa[:, c, 0:256]), reads=[B['pa', c]], writes=[B['HTc', c]])
                    P.op('dve', lambda e, c=c: e.tensor_copy(out=HTc[:, c, 256:512], in_=pb_[:, c, 0:256]), reads=[B['pb', c, 0]], writes=[B['HTc', c]])
                P.flush()
            with contextlib.ExitStack() as s3:
                P.stack = s3
                stm = P.sb("stm", [128, NT_A, 256], BF16)
                YhT = P.sb("YhT", [128, 2, 1024], BF16)
                Ytm = P.sb("Ytm", [128, 68, 256], BF16)
                Gt = [P.sb(f"Gt{i}", [128, 1024], BF16) for i in range(NFB)]
                pr1 = P.sb("pr1", [128, 512], F32)
                pr2 = P.sb("pr2", [128, 512], F32)
                yst = P.sb("yst", [128, 512], F32)
                ysk = P.sb("ysk", [128, 512], F32)
                for t in range(NT_A):
                    for c in range(2):
                        P.op('pe', lambda e, t=t, c=c: e.transpose(ptb[:, c * 128:(c + 1) * 128], zH[:, c, t * 128:(t + 1) * 128], idb[:]),
                             reads=[B['zH', c], B['idb']], writes=[B['ptb']])
                    if t % 2 == 0:
                        P.op('act', lambda e, t=t: e.copy(out=stm[:, t, :], in_=ptb[:, 0:256]), reads=[B['ptb']], writes=[B['stm', t]])
                    else:
                        P.op('dve', lambda e, t=t: e.tensor_copy(out=stm[:, t, :], in_=ptb[:, 0:256]), reads=[B['ptb']], writes=[B['stm', t]])

                def product(c, ucs, uss, hc, hs, n, keyc, keys_):
                    P.op('dve', lambda e: e.tensor_tensor(out=pr1[:, 0:n], in0=ucs, in1=hc, op=ALU.mult), reads=[B[keyc]], writes=[B['pr1']])
                    P.op('dve', lambda e: e.tensor_tensor(out=pr2[:, 0:n], in0=uss, in1=hs, op=ALU.mult), reads=[B[keys_]], writes=[B['pr2']])
                    P.op('pool', lambda e: e.tensor_tensor(out=YhT[:, c, 0:n], in0=pr1[:, 0:n], in1=pr2[:, 0:n], op=ALU.subtract), reads=[B['pr1'], B['pr2']], writes=[B['YhT', c]])
                    P.op('dve', lambda e: e.tensor_tensor(out=pr1[:, 0:n], in0=ucs, in1=hs, op=ALU.mult), reads=[B[keyc], B['pr1']], writes=[B['pr1']])
                    P.op('dve', lambda e: e.tensor_tensor(out=pr2[:, 0:n], in0=uss, in1=hc, op=ALU.mult), reads=[B[keys_], B['pr2']], writes=[B['pr2']])
                    P.op('pool', lambda e: e.tensor_tensor(out=YhT[:, c, 512:512 + n], in0=pr1[:, 0:n], in1=pr2[:, 0:n], op=ALU.add), reads=[B['pr1'], B['pr2']], writes=[B['YhT', c]])

                def ytrans(kts, cols):
                    for kt, c0 in zip(kts, cols):
                        for c in range(2):
                            P.op('pe', lambda e, c=c, c0=c0: e.transpose(ptb[:, c * 128:(c + 1) * 128], YhT[:, c, c0:c0 + 128], idb[:]),
                                 reads=[B['YhT', c], B['idb']], writes=[B['ptb']])
                        if kt % 2 == 0:
                            P.op('act', lambda e, kt=kt: e.copy(out=Ytm[:, kt, :], in_=ptb[:, 0:256]), reads=[B['ptb']], writes=[B['Ytm', kt]])
                        else:
                            P.op('dve', lambda e, kt=kt: e.tensor_copy(out=Ytm[:, kt, :], in_=ptb[:, 0:256]), reads=[B['ptb']], writes=[B['Ytm', kt]])

                for j in range(8):
                    for t in range(32):
                        fs = fcnt['n'] % NFB
                        fcnt['n'] += 1
                        P.dma('sp' if fcnt['n'] % 2 else 'act', lambda e, t=t, j=j, fs=fs: e.dma_start(out=Ft[fs][:], in_=Fm[t, :, j, :]), writes=[B['Ft', fs]])
                        for c in range(2):
                            P.op('pe', lambda e, t=t, c=c, fs=fs: e.matmul(pa[:, c, :], lhsT=stm[:, t, c * 128:(c + 1) * 128], rhs=Ft[fs][:, 0:512], start=(t == 0), stop=(t == 31)),
                                 reads=[B['stm', t], B['Ft', fs]], writes=[B['pa', c]])
                            P.op('pe', lambda e, t=t, c=c, fs=fs: e.matmul(pb_[:, c, :], lhsT=stm[:, t, c * 128:(c + 1) * 128], rhs=Ft[fs][:, 512:1024], start=(t == 0), stop=(t == 31)),
                                 reads=[B['stm', t], B['Ft', fs]], writes=[B['pb', c, 0]])
                    for c in range(2):
                        product(c, pa[:, c, :], pb_[:, c, :], HT[:, c, j * 1024:j * 1024 + 512], HT[:, c, j * 1024 + 512:(j + 1) * 1024], 512, ('pa', c), ('pb', c, 0))
                    ytrans([j * 4 + i for i in range(4)] + [32 + j * 4 + i for i in range(4)], [i * 128 for i in range(4)] + [512 + i * 128 for i in range(4)])
                for c in range(2):
                    for t in range(2):
                        P.op('pe', lambda e, t=t, c=c: e.matmul(pa[:, c, 0:256], lhsT=stm[:, 32 + t, c * 128:(c + 1) * 128], rhs=Fcs[:, t, 0:256], start=(t == 0), stop=(t == 1)),
                             reads=[B['stm', 32 + t], B['Fcs']], writes=[B['pa', c]])
                        P.op('pe', lambda e, t=t, c=c: e.matmul(pb_[:, c, 0:256], lhsT=stm[:, 32 + t, c * 128:(c + 1) * 128], rhs=Fcs[:, t, 256:512], start=(t == 0), stop=(t == 1)),
                             reads=[B['stm', 32 + t], B['Fcs']], writes=[B['pb', c, 0]])
                    product(c, pa[:, c, 0:256], pb_[:, c, 0:256], HTc[:, c, 0:256], HTc[:, c, 256:512], 256, ('pa', c), ('pb', c, 0))
                ytrans([64, 65, 66, 67], [0, 128, 512, 640])

                def finish_blk(c, ps_ap, pkey, tok0, n, scale):
                    P.op('pool', lambda e: e.tensor_scalar(out=ysk[:, 0:n], in0=zH[:, c, tok0:tok0 + n], scalar1=hsks[:, o, c:c + 1], scalar2=None, op0=ALU.mult),
                         reads=[B['zH', c], B['hsks']], writes=[B['ysk']])
                    P.op('dve', lambda e: e.scalar_tensor_tensor(out=yst[:, 0:n], in0=ps_ap, scalar=scale, in1=ysk[:, 0:n], op0=ALU.mult, op1=ALU.add),
                         reads=[B[pkey], B['ysk']], writes=[B['yst']])
                    if o == 0:
                        P.op('pool', lambda e: e.tensor_tensor(out=zH[:, c, tok0:tok0 + n], in0=yst[:, 0:n], in1=zH[:, 2 + c, tok0:tok0 + n], op=ALU.mult),
                             reads=[B['yst'], B['zH', 2 + c]], writes=[B['zH', c]])
                    else:
                        P.op('pool', lambda e: e.tensor_tensor(out=yst[:, 0:n], in0=yst[:, 0:n], in1=zH[:, 4 + c, tok0:tok0 + n], op=ALU.mult),
                             reads=[B['yst'], B['zH', 4 + c]], writes=[B['yst']])
                        P.dma('sp', lambda e: e.dma_start(out=YT[4 + c, :, tok0:tok0 + n], in_=yst[:, 0:n]), reads=[B['yst']], is_output=True)

                for ps_ in range(4):
                    for kt in range(64):
                        gs = gcnt['n'] % NFB
                        gcnt['n'] += 1
                        P.dma('sp' if gcnt['n'] % 2 else 'act', lambda e, ps_=ps_, kt=kt, gs=gs: e.dma_start(out=Gt[gs][:], in_=Gm[ps_, kt]), writes=[B['Gt', gs]])
                        for c in range(2):
                            for nb in range(2):
                                acc_ap = (pa if c == 0 else pb_)[:, nb, :]
                                P.op('pe', lambda e, kt=kt, c=c, nb=nb, gs=gs, acc_ap=acc_ap: e.matmul(acc_ap, lhsT=Ytm[:, kt, c * 128:(c + 1) * 128], rhs=Gt[gs][:, nb * 512:(nb + 1) * 512],
                                                                                                  start=(kt == 0), stop=(kt == 63)),
                                     reads=[B['Ytm', kt], B['Gt', gs]], writes=[B['pa', nb] if c == 0 else B['pb', nb, 0]])
                    for c in range(2):
                        for nb in range(2):
                            finish_blk(c, (pa if c == 0 else pb_)[:, nb, :], ('pa', nb) if c == 0 else ('pb', nb, 0), ps_ * 1024 + nb * 512, 512, 2.0 / 8192.0)
                for c in range(2):
                    for kt in range(4):
                        P.op('pe', lambda e, kt=kt, c=c: e.matmul(pc[:, 0:256], lhsT=Ytm[:, 64 + kt, c * 128:(c + 1) * 128], rhs=Gcs[:, kt, :], start=(kt == 0), stop=(kt == 3)),
                             reads=[B['Ytm', 64 + kt], B['Gcs']], writes=[B['pc']])
                    finish_blk(c, pc[:, 0:256], 'pc', 4096, 256, 2.0 / 512.0)
                P.flush()
        sH.close()
        P.stack = st
        P.mute = ('noR' in _dbg)
        build_A_rest(nc, P, B, st, x, wA, wR, wD, caw, ropec, ropes, qkn, rdec, rtab, rcol, YT, ids, idb, mcol, pt, ptb, pa, pb_, pc, build_uT, inproj_fm, dwconv)


def build_A():
    nc = bass.Bass("TRN2", target_bir_lowering=False)
    x = _din(nc, "x", [NT_A, 128, D])
    modcols = _din(nc, "modcols", [128, 2, 2, 8])
    ident = _din(nc, "ident", [128, 128])
    wA = _din(nc, "wA", [128, 8, 768])
    wR = _din(nc, "wR", [128, 8, 1024])
    wH = _din(nc, "wH", [128, 8, 768])
    wD = _din(nc, "wD", [128, 8, 512])
    caw = _din(nc, "caw", [128, 2, 4])
    hcw = _din(nc, "hcw", [128, 6, 4])
    hsk = _din(nc, "hsk", [128, 2, 2])
    ropec = _din(nc, "ropec", [128, 32, 64])
    ropes = _din(nc, "ropes", [128, 32, 64])
    qkn = _din(nc, "qkn", [128, 2, 128])
    rdec = _din(nc, "rdec", [128, 4])
    rtab = _din(nc, "rtab", [128, 6, 128])
    rcol = _din(nc, "rcol", [128, 4])
    featsT = _din(nc, "featsT", [33, 4096])
    featsTc = _din(nc, "featsTc", [33, 256])
    hw1 = _din(nc, "hw1", [33, 64])
    hw2 = _din(nc, "hw2", [64, 64])
    hcols = _din(nc, "hcols", [64, 4])
    hw3 = _din(nc, "hw3", [64, 2, 2, 256])
    negt = _din(nc, "negt", [128, 34])
    adel = _din(nc, "adel", [128, 2, 2, 256])
    Fm = _din(nc, "Fm", [32, 128, 8, 1024], BF16)
    Gm = _din(nc, "Gm", [4, 64, 128, 1024], BF16)
    Fc = _din(nc, "Fc", [128, 2, 512], BF16)
    Gc = _din(nc, "Gc", [128, 4, 256], BF16)
    YT = _dout(nc, "YT", [8, 128, NTOK_A])

    Dm = dict(x=x, modcols=modcols, ident=ident, wA=wA, wR=wR, wH=wH, wD=wD, caw=caw, hcw=hcw, hsk=hsk, ropec=ropec, ropes=ropes, qkn=qkn, rdec=rdec, rtab=rtab, rcol=rcol, featsT=featsT, featsTc=featsTc, hw1=hw1, hw2=hw2, hcols=hcols, hw3=hw3, negt=negt, adel=adel, Fm=Fm, Gm=Gm, Fc=Fc, Gc=Gc, YT=YT)
    with contextlib.ExitStack() as st0:
        P = Prog(nc, st0)
        emit_A(nc, P, Dm)
    return nc


def build_A_rest(nc, P, B, st, x, wA, wR, wD, caw, ropec, ropes, qkn, rdec, rtab, rcol, YT, ids, idb, mcol, pt, ptb, pa, pb_, pc, build_uT, inproj_fm, dwconv):
    SC = 128.0 ** -0.5
    sG = st.enter_context(contextlib.ExitStack())
    P.stack = sG
    uT = P.sb("uT2", [128, 8, NTOK_A], BF16)
    rc = P.sb("rc", [128, 32, 64], F32)
    rs = P.sb("rs", [128, 32, 64], F32)
    P.dma('sp', lambda e: e.dma_start(out=rc[:], in_=ropec), writes=[B['rope']])
    P.dma('sp', lambda e: e.dma_start(out=rs[:], in_=ropes), writes=[B['rope']])
    with contextlib.ExitStack() as s0:
        build_uT(uT, s0)
        P.flush()

    def rope(src, skey, t, tmp, tkey):
        for ax in range(2):
            x1 = src[:, ax * 64:ax * 64 + 32]
            x2 = src[:, ax * 64 + 32:ax * 64 + 64]
            c_ = rc[:, t, ax * 32:(ax + 1) * 32]
            s_ = rs[:, t, ax * 32:(ax + 1) * 32]
            P.op('dve', lambda e, x1=x1, c_=c_: e.tensor_tensor(out=tmp[:, 0, :], in0=x1, in1=c_, op=ALU.mult), reads=[B[skey], B['rope']], writes=[B[tkey, 0]])
            P.op('dve', lambda e, x2=x2, s_=s_: e.tensor_tensor(out=tmp[:, 1, :], in0=x2, in1=s_, op=ALU.mult), reads=[B[skey], B['rope']], writes=[B[tkey, 1]])
            P.op('pool', lambda e, x2=x2, c_=c_: e.tensor_tensor(out=tmp[:, 2, :], in0=x2, in1=c_, op=ALU.mult), reads=[B[skey], B['rope']], writes=[B[tkey, 2]])
            P.op('pool', lambda e, x1=x1, s_=s_: e.tensor_tensor(out=tmp[:, 3, :], in0=x1, in1=s_, op=ALU.mult), reads=[B[skey], B['rope']], writes=[B[tkey, 3]])
            P.op('dve', lambda e, x1=x1: e.tensor_tensor(out=x1, in0=tmp[:, 0, :], in1=tmp[:, 1, :], op=ALU.subtract), reads=[B[tkey, 0], B[tkey, 1], B[tkey, 3], B[skey]], writes=[B[skey]])
            P.op('pool', lambda e, x2=x2: e.tensor_tensor(out=x2, in0=tmp[:, 2, :], in1=tmp[:, 3, :], op=ALU.add), reads=[B[tkey, 2], B[tkey, 3], B[tkey, 1], B[skey]], writes=[B[skey]])

    import os as _os
    _dbg = _os.environ.get('A_DBG', '').split(',')
    _base = P.mute
    P.mute = _base or ('noS' in _dbg)
    with contextlib.ExitStack() as s1:
        P.stack = s1
        wAs = P.sb("wAs", [128, 8, 768], BF16)
        caws = P.sb("caws", [128, 2, 4], F32)
        P.dma('pool', lambda e: e.dma_start(out=wAs[:], in_=wA), writes=[B['wAs']])
        P.dma('sp', lambda e: e.dma_start(out=caws[:], in_=caw), writes=[B['caws']])
        zA = P.sb("zA", [128, 3, NTOK_A], BF16)
        pp_ = P.sb("pp_", [128, NTOK_A], BF16)
        acc = P.sb("accA", [128, NTOK_A], F32)
        for c in range(2):
            for g in range(3):
                inproj_fm(uT, wAs, 'wAs', g * 256 + c * 128, zA[:, g, :], ('zA', g))
            P.op('pool', lambda e: e.tensor_tensor(out=pp_[:], in0=zA[:, 1, :], in1=zA[:, 2, :], op=ALU.mult), reads=[B['zA', 1], B['zA', 2]], writes=[B['pp_']])
            dwconv(pp_[:], 'pp_', caws[:, c, :], 'caws', acc, 'accA', None, None)
            P.op('dve', lambda e: e.tensor_tensor(out=acc[:], in0=acc[:], in1=zA[:, 0, :], op=ALU.mult), reads=[B['accA'], B['zA', 0]], writes=[B['accA']])
            P.dma('sp', lambda e, c=c: e.dma_start(out=YT[0 + c], in_=acc[:]), reads=[B['accA']], is_output=True)
        P.flush()

    P.mute = _base or ('noD' in _dbg)
    with contextlib.ExitStack() as s2:
        P.stack = s2
        wDs = P.sb("wDs", [128, 8, 512], BF16)
        qkns = P.sb("qkns", [128, 2, 128], F32)
        P.dma('pool', lambda e: e.dma_start(out=wDs[:], in_=wD), writes=[B['wDs']])
        P.dma('sp', lambda e: e.dma_start(out=qkns[:], in_=qkn), writes=[B['qkns']])
        qT = P.sb("qT", [128, 2, NTOK_A], BF16)
        kT = P.sb("kT", [128, NTOK_A], BF16)
        vtm = P.sb("vtm", [128, NT_A, 128], BF16)
        onesb = P.sb("onesb", [128, 128], BF16)
        P.op('dve', lambda e: e.memset(onesb[:], 1.0), writes=[B['onesb']])
        qn = [P.sb(f"qn{i}", [128, 3, 128], F32) for i in range(2)]
        qb = [P.sb(f"qb{i}", [128, 3, 128], BF16) for i in range(2)]
        sq3 = P.sb("sq3", [128, 384], F32)
        ss = [P.sb(f"ss{i}", [128, 4], F32) for i in range(2)]
        tmp = P.sb("tmpr", [128, 4, 32], F32)
        for t in range(NT_A):
            s = t % 2
            for kt in range(8):
                P.op('pe', lambda e, t=t, kt=kt, s=s: e.matmul(pa[:, s, :], lhsT=uT[:, kt, t * 128:(t + 1) * 128], rhs=wDs[:, kt, :], start=(kt == 0), stop=(kt == 7)),
                     reads=[B['uT'], B['wDs']], writes=[B['pa', s]])
            P.op('act', lambda e, s=s: e.activation(out=sq3[:], in_=pa[:, s, 0:384], func=AF.Square), reads=[B['pa', s]], writes=[B['sq3']])
            P.op('dve', lambda e, s=s: e.reduce_sum(out=ss[s][:, 0:3], in_=sq3[:].rearrange("p (h d) -> p h d", h=3), axis=AX.X),
                 reads=[B['sq3']], writes=[B['ss', s, 0], B['ss', s, 1], B['ss', s, 2]])
            P.op('dve', lambda e, s=s: e.tensor_scalar(out=ss[s][:, 0:3], in0=ss[s][:, 0:3], scalar1=1.0 / 128.0, scalar2=1e-6, op0=ALU.mult, op1=ALU.add),
                 reads=[B['ss', s, h] for h in range(3)], writes=[B['ss', s, 'a']])
            P.op('act', lambda e, s=s: e.sqrt(out=ss[s][:, 0:3], in_=ss[s][:, 0:3]), reads=[B['ss', s, 'a']], writes=[B['ss', s, 'a']])
            P.op('dve', lambda e, s=s: e.reciprocal(out=ss[s][:, 0:3], in_=ss[s][:, 0:3]), reads=[B['ss', s, 'a']], writes=[B['ss', s, 'a']])
            for h in range(3):
                P.op('dve', lambda e, h=h, s=s: e.scalar_tensor_tensor(out=qn[s][:, h, :], in0=pa[:, s, h * 128:(h + 1) * 128], scalar=ss[s][:, h:h + 1], in1=qkns[:, 0 if h < 2 else 1, :],
                                                                     op0=ALU.mult, op1=ALU.mult), reads=[B['pa', s], B['ss', s, 'a'], B['qkns']], writes=[B['qn', s, h]])
                if t < 32:
                    rope(qn[s][:, h, :], ('qn', s, h), t, tmp, 'tmpr')
                P.op('pool', lambda e, h=h, s=s: e.tensor_copy(out=qb[s][:, h, :], in_=qn[s][:, h, :]), reads=[B['qn', s, h]], writes=[B['qb', s, h]])
            P.op('act', lambda e, t=t, s=s: e.copy(out=vtm[:, t, :], in_=pa[:, s, 384:512]), reads=[B['pa', s]], writes=[B['vtm', t]])
            for h in range(3):
                P.op('pe', lambda e, h=h, s=s: e.transpose(ptb[:, h * 128:(h + 1) * 128], qb[s][:, h, :], idb[:]), reads=[B['qb', s, h], B['idb']], writes=[B['ptb']])
            P.op('dve', lambda e, t=t: e.tensor_copy(out=qT[:, :, t * 128:(t + 1) * 128], in_=ptb[:, 0:256].rearrange("p (h d) -> p h d", h=2)), reads=[B['ptb']], writes=[B['qT']])
            P.op('act', lambda e, t=t: e.copy(out=kT[:, t * 128:(t + 1) * 128], in_=ptb[:, 256:384]), reads=[B['ptb']], writes=[B['kT']])
        if 'dumpQK' in _dbg:
            dq = P.sb("dq", [128, NTOK_A], F32)
            for i_, src_ in enumerate((qT[:, 0, :], qT[:, 1, :], kT[:])):
                P.op('dve', lambda e, src_=src_: e.tensor_copy(out=dq[:], in_=src_), reads=[B['qT'], B['kT']], writes=[B['dq']])
                P.dma('sp', lambda e, i_=i_: e.dma_start(out=YT[5 + i_], in_=dq[:]), reads=[B['dq']], writes=[B['dqo', i_]], is_output=True)
            P.op('dve', lambda e: e.tensor_copy(out=dq[:].rearrange("p (t d) -> p t d", d=128), in_=vtm[:]), reads=[B['vtm', t_] for t_ in range(NT_A)], writes=[B['dq']])
            P.dma('sp', lambda e: e.dma_start(out=YT[4], in_=dq[:]), reads=[B['dq']], is_output=True)
        P.mute = P.mute or ('noD2' in _dbg)
        pT = [P.sb(f"pT{i}", [128, 512], BF16) for i in range(3)]
        rd = P.sb("rd", [128, 512], F32)
        yo = [P.sb(f"yo{i}", [128, 512], F32) for i in range(2)]
        pcnt = 0
        ocnt = 0
        for h in range(2):
            for bi, (tok0, ntok) in enumerate(TBLK_A):
                if 'cOne' in _dbg and (h, bi) != (0, 0):
                    continue
                ktiles = list(range(34)) if bi < 8 else [32, 33]
                nkt = len(ktiles)
                slots = []
                for ki, kt in enumerate(ktiles):
                    slots.append((pcnt % 2, pcnt % 3))
                    pcnt += 1

                def qk_exp(ki):
                    kt = ktiles[ki]
                    s, p3 = slots[ki]
                    P.op('pe', lambda e, kt=kt, h=h, s=s, tok0=tok0, ntok=ntok: e.matmul(pa[:, s, 0:ntok], lhsT=kT[:, kt * 128:(kt + 1) * 128], rhs=qT[:, h, tok0:tok0 + ntok], start=True, stop=True),
                         reads=[B['kT'], B['qT']], writes=[B['pa', s]])
                    P.op('act', lambda e, s=s, p3=p3, ntok=ntok: e.activation(out=pT[p3][:, 0:ntok], in_=pa[:, s, 0:ntok], func=AF.Exp, scale=SC), reads=[B['pa', s]], writes=[B['pT', p3]])

                def pv(ki):
                    kt = ktiles[ki]
                    s, p3 = slots[ki]
                    P.op('pe', lambda e, kt=kt, p3=p3, ntok=ntok, ki=ki, nkt=nkt: e.matmul(pb_[:, 0, 0:ntok], lhsT=vtm[:, kt, :], rhs=pT[p3][:, 0:ntok], start=(ki == 0), stop=(ki == nkt - 1)),
                         reads=[B['vtm', kt], B['pT', p3]], writes=[B['pb', 0, 0]])
                    P.op('pe', lambda e, p3=p3, ntok=ntok, ki=ki, nkt=nkt: e.matmul(pb_[:, 1, 0:ntok], lhsT=onesb[:], rhs=pT[p3][:, 0:ntok], start=(ki == 0), stop=(ki == nkt - 1)),
                         reads=[B['onesb'], B['pT', p3]], writes=[B['pb', 1, 0]])

                qk_exp(0)
                for ki in range(1, nkt):
                    qk_exp(ki)
                    pv(ki - 1)
                pv(nkt - 1)
                if 'cQK' in _dbg:
                    continue
                P.op('dve', lambda e, ntok=ntok: e.reciprocal(out=rd[:, 0:ntok], in_=pb_[:, 1, 0:ntok]), reads=[B['pb', 1, 0]], writes=[B['rd']])
                y = yo[ocnt % 2]
                yk = ('yo', ocnt % 2)
                ocnt += 1
                P.op('dve', lambda e, ntok=ntok, y=y: e.tensor_tensor(out=y[:, 0:ntok], in0=pb_[:, 0, 0:ntok], in1=rd[:, 0:ntok], op=ALU.mult), reads=[B['pb', 0, 0], B['rd']], writes=[B[yk]])
                P.dma('sp', lambda e, h=h, tok0=tok0, ntok=ntok, y=y: e.dma_start(out=YT[6 + h, :, tok0:tok0 + ntok], in_=y[:, 0:ntok]), reads=[B[yk]], is_output=True)
        P.flush()

    P.mute = _base or ('noT' in _dbg)
    with contextlib.ExitStack() as s3:
        P.stack = s3
        wRs = P.sb("wRs", [128, 8, 1024], BF16)
        P.dma('pool', lambda e: e.dma_start(out=wRs[:], in_=wR), writes=[B['wRs']])
        rds = P.sb("rds", [128, 4], F32)
        lgam = P.sb("lgam", [128, 4], F32)
        rtabs = P.sb("rtabs", [128, 6, 128], F32)
        rcols = P.sb("rcols", [128, 4], F32)
        P.dma('sp', lambda e: e.dma_start(out=rds[:], in_=rdec), writes=[B['rds']])
        P.dma('sp', lambda e: e.dma_start(out=rtabs[:], in_=rtab), writes=[B['rtabs']])
        P.dma('sp', lambda e: e.dma_start(out=rcols[:], in_=rcol), writes=[B['rcols']])
        P.op('act', lambda e: e.activation(out=lgam[:], in_=rds[:], func=AF.Exp), reads=[B['rds']], writes=[B['lgam']])
        P.op('dve', lambda e: e.tensor_scalar(out=lgam[:], in0=lgam[:], scalar1=-1.0, scalar2=None, op0=ALU.mult), reads=[B['lgam']], writes=[B['lgam']])
        mask = P.sb("mask", [128, 128], F32)
        mtmp = P.sb("mtmpR", [128, 128], F32)
        qdf = P.sb("qdf", [128, 128], F32)
        qdb = P.sb("qdb", [128, 128], F32)
        kdc = P.sb("kdc", [128, 4], F32)
        qT = P.sb("rqT", [128, NTOK_A], BF16)
        kT = P.sb("rkT", [128, NTOK_A], BF16)
        qdfT = P.sb("qdfT", [128, NTOK_A], BF16)
        qdbT = P.sb("qdbT", [128, NTOK_A], BF16)
        kdf = P.sb("kdf", [128, NT_A, 128], BF16)
        kdb = P.sb("kdb", [128, NT_A, 128], BF16)
        vtm = P.sb("rvtm", [128, NT_A, 128], BF16)
        sgt = P.sb("sgt", [128, NT_A, 128], BF16)
        Sf = P.sb("Sf", [128, NT_A, 128], BF16)
        Sb = P.sb("Sb", [128, NT_A, 128], BF16)
        stf = [P.sb(f"stf{i}", [128, 128], F32) for i in range(2)]
        qk = [P.sb(f"rqk{i}", [128, 2, 128], F32) for i in range(2)]
        qkb = [P.sb(f"rqkb{i}", [128, 2, 128], BF16) for i in range(2)]
        tmp = P.sb("tmpr2", [128, 4, 32], F32)
        smT = [P.sb(f"smT{i}", [128, 128], BF16) for i in range(2)]
        stt = P.sb("rstt", [128, 6], F32)
        mv = P.sb("rmv", [128, 2], F32)
        rstd = P.sb("rrstd", [128, 1], F32)
        on = [P.sb(f"on{i}", [128, 128], F32) for i in range(2)]
        ob = [P.sb(f"ob{i}", [128, 128], BF16) for i in range(2)]
        ysg = [P.sb(f"rysg{i}", [128, 512], F32) for i in range(2)]
        for h in range(2):
            lf = lgam[:, h:h + 1]
            lb = lgam[:, 2 + h:3 + h]
            P.op('act', lambda e, lf=lf: e.activation(out=mask[:], in_=rtabs[:, 0, :], func=AF.Exp, scale=lf), reads=[B['rtabs'], B['lgam']], writes=[B['mask']])
            P.op('dve', lambda e: e.tensor_tensor(out=mask[:], in0=mask[:], in1=rtabs[:, 1, :], op=ALU.mult), reads=[B['mask'], B['rtabs']], writes=[B['mask']])
            P.op('act', lambda e, lb=lb: e.activation(out=mtmp[:], in_=rtabs[:, 2, :], func=AF.Exp, scale=lb), reads=[B['rtabs'], B['lgam']], writes=[B['mtmpR']])
            P.op('dve', lambda e: e.tensor_tensor(out=mtmp[:], in0=mtmp[:], in1=rtabs[:, 3, :], op=ALU.mult), reads=[B['mtmpR'], B['rtabs']], writes=[B['mtmpR']])
            P.op('dve', lambda e: e.tensor_tensor(out=mask[:], in0=mask[:], in1=mtmp[:], op=ALU.add), reads=[B['mask'], B['mtmpR']], writes=[B['mask']])
            P.op('act', lambda e, lf=lf: e.activation(out=qdf[:], in_=rtabs[:, 4, :], func=AF.Exp, scale=lf), reads=[B['rtabs'], B['lgam']], writes=[B['qdf']])
            P.op('act', lambda e, lb=lb: e.activation(out=qdb[:], in_=rtabs[:, 5, :], func=AF.Exp, scale=lb), reads=[B['rtabs'], B['lgam']], writes=[B['qdb']])
            P.op('act', lambda e, lf=lf: e.activation(out=kdc[:, 0:1], in_=rcols[:, 0:1], func=AF.Exp, scale=lf), reads=[B['rcols'], B['lgam']], writes=[B['kdc', 0]])
            P.op('act', lambda e, lb=lb: e.activation(out=kdc[:, 1:2], in_=rcols[:, 1:2], func=AF.Exp, scale=lb), reads=[B['rcols'], B['lgam']], writes=[B['kdc', 1]])
            P.op('act', lambda e, lf=lf: e.activation(out=kdc[:, 2:3], in_=rcols[:, 2:3], func=AF.Exp, scale=lf), reads=[B['rcols'], B['lgam']], writes=[B['kdc', 2]])
            P.op('act', lambda e, lb=lb: e.activation(out=kdc[:, 3:4], in_=rcols[:, 2:3], func=AF.Exp, scale=lb), reads=[B['rcols'], B['lgam']], writes=[B['kdc', 3]])
            kd = [B['kdc', i] for i in range(4)]
            for t in range(NT_A):
                s = t % 2
                for g in range(4):
                    for kt in range(8):
                        P.op('pe', lambda e, t=t, kt=kt, s=s, g=g, h=h: e.matmul(pa[:, s, g * 128:(g + 1) * 128], lhsT=uT[:, kt, t * 128:(t + 1) * 128], rhs=wRs[:, kt, g * 256 + h * 128:g * 256 + (h + 1) * 128],
                                                                        start=(kt == 0), stop=(kt == 7)), reads=[B['uT'], B['wRs']], writes=[B['pa', s]])
                P.op('act', lambda e, s=s: e.copy(out=qk[s][:, 0, :], in_=pa[:, s, 0:128]), reads=[B['pa', s]], writes=[B['rqk', s, 0]])
                P.op('act', lambda e, s=s: e.activation(out=qk[s][:, 1, :], in_=pa[:, s, 128:256], func=AF.Copy, scale=SC), reads=[B['pa', s]], writes=[B['rqk', s, 1]])
                P.op('act', lambda e, s=s, t=t: e.copy(out=vtm[:, t, :], in_=pa[:, s, 256:384]), reads=[B['pa', s]], writes=[B['rvtm', t]])
                P.op('act', lambda e, s=s, t=t: e.activation(out=sgt[:, t, :], in_=pa[:, s, 384:512], func=AF.Silu), reads=[B['pa', s]], writes=[B['sgt', t]])
                for j in range(2):
                    if t < 32:
                        rope(qk[s][:, j, :], ('rqk', s, j), t, tmp, 'tmpr2')
                    P.op('pool', lambda e, s=s, j=j: e.tensor_copy(out=qkb[s][:, j, :], in_=qk[s][:, j, :]), reads=[B['rqk', s, j]], writes=[B['rqkb', s, j]])
                P.op('dve', lambda e, s=s, t=t: e.tensor_scalar(out=kdf[:, t, :], in0=qk[s][:, 1, :], scalar1=kdc[:, 0:1], scalar2=None, op0=ALU.mult), reads=[B['rqk', s, 1], kd[0]], writes=[B['kdf', t]])
                P.op('dve', lambda e, s=s, t=t: e.tensor_scalar(out=kdb[:, t, :], in0=qk[s][:, 1, :], scalar1=kdc[:, 1:2], scalar2=None, op0=ALU.mult), reads=[B['rqk', s, 1], kd[1]], writes=[B['kdb', t]])
                for j in range(2):
                    P.op('pe', lambda e, s=s, j=j: e.transpose(ptb[:, j * 128:(j + 1) * 128], qkb[s][:, j, :], idb[:]), reads=[B['rqkb', s, j], B['idb']], writes=[B['ptb']])
                sl = slice(t * 128, (t + 1) * 128)
                P.op('act', lambda e, sl=sl: e.copy(out=qT[:, sl], in_=ptb[:, 0:128]), reads=[B['ptb']], writes=[B['rqT', t]])
                P.op('act', lambda e, sl=sl: e.copy(out=kT[:, sl], in_=ptb[:, 128:256]), reads=[B['ptb']], writes=[B['rkT', t]])
                P.op('dve', lambda e, sl=sl: e.tensor_tensor(out=qdfT[:, sl], in0=ptb[:, 0:128], in1=qdf[:], op=ALU.mult), reads=[B['ptb'], B['qdf']], writes=[B['qdfT', t]])
                P.op('dve', lambda e, sl=sl: e.tensor_tensor(out=qdbT[:, sl], in0=ptb[:, 0:128], in1=qdb[:], op=ALU.mult), reads=[B['ptb'], B['qdb']], writes=[B['qdbT', t]])

            def chain(order, kdx, Sx, cd, ckey, name):
                cur = None
                for n_, t in enumerate(order):
                    if cur is None:
                        P.op('pool', lambda e, t=t: e.memset(Sx[:, t, :], 0.0), writes=[B[name, t]])
                    else:
                        P.op('pool', lambda e, t=t, cur=cur: e.tensor_copy(out=Sx[:, t, :], in_=stf[cur][:]), reads=[B['stf', cur]], writes=[B[name, t]])
                    if n_ == len(order) - 1:
                        break
                    P.op('pe', lambda e, t=t: e.matmul(pc[:, 0:128], lhsT=kdx[:, t, :], rhs=vtm[:, t, :], start=True, stop=True), reads=[B[name + 'k', t], B['rvtm', t]], writes=[B['pc']])
                    nxt = 0 if cur is None else 1 - cur
                    if cur is None:
                        P.op('dve', lambda e, nxt=nxt: e.tensor_copy(out=stf[nxt][:], in_=pc[:, 0:128]), reads=[B['pc']], writes=[B['stf', nxt]])
                    else:
                        P.op('dve', lambda e, nxt=nxt, cur=cur: e.scalar_tensor_tensor(out=stf[nxt][:], in0=stf[cur][:], scalar=cd, in1=pc[:, 0:128], op0=ALU.mult, op1=ALU.add),
                             reads=[B['stf', cur], B['pc'], ckey], writes=[B['stf', nxt]])
                    cur = nxt
            for t in range(NT_A):
                B.d[('Sfk', t)] = B['kdf', t]
                B.d[('Sbk', t)] = B['kdb', t]
            chain([32, 33] + list(range(32)), kdf, Sf, kdc[:, 2:3], kd[2], 'Sf')
            chain([33, 32] + list(range(31, -1, -1)), kdb, Sb, kdc[:, 3:4], kd[3], 'Sb')
            for t in range(NT_A):
                s = t % 2
                sl = slice(t * 128, (t + 1) * 128)
                P.op('pe', lambda e, sl=sl, s=s: e.matmul(pa[:, s, 0:128], lhsT=kT[:, sl], rhs=qT[:, sl], start=True, stop=True), reads=[B['rkT', t], B['rqT', t]], writes=[B['pa', s]])
                P.op('dve', lambda e, s=s: e.tensor_tensor(out=smT[s][:], in0=pa[:, s, 0:128], in1=mask[:], op=ALU.mult), reads=[B['pa', s], B['mask']], writes=[B['smT', s]])
                P.op('pe', lambda e, t=t, s=s: e.matmul(pb_[:, s, 0:128], lhsT=smT[s][:], rhs=vtm[:, t, :], start=True, stop=False), reads=[B['smT', s], B['rvtm', t]], writes=[B['pb', s, 0]])
                P.op('pe', lambda e, t=t, s=s, sl=sl: e.matmul(pb_[:, s, 0:128], lhsT=qdfT[:, sl], rhs=Sf[:, t, :], start=False, stop=False), reads=[B['qdfT', t], B['Sf', t]], writes=[B['pb', s, 0]])
                P.op('pe', lambda e, t=t, s=s, sl=sl: e.matmul(pb_[:, s, 0:128], lhsT=qdbT[:, sl], rhs=Sb[:, t, :], start=False, stop=True), reads=[B['qdbT', t], B['Sb', t]], writes=[B['pb', s, 0]])
                P.op('act', lambda e, s=s: e.copy(out=on[s][:], in_=pb_[:, s, 0:128]), reads=[B['pb', s, 0]], writes=[B['on', s]])
                P.op('dve', lambda e, s=s: e.bn_stats(out=stt[:], in_=on[s][:]), reads=[B['on', s]], writes=[B['rstt']])
                P.op('dve', lambda e: e.bn_aggr(out=mv[:], in_=stt[:]), reads=[B['rstt']], writes=[B['rmv']])
                P.op('dve', lambda e: e.tensor_scalar_add(out=rstd[:], in0=mv[:, 1:2], scalar1=1e-6), reads=[B['rmv']], writes=[B['rrstd']])
                P.op('act', lambda e: e.sqrt(out=rstd[:], in_=rstd[:]), reads=[B['rrstd']], writes=[B['rrstd']])
                P.op('dve', lambda e: e.reciprocal(out=rstd[:], in_=rstd[:]), reads=[B['rrstd']], writes=[B['rrstd']])
                P.op('dve', lambda e, s=s: e.tensor_scalar(out=on[s][:], in0=on[s][:], scalar1=mv[:, 0:1], scalar2=rstd[:, 0:1], op0=ALU.subtract, op1=ALU.mult),
                     reads=[B['on', s], B['rmv'], B['rrstd']], writes=[B['on', s]])
                P.op('pool', lambda e, s=s, t=t: e.tensor_tensor(out=ob[s][:], in0=on[s][:], in1=sgt[:, t, :], op=ALU.mult), reads=[B['on', s], B['sgt', t]], writes=[B['ob', s]])
                P.op('pe', lambda e, s=s: e.transpose(ptb[:, 512:640], ob[s][:], idb[:]), reads=[B['ob', s], B['idb']], writes=[B['ptb2']])
                g4 = t // 4
                yg = ysg[g4 % 2]
                P.op('act', lambda e, t=t, yg=yg: e.copy(out=yg[:, (t % 4) * 128:(t % 4 + 1) * 128], in_=ptb[:, 512:640]), reads=[B['ptb2']], writes=[B['rysg', g4 % 2]])
                if t % 4 == 3 or t == NT_A - 1:
                    n_ = (t % 4 + 1) * 128
                    P.dma('sp', lambda e, h=h, g4=g4, yg=yg, n_=n_: e.dma_start(out=YT[2 + h, :, g4 * 512:g4 * 512 + n_], in_=yg[:, 0:n_]), reads=[B['rysg', g4 % 2]], is_output=True)
        P.flush()


import ml_dtypes
_BF = ml_dtypes.bfloat16
_CONST = {}


def _consts():
    if _CONST:
        return _CONST
    C = _CONST
    rows = 64
    row = np.repeat(np.arange(rows, dtype=np.float32), 64)
    col = np.tile(np.arange(64, dtype=np.float32), rows)
    inv = (10000.0 ** (-np.arange(32, dtype=np.float32) / 32)).astype(np.float32)
    ang = np.stack([row[:, None] * inv, col[:, None] * inv], axis=1).astype(np.float32)
    C['ropec'] = np.ascontiguousarray(np.cos(ang).astype(np.float32).reshape(32, 128, 64).transpose(1, 0, 2))
    C['ropes'] = np.ascontiguousarray(np.sin(ang).astype(np.float32).reshape(32, 128, 64).transpose(1, 0, 2))
    j = np.arange(128, dtype=np.float32)[:, None]
    i = np.arange(128, dtype=np.float32)[None, :]
    rtab = np.stack([np.maximum(i - j, 0), (i >= j).astype(np.float32), np.maximum(j - i, 0), (j >= i).astype(np.float32),
                     np.broadcast_to(i + 1, (128, 128)), np.broadcast_to(128 - i, (128, 128))], axis=1).astype(np.float32)
    C['rtab'] = np.ascontiguousarray(rtab)
    jj = np.arange(128, dtype=np.float32)
    C['rcol'] = np.ascontiguousarray(np.stack([127 - jj, jj, np.full(128, 128.0), np.zeros(128)], 1).astype(np.float32))

    def feats(l):
        t = np.linspace(0.0, 1.0, l, dtype=np.float32)[:, None]
        w = (2.0 * np.pi * np.arange(l, dtype=np.float32) / l).astype(np.float32)
        f = np.linspace(1e-4, 15, 16, dtype=np.float32)
        a = (w[:, None] * f[None, :]).astype(np.float32)
        return np.concatenate([t, np.cos(a), -np.sin(a)], axis=-1).astype(np.float32), t[:, 0]
    f4, t4 = feats(4096)
    fc, tc = feats(256)
    C['featsT'] = np.ascontiguousarray(f4.T)
    C['featsTc'] = np.ascontiguousarray(fc.T)
    negt = np.concatenate([-t4.reshape(32, 128), -tc.reshape(2, 128)], 0).T
    C['negt'] = np.ascontiguousarray(negt.astype(np.float32))
    mx = np.log(1e-2) / 0.3
    mn = np.log(1e-2) / 1.5
    C['absdelta'] = np.abs(np.linspace(mn, mx, 2048, dtype=np.float32)).reshape(2, 2, 512)
    n = np.arange(4096, dtype=np.float64)
    k = np.arange(4096, dtype=np.float64) + 0.5
    Fm = np.empty((32, 128, 8, 1024), dtype=_BF)
    for nt in range(32):
        th = 2 * np.pi * np.outer(n[nt * 128:(nt + 1) * 128], k) / 8192.0
        Fm[nt, :, :, 0:512] = np.cos(th).reshape(128, 8, 512).astype(_BF)
        Fm[nt, :, :, 512:1024] = np.sin(th).reshape(128, 8, 512).astype(_BF)
    C['Fm'] = Fm
    Gm = np.empty((4, 64, 128, 1024), dtype=_BF)
    for kt in range(32):
        th = 2 * np.pi * np.outer(k[kt * 128:(kt + 1) * 128], n) / 8192.0
        Gm[:, kt] = np.cos(th).reshape(128, 4, 1024).transpose(1, 0, 2).astype(_BF)
        Gm[:, 32 + kt] = np.sin(th).reshape(128, 4, 1024).transpose(1, 0, 2).astype(_BF)
    C['Gm'] = Gm
    nc_ = np.arange(256, dtype=np.float64)
    kc = np.arange(256, dtype=np.float64) + 0.5
    th = 2 * np.pi * np.outer(nc_, kc) / 512.0
    Fc = np.concatenate([np.cos(th), np.sin(th)], 1).reshape(2, 128, 512).transpose(1, 0, 2)
    C['Fc'] = np.ascontiguousarray(Fc).astype(_BF)
    Gc = np.concatenate([np.cos(th.T), np.sin(th.T)], 0).reshape(4, 128, 256).transpose(1, 0, 2)
    C['Gc'] = np.ascontiguousarray(Gc).astype(_BF)
    return C


def run_A(l, x_cur, h_cur, mod, inp):
    C = _consts()
    w_in = inp['w_in'][l]
    ins = []
    for b in range(4):
        xt = np.ascontiguousarray(np.concatenate([x_cur[b], h_cur[b]], 0).reshape(NT_A, 128, 1024))
        ms = [mod[b], mod[4]]
        modcols = np.ascontiguousarray(np.stack([np.stack([_cols(m[k_ * 1024:(k_ + 1) * 1024]) for k_ in (0, 1)], 1) for m in ms], 1))
        for half in range(2):
            r256 = half * 256 + np.arange(256)
            colsA = np.concatenate([g * 512 + r256 for g in range(3)])
            colsR = np.concatenate([1536 + g * 512 + r256 for g in range(4)])
            colsH = np.concatenate([3584 + g * 512 + r256 for g in range(3)])
            colsD = np.concatenate([5120 + r256, 5120 + 512 + half * 128 + np.arange(128), 5120 + 768 + half * 128 + np.arange(128)])
            ch = half * 256 + np.arange(256)
            caw = np.stack([inp['conv_a_w'][l][0, ch], inp['conv_a_w'][l][1, ch], inp['conv_a_w'][l][2, ch], inp['conv_a_b'][l][ch]], -1)
            hch = np.concatenate([g * 512 + ch for g in range(3)])
            hcw = np.stack([inp['hy_conv_w'][l][0, hch], inp['hy_conv_w'][l][1, hch], inp['hy_conv_w'][l][2, hch], inp['hy_conv_b'][l][hch]], -1)
            d = dict(
                x=xt, modcols=modcols, ident=_IDENT,
                wA=_tile_rows(w_in[:, colsA]), wR=_tile_rows(w_in[:, colsR]), wH=_tile_rows(w_in[:, colsH]), wD=_tile_rows(w_in[:, colsD]),
                caw=np.ascontiguousarray(caw.reshape(2, 128, 4).transpose(1, 0, 2)),
                hcw=np.ascontiguousarray(hcw.reshape(6, 128, 4).transpose(1, 0, 2)),
                hsk=np.ascontiguousarray(inp['hy_skip'][l][:, ch].reshape(2, 2, 128).transpose(2, 0, 1)),
                ropec=C['ropec'], ropes=C['ropes'],
                qkn=_rep(np.stack([inp['q_norm'][l], inp['k_norm'][l]], 0)),
                rdec=_rep(inp['ret_decay'][l][:, half * 2:half * 2 + 2].reshape(4)),
                rtab=C['rtab'], rcol=C['rcol'], featsT=C['featsT'], featsTc=C['featsTc'],
                hw1=np.ascontiguousarray(inp['hy_w1'][l]), hw2=np.ascontiguousarray(inp['hy_w2'][l]),
                hcols=np.ascontiguousarray(np.stack([inp['hy_b1'][l], inp['hy_b2'][l], inp['hy_freq'][l][0], inp['hy_freq'][l][1]], 1)),
                hw3=np.ascontiguousarray(inp['hy_w3'][l].reshape(64, 2, 2, 512)[:, :, :, half * 256:(half + 1) * 256]),
                negt=C['negt'], adel=_rep(np.ascontiguousarray(C['absdelta'][:, :, half * 256:(half + 1) * 256])),
                Fm=C['Fm'], Gm=C['Gm'], Fc=C['Fc'], Gc=C['Gc'],
            )
            ins.append(d)
    res = run_bass_kernel_spmd(_prog('A', build_A), ins, core_ids=list(range(8)))
    YT = []
    for b in range(4):
        y = np.empty((4, 2, 2, 128, NTOK_A), np.float32)
        for half in range(2):
            o = res.results[b * 2 + half]['YT'].reshape(4, 2, 128, NTOK_A)
            y[:, half] = o
        YT.append(y.reshape(2048, NTOK_A))
    return YT


class _YTView:
    def __init__(self, base, half):
        self.base = base
        self.half = half

    def _m(self, i8):
        return (i8 // 2) * 4 + self.half * 2 + (i8 % 2)

    def __getitem__(self, idx):
        if isinstance(idx, tuple):
            return self.base[(self._m(idx[0]),) + tuple(idx[1:])]
        return self.base[self._m(idx)]


def emit_M2(nc, P, Dm):
    cT, wm, bcol, brow, modc, modr = Dm['cT'], Dm['wm'], Dm['bcol'], Dm['brow'], Dm['modc'], Dm['modr']
    with contextlib.ExitStack() as st:
        P.stack = st
        B = Bufs()
        cs = P.sb("cs", [128, 8, 2], F32)
        sc = P.sb("sc", [128, 8, 2], F32)
        ones = P.sb("ones", [128, 128], F32)
        scb = P.sb("scb", [128, 8, 2, 128], F32)
        wch = [P.sb(f"wch{i}", [128, 8, 1024], F32) for i in range(2)]
        bc = P.sb("bc", [128, 48], F32)
        br = P.sb("br", [128, 2, 1024], F32)
        mc = P.sb("mc", [128, 2, 48], F32)
        mr = P.sb("mr", [128, 2, 2, 1024], F32)
        pm = P.ps("pm", [128, 2, 512], F32)
        pcl = P.ps("pc", [128, 512], F32)
        P.dma('sp', lambda e: e.dma_start(out=cs[:], in_=cT), writes=[B['cs']])
        P.op('act', lambda e: e.activation(out=sc[:], in_=cs[:], func=AF.Silu), reads=[B['cs']], writes=[B['sc']])
        P.op('dve', lambda e: e.memset(ones[:], 1.0), writes=[B['ones']])
        for kt in range(8):
            for ms in range(2):
                P.op('dve', lambda e, kt=kt, ms=ms: e.tensor_scalar(out=scb[:, kt, ms, :], in0=ones[:], scalar1=sc[:, kt, ms:ms + 1], scalar2=None, op0=ALU.mult),
                     reads=[B['ones'], B['sc']], writes=[B['scb']])
        wc = 0
        for l in range(4):
            P.dma('sp', lambda e, l=l: e.dma_start(out=bc[:], in_=bcol[l]), writes=[B['bc']])
            P.dma('sp', lambda e, l=l: e.dma_start(out=br[:], in_=brow[l]), writes=[B['br']])
            for k in range(6):
                ws_ = wc % 2
                wc += 1
                P.dma('sp', lambda e, l=l, k=k, ws_=ws_: e.dma_start(out=wch[ws_][:], in_=wm[l, k]), writes=[B['wch', ws_]])
                for f in range(8):
                    for kt in range(8):
                        P.op('pe', lambda e, f=f, kt=kt, ws_=ws_: e.matmul(pcl[:, f * 2:f * 2 + 2], lhsT=wch[ws_][:, kt, f * 128:(f + 1) * 128], rhs=sc[:, kt, :], start=(kt == 0), stop=(kt == 7)),
                             reads=[B['wch', ws_], B['sc']], writes=[B['pc']])
                for ms in range(2):
                    P.op('dve', lambda e, k=k, ms=ms: e.tensor_tensor(out=mc[:, ms, k * 8:(k + 1) * 8], in0=pcl[:, 0:16].rearrange("p (f m) -> p m f", m=2)[:, ms, :], in1=bc[:, k * 8:(k + 1) * 8], op=ALU.add),
                         reads=[B['pc'], B['bc']], writes=[B['mc']])
                if k in (2, 5):
                    j = 0 if k == 2 else 1
                    for ms in range(2):
                        for nb in range(2):
                            for kt in range(8):
                                P.op('pe', lambda e, ms=ms, nb=nb, kt=kt, ws_=ws_: e.matmul(pm[:, nb, :], lhsT=scb[:, kt, ms, :], rhs=wch[ws_][:, kt, nb * 512:(nb + 1) * 512], start=(kt == 0), stop=(kt == 7)),
                                     reads=[B['scb'], B['wch', ws_]], writes=[B['pm', nb]])
                            P.op('dve', lambda e, ms=ms, nb=nb, j=j: e.tensor_tensor(out=mr[:, ms, j, nb * 512:(nb + 1) * 512], in0=pm[:, nb, :], in1=br[:, j, nb * 512:(nb + 1) * 512], op=ALU.add),
                                 reads=[B['pm', nb], B['br']], writes=[B['mr']])
            P.dma('sp', lambda e, l=l: e.dma_start(out=modc[l], in_=mc[:]), reads=[B['mc']], writes=[B['modc', l]])
            P.dma('sp', lambda e, l=l: e.dma_start(out=modr[l], in_=mr[:]), reads=[B['mr']], writes=[B['modr', l]])
        P.flush()


def emit_cast(nc, P, pairs):
    with contextlib.ExitStack() as st:
        P.stack = st
        B = Bufs()
        cb = [P.sb(f"cb{i}", [128, 4096], BF16) for i in range(3)]
        for i, (src, dst) in enumerate(pairs):
            c = cb[i % 3]
            n = src.shape[-1]
            P.dma('pool', lambda e, c=c, src=src, n=n: e.dma_start(out=c[:, 0:n], in_=src), writes=[B['cb', i % 3]])
            P.dma('sp', lambda e, c=c, dst=dst, n=n: e.dma_start(out=dst, in_=c[:, 0:n]), reads=[B['cb', i % 3]], writes=[B['dst', i]])
        P.flush()


def build_F():
    nc = bass.Bass("TRN2", target_bir_lowering=False)
    I = lambda name, shape, dt=F32: _din(nc, name, shape, dt)
    T = lambda name, shape, dt=F32: nc.dram_tensor(name, list(shape), dt, kind="Internal").ap()
    x0 = I("x0", [NT_A, 128, D])
    cT = I("cT", [128, 8, 2])
    wm = I("wm", [4, 6, 128, 8, 1024])
    bcol = I("bcol", [4, 128, 48])
    brow = I("brow", [4, 128, 2, 1024])
    ident = I("ident", [128, 128])
    wA = I("wA", [4, 2, 128, 8, 768])
    wR = I("wR", [4, 2, 128, 8, 1024])
    wH = I("wH", [4, 2, 128, 8, 768])
    wD = I("wD", [4, 2, 128, 8, 512])
    caw = I("caw", [4, 2, 128, 2, 4])
    hcw = I("hcw", [4, 2, 128, 6, 4])
    hsk = I("hsk", [4, 2, 128, 2, 2])
    ropec = I("ropec", [128, 32, 64])
    ropes = I("ropes", [128, 32, 64])
    qkn = I("qkn", [4, 128, 2, 128])
    rdec = I("rdec", [4, 2, 128, 4])
    rtab = I("rtab", [128, 6, 128])
    rcol = I("rcol", [128, 4])
    featsT = I("featsT", [33, 4096])
    featsTc = I("featsTc", [33, 256])
    hw1 = I("hw1", [4, 33, 64])
    hw2 = I("hw2", [4, 64, 64])
    hcols = I("hcols", [4, 64, 4])
    hw3 = I("hw3", [4, 2, 64, 2, 2, 256])
    negt = I("negt", [128, 34])
    adel = I("adel", [2, 128, 2, 2, 256])
    Fm = I("Fm", [32, 128, 8, 1024], BF16)
    Gm = I("Gm", [4, 64, 128, 1024], BF16)
    Fc = I("Fc", [128, 2, 512], BF16)
    Gc = I("Gc", [128, 4, 256], BF16)
    wg = I("wg", [4, 8, 128, 4, 8, 128])
    wb = I("wb", [4, 8, 128, 4, 4, 128])
    wo = I("wo", [4, 128, 8, D])
    lnrows = I("lnrows", [4, 128, 4, D])
    bgc = I("bgc", [4, 128, 4, 8])
    w1d = I("w1d", [2, 11, 128, 8, 256])
    w3d = I("w3d", [2, 11, 128, 8, 256])
    w2d = I("w2d", [2, 11, 128, 2, D])
    w1m = I("w1m", [2, 56, 128, 8, 512])
    w3m = I("w3m", [2, 56, 128, 8, 512])
    w2m = I("w2m", [2, 56, 128, 4, D])
    rt = I("rt", [2, 128, 8, 8])
    sel = I("sel", [8, 8, 128])
    out = _dout(nc, "out", [NT_A, 128, D])
    xb = [T("xbuf0", [NT_A, 128, D]), T("xbuf1", [NT_A, 128, D])]
    x1s = T("x1s", [NT_A, 128, D])
    YTs = T("YTs", [16, 128, NTOK_A])
    modc = T("modc", [4, 128, 2, 48])
    modr = T("modr", [4, 128, 2, 2, D])
    uTs = T("uTs", [128, 8, NTOK_A], BF16)
    wgb = T("wgb", [8, 128, 4, 8, 128], BF16)
    wbb = T("wbb", [8, 128, 4, 4, 128], BF16)
    wob = T("wob", [128, 8, D], BF16)
    with contextlib.ExitStack() as st0:
        P = Prog(nc, st0)
        emit_M2(nc, P, dict(cT=cT, wm=wm, bcol=bcol, brow=brow, modc=modc, modr=modr))
        import os as _os
        NL = int(_os.environ.get('F_LAYERS', '4'))
        for l in range(NL):
            xin = x0 if l == 0 else xb[l % 2]
            xout = out if l == NL - 1 else xb[(l + 1) % 2]
            mcA = modc[l][:, :, 0:16].rearrange("p m (k f) -> p m k f", k=2)
            mcB = (mcA, modc[l][:, :, 24:40].rearrange("p m (k f) -> p m k f", k=2))
            ustate = {'have': False}
            for half in range(2):
                emit_A(nc, P, dict(uTs=uTs, ustate=ustate, x=xin, modcols=mcA, ident=ident, wA=wA[l, half], wR=wR[l, half], wH=wH[l, half], wD=wD[l, half],
                                   caw=caw[l, half], hcw=hcw[l, half], hsk=hsk[l, half], ropec=ropec, ropes=ropes, qkn=qkn[l], rdec=rdec[l, half],
                                   rtab=rtab, rcol=rcol, featsT=featsT, featsTc=featsTc, hw1=hw1[l], hw2=hw2[l], hcols=hcols[l], hw3=hw3[l, half],
                                   negt=negt, adel=adel[half], Fm=Fm, Gm=Gm, Fc=Fc, Gc=Gc, YT=_YTView(YTs, half)))
            moe = (l % 2 == 1)
            i = l // 2
            pairs = []
            for fo in range(8):
                pairs.append((wg[l, fo].rearrange("p a b c -> p (a b c)"), wgb[fo].rearrange("p a b c -> p (a b c)")))
                pairs.append((wb[l, fo].rearrange("p a b c -> p (a b c)"), wbb[fo].rearrange("p a b c -> p (a b c)")))
            for kh in range(2):
                pairs.append((wo[l][:, kh * 4:(kh + 1) * 4, :].rearrange("p a b -> p (a b)"), wob[:, kh * 4:(kh + 1) * 4, :].rearrange("p a b -> p (a b)")))
            emit_cast(nc, P, pairs)
            for hb in range(2):
                Dm = dict(x=xin, yT=YTs, modcols=mcB, modrows=modr[l], lnrows=lnrows[l], bgc=bgc[l], ident=ident, wg=wgb, wb=wbb, wo=wob, wbf16=True,
                          x1o=x1s, xo=xout)
                if moe:
                    Dm.update(w1=w1m[i], w3=w3m[i], w2=w2m[i], rt=rt[i], sel=sel)
                else:
                    Dm.update(w1=w1d[i], w3=w3d[i], w2=w2d[i])
                emit_B(nc, P, Dm, moe, gt=(lambda t, hb=hb: hb * 16 + t if t < 16 else 32 + hb))
    return nc


def _pack_F(inp):
    C = _consts()
    sh = dict(ident=_IDENT, ropec=C['ropec'], ropes=C['ropes'], rtab=C['rtab'], rcol=C['rcol'], featsT=C['featsT'], featsTc=C['featsTc'],
              negt=C['negt'], Fm=C['Fm'], Gm=C['Gm'], Fc=C['Fc'], Gc=C['Gc'])
    sh['adel'] = np.stack([_rep(np.ascontiguousarray(C['absdelta'][:, :, h * 256:(h + 1) * 256])) for h in range(2)], 0)
    w_mod = inp['w_mod']
    sh['wm'] = np.ascontiguousarray(w_mod.reshape(4, 8, 128, 6, 1024).transpose(0, 3, 2, 1, 4))
    sh['bcol'] = np.ascontiguousarray(inp['b_mod'].reshape(4, 48, 128).transpose(0, 2, 1))
    sh['brow'] = np.stack([_rep(np.stack([inp['b_mod'][l, 2048:3072], inp['b_mod'][l, 5120:6144]], 0)) for l in range(4)], 0)
    wA, wR, wH, wD, caw, hcw, hsk, rdec, hw3 = [], [], [], [], [], [], [], [], []
    for l in range(4):
        w_in = inp['w_in'][l]
        rows = [[] for _ in range(9)]
        for half in range(2):
            r256 = half * 256 + np.arange(256)
            colsA = np.concatenate([g * 512 + r256 for g in range(3)])
            colsR = np.concatenate([1536 + g * 512 + r256 for g in range(4)])
            colsH = np.concatenate([3584 + g * 512 + r256 for g in range(3)])
            colsD = np.concatenate([5120 + r256, 5120 + 512 + half * 128 + np.arange(128), 5120 + 768 + half * 128 + np.arange(128)])
            ch = r256
            cawv = np.stack([inp['conv_a_w'][l][0, ch], inp['conv_a_w'][l][1, ch], inp['conv_a_w'][l][2, ch], inp['conv_a_b'][l][ch]], -1)
            hch = np.concatenate([g * 512 + ch for g in range(3)])
            hcwv = np.stack([inp['hy_conv_w'][l][0, hch], inp['hy_conv_w'][l][1, hch], inp['hy_conv_w'][l][2, hch], inp['hy_conv_b'][l][hch]], -1)
            vals = [_tile_rows(w_in[:, colsA]), _tile_rows(w_in[:, colsR]), _tile_rows(w_in[:, colsH]), _tile_rows(w_in[:, colsD]),
                    cawv.reshape(2, 128, 4).transpose(1, 0, 2), hcwv.reshape(6, 128, 4).transpose(1, 0, 2),
                    inp['hy_skip'][l][:, ch].reshape(2, 2, 128).transpose(2, 0, 1),
                    _rep(inp['ret_decay'][l][:, half * 2:half * 2 + 2].reshape(4)),
                    inp['hy_w3'][l].reshape(64, 2, 2, 512)[:, :, :, half * 256:(half + 1) * 256]]
            for r_, v_ in zip(rows, vals):
                r_.append(v_)
        for lst, r_ in zip((wA, wR, wH, wD, caw, hcw, hsk, rdec, hw3), rows):
            lst.append(np.stack(r_, 0))
    for n_, lst in zip(('wA', 'wR', 'wH', 'wD', 'caw', 'hcw', 'hsk', 'rdec', 'hw3'), (wA, wR, wH, wD, caw, hcw, hsk, rdec, hw3)):
        sh[n_] = np.ascontiguousarray(np.stack(lst, 0))
    sh['qkn'] = np.stack([_rep(np.stack([inp['q_norm'][l], inp['k_norm'][l]], 0)) for l in range(4)], 0)
    sh['hw1'] = np.ascontiguousarray(inp['hy_w1'])
    sh['hw2'] = np.ascontiguousarray(inp['hy_w2'])
    sh['hcols'] = np.ascontiguousarray(np.stack([inp['hy_b1'], inp['hy_b2'], inp['hy_freq'][:, 0], inp['hy_freq'][:, 1]], -1))
    pw = [prep_B_weights(l, inp) for l in range(4)]
    for n_ in ('wg', 'wb', 'wo', 'lnrows', 'bgc'):
        sh[n_] = np.stack([pw[l][n_] for l in range(4)], 0)
    for n_ in ('w1', 'w3', 'w2'):
        sh[n_ + 'd'] = np.stack([pw[0][n_], pw[2][n_]], 0)
        sh[n_ + 'm'] = np.stack([pw[1][n_], pw[3][n_]], 0)
    sh['rt'] = np.stack([pw[1]['rt'], pw[3]['rt']], 0)
    sh['sel'] = pw[1]['sel']
    per = []
    for b in range(4):
        cc = np.stack([inp['c'][b], inp['c_ctx']], 0)
        per.append(dict(x0=np.ascontiguousarray(np.concatenate([inp['x'][b], inp['ctx'][b]], 0).reshape(NT_A, 128, 1024)),
                        cT=np.ascontiguousarray(cc.T.reshape(8, 128, 2).transpose(1, 0, 2))))
    return sh, per


def kernel_fused(**inp):
    inp = {k_: np.asarray(v) for k_, v in inp.items()}
    sh, per = _pack_F(inp)
    ins = [dict(sh, **per[b]) for b in range(4)]
    res = run_bass_kernel_spmd(_prog('F', build_F), ins, core_ids=list(range(4)))
    out = np.stack([res.results[b]['out'].reshape(NTOK_A, 1024)[:4096] for b in range(4)], 0)
    return np.ascontiguousarray(out, dtype=np.float32)


def kernel_unfused(**inp):
    inp = {k_: np.asarray(v) for k_, v in inp.items()}
    mod = run_M(inp['c'], inp['c_ctx'], inp['w_mod'], inp['b_mod'])
    x_cur = np.ascontiguousarray(inp['x'], dtype=np.float32)
    h_cur = np.ascontiguousarray(inp['ctx'], dtype=np.float32)
    for l in range(4):
        YT = run_A(l, x_cur, h_cur, mod[:, l], inp)
        x_cur, h_cur, _ = run_B(l, x_cur, h_cur, YT, mod[:, l], inp)
    return x_cur


def kernel(**inp):
    return kernel_fused(**inp)
```

```python
import contextlib
import numpy as np
import concourse.bass as bass
import concourse.mybir as mybir

F32 = mybir.dt.float32
BF16 = mybir.dt.bfloat16
AF = mybir.ActivationFunctionType
ALU = mybir.AluOpType
AX = mybir.AxisListType

NDMA = 24
ENGS = ['pe', 'act', 'dve', 'pool', 'sp']


class Buf:
    __slots__ = ('w', 'r', 'name', 'excl')

    def __init__(self, name=''):
        self.w = None
        self.r = {}
        self.name = name
        self.excl = False


PSUM_KEYS = {'pt', 'ptb', 'pa', 'pb', 'pc', 'pg', 'pp', 'pmx', 'pg1', 'pg3', 'pf', 'pw', 'pm'}


class Bufs:
    def __init__(self, name=''):
        self.d = {}
        self.name = name

    def __getitem__(self, k):
        if k == 'ptb2':
            k = 'ptb'
        b = self.d.get(k)
        if b is None:
            b = Buf(f"{self.name}{k}")
            k0 = k[0] if isinstance(k, tuple) else k
            b.excl = k0 in PSUM_KEYS
            self.d[k] = b
        return b


class Prog:
    def __init__(self, nc, stack, same_engine_sync=True):
        self.nc = nc
        self.stack = stack
        self.q = {e: [] for e in ENGS}
        self.sem = {e: stack.enter_context(nc.semaphore(f"s_{e}")) for e in ENGS}
        self.cnt = {e: 0 for e in ENGS}
        self.seen = {e: {} for e in ENGS}
        self.dsem = [stack.enter_context(nc.semaphore(f"dq{i}")) for i in range(NDMA)]
        self.dval = [0] * NDMA
        self.dnext = 0
        self.ses = same_engine_sync
        self.out_tokens = []

    def sb(self, name, shape, dt):
        self._uid = getattr(self, '_uid', 0) + 1
        return self.stack.enter_context(self.nc.sbuf_tensor(f"{name}_u{self._uid}", list(shape), dt))

    def ps(self, name, shape, dt=F32):
        self._uid = getattr(self, '_uid', 0) + 1
        return self.stack.enter_context(self.nc.psum_tensor(f"{name}_u{self._uid}", list(shape), dt))

    def _semh(self, k):
        return self.sem[k] if isinstance(k, str) else self.dsem[k[1]]

    def _deps(self, e, reads, writes):
        need = {}
        xr = [b for b in reads if b.excl]
        if xr:
            writes = list(writes) + xr

        def add(tok):
            if tok is None:
                return
            k, v = tok
            if need.get(k, 0) < v:
                need[k] = v

        for b in reads:
            add(b.w)
        for b in writes:
            add(b.w)
            for k, v in b.r.items():
                add((k, v))
        waits = []
        for k, v in need.items():
            if k == e and (e == 'pe' or not self.ses):
                continue
            if self.seen[e].get(k, 0) >= v:
                continue
            self.seen[e][k] = v
            waits.append((k, v))
        return waits

    def _mark(self, tok, reads, writes):
        k, v = tok
        xr = [b for b in reads if b.excl]
        if xr:
            writes = list(writes) + xr
        for b in reads:
            if b.r.get(k, 0) < v:
                b.r[k] = v
        for b in writes:
            b.w = tok
            b.r = {}

    mute = False

    def op(self, e, fn, reads=(), writes=()):
        if self.mute:
            return
        waits = self._deps(e, reads, writes)
        self.cnt[e] += 1
        tok = (e, self.cnt[e])
        self._mark(tok, reads, writes)
        self.q[e].append((waits, fn, (self.sem[e], 1)))

    def dma(self, e, fn, reads=(), writes=(), is_output=False):
        if self.mute:
            return
        if e == 'pool':
            i = self._dpool = (getattr(self, '_dpool', -1) + 1) % 8
        else:
            i = 8 + self.dnext
            self.dnext = (self.dnext + 1) % (NDMA - 8)
        waits = self._deps(e, reads, writes)
        k = ('d', i)
        if self.dval[i] > 0 and self.seen[e].get(k, 0) < self.dval[i]:
            waits.append((k, self.dval[i]))
            self.seen[e][k] = self.dval[i]
        self.dval[i] += 16
        tok = (k, self.dval[i])
        self._mark(tok, reads, writes)
        self.q[e].append((waits, fn, (self.dsem[i], 16)))
        if is_output:
            self.out_tokens.append(tok)

    def flush(self):
        nc = self.nc
        q = self.q
        semh = self._semh
        ex = []
        for e in ['pe', 'act', 'dve', 'pool']:
            if self.cnt[e] > 0:
                ex.append((e, self.cnt[e]))
        for i in range(NDMA):
            if self.dval[i] > 0:
                ex.append((('d', i), self.dval[i]))

        def emit(eng, ename):
            for waits, fn, (sem, n) in q[ename]:
                for (k, v) in waits:
                    eng.wait_ge(semh(k), v)
                fn(eng).then_inc(sem, n)
            for (k, v) in ex:
                if self.seen[ename].get(k, 0) < v:
                    eng.wait_ge(semh(k), v)
                    self.seen[ename][k] = v

        with nc.Block() as block:
            @block.tensor
            def _(t):
                emit(t, 'pe')

            @block.scalar
            def _(t):
                emit(t, 'act')

            @block.vector
            def _(t):
                emit(t, 'dve')

            @block.gpsimd
            def _(t):
                emit(t, 'pool')

            @block.sync
            def _(t):
                emit(t, 'sp')
        self.q = {e: [] for e in ENGS}

from concourse.bass_utils import run_bass_kernel_spmd

D = 1024
ALPHA = (2 * 4) ** 0.25
NTILE = 17
NTOK = NTILE * 128
BLOCKS = [(0, 4), (4, 4), (8, 4), (12, 4), (16, 1)]


def _din(nc, name, shape, dt=F32):
    return nc.dram_tensor(name, list(shape), dt, kind="ExternalInput").ap()


def _dout(nc, name, shape, dt=F32):
    return nc.dram_tensor(name, list(shape), dt, kind="ExternalOutput").ap()


def build_M():
    nc = bass.Bass("TRN2", target_bir_lowering=False)
    cT = _din(nc, "cT", [128, 8, 5])
    w = _din(nc, "w", [128, 8, 3072])
    b = _din(nc, "b", [5, 3072])
    o = _dout(nc, "o", [5, 3072])
    with contextlib.ExitStack() as st:
        P = Prog(nc, st)
        B = Bufs()
        cs = P.sb("cs", [128, 8, 5], F32)
        sc = P.sb("sc", [128, 8, 5], F32)
        ws = P.sb("ws", [128, 8, 3072], F32)
        bs = P.sb("bs", [5, 3072], F32)
        os_ = P.sb("os", [5, 3072], F32)
        pm = P.ps("pm", [128, 2, 512], F32)
        P.dma('sp', lambda e: e.dma_start(out=cs[:], in_=cT), writes=[B['cs']])
        P.dma('sp', lambda e: e.dma_start(out=bs[:], in_=b), writes=[B['bs']])
        for kt in range(8):
            P.dma('sp', lambda e, kt=kt: e.dma_start(out=ws[:, kt, :], in_=w[:, kt, :]), writes=[B['ws', kt]])
        P.op('act', lambda e: e.activation(out=sc[:], in_=cs[:], func=AF.Silu), reads=[B['cs']], writes=[B['sc']])
        for nb in range(6):
            pb = B['pm', nb % 2]
            for kt in range(8):
                P.op('pe', lambda e, nb=nb, kt=kt: e.matmul(pm[0:5, nb % 2, :], lhsT=sc[:, kt, :], rhs=ws[:, kt, nb * 512:(nb + 1) * 512],
                                                          start=(kt == 0), stop=(kt == 7)),
                     reads=[B['sc'], B['ws', kt]], writes=[pb])
            P.op('dve', lambda e, nb=nb: e.tensor_tensor(out=os_[:, nb * 512:(nb + 1) * 512], in0=pm[0:5, nb % 2, :],
                                                        in1=bs[:, nb * 512:(nb + 1) * 512], op=ALU.add),
                 reads=[pb, B['bs']], writes=[B['os', nb]])
        P.dma('sp', lambda e: e.dma_start(out=o, in_=os_[:]), reads=[B['os', nb] for nb in range(6)], is_output=True)
        P.flush()
    return nc


def emit_B(nc, P, Dm, moe, gt=None):
    if gt is None:
        gt = lambda t: t
    KC = 4 if moe else 2
    NCH = 56 if moe else 11
    CPE = 7 if moe else 11
    x = Dm['x']
    yT = Dm['yT']
    modcols = Dm['modcols']
    modrows = Dm['modrows']
    lnrows = Dm['lnrows']
    bgc = Dm['bgc']
    ident = Dm['ident']
    wg = Dm['wg']
    wb = Dm['wb']
    wo = Dm['wo']
    w1 = Dm['w1']
    w3 = Dm['w3']
    w2 = Dm['w2']
    x1o = Dm['x1o']
    xo = Dm['xo']
    if moe:
        rt = Dm['rt']
        sel = Dm['sel']
    with contextlib.ExitStack() as st:
        P.stack = st
        B = Bufs()
        ids = P.sb("ids", [128, 128], F32)
        mcol = P.sb("mcol", [128, 2, 4, 8], F32)
        bgs = P.sb("bgs", [128, 4, 8], F32)
        u2T = P.sb("u2T", [128, 8, NTOK], BF16)
        P.dma('sp', lambda e: e.dma_start(out=ids[:], in_=ident), writes=[B['ids']])
        if isinstance(modcols, tuple):
            P.dma('sp', lambda e: e.dma_start(out=mcol[:, :, 0:2, :], in_=modcols[0]), writes=[B['mcol']])
            P.dma('sp', lambda e: e.dma_start(out=mcol[:, :, 2:4, :], in_=modcols[1]), writes=[B['mcol']])
        else:
            P.dma('sp', lambda e: e.dma_start(out=mcol[:], in_=modcols), writes=[B['mcol']])
        P.dma('sp', lambda e: e.dma_start(out=bgs[:], in_=bgc), writes=[B['bgs']])
        P.op('dve', lambda e: e.tensor_scalar_add(out=mcol[:, :, 1, :], in0=mcol[:, :, 1, :], scalar1=1.0), reads=[B['mcol']], writes=[B['mcol']])
        P.op('dve', lambda e: e.tensor_scalar_add(out=mcol[:, :, 3, :], in0=mcol[:, :, 3, :], scalar1=1.0), reads=[B['mcol']], writes=[B['mcol']])
        if moe:
            rts = P.sb("rts", [128, 8, 8], F32)
            sels = P.sb("sels", [8, 8, 128], F32)
            wall = P.sb("wall", [128, NTILE, 8], F32)
            WT = P.sb("WT", [8, NTOK], F32)
            P.dma('sp', lambda e: e.dma_start(out=rts[:], in_=rt), writes=[B['rts']])
            P.dma('sp', lambda e: e.dma_start(out=sels[:], in_=sel), writes=[B['sels']])

        def ln_tile(r, stt, mv, rstd, gi, key):
            rb = B[key]
            for hf in range(2):
                P.op('dve', lambda e, hf=hf: e.bn_stats(out=stt[:, hf, :], in_=r[:, hf * 512:(hf + 1) * 512]), reads=[rb], writes=[B[key, 'st', hf]])
            P.op('dve', lambda e: e.bn_aggr(out=mv[:], in_=stt[:].rearrange("p a b -> p (a b)")), reads=[B[key, 'st', 0], B[key, 'st', 1]], writes=[B[key, 'mv']])
            P.op('dve', lambda e: e.tensor_scalar_add(out=rstd[:], in0=mv[:, 1:2], scalar1=1e-6), reads=[B[key, 'mv']], writes=[B[key, 'rstd']])
            P.op('act', lambda e: e.sqrt(out=rstd[:], in_=rstd[:]), reads=[B[key, 'rstd']], writes=[B[key, 'rstd']])
            P.op('dve', lambda e: e.reciprocal(out=rstd[:], in_=rstd[:]), reads=[B[key, 'rstd']], writes=[B[key, 'rstd']])
            P.op('dve', lambda e: e.tensor_scalar(out=r[:], in0=r[:], scalar1=mv[:, 0:1], scalar2=rstd[:, 0:1], op0=ALU.subtract, op1=ALU.mult),
                 reads=[rb, B[key, 'mv'], B[key, 'rstd']], writes=[rb])
            P.op('pool', lambda e: e.tensor_tensor(out=r[:], in0=r[:], in1=lnr[:, gi, :], op=ALU.mult), reads=[rb, B['lnr']], writes=[rb])
            P.op('pool', lambda e: e.tensor_tensor(out=r[:], in0=r[:], in1=lnr[:, gi + 1, :], op=ALU.add), reads=[rb, B['lnr']], writes=[rb])

        with contextlib.ExitStack() as s1:
            P.stack = s1
            mrow = P.sb("mrow", [128, 2, 2, D], F32)
            lnr = P.sb("lnr", [128, 4, D], F32)
            P.dma('sp', lambda e: e.dma_start(out=mrow[:], in_=modrows), writes=[B['mrow']])
            P.dma('sp', lambda e: e.dma_start(out=lnr[:], in_=lnrows), writes=[B['lnr']])
            xs = P.sb("xs", [128, 4, D], F32)
            uT = P.sb("uT", [128, 8, 512], BF16)
            yTb = P.sb("yTb", [128, 16, 512], BF16)
            mT = P.sb("mT", [128, 8, 512], BF16)
            wgs = [P.sb(f"wgs{i}", [128, 4, 8, 128], BF16) for i in range(2)]
            wbs = [P.sb(f"wbs{i}", [128, 4, 4, 128], BF16) for i in range(2)]
            wos = P.sb("wos", [128, 8, D], BF16)
            sg = [P.sb(f"sg{i}", [128, 512], F32) for i in range(2)]
            macc = P.sb("macc", [128, 512], F32)
            mtmp = P.sb("mtmp", [128, 512], F32)
            rr = [P.sb(f"rr{i}", [128, D], F32) for i in range(2)]
            t1 = P.sb("t1", [128, D], F32)
            stt = P.sb("stt", [128, 2, 6], F32)
            mv = P.sb("mv", [128, 2], F32)
            rstd = P.sb("rstd", [128, 1], F32)
            u2f = P.sb("u2f", [128, 8, 128], F32)
            pt = P.ps("pt", [128, 2, 512], F32)
            pg = P.ps("pg", [128, 2, 512], F32)
            pp = P.ps("pp", [128, 2, 512], F32)
            pmx = P.ps("pmx", [128, 2, 512], F32)
            if moe:
                lg = P.sb("lg", [128, 8], F32)
                m8 = P.sb("m8", [128, 8], F32)
                msk = P.sb("msk", [128, 8], F32)
                ex = P.sb("ex", [128, 8], F32)
                sm = P.sb("sm", [128, 4], F32)

            P.dma('sp' if Dm.get('wbf16') else 'pool', lambda e: e.dma_start(out=wos[:], in_=wo), writes=[B['wos']])
            wcnt = 0
            tcnt = 0
            for bi, (t0, nt) in enumerate(BLOCKS):
                ms = 1 if bi == 4 else 0
                ntok = nt * 128
                tok0 = gt(t0) * 128
                P.dma('sp', lambda e, t0=t0, nt=nt: e.dma_start(out=xs[:, 0:nt, :], in_=x[gt(t0):gt(t0) + nt].rearrange("t p f -> p t f")),
                      writes=[B['xs', j] for j in range(nt)])
                P.dma('pool', lambda e, tok0=tok0, ntok=ntok: e.dma_start(out=yTb[:, :, 0:ntok], in_=yT[:, :, tok0:tok0 + ntok].rearrange("c p t -> p c t")),
                      writes=[B['yTb']])
                for j in range(nt):
                    for f in range(8):
                        pb = B['pt', tcnt % 2]
                        P.op('pe', lambda e, j=j, f=f, s=tcnt % 2: e.transpose(pt[:, s, 0:128], xs[:, j, f * 128:(f + 1) * 128], ids[:]),
                             reads=[B['xs', j], B['ids']], writes=[pb])
                        P.op('act', lambda e, j=j, f=f, s=tcnt % 2, ms=ms: e.activation(out=uT[:, f, j * 128:(j + 1) * 128], in_=pt[:, s, 0:128], func=AF.Identity,
                                                                                      bias=mcol[:, ms, 0, f:f + 1], scale=mcol[:, ms, 1, f:f + 1]),
                             reads=[pb, B['mcol']], writes=[B['uT', f]])
                        tcnt += 1
                for fo in range(8):
                    ws_ = wcnt % 2
                    wq = 'sp' if Dm.get('wbf16') else 'pool'
                    P.dma(wq, lambda e, fo=fo, ws_=ws_: e.dma_start(out=wgs[ws_][:], in_=wg[fo]), writes=[B['wgs', ws_]])
                    P.dma(wq, lambda e, fo=fo, ws_=ws_: e.dma_start(out=wbs[ws_][:], in_=wb[fo]), writes=[B['wbs', ws_]])
                    wcnt += 1
                    for br in range(4):
                        s = br % 2
                        for kt in range(8):
                            P.op('pe', lambda e, br=br, kt=kt, s=s, ws_=ws_, ntok=ntok: e.matmul(pg[:, s, 0:ntok], lhsT=wgs[ws_][:, br, kt, :], rhs=uT[:, kt, 0:ntok],
                                                                                               start=(kt == 0), stop=(kt == 7)),
                                 reads=[B['wgs', ws_], B['uT', kt]], writes=[B['pg', s]])
                        for kt in range(4):
                            P.op('pe', lambda e, br=br, kt=kt, s=s, ws_=ws_, ntok=ntok: e.matmul(pp[:, s, 0:ntok], lhsT=wbs[ws_][:, br, kt, :], rhs=yTb[:, br * 4 + kt, 0:ntok],
                                                                                               start=(kt == 0), stop=(kt == 3)),
                                 reads=[B['wbs', ws_], B['yTb']], writes=[B['pp', s]])
                        P.op('act', lambda e, br=br, fo=fo, s=s, ntok=ntok: e.activation(out=sg[s][:, 0:ntok], in_=pg[:, s, 0:ntok], func=AF.Sigmoid,
                                                                                       bias=bgs[:, br, fo:fo + 1], scale=1.0),
                             reads=[B['pg', s], B['bgs']], writes=[B['sg', s]])
                        if br == 0:
                            P.op('dve', lambda e, s=s, ntok=ntok: e.tensor_tensor(out=macc[:, 0:ntok], in0=sg[s][:, 0:ntok], in1=pp[:, s, 0:ntok], op=ALU.mult),
                                 reads=[B['sg', s], B['pp', s]], writes=[B['macc']])
                        else:
                            P.op('dve', lambda e, s=s, ntok=ntok: e.tensor_tensor(out=mtmp[:, 0:ntok], in0=sg[s][:, 0:ntok], in1=pp[:, s, 0:ntok], op=ALU.mult),
                                 reads=[B['sg', s], B['pp', s]], writes=[B['mtmp']])
                            if br < 3:
                                P.op('pool', lambda e, ntok=ntok: e.tensor_tensor(out=macc[:, 0:ntok], in0=macc[:, 0:ntok], in1=mtmp[:, 0:ntok], op=ALU.add),
                                     reads=[B['macc'], B['mtmp']], writes=[B['macc']])
                            else:
                                P.op('pool', lambda e, fo=fo, ntok=ntok: e.tensor_tensor(out=mT[:, fo, 0:ntok], in0=macc[:, 0:ntok], in1=mtmp[:, 0:ntok], op=ALU.add),
                                     reads=[B['macc'], B['mtmp']], writes=[B['mT', fo]])
                for j in range(nt):
                    tile = t0 + j
                    r = rr[tile % 2]
                    rk = ('rr', tile % 2)
                    for hf in range(2):
                        for kt in range(8):
                            P.op('pe', lambda e, j=j, hf=hf, kt=kt: e.matmul(pmx[:, hf, :], lhsT=mT[:, kt, j * 128:(j + 1) * 128], rhs=wos[:, kt, hf * 512:(hf + 1) * 512],
                                                                           start=(kt == 0), stop=(kt == 7)),
                                 reads=[B['mT', kt], B['wos']], writes=[B['pmx', hf]])
                        P.op('dve', lambda e, hf=hf, ms=ms: e.tensor_tensor(out=t1[:, hf * 512:(hf + 1) * 512], in0=pmx[:, hf, :], in1=mrow[:, ms, 0, hf * 512:(hf + 1) * 512], op=ALU.mult),
                             reads=[B['pmx', hf], B['mrow']], writes=[B['t1', hf]])
                    P.op('dve', lambda e, j=j, r=r: e.scalar_tensor_tensor(out=r[:], in0=xs[:, j, :], scalar=ALPHA, in1=t1[:], op0=ALU.mult, op1=ALU.add),
                         reads=[B['xs', j], B['t1', 0], B['t1', 1]], writes=[B[rk]])
                    ln_tile(r, stt, mv, rstd, 0, rk)
                    P.dma('sp', lambda e, tile=tile, r=r: e.dma_start(out=x1o[gt(tile)], in_=r[:]), reads=[B[rk]], writes=[B['x1o', tile]])
                    for f in range(8):
                        pb = B['pt', tcnt % 2]
                        P.op('pe', lambda e, r=r, f=f, s=tcnt % 2: e.transpose(pt[:, s, 0:128], r[:, f * 128:(f + 1) * 128], ids[:]),
                             reads=[B[rk], B['ids']], writes=[pb])
                        if moe:
                            P.op('act', lambda e, f=f, s=tcnt % 2, ms=ms: e.activation(out=u2f[:, f, :], in_=pt[:, s, 0:128], func=AF.Identity,
                                                                                     bias=mcol[:, ms, 2, f:f + 1], scale=mcol[:, ms, 3, f:f + 1]),
                                 reads=[pb, B['mcol']], writes=[B['u2f', f]])
                            P.op('dve', lambda e, f=f, tile=tile: e.tensor_copy(out=u2T[:, f, tile * 128:(tile + 1) * 128], in_=u2f[:, f, :]),
                                 reads=[B['u2f', f]], writes=[B['u2T', f, tile]])
                        else:
                            P.op('act', lambda e, f=f, s=tcnt % 2, ms=ms, tile=tile: e.activation(out=u2T[:, f, tile * 128:(tile + 1) * 128], in_=pt[:, s, 0:128], func=AF.Identity,
                                                                                                bias=mcol[:, ms, 2, f:f + 1], scale=mcol[:, ms, 3, f:f + 1]),
                                 reads=[pb, B['mcol']], writes=[B['u2T', f, tile]])
                        tcnt += 1
                    if moe:
                        for kt in range(8):
                            P.op('pe', lambda e, kt=kt: e.matmul(pg[:, 0, 0:8], lhsT=u2f[:, kt, :], rhs=rts[:, kt, :], start=(kt == 0), stop=(kt == 7)),
                                 reads=[B['u2f', kt], B['rts']], writes=[B['pg', 0]])
                        P.op('dve', lambda e: e.tensor_copy(out=lg[:], in_=pg[:, 0, 0:8]), reads=[B['pg', 0]], writes=[B['lg']])
                        P.op('dve', lambda e: e.max(out=m8[:], in_=lg[:]), reads=[B['lg']], writes=[B['m8']])
                        P.op('dve', lambda e: e.tensor_scalar(out=msk[:], in0=lg[:], scalar1=m8[:, 1:2], scalar2=None, op0=ALU.is_ge), reads=[B['lg'], B['m8']], writes=[B['msk']])
                        P.op('dve', lambda e: e.tensor_scalar(out=sm[:, 0:1], in0=m8[:, 0:1], scalar1=-1.0, scalar2=None, op0=ALU.mult), reads=[B['m8']], writes=[B['sm', 0]])
                        P.op('dve', lambda e: e.tensor_tensor(out=sm[:, 1:2], in0=m8[:, 1:2], in1=m8[:, 0:1], op=ALU.subtract), reads=[B['m8']], writes=[B['sm', 1]])
                        P.op('act', lambda e: e.activation(out=ex[:], in_=lg[:], func=AF.Exp, bias=sm[:, 0:1], scale=1.0), reads=[B['lg'], B['sm', 0]], writes=[B['ex']])
                        P.op('act', lambda e: e.activation(out=sm[:, 2:3], in_=sm[:, 1:2], func=AF.Exp), reads=[B['sm', 1]], writes=[B['sm', 2]])
                        P.op('dve', lambda e: e.tensor_scalar_add(out=sm[:, 2:3], in0=sm[:, 2:3], scalar1=1.0), reads=[B['sm', 2]], writes=[B['sm', 2]])
                        P.op('dve', lambda e: e.reciprocal(out=sm[:, 3:4], in_=sm[:, 2:3]), reads=[B['sm', 2]], writes=[B['sm', 3]])
                        P.op('dve', lambda e: e.tensor_tensor(out=ex[:], in0=ex[:], in1=msk[:], op=ALU.mult), reads=[B['ex'], B['msk']], writes=[B['ex']])
                        P.op('dve', lambda e, tile=tile: e.tensor_scalar(out=wall[:, tile, :], in0=ex[:], scalar1=sm[:, 3:4], scalar2=None, op0=ALU.mult),
                             reads=[B['ex'], B['sm', 3]], writes=[B['wall', tile]])
                        P.op('pe', lambda e, tile=tile: e.transpose(pp[0:8, 0, 0:128], wall[:, tile, :], ids[:]), reads=[B['wall', tile], B['ids']], writes=[B['pp', 0]])
                        P.op('act', lambda e, tile=tile: e.copy(out=WT[:, tile * 128:(tile + 1) * 128], in_=pp[0:8, 0, 0:128]), reads=[B['pp', 0]], writes=[B['WT']])
            P.flush()
        s2o = st.enter_context(contextlib.ExitStack())
        P.stack = s2o
        facc = P.sb("facc", [128, NTILE, D], F32)
        pg1 = P.ps("pg1", [128, 2, 512], F32)
        pg3 = P.ps("pg3", [128, 2, 512], F32)
        pf = P.ps("pf", [128, 2, 512], F32)
        pw = P.ps("pw", [128, 512], F32)
        with contextlib.ExitStack() as s2:
            P.stack = s2
            w1s = [P.sb(f"w1s{i}", [128, 8, KC * 128], BF16) for i in range(2)]
            w3s = [P.sb(f"w3s{i}", [128, 8, KC * 128], BF16) for i in range(2)]
            w2s = [P.sb(f"w2s{i}", [128, KC, D], BF16) for i in range(2)]
            s1b = [P.sb(f"s1b{i}", [128, 512], F32) for i in range(2)]
            hT = [P.sb(f"hT{i}", [128, KC, 512], BF16) for i in range(2)]
            gcnt = 0
            fcnt = 0
            hcnt = 0
            for ci in range(NCH):
                wsl = ci % 2
                ex_ = ci // CPE
                P.dma('pool', lambda e, ci=ci, wsl=wsl: e.dma_start(out=w1s[wsl][:], in_=w1[ci]), writes=[B['w1s', wsl]])
                P.dma('pool', lambda e, ci=ci, wsl=wsl: e.dma_start(out=w3s[wsl][:], in_=w3[ci]), writes=[B['w3s', wsl]])
                P.dma('pool', lambda e, ci=ci, wsl=wsl: e.dma_start(out=w2s[wsl][:], in_=w2[ci]), writes=[B['w2s', wsl]])
                for bi, (t0, nt) in enumerate(BLOCKS):
                    ntok = nt * 128
                    tok0 = t0 * 128
                    hs = hcnt % 2
                    hcnt += 1
                    if moe:
                        P.op('pe', lambda e, ex_=ex_, tok0=tok0, ntok=ntok: e.matmul(pw[:, 0:ntok], lhsT=sels[:, ex_, :], rhs=WT[:, tok0:tok0 + ntok], start=True, stop=True),
                             reads=[B['sels'], B['WT']], writes=[B['pw']])
                    for kk in range(KC):
                        g = gcnt % 2
                        gcnt += 1
                        for kt in range(8):
                            P.op('pe', lambda e, kk=kk, kt=kt, g=g, wsl=wsl, tok0=tok0, ntok=ntok: e.matmul(pg1[:, g, 0:ntok], lhsT=w1s[wsl][:, kt, kk * 128:(kk + 1) * 128],
                                                                                                       rhs=u2T[:, kt, tok0:tok0 + ntok], start=(kt == 0), stop=(kt == 7)),
                                 reads=[B['w1s', wsl], B['u2T']], writes=[B['pg1', g]])
                        for kt in range(8):
                            P.op('pe', lambda e, kk=kk, kt=kt, g=g, wsl=wsl, tok0=tok0, ntok=ntok: e.matmul(pg3[:, g, 0:ntok], lhsT=w3s[wsl][:, kt, kk * 128:(kk + 1) * 128],
                                                                                                       rhs=u2T[:, kt, tok0:tok0 + ntok], start=(kt == 0), stop=(kt == 7)),
                                 reads=[B['w3s', wsl], B['u2T']], writes=[B['pg3', g]])
                        P.op('act', lambda e, g=g, ntok=ntok: e.activation(out=s1b[g][:, 0:ntok], in_=pg1[:, g, 0:ntok], func=AF.Silu), reads=[B['pg1', g]], writes=[B['s1b', g]])
                        if moe:
                            P.op('dve', lambda e, g=g, ntok=ntok: e.tensor_tensor(out=s1b[g][:, 0:ntok], in0=s1b[g][:, 0:ntok], in1=pg3[:, g, 0:ntok], op=ALU.mult),
                                 reads=[B['s1b', g], B['pg3', g]], writes=[B['s1b', g]])
                            P.op('dve', lambda e, g=g, kk=kk, hs=hs, ntok=ntok: e.tensor_tensor(out=hT[hs][:, kk, 0:ntok], in0=s1b[g][:, 0:ntok], in1=pw[:, 0:ntok], op=ALU.mult),
                                 reads=[B['s1b', g], B['pw']], writes=[B['hT', hs, kk]])
                        else:
                            P.op('dve', lambda e, g=g, kk=kk, hs=hs, ntok=ntok: e.tensor_tensor(out=hT[hs][:, kk, 0:ntok], in0=s1b[g][:, 0:ntok], in1=pg3[:, g, 0:ntok], op=ALU.mult),
                                 reads=[B['s1b', g], B['pg3', g]], writes=[B['hT', hs, kk]])
                    for j in range(nt):
                        tile = t0 + j
                        for hf in range(2):
                            fs = fcnt % 2
                            fcnt += 1
                            for kk in range(KC):
                                P.op('pe', lambda e, j=j, hf=hf, kk=kk, fs=fs, hs=hs, wsl=wsl: e.matmul(pf[:, fs, :], lhsT=hT[hs][:, kk, j * 128:(j + 1) * 128],
                                                                                                    rhs=w2s[wsl][:, kk, hf * 512:(hf + 1) * 512], start=(kk == 0), stop=(kk == KC - 1)),
                                     reads=[B['hT', hs, kk], B['w2s', wsl]], writes=[B['pf', fs]])
                            fb = B['facc', tile, hf]
                            if ci == 0:
                                P.op('act', lambda e, tile=tile, hf=hf, fs=fs: e.copy(out=facc[:, tile, hf * 512:(hf + 1) * 512], in_=pf[:, fs, :]), reads=[B['pf', fs]], writes=[fb])
                            else:
                                P.op('dve', lambda e, tile=tile, hf=hf, fs=fs: e.tensor_tensor(out=facc[:, tile, hf * 512:(hf + 1) * 512], in0=facc[:, tile, hf * 512:(hf + 1) * 512],
                                                                                              in1=pf[:, fs, :], op=ALU.add), reads=[B['pf', fs], fb], writes=[fb])
            P.flush()
        with contextlib.ExitStack() as s3:
            P.stack = s3
            mrow = P.sb("mrow3", [128, 2, 2, D], F32)
            lnr = P.sb("lnr3", [128, 4, D], F32)
            P.dma('sp', lambda e: e.dma_start(out=mrow[:], in_=modrows), writes=[B['mrow']])
            P.dma('sp', lambda e: e.dma_start(out=lnr[:], in_=lnrows), writes=[B['lnr']])
            xr = [P.sb(f"xr{i}", [128, D], F32) for i in range(2)]
            t1 = P.sb("t1b", [128, D], F32)
            stt = P.sb("stt2", [128, 2, 6], F32)
            mv = P.sb("mv2", [128, 2], F32)
            rstd = P.sb("rstd2", [128, 1], F32)
            for tile in range(NTILE):
                ms = 1 if tile == 16 else 0
                r = xr[tile % 2]
                rk = ('xr', tile % 2)
                P.dma('sp', lambda e, tile=tile, r=r: e.dma_start(out=r[:], in_=x1o[gt(tile)]), reads=[B['x1o', tile]], writes=[B[rk]])
                P.op('dve', lambda e, tile=tile, ms=ms: e.tensor_tensor(out=t1[:], in0=facc[:, tile, :], in1=mrow[:, ms, 1, :], op=ALU.mult),
                     reads=[B['facc', tile, 0], B['facc', tile, 1], B['mrow']], writes=[B['t1b']])
                P.op('dve', lambda e, r=r: e.scalar_tensor_tensor(out=r[:], in0=r[:], scalar=ALPHA, in1=t1[:], op0=ALU.mult, op1=ALU.add),
                     reads=[B[rk], B['t1b']], writes=[B[rk]])
                ln_tile(r, stt, mv, rstd, 2, rk)
                P.dma('sp', lambda e, tile=tile, r=r: e.dma_start(out=xo[gt(tile)], in_=r[:]), reads=[B[rk]], is_output=True)
            P.flush()


def build_B(moe):
    KC = 4 if moe else 2
    NCH = 56 if moe else 11
    CPE = 7 if moe else 11
    nc = bass.Bass("TRN2", target_bir_lowering=False)
    x = _din(nc, "x", [NTILE, 128, D])
    yT = _din(nc, "yT", [16, 128, NTOK])
    modcols = _din(nc, "modcols", [128, 2, 4, 8])
    modrows = _din(nc, "modrows", [128, 2, 2, D])
    lnrows = _din(nc, "lnrows", [128, 4, D])
    bgc = _din(nc, "bgc", [128, 4, 8])
    ident = _din(nc, "ident", [128, 128])
    wg = _din(nc, "wg", [8, 128, 4, 8, 128])
    wb = _din(nc, "wb", [8, 128, 4, 4, 128])
    wo = _din(nc, "wo", [128, 8, D])
    w1 = _din(nc, "w1", [NCH, 128, 8, KC * 128])
    w3 = _din(nc, "w3", [NCH, 128, 8, KC * 128])
    w2 = _din(nc, "w2", [NCH, 128, KC, D])
    if moe:
        rt = _din(nc, "rt", [128, 8, 8])
        sel = _din(nc, "sel", [8, 8, 128])
    x1o = _dout(nc, "x1o", [NTILE, 128, D])
    xo = _dout(nc, "xo", [NTILE, 128, D])

    Dm = dict(x=x, yT=yT, modcols=modcols, modrows=modrows, lnrows=lnrows, bgc=bgc, ident=ident, wg=wg, wb=wb, wo=wo, w1=w1, w3=w3, w2=w2, x1o=x1o, xo=xo)
    if moe:
        Dm['rt'] = rt
        Dm['sel'] = sel
    with contextlib.ExitStack() as st0:
        P = Prog(nc, st0)
        emit_B(nc, P, Dm, moe)
    return nc


_CACHE = {}


def _prog(name, fn, *a):
    k = (name,) + a
    if k not in _CACHE:
        _CACHE[k] = fn(*a)
    return _CACHE[k]


def _tile_rows(w):
    K, N = w.shape
    return np.ascontiguousarray(w.reshape(K // 128, 128, N).transpose(1, 0, 2))


def _cols(v):
    return np.ascontiguousarray(v.reshape(-1, 128).T)


def _rep(v):
    return np.ascontiguousarray(np.broadcast_to(v[None], (128,) + v.shape))


_IDENT = np.eye(128, dtype=np.float32)


def run_M(c, c_ctx, w_mod, b_mod):
    cc = np.concatenate([c, c_ctx[None]], 0)
    cT = np.ascontiguousarray(cc.T.reshape(8, 128, 5).transpose(1, 0, 2))
    Wm = w_mod.transpose(1, 0, 2).reshape(1024, 4 * 6144)
    bm = b_mod.reshape(4 * 6144)
    ins = []
    for i in range(8):
        sl = slice(i * 3072, (i + 1) * 3072)
        ins.append(dict(cT=cT, w=_tile_rows(Wm[:, sl]), b=np.ascontiguousarray(np.broadcast_to(bm[None, sl], (5, 3072)))))
    res = run_bass_kernel_spmd(_prog('M', build_M), ins, core_ids=list(range(8)))
    mod = np.concatenate([res.results[i]['o'] for i in range(8)], axis=1)
    return mod.reshape(5, 4, 6144)


def prep_B_weights(l, inp):
    moe = (l % 2 == 1)
    i = l // 2
    d = {}
    d['wg'] = np.ascontiguousarray(inp['w_gate'][l].reshape(4, 8, 128, 8, 128).transpose(3, 2, 0, 1, 4))
    d['wb'] = np.ascontiguousarray(inp['w_branch'][l].reshape(4, 4, 128, 8, 128).transpose(3, 2, 0, 1, 4))
    d['wo'] = _tile_rows(inp['w_o'][l])
    d['lnrows'] = _rep(np.stack([inp['ln_g'][l, 0], inp['ln_b'][l, 0], inp['ln_g'][l, 1], inp['ln_b'][l, 1]], 0))
    d['bgc'] = np.ascontiguousarray(inp['b_gate'][l].reshape(4, 8, 128).transpose(2, 0, 1))
    d['ident'] = _IDENT
    if not moe:
        KC, NCH = 2, 11
        d['w1'] = np.ascontiguousarray(inp['ffn_w1'][i].reshape(8, 128, NCH, KC * 128).transpose(2, 1, 0, 3))
        d['w3'] = np.ascontiguousarray(inp['ffn_w3'][i].reshape(8, 128, NCH, KC * 128).transpose(2, 1, 0, 3))
        d['w2'] = np.ascontiguousarray(inp['ffn_w2'][i].reshape(NCH, KC, 128, 1024).transpose(0, 2, 1, 3))
    else:
        KC, CPE = 4, 7
        d['w1'] = np.ascontiguousarray(inp['moe_w1'][i].reshape(8, 8, 128, CPE, KC * 128).transpose(0, 3, 2, 1, 4)).reshape(56, 128, 8, KC * 128)
        d['w3'] = np.ascontiguousarray(inp['moe_w3'][i].reshape(8, 8, 128, CPE, KC * 128).transpose(0, 3, 2, 1, 4)).reshape(56, 128, 8, KC * 128)
        d['w2'] = np.ascontiguousarray(inp['moe_w2'][i].reshape(8, CPE, KC, 128, 1024).transpose(0, 1, 3, 2, 4)).reshape(56, 128, KC, 1024)
        d['rt'] = _tile_rows(inp['router'][i])
        sel = np.zeros((8, 8, 128), np.float32)
        for e in range(8):
            sel[e, e, :] = 1.0
        d['sel'] = sel
    return d


def run_B(l, x_cur, h_cur, YT, mod, inp):
    moe = (l % 2 == 1)
    wd = prep_B_weights(l, inp)
    ins = []
    for b in range(4):
        for half in range(2):
            d = dict(wd)
            xt = np.concatenate([x_cur[b, half * 2048:(half + 1) * 2048], h_cur[b, half * 128:(half + 1) * 128]], 0)
            d['x'] = np.ascontiguousarray(xt.reshape(17, 128, 1024))
            toks = np.concatenate([np.arange(half * 2048, (half + 1) * 2048), 4096 + np.arange(half * 128, (half + 1) * 128)])
            d['yT'] = np.ascontiguousarray(YT[b][:, toks].reshape(16, 128, NTOK))
            ms = [mod[b], mod[4]]
            d['modcols'] = np.ascontiguousarray(np.stack([np.stack([_cols(m[k * 1024:(k + 1) * 1024]) for k in (0, 1, 3, 4)], 1) for m in ms], 1))
            d['modrows'] = _rep(np.stack([np.stack([m[2048:3072], m[5120:6144]], 0) for m in ms], 0))
            ins.append(d)
    res = run_bass_kernel_spmd(_prog('B', build_B, moe), ins, core_ids=list(range(8)))
    x_new = np.empty_like(x_cur)
    h_new = np.empty_like(h_cur)
    x1 = np.empty_like(x_cur)
    for b in range(4):
        for half in range(2):
            o = res.results[b * 2 + half]['xo'].reshape(NTOK, 1024)
            x_new[b, half * 2048:(half + 1) * 2048] = o[:2048]
            h_new[b, half * 128:(half + 1) * 128] = o[2048:]
            x1[b, half * 2048:(half + 1) * 2048] = res.results[b * 2 + half]['x1o'].reshape(NTOK, 1024)[:2048]
    return x_new, h_new, x1


NT_A = 34
NFB = 6
NTOK_A = NT_A * 128
TBLK_A = [(i * 512, 512) for i in range(8)] + [(4096, 256)]
PI = float(np.pi)


def emit_A(nc, P, Dm):
    x = Dm['x']
    modcols = Dm['modcols']
    ident = Dm['ident']
    wA = Dm['wA']
    wR = Dm['wR']
    wH = Dm['wH']
    wD = Dm['wD']
    caw = Dm['caw']
    hcw = Dm['hcw']
    hsk = Dm['hsk']
    ropec = Dm['ropec']
    ropes = Dm['ropes']
    qkn = Dm['qkn']
    rdec = Dm['rdec']
    rtab = Dm['rtab']
    rcol = Dm['rcol']
    featsT = Dm['featsT']
    featsTc = Dm['featsTc']
    hw1 = Dm['hw1']
    hw2 = Dm['hw2']
    hcols = Dm['hcols']
    hw3 = Dm['hw3']
    negt = Dm['negt']
    adel = Dm['adel']
    Fm = Dm['Fm']
    Gm = Dm['Gm']
    Fc = Dm['Fc']
    Gc = Dm['Gc']
    YT = Dm['YT']
    with contextlib.ExitStack() as st:
        P.stack = st
        B = Bufs()
        ids = P.sb("ids", [128, 128], F32)
        idb = P.sb("idb", [128, 128], BF16)
        mcol = P.sb("mcol", [128, 2, 2, 8], F32)
        P.dma('sp', lambda e: e.dma_start(out=ids[:], in_=ident), writes=[B['ids']])
        P.dma('sp', lambda e: e.dma_start(out=mcol[:], in_=modcols), writes=[B['mcol']])
        P.op('dve', lambda e: e.tensor_copy(out=idb[:], in_=ids[:]), reads=[B['ids']], writes=[B['idb']])
        P.op('dve', lambda e: e.tensor_scalar_add(out=mcol[:, :, 1, :], in0=mcol[:, :, 1, :], scalar1=1.0), reads=[B['mcol']], writes=[B['mcol']])
        pt = P.ps("pt", [128, 2, 512], F32)
        ptb = P.ps("ptb", [128, 1024], BF16)
        pa = P.ps("pa", [128, 2, 512], F32)
        pb_ = P.ps("pb", [128, 2, 512], F32)
        pc = P.ps("pc", [128, 512], F32)
        cnt = {'t': 0}

        uTs = Dm.get('uTs')
        ustate = Dm.get('ustate', {'have': False})

        def build_uT(uT, stk):
            P.stack = stk
            if uTs is not None and ustate['have']:
                for f in range(8):
                    P.dma('sp', lambda e, f=f: e.dma_start(out=uT[:, f, :], in_=uTs[:, f, :]), writes=[B['uT']])
                return
            xb = [P.sb(f"xb{i}", [128, D], F32) for i in range(2)]
            for t in range(NT_A):
                ms = 1 if t >= 32 else 0
                xs = xb[t % 2]
                P.dma('sp', lambda e, t=t, xs=xs: e.dma_start(out=xs[:], in_=x[t]), writes=[B['xb', t % 2]])
                for f in range(8):
                    s = cnt['t'] % 2
                    cnt['t'] += 1
                    P.op('pe', lambda e, xs=xs, f=f, s=s: e.transpose(pt[:, s, 0:128], xs[:, f * 128:(f + 1) * 128], ids[:]),
                         reads=[B['xb', t % 2], B['ids']], writes=[B['pt', s]])
                    P.op('act', lambda e, t=t, f=f, s=s, ms=ms: e.activation(out=uT[:, f, t * 128:(t + 1) * 128], in_=pt[:, s, 0:128], func=AF.Identity,
                                                                           bias=mcol[:, ms, 0, f:f + 1], scale=mcol[:, ms, 1, f:f + 1]),
                         reads=[B['pt', s], B['mcol']], writes=[B['uT']])
            if uTs is not None:
                for f in range(8):
                    P.dma('sp', lambda e, f=f: e.dma_start(out=uTs[:, f, :], in_=uT[:, f, :]), reads=[B['uT']], writes=[B['uTs']])
                ustate['have'] = True

        def inproj_fm(uT, wsb, wkey, col0, dst, dkey):
            for bi, (tok0, ntok) in enumerate(TBLK_A):
                s = bi % 2
                for kt in range(8):
                    P.op('pe', lambda e, kt=kt, s=s, tok0=tok0, ntok=ntok: e.matmul(pa[:, s, 0:ntok], lhsT=wsb[:, kt, col0:col0 + 128], rhs=uT[:, kt, tok0:tok0 + ntok],
                                                                                  start=(kt == 0), stop=(kt == 7)),
                         reads=[B[wkey], B['uT']], writes=[B['pa', s]])
                eng = 'act' if bi % 2 == 0 else 'dve'
                if eng == 'act':
                    P.op('act', lambda e, s=s, tok0=tok0, ntok=ntok: e.copy(out=dst[:, tok0:tok0 + ntok], in_=pa[:, s, 0:ntok]), reads=[B['pa', s]], writes=[B[dkey]])
                else:
                    P.op('dve', lambda e, s=s, tok0=tok0, ntok=ntok: e.tensor_copy(out=dst[:, tok0:tok0 + ntok], in_=pa[:, s, 0:ntok]), reads=[B['pa', s]], writes=[B[dkey]])

        def dwconv(z, zkey, wc, wkey, acc, akey, out, okey):
            P.op('dve', lambda e: e.tensor_scalar(out=acc[:], in0=z, scalar1=wc[:, 1:2], scalar2=wc[:, 3:4], op0=ALU.mult, op1=ALU.add),
                 reads=[B[zkey], B[wkey]], writes=[B[akey]])
            for (a, n) in ((0, 4096), (4096, 256)):
                P.op('dve', lambda e, a=a, n=n: e.scalar_tensor_tensor(out=acc[:, a + 1:a + n], in0=z[:, a:a + n - 1], scalar=wc[:, 0:1], in1=acc[:, a + 1:a + n],
                                                                      op0=ALU.mult, op1=ALU.add), reads=[B[zkey], B[wkey], B[akey]], writes=[B[akey]])
                P.op('dve', lambda e, a=a, n=n: e.scalar_tensor_tensor(out=acc[:, a:a + n - 1], in0=z[:, a + 1:a + n], scalar=wc[:, 2:3], in1=acc[:, a:a + n - 1],
                                                                      op0=ALU.mult, op1=ALU.add), reads=[B[zkey], B[wkey], B[akey]], writes=[B[akey]])
            if out is not None:
                P.op('pool', lambda e: e.tensor_copy(out=out, in_=acc[:]), reads=[B[akey]], writes=[B[okey]])

        import os as _os
        _dbg = _os.environ.get('A_DBG', '').split(',')
        P.mute = ('noH' in _dbg)
        sH = st.enter_context(contextlib.ExitStack())
        P.stack = sH
        zH = P.sb("zH", [128, 6, NTOK_A], BF16)
        with contextlib.ExitStack() as s0:
            P.stack = s0
            uT = P.sb("uT", [128, 8, NTOK_A], BF16)
            wHs = P.sb("wHs", [128, 8, 768], BF16)
            P.dma('pool', lambda e: e.dma_start(out=wHs[:], in_=wH), writes=[B['wHs']])
            build_uT(uT, s0)
            for c in range(6):
                inproj_fm(uT, wHs, 'wHs', c * 128, zH[:, c, :], ('zH', c))
            P.flush()
        P.stack = sH
        hsks = P.sb("hsks", [128, 2, 2], F32)
        P.dma('sp', lambda e: e.dma_start(out=hsks[:], in_=hsk), writes=[B['hsks']])
        with contextlib.ExitStack() as s1:
            P.stack = s1
            hcws = P.sb("hcws", [128, 6, 4], F32)
            acc = P.sb("acc", [128, NTOK_A], F32)
            P.dma('sp', lambda e: e.dma_start(out=hcws[:], in_=hcw), writes=[B['hcws']])
            for c in range(6):
                dwconv(zH[:, c, :], ('zH', c), hcws[:, c, :], 'hcws', acc, 'acc', zH[:, c, :], ('zH', c))
            P.flush()
        P.stack = sH
        HTc = P.sb("HTc", [128, 2, 512], BF16)
        Fcs = P.sb("Fcs", [128, 2, 512], BF16)
        Gcs = P.sb("Gcs", [128, 4, 256], BF16)
        P.dma('sp', lambda e: e.dma_start(out=Fcs[:], in_=Fc), writes=[B['Fcs']])
        P.dma('sp', lambda e: e.dma_start(out=Gcs[:], in_=Gc), writes=[B['Gcs']])
        Ft = [P.sb(f"Ft{i}", [128, 1024], BF16) for i in range(NFB)]
        fcnt = {'n': 0}
        h2T = P.sb("h2T", [64, 4352], F32)
        hcs = P.sb("hcs", [64, 6], F32)
        w3s = P.sb("w3s", [64, 2, 3, 256], F32)
        negts = P.sb("negts", [128, 34], F32)
        adels = P.sb("adels", [128, 2, 3, 256], F32)
        npi = P.sb("npi", [64, 1], F32)
        P.dma('sp', lambda e: e.dma_start(out=hcs[:, 0:4], in_=hcols), writes=[B['hcs']])
        P.dma('sp', lambda e: e.dma_start(out=w3s[:, :, 0:2, :], in_=hw3), writes=[B['w3s']])
        P.dma('sp', lambda e: e.dma_start(out=negts[:], in_=negt), writes=[B['negts']])
        P.dma('sp', lambda e: e.dma_start(out=adels[:, :, 0:2, :], in_=adel), writes=[B['adels']])
        P.op('dve', lambda e: e.tensor_scalar(out=w3s[:, :, 2, :], in0=w3s[:, :, 1, :], scalar1=-1.0, scalar2=None, op0=ALU.mult), reads=[B['w3s']], writes=[B['w3s']])
        P.op('dve', lambda e: e.tensor_copy(out=adels[:, :, 2, :], in_=adels[:, :, 1, :]), reads=[B['adels']], writes=[B['adels']])
        P.op('dve', lambda e: e.tensor_tensor(out=hcs[:, 4:6], in0=hcs[:, 0:2], in1=hcs[:, 2:4], op=ALU.mult), reads=[B['hcs']], writes=[B['hcs']])
        P.op('dve', lambda e: e.memset(npi[:], -PI), writes=[B['npi']])

        def sin_layer(src, skey, wmat, wkey2, kdim, li, dst, dkey2, argb):
            for bi, (tok0, ntok) in enumerate(TBLK_A):
                s = bi % 2
                P.op('pe', lambda e, s=s, tok0=tok0, ntok=ntok: e.matmul(pa[0:64, s, 0:ntok], lhsT=wmat[0:kdim, :], rhs=src[0:kdim, tok0:tok0 + ntok], start=True, stop=True),
                     reads=[B[skey], B[wkey2]], writes=[B['pa', s]])
                ab = argb[s]
                P.op('dve', lambda e, s=s, ntok=ntok, ab=ab: e.tensor_scalar(out=ab[:, 0:ntok], in0=pa[0:64, s, 0:ntok], scalar1=hcs[:, 2 + li:3 + li], scalar2=hcs[:, 4 + li:5 + li],
                                                                            op0=ALU.mult, op1=ALU.add), reads=[B['pa', s], B['hcs']], writes=[B['argb', s]])
                m1 = argb[2 + s]
                P.op('dve', lambda e, ntok=ntok, ab=ab, m1=m1: e.tensor_scalar(out=m1[:, 0:ntok], in0=ab[:, 0:ntok], scalar1=PI, scalar2=None, op0=ALU.is_gt),
                     reads=[B['argb', s]], writes=[B['argm', s]])
                P.op('dve', lambda e, ntok=ntok, ab=ab, m1=m1: e.scalar_tensor_tensor(out=ab[:, 0:ntok], in0=m1[:, 0:ntok], scalar=-2.0 * PI, in1=ab[:, 0:ntok], op0=ALU.mult, op1=ALU.add),
                     reads=[B['argb', s], B['argm', s]], writes=[B['argb', s]])
                P.op('dve', lambda e, ntok=ntok, ab=ab, m1=m1: e.tensor_scalar(out=m1[:, 0:ntok], in0=ab[:, 0:ntok], scalar1=-PI, scalar2=None, op0=ALU.is_lt),
                     reads=[B['argb', s], B['argm', s]], writes=[B['argm', s]])
                P.op('dve', lambda e, ntok=ntok, ab=ab, m1=m1: e.scalar_tensor_tensor(out=ab[:, 0:ntok], in0=m1[:, 0:ntok], scalar=2.0 * PI, in1=ab[:, 0:ntok], op0=ALU.mult, op1=ALU.add),
                     reads=[B['argb', s], B['argm', s]], writes=[B['argb', s]])
                P.op('act', lambda e, tok0=tok0, ntok=ntok, ab=ab: e.activation(out=dst[:, tok0:tok0 + ntok], in_=ab[:, 0:ntok], func=AF.Sin),
                     reads=[B['argb', s]], writes=[B[dkey2]])

        with contextlib.ExitStack() as sm:
            P.stack = sm
            h1T = P.sb("h1T", [64, 4352], F32)
            w2s = P.sb("w2s", [64, 64], F32)
            argb = [P.sb(f"argb{i}", [64, 512], F32) for i in range(4)]
            P.dma('sp', lambda e: e.dma_start(out=w2s[:], in_=hw2), writes=[B['w2s']])
            with contextlib.ExitStack() as sm2:
                P.stack = sm2
                fT = P.sb("fT", [33, 4352], F32)
                w1s = P.sb("w1s", [33, 64], F32)
                P.dma('sp', lambda e: e.dma_start(out=fT[:, 0:4096], in_=featsT), writes=[B['fT']])
                P.dma('sp', lambda e: e.dma_start(out=fT[:, 4096:4352], in_=featsTc), writes=[B['fT']])
                P.dma('sp', lambda e: e.dma_start(out=w1s[:], in_=hw1), writes=[B['w1s']])
                sin_layer(fT, 'fT', w1s, 'w1s', 33, 0, h1T, 'h1T', argb)
                P.flush()
            sin_layer(h1T, 'h1T', w2s, 'w2s', 64, 1, h2T, 'h2T', argb)
            P.flush()

        gcnt = {'n': 0}
        for o in range(2):
          with contextlib.ExitStack() as so:
            P.stack = so
            HT = P.sb(f"HT{o}", [128, 2, 8192], BF16)
            with contextlib.ExitStack() as sf:
                P.stack = sf
                env = [P.sb(f"env{i}", [128, 768], F32) for i in range(2)]
                filt = P.sb("filt", [128, 34, 768], BF16)
                for t in range(NT_A):
                    s = t % 2
                    ev = env[s]
                    P.op('act', lambda e, t=t, o=o, ev=ev: e.activation(out=ev[:], in_=adels[:, o, :, :].rearrange("p a b -> p (a b)"), func=AF.Exp, scale=negts[:, t:t + 1]),
                         reads=[B['adels'], B['negts']], writes=[B['env', s]])
                    P.op('pe', lambda e, t=t, o=o, s=s: e.matmul(pb_[:, s, 0:512], lhsT=h2T[:, t * 128:(t + 1) * 128], rhs=w3s[:, o, 0:2, :].rearrange("p a b -> p (a b)"), start=True, stop=True),
                         reads=[B['h2T'], B['w3s']], writes=[B['pb', s, 0]])
                    P.op('dve', lambda e, t=t, s=s, ev=ev: e.tensor_tensor(out=filt[:, t, 0:512], in0=pb_[:, s, 0:512], in1=ev[:, 0:512], op=ALU.mult),
                         reads=[B['pb', s, 0], B['env', s]], writes=[B['filt', t]])
                P.op('dve', lambda e: e.memset(filt[0:1, 0, 256:512], 0.0), reads=[B['filt', 0]], writes=[B['filt', 0]])
                P.op('dve', lambda e: e.memset(filt[0:1, 32, 256:512], 0.0), reads=[B['filt', 32]], writes=[B['filt', 32]])
                for t in range(NT_A):
                    P.op('pool', lambda e, t=t: e.tensor_tensor(out=filt[:, t, 512:768], in0=filt[:, t, 0:256], in1=filt[:, t, 256:512], op=ALU.add),
                         reads=[B['filt', t]], writes=[B['filt', t]])
                    P.op('pool', lambda e, t=t: e.tensor_tensor(out=filt[:, t, 0:256], in0=filt[:, t, 0:256], in1=filt[:, t, 256:512], op=ALU.subtract),
                         reads=[B['filt', t]], writes=[B['filt', t]])
                for j in range(8):
                    for t in range(32):
                        fs = fcnt['n'] % NFB
                        fcnt['n'] += 1
                        P.dma('sp', lambda e, t=t, j=j, fs=fs: e.dma_start(out=Ft[fs][:], in_=Fm[t, :, j, :]), writes=[B['Ft', fs]])
                        for c in range(2):
                            P.op('pe', lambda e, t=t, c=c, fs=fs: e.matmul(pa[:, c, :], lhsT=filt[:, t, 512 + c * 128:512 + (c + 1) * 128], rhs=Ft[fs][:, 0:512], start=(t == 0), stop=(t == 31)),
                                 reads=[B['filt', t], B['Ft', fs]], writes=[B['pa', c]])
                            P.op('pe', lambda e, t=t, c=c, fs=fs: e.matmul(pb_[:, c, :], lhsT=filt[:, t, c * 128:(c + 1) * 128], rhs=Ft[fs][:, 512:1024], start=(t == 0), stop=(t == 31)),
                                 reads=[B['filt', t], B['Ft', fs]], writes=[B['pb', c, 0]])
                    for c in range(2):
                        P.op('act', lambda e, c=c, j=j: e.copy(out=HT[:, c, j * 1024:j * 1024 + 512], in_=pa[:, c, :]), reads=[B['pa', c]], writes=[B['HT', c]])
                        P.op('dve', lambda e, c=c, j=j: e.tensor_copy(out=HT[:, c, j * 1024 + 512:(j + 1) * 1024], in_=pb_[:, c, :]), reads=[B['pb', c, 0]], writes=[B['HT', c]])
                for c in range(2):
                    for t in range(2):
                        tt = 32 + t
                        P.op('pe', lambda e, t=t, tt=tt, c=c: e.matmul(pa[:, c, 0:256], lhsT=filt[:, tt, 512 + c * 128:512 + (c + 1) * 128], rhs=Fcs[:, t, 0:256], start=(t == 0), stop=(t == 1)),
                             reads=[B['filt', tt], B['Fcs']], writes=[B['pa', c]])
                        P.op('pe', lambda e, t=t, tt=tt, c=c: e.matmul(pb_[:, c, 0:256], lhsT=filt[:, tt, c * 128:(c + 1) * 128], rhs=Fcs[:, t, 256:512], start=(t == 0), stop=(t == 1)),
                             reads=[B['filt', tt], B['Fcs']], writes=[B['pb', c, 0]])
                    P.op('act', lambda e, c=c: e.copy(out=HTc[:, c, 0:256], in_=pa[:, c, 0:256]), reads=[B['pa', c]], writes=[B['HTc', c]])
                    P.op('dve', lambda e, c=c: e.tensor_copy(out=HTc[:, c, 256:512], in_=pb_[:, c, 0:256]), reads=[B['pb', c, 0]], writes=[B['HTc', c]])
                P.flush()
            with contextlib.ExitStack() as s3:
                P.stack = s3
                stm = P.sb("stm", [128, NT_A, 256], BF16)
                YhT = P.sb("YhT", [128, 2, 1024], BF16)
                Ytm = P.sb("Ytm", [128, 68, 256], BF16)
                Gt = [P.sb(f"Gt{i}", [128, 1024], BF16) for i in range(NFB)]
                pr1 = P.sb("pr1", [128, 512], F32)
                pr2 = P.sb("pr2", [128, 512], F32)
                yst = P.sb("yst", [128, 512], F32)
                ysk = P.sb("ysk", [128, 512], F32)
                for t in range(NT_A):
                    for c in range(2):
                        P.op('pe', lambda e, t=t, c=c: e.transpose(ptb[:, c * 128:(c + 1) * 128], zH[:, c, t * 128:(t + 1) * 128], idb[:]),
                             reads=[B['zH', c], B['idb']], writes=[B['ptb']])
                    if t % 2 == 0:
                        P.op('act', lambda e, t=t: e.copy(out=stm[:, t, :], in_=ptb[:, 0:256]), reads=[B['ptb']], writes=[B['stm', t]])
                    else:
                        P.op('dve', lambda e, t=t: e.tensor_copy(out=stm[:, t, :], in_=ptb[:, 0:256]), reads=[B['ptb']], writes=[B['stm', t]])

                def product(c, ucs, uss, hc, hs, n, keyc, keys_):
                    P.op('dve', lambda e: e.tensor_tensor(out=pr1[:, 0:n], in0=ucs, in1=hc, op=ALU.mult), reads=[B[keyc]], writes=[B['pr1']])
                    P.op('dve', lambda e: e.tensor_tensor(out=pr2[:, 0:n], in0=uss, in1=hs, op=ALU.mult), reads=[B[keys_]], writes=[B['pr2']])
                    P.op('pool', lambda e: e.tensor_tensor(out=YhT[:, c, 0:n], in0=pr1[:, 0:n], in1=pr2[:, 0:n], op=ALU.subtract), reads=[B['pr1'], B['pr2']], writes=[B['YhT', c]])
                    P.op('dve', lambda e: e.tensor_tensor(out=pr1[:, 0:n], in0=ucs, in1=hs, op=ALU.mult), reads=[B[keyc], B['pr1']], writes=[B['pr1']])
                    P.op('dve', lambda e: e.tensor_tensor(out=pr2[:, 0:n], in0=uss, in1=hc, op=ALU.mult), reads=[B[keys_], B['pr2']], writes=[B['pr2']])
                    P.op('pool', lambda e: e.tensor_tensor(out=YhT[:, c, 512:512 + n], in0=pr1[:, 0:n], in1=pr2[:, 0:n], op=ALU.add), reads=[B['pr1'], B['pr2']], writes=[B['YhT', c]])

                def ytrans(kts, cols):
                    for kt, c0 in zip(kts, cols):
                        for c in range(2):
                            P.op('pe', lambda e, c=c, c0=c0: e.transpose(ptb[:, c * 128:(c + 1) * 128], YhT[:, c, c0:c0 + 128], idb[:]),
                                 reads=[B['YhT', c], B['idb']], writes=[B['ptb']])
                        if kt % 2 == 0:
                            P.op('act', lambda e, kt=kt: e.copy(out=Ytm[:, kt, :], in_=ptb[:, 0:256]), reads=[B['ptb']], writes=[B['Ytm', kt]])
                        else:
                            P.op('dve', lambda e, kt=kt: e.tensor_copy(out=Ytm[:, kt, :], in_=ptb[:, 0:256]), reads=[B['ptb']], writes=[B['Ytm', kt]])

                for j in range(8):
                    for t in range(32):
                        fs = fcnt['n'] % NFB
                        fcnt['n'] += 1
                        P.dma('sp', lambda e, t=t, j=j, fs=fs: e.dma_start(out=Ft[fs][:], in_=Fm[t, :, j, :]), writes=[B['Ft', fs]])
                        for c in range(2):
                            P.op('pe', lambda e, t=t, c=c, fs=fs: e.matmul(pa[:, c, :], lhsT=stm[:, t, c * 128:(c + 1) * 128], rhs=Ft[fs][:, 0:512], start=(t == 0), stop=(t == 31)),
                                 reads=[B['stm', t], B['Ft', fs]], writes=[B['pa', c]])
                            P.op('pe', lambda e, t=t, c=c, fs=fs: e.matmul(pb_[:, c, :], lhsT=stm[:, t, c * 128:(c + 1) * 128], rhs=Ft[fs][:, 512:1024], start=(t == 0), stop=(t == 31)),
                                 reads=[B['stm', t], B['Ft', fs]], writes=[B['pb', c, 0]])
                    for c in range(2):
                        product(c, pa[:, c, :], pb_[:, c, :], HT[:, c, j * 1024:j * 1024 + 512], HT[:, c, j * 1024 + 512:(j + 1) * 1024], 512, ('pa', c), ('pb', c, 0))
                    ytrans([j * 4 + i for i in range(4)] + [32 + j * 4 + i for i in range(4)], [i * 128 for i in range(4)] + [512 + i * 128 for i in range(4)])
                for c in range(2):
                    for t in range(2):
                        P.op('pe', lambda e, t=t, c=c: e.matmul(pa[:, c, 0:256], lhsT=stm[:, 32 + t, c * 128:(c + 1) * 128], rhs=Fcs[:, t, 0:256], start=(t == 0), stop=(t == 1)),
                             reads=[B['stm', 32 + t], B['Fcs']], writes=[B['pa', c]])
                        P.op('pe', lambda e, t=t, c=c: e.matmul(pb_[:, c, 0:256], lhsT=stm[:, 32 + t, c * 128:(c + 1) * 128], rhs=Fcs[:, t, 256:512], start=(t == 0), stop=(t == 1)),
                             reads=[B['stm', 32 + t], B['Fcs']], writes=[B['pb', c, 0]])
                    product(c, pa[:, c, 0:256], pb_[:, c, 0:256], HTc[:, c, 0:256], HTc[:, c, 256:512], 256, ('pa', c), ('pb', c, 0))
                ytrans([64, 65, 66, 67], [0, 128, 512, 640])

                def finish_blk(c, ps_ap, pkey, tok0, n, scale):
                    P.op('pool', lambda e: e.tensor_scalar(out=ysk[:, 0:n], in0=zH[:, c, tok0:tok0 + n], scalar1=hsks[:, o, c:c + 1], scalar2=None, op0=ALU.mult),
                         reads=[B['zH', c], B['hsks']], writes=[B['ysk']])
                    P.op('dve', lambda e: e.scalar_tensor_tensor(out=yst[:, 0:n], in0=ps_ap, scalar=scale, in1=ysk[:, 0:n], op0=ALU.mult, op1=ALU.add),
                         reads=[B[pkey], B['ysk']], writes=[B['yst']])
                    if o == 0:
                        P.op('pool', lambda e: e.tensor_tensor(out=zH[:, c, tok0:tok0 + n], in0=yst[:, 0:n], in1=zH[:, 2 + c, tok0:tok0 + n], op=ALU.mult),
                             reads=[B['yst'], B['zH', 2 + c]], writes=[B['zH', c]])
                    else:
                        P.op('pool', lambda e: e.tensor_tensor(out=yst[:, 0:n], in0=yst[:, 0:n], in1=zH[:, 4 + c, tok0:tok0 + n], op=ALU.mult),
                             reads=[B['yst'], B['zH', 4 + c]], writes=[B['yst']])
                        P.dma('sp', lambda e: e.dma_start(out=YT[4 + c, :, tok0:tok0 + n], in_=yst[:, 0:n]), reads=[B['yst']], is_output=True)

                for ps_ in range(4):
                    for kt in range(64):
                        gs = gcnt['n'] % NFB
                        gcnt['n'] += 1
                        P.dma('sp', lambda e, ps_=ps_, kt=kt, gs=gs: e.dma_start(out=Gt[gs][:], in_=Gm[ps_, kt]), writes=[B['Gt', gs]])
                        for c in range(2):
                            for nb in range(2):
                                acc_ap = (pa if c == 0 else pb_)[:, nb, :]
                                P.op('pe', lambda e, kt=kt, c=c, nb=nb, gs=gs, acc_ap=acc_ap: e.matmul(acc_ap, lhsT=Ytm[:, kt, c * 128:(c + 1) * 128], rhs=Gt[gs][:, nb * 512:(nb + 1) * 512],
                                                                                                  start=(kt == 0), stop=(kt == 63)),
                                     reads=[B['Ytm', kt], B['Gt', gs]], writes=[B['pa', nb] if c == 0 else B['pb', nb, 0]])
                    for c in range(2):
                        for nb in range(2):
                            finish_blk(c, (pa if c == 0 else pb_)[:, nb, :], ('pa', nb) if c == 0 else ('pb', nb, 0), ps_ * 1024 + nb * 512, 512, 2.0 / 8192.0)
                for c in range(2):
                    for kt in range(4):
                        P.op('pe', lambda e, kt=kt, c=c: e.matmul(pc[:, 0:256], lhsT=Ytm[:, 64 + kt, c * 128:(c + 1) * 128], rhs=Gcs[:, kt, :], start=(kt == 0), stop=(kt == 3)),
                             reads=[B['Ytm', 64 + kt], B['Gcs']], writes=[B['pc']])
                    finish_blk(c, pc[:, 0:256], 'pc', 4096, 256, 2.0 / 512.0)
                P.flush()
        sH.close()
        P.stack = st
        P.mute = ('noR' in _dbg)
        build_A_rest(nc, P, B, st, x, wA, wR, wD, caw, ropec, ropes, qkn, rdec, rtab, rcol, YT, ids, idb, mcol, pt, ptb, pa, pb_, pc, build_uT, inproj_fm, dwconv)


def build_A():
    nc = bass.Bass("TRN2", target_bir_lowering=False)
    x = _din(nc, "x", [NT_A, 128, D])
    modcols = _din(nc, "modcols", [128, 2, 2, 8])
    ident = _din(nc, "ident", [128, 128])
    wA = _din(nc, "wA", [128, 8, 768])
    wR = _din(nc, "wR", [128, 8, 1024])
    wH = _din(nc, "wH", [128, 8, 768])
    wD = _din(nc, "wD", [128, 8, 512])
    caw = _din(nc, "caw", [128, 2, 4])
    hcw = _din(nc, "hcw", [128, 6, 4])
    hsk = _din(nc, "hsk", [128, 2, 2])
    ropec = _din(nc, "ropec", [128, 32, 64])
    ropes = _din(nc, "ropes", [128, 32, 64])
    qkn = _din(nc, "qkn", [128, 2, 128])
    rdec = _din(nc, "rdec", [128, 4])
    rtab = _din(nc, "rtab", [128, 6, 128])
    rcol = _din(nc, "rcol", [128, 4])
    featsT = _din(nc, "featsT", [33, 4096])
    featsTc = _din(nc, "featsTc", [33, 256])
    hw1 = _din(nc, "hw1", [33, 64])
    hw2 = _din(nc, "hw2", [64, 64])
    hcols = _din(nc, "hcols", [64, 4])
    hw3 = _din(nc, "hw3", [64, 2, 2, 256])
    negt = _din(nc, "negt", [128, 34])
    adel = _din(nc, "adel", [128, 2, 2, 256])
    Fm = _din(nc, "Fm", [32, 128, 8, 1024], BF16)
    Gm = _din(nc, "Gm", [4, 64, 128, 1024], BF16)
    Fc = _din(nc, "Fc", [128, 2, 512], BF16)
    Gc = _din(nc, "Gc", [128, 4, 256], BF16)
    YT = _dout(nc, "YT", [8, 128, NTOK_A])

    Dm = dict(x=x, modcols=modcols, ident=ident, wA=wA, wR=wR, wH=wH, wD=wD, caw=caw, hcw=hcw, hsk=hsk, ropec=ropec, ropes=ropes, qkn=qkn, rdec=rdec, rtab=rtab, rcol=rcol, featsT=featsT, featsTc=featsTc, hw1=hw1, hw2=hw2, hcols=hcols, hw3=hw3, negt=negt, adel=adel, Fm=Fm, Gm=Gm, Fc=Fc, Gc=Gc, YT=YT)
    with contextlib.ExitStack() as st0:
        P = Prog(nc, st0)
        emit_A(nc, P, Dm)
    return nc


def build_A_rest(nc, P, B, st, x, wA, wR, wD, caw, ropec, ropes, qkn, rdec, rtab, rcol, YT, ids, idb, mcol, pt, ptb, pa, pb_, pc, build_uT, inproj_fm, dwconv):
    SC = 128.0 ** -0.5
    sG = st.enter_context(contextlib.ExitStack())
    P.stack = sG
    uT = P.sb("uT2", [128, 8, NTOK_A], BF16)
    rc = P.sb("rc", [128, 32, 64], F32)
    rs = P.sb("rs", [128, 32, 64], F32)
    P.dma('sp', lambda e: e.dma_start(out=rc[:], in_=ropec), writes=[B['rope']])
    P.dma('sp', lambda e: e.dma_start(out=rs[:], in_=ropes), writes=[B['rope']])
    with contextlib.ExitStack() as s0:
        build_uT(uT, s0)
        P.flush()

    def rope(src, skey, t, tmp, tkey):
        v = src.rearrange("p (a h f) -> p a h f", a=2, h=2)
        x1 = v[:, :, 0, :]
        x2 = v[:, :, 1, :]
        c_ = rc[:, t, :].rearrange("p (a f) -> p a f", a=2)
        s_ = rs[:, t, :].rearrange("p (a f) -> p a f", a=2)
        tv = [tmp[:, i, :].rearrange("p (a f) -> p a f", a=2) for i in range(4)]
        P.op('dve', lambda e: e.tensor_tensor(out=tv[0], in0=x1, in1=c_, op=ALU.mult), reads=[B[skey], B['rope']], writes=[B[tkey, 0]])
        P.op('dve', lambda e: e.tensor_tensor(out=tv[1], in0=x2, in1=s_, op=ALU.mult), reads=[B[skey], B['rope']], writes=[B[tkey, 1]])
        P.op('pool', lambda e: e.tensor_tensor(out=tv[2], in0=x2, in1=c_, op=ALU.mult), reads=[B[skey], B['rope']], writes=[B[tkey, 2]])
        P.op('pool', lambda e: e.tensor_tensor(out=tv[3], in0=x1, in1=s_, op=ALU.mult), reads=[B[skey], B['rope']], writes=[B[tkey, 3]])
        P.op('dve', lambda e: e.tensor_tensor(out=x1, in0=tv[0], in1=tv[1], op=ALU.subtract), reads=[B[tkey, 0], B[tkey, 1], B[tkey, 3], B[skey]], writes=[B[skey]])
        P.op('pool', lambda e: e.tensor_tensor(out=x2, in0=tv[2], in1=tv[3], op=ALU.add), reads=[B[tkey, 2], B[tkey, 3], B[tkey, 1], B[skey]], writes=[B[skey]])

    import os as _os
    _dbg = _os.environ.get('A_DBG', '').split(',')
    _base = P.mute
    P.mute = _base or ('noS' in _dbg)
    with contextlib.ExitStack() as s1:
        P.stack = s1
        wAs = P.sb("wAs", [128, 8, 768], BF16)
        caws = P.sb("caws", [128, 2, 4], F32)
        P.dma('pool', lambda e: e.dma_start(out=wAs[:], in_=wA), writes=[B['wAs']])
        P.dma('sp', lambda e: e.dma_start(out=caws[:], in_=caw), writes=[B['caws']])
        zA = P.sb("zA", [128, 3, NTOK_A], BF16)
        pp_ = P.sb("pp_", [128, NTOK_A], BF16)
        acc = P.sb("accA", [128, NTOK_A], F32)
        for c in range(2):
            for g in range(3):
                inproj_fm(uT, wAs, 'wAs', g * 256 + c * 128, zA[:, g, :], ('zA', g))
            P.op('pool', lambda e: e.tensor_tensor(out=pp_[:], in0=zA[:, 1, :], in1=zA[:, 2, :], op=ALU.mult), reads=[B['zA', 1], B['zA', 2]], writes=[B['pp_']])
            dwconv(pp_[:], 'pp_', caws[:, c, :], 'caws', acc, 'accA', None, None)
            P.op('dve', lambda e: e.tensor_tensor(out=acc[:], in0=acc[:], in1=zA[:, 0, :], op=ALU.mult), reads=[B['accA'], B['zA', 0]], writes=[B['accA']])
            P.dma('sp', lambda e, c=c: e.dma_start(out=YT[0 + c], in_=acc[:]), reads=[B['accA']], is_output=True)
        P.flush()

    P.mute = _base or ('noD' in _dbg)
    with contextlib.ExitStack() as s2:
        P.stack = s2
        wDs = P.sb("wDs", [128, 8, 512], BF16)
        qkns = P.sb("qkns", [128, 2, 128], F32)
        P.dma('pool', lambda e: e.dma_start(out=wDs[:], in_=wD), writes=[B['wDs']])
        P.dma('sp', lambda e: e.dma_start(out=qkns[:], in_=qkn), writes=[B['qkns']])
        qT = P.sb("qT", [128, 2, NTOK_A], BF16)
        kT = P.sb("kT", [128, NTOK_A], BF16)
        vtm = P.sb("vtm", [128, NT_A, 128], BF16)
        onesb = P.sb("onesb", [128, 128], BF16)
        P.op('dve', lambda e: e.memset(onesb[:], 1.0), writes=[B['onesb']])
        qn = [P.sb(f"qn{i}", [128, 3, 128], F32) for i in range(2)]
        qb = [P.sb(f"qb{i}", [128, 3, 128], BF16) for i in range(2)]
        sq3 = P.sb("sq3", [128, 384], F32)
        ss = [P.sb(f"ss{i}", [128, 4], F32) for i in range(2)]
        tmp = P.sb("tmpr", [128, 4, 64], F32)
        for t in range(NT_A):
            s = t % 2
            for kt in range(8):
                P.op('pe', lambda e, t=t, kt=kt, s=s: e.matmul(pa[:, s, :], lhsT=uT[:, kt, t * 128:(t + 1) * 128], rhs=wDs[:, kt, :], start=(kt == 0), stop=(kt == 7)),
                     reads=[B['uT'], B['wDs']], writes=[B['pa', s]])
            P.op('act', lambda e, s=s: e.activation(out=sq3[:], in_=pa[:, s, 0:384], func=AF.Square), reads=[B['pa', s]], writes=[B['sq3']])
            P.op('dve', lambda e, s=s: e.reduce_sum(out=ss[s][:, 0:3], in_=sq3[:].rearrange("p (h d) -> p h d", h=3), axis=AX.X),
                 reads=[B['sq3']], writes=[B['ss', s, 0], B['ss', s, 1], B['ss', s, 2]])
            P.op('dve', lambda e, s=s: e.tensor_scalar(out=ss[s][:, 0:3], in0=ss[s][:, 0:3], scalar1=1.0 / 128.0, scalar2=1e-6, op0=ALU.mult, op1=ALU.add),
                 reads=[B['ss', s, h] for h in range(3)], writes=[B['ss', s, 'a']])
            P.op('act', lambda e, s=s: e.sqrt(out=ss[s][:, 0:3], in_=ss[s][:, 0:3]), reads=[B['ss', s, 'a']], writes=[B['ss', s, 'a']])
            P.op('dve', lambda e, s=s: e.reciprocal(out=ss[s][:, 0:3], in_=ss[s][:, 0:3]), reads=[B['ss', s, 'a']], writes=[B['ss', s, 'a']])
            for h in range(3):
                P.op('dve', lambda e, h=h, s=s: e.scalar_tensor_tensor(out=qn[s][:, h, :], in0=pa[:, s, h * 128:(h + 1) * 128], scalar=ss[s][:, h:h + 1], in1=qkns[:, 0 if h < 2 else 1, :],
                                                                     op0=ALU.mult, op1=ALU.mult), reads=[B['pa', s], B['ss', s, 'a'], B['qkns']], writes=[B['qn', s, h]])
                if t < 32:
                    rope(qn[s][:, h, :], ('qn', s, h), t, tmp, 'tmpr')
                P.op('pool', lambda e, h=h, s=s: e.tensor_copy(out=qb[s][:, h, :], in_=qn[s][:, h, :]), reads=[B['qn', s, h]], writes=[B['qb', s, h]])
            P.op('act', lambda e, t=t, s=s: e.copy(out=vtm[:, t, :], in_=pa[:, s, 384:512]), reads=[B['pa', s]], writes=[B['vtm', t]])
            for h in range(3):
                P.op('pe', lambda e, h=h, s=s: e.transpose(ptb[:, h * 128:(h + 1) * 128], qb[s][:, h, :], idb[:]), reads=[B['qb', s, h], B['idb']], writes=[B['ptb']])
            P.op('dve', lambda e, t=t: e.tensor_copy(out=qT[:, :, t * 128:(t + 1) * 128], in_=ptb[:, 0:256].rearrange("p (h d) -> p h d", h=2)), reads=[B['ptb']], writes=[B['qT']])
            P.op('act', lambda e, t=t: e.copy(out=kT[:, t * 128:(t + 1) * 128], in_=ptb[:, 256:384]), reads=[B['ptb']], writes=[B['kT']])
        if 'dumpQK' in _dbg:
            dq = P.sb("dq", [128, NTOK_A], F32)
            for i_, src_ in enumerate((qT[:, 0, :], qT[:, 1, :], kT[:])):
                P.op('dve', lambda e, src_=src_: e.tensor_copy(out=dq[:], in_=src_), reads=[B['qT'], B['kT']], writes=[B['dq']])
                P.dma('sp', lambda e, i_=i_: e.dma_start(out=YT[5 + i_], in_=dq[:]), reads=[B['dq']], writes=[B['dqo', i_]], is_output=True)
            P.op('dve', lambda e: e.tensor_copy(out=dq[:].rearrange("p (t d) -> p t d", d=128), in_=vtm[:]), reads=[B['vtm', t_] for t_ in range(NT_A)], writes=[B['dq']])
            P.dma('sp', lambda e: e.dma_start(out=YT[4], in_=dq[:]), reads=[B['dq']], is_output=True)
        P.mute = P.mute or ('noD2' in _dbg)
        pT = [P.sb(f"pT{i}", [128, 512], BF16) for i in range(3)]
        rd = P.sb("rd", [128, 512], F32)
        yo = [P.sb(f"yo{i}", [128, 512], F32) for i in range(2)]
        pcnt = 0
        ocnt = 0
        for h in range(2):
            for bi, (tok0, ntok) in enumerate(TBLK_A):
                if 'cOne' in _dbg and (h, bi) != (0, 0):
                    continue
                ktiles = list(range(34)) if bi < 8 else [32, 33]
                nkt = len(ktiles)
                slots = []
                for ki, kt in enumerate(ktiles):
                    slots.append((pcnt % 2, pcnt % 3))
                    pcnt += 1

                def qk_exp(ki):
                    kt = ktiles[ki]
                    s, p3 = slots[ki]
                    P.op('pe', lambda e, kt=kt, h=h, s=s, tok0=tok0, ntok=ntok: e.matmul(pa[:, s, 0:ntok], lhsT=kT[:, kt * 128:(kt + 1) * 128], rhs=qT[:, h, tok0:tok0 + ntok], start=True, stop=True),
                         reads=[B['kT'], B['qT']], writes=[B['pa', s]])
                    P.op('act', lambda e, s=s, p3=p3, ntok=ntok: e.activation(out=pT[p3][:, 0:ntok], in_=pa[:, s, 0:ntok], func=AF.Exp, scale=SC), reads=[B['pa', s]], writes=[B['pT', p3]])

                def pv(ki):
                    kt = ktiles[ki]
                    s, p3 = slots[ki]
                    P.op('pe', lambda e, kt=kt, p3=p3, ntok=ntok, ki=ki, nkt=nkt: e.matmul(pb_[:, 0, 0:ntok], lhsT=vtm[:, kt, :], rhs=pT[p3][:, 0:ntok], start=(ki == 0), stop=(ki == nkt - 1)),
                         reads=[B['vtm', kt], B['pT', p3]], writes=[B['pb', 0, 0]])
                    P.op('pe', lambda e, p3=p3, ntok=ntok, ki=ki, nkt=nkt: e.matmul(pb_[:, 1, 0:ntok], lhsT=onesb[:], rhs=pT[p3][:, 0:ntok], start=(ki == 0), stop=(ki == nkt - 1)),
                         reads=[B['onesb'], B['pT', p3]], writes=[B['pb', 1, 0]])

                qk_exp(0)
                for ki in range(1, nkt):
                    qk_exp(ki)
                    pv(ki - 1)
                pv(nkt - 1)
                if 'cQK' in _dbg:
                    continue
                P.op('dve', lambda e, ntok=ntok: e.reciprocal(out=rd[:, 0:ntok], in_=pb_[:, 1, 0:ntok]), reads=[B['pb', 1, 0]], writes=[B['rd']])
                y = yo[ocnt % 2]
                yk = ('yo', ocnt % 2)
                ocnt += 1
                P.op('dve', lambda e, ntok=ntok, y=y: e.tensor_tensor(out=y[:, 0:ntok], in0=pb_[:, 0, 0:ntok], in1=rd[:, 0:ntok], op=ALU.mult), reads=[B['pb', 0, 0], B['rd']], writes=[B[yk]])
                P.dma('sp', lambda e, h=h, tok0=tok0, ntok=ntok, y=y: e.dma_start(out=YT[6 + h, :, tok0:tok0 + ntok], in_=y[:, 0:ntok]), reads=[B[yk]], is_output=True)
        P.flush()

    P.mute = _base or ('noT' in _dbg)
    with contextlib.ExitStack() as s3:
        P.stack = s3
        wRs = P.sb("wRs", [128, 8, 1024], BF16)
        P.dma('pool', lambda e: e.dma_start(out=wRs[:], in_=wR), writes=[B['wRs']])
        rds = P.sb("rds", [128, 4], F32)
        lgam = P.sb("lgam", [128, 4], F32)
        rtabs = P.sb("rtabs", [128, 6, 128], F32)
        rcols = P.sb("rcols", [128, 4], F32)
        P.dma('sp', lambda e: e.dma_start(out=rds[:], in_=rdec), writes=[B['rds']])
        P.dma('sp', lambda e: e.dma_start(out=rtabs[:], in_=rtab), writes=[B['rtabs']])
        P.dma('sp', lambda e: e.dma_start(out=rcols[:], in_=rcol), writes=[B['rcols']])
        P.op('act', lambda e: e.activation(out=lgam[:], in_=rds[:], func=AF.Exp), reads=[B['rds']], writes=[B['lgam']])
        P.op('dve', lambda e: e.tensor_scalar(out=lgam[:], in0=lgam[:], scalar1=-1.0, scalar2=None, op0=ALU.mult), reads=[B['lgam']], writes=[B['lgam']])
        mask = P.sb("mask", [128, 128], F32)
        mtmp = P.sb("mtmpR", [128, 128], F32)
        qdf = P.sb("qdf", [128, 128], F32)
        qdb = P.sb("qdb", [128, 128], F32)
        kdc = P.sb("kdc", [128, 4], F32)
        qT = P.sb("rqT", [128, NTOK_A], BF16)
        kT = P.sb("rkT", [128, NTOK_A], BF16)
        qdfT = P.sb("qdfT", [128, NTOK_A], BF16)
        qdbT = P.sb("qdbT", [128, NTOK_A], BF16)
        kdf = P.sb("kdf", [128, NT_A, 128], BF16)
        kdb = P.sb("kdb", [128, NT_A, 128], BF16)
        vtm = P.sb("rvtm", [128, NT_A, 128], BF16)
        sgt = P.sb("sgt", [128, NT_A, 128], BF16)
        Sf = P.sb("Sf", [128, NT_A, 128], BF16)
        Sb = P.sb("Sb", [128, NT_A, 128], BF16)
        stf = [P.sb(f"stf{i}", [128, 128], F32) for i in range(2)]
        qk = [P.sb(f"rqk{i}", [128, 2, 128], F32) for i in range(2)]
        qkb = [P.sb(f"rqkb{i}", [128, 2, 128], BF16) for i in range(2)]
        tmp = P.sb("tmpr2", [128, 4, 64], F32)
        smT = [P.sb(f"smT{i}", [128, 128], BF16) for i in range(2)]
        stt = P.sb("rstt", [128, 6], F32)
        mv = P.sb("rmv", [128, 2], F32)
        rstd = P.sb("rrstd", [128, 1], F32)
        on = [P.sb(f"on{i}", [128, 128], F32) for i in range(2)]
        ob = [P.sb(f"ob{i}", [128, 128], BF16) for i in range(2)]
        ysg = [P.sb(f"rysg{i}", [128, 512], F32) for i in range(2)]
        for h in range(2):
            lf = lgam[:, h:h + 1]
            lb = lgam[:, 2 + h:3 + h]
            P.op('act', lambda e, lf=lf: e.activation(out=mask[:], in_=rtabs[:, 0, :], func=AF.Exp, scale=lf), reads=[B['rtabs'], B['lgam']], writes=[B['mask']])
            P.op('dve', lambda e: e.tensor_tensor(out=mask[:], in0=mask[:], in1=rtabs[:, 1, :], op=ALU.mult), reads=[B['mask'], B['rtabs']], writes=[B['mask']])
            P.op('act', lambda e, lb=lb: e.activation(out=mtmp[:], in_=rtabs[:, 2, :], func=AF.Exp, scale=lb), reads=[B['rtabs'], B['lgam']], writes=[B['mtmpR']])
            P.op('dve', lambda e: e.tensor_tensor(out=mtmp[:], in0=mtmp[:], in1=rtabs[:, 3, :], op=ALU.mult), reads=[B['mtmpR'], B['rtabs']], writes=[B['mtmpR']])
            P.op('dve', lambda e: e.tensor_tensor(out=mask[:], in0=mask[:], in1=mtmp[:], op=ALU.add), reads=[B['mask'], B['mtmpR']], writes=[B['mask']])
            P.op('act', lambda e, lf=lf: e.activation(out=qdf[:], in_=rtabs[:, 4, :], func=AF.Exp, scale=lf), reads=[B['rtabs'], B['lgam']], writes=[B['qdf']])
            P.op('act', lambda e, lb=lb: e.activation(out=qdb[:], in_=rtabs[:, 5, :], func=AF.Exp, scale=lb), reads=[B['rtabs'], B['lgam']], writes=[B['qdb']])
            P.op('act', lambda e, lf=lf: e.activation(out=kdc[:, 0:1], in_=rcols[:, 0:1], func=AF.Exp, scale=lf), reads=[B['rcols'], B['lgam']], writes=[B['kdc', 0]])
            P.op('act', lambda e, lb=lb: e.activation(out=kdc[:, 1:2], in_=rcols[:, 1:2], func=AF.Exp, scale=lb), reads=[B['rcols'], B['lgam']], writes=[B['kdc', 1]])
            P.op('act', lambda e, lf=lf: e.activation(out=kdc[:, 2:3], in_=rcols[:, 2:3], func=AF.Exp, scale=lf), reads=[B['rcols'], B['lgam']], writes=[B['kdc', 2]])
            P.op('act', lambda e, lb=lb: e.activation(out=kdc[:, 3:4], in_=rcols[:, 2:3], func=AF.Exp, scale=lb), reads=[B['rcols'], B['lgam']], writes=[B['kdc', 3]])
            kd = [B['kdc', i] for i in range(4)]
            for t in range(NT_A):
                s = t % 2
                for g in range(4):
                    for kt in range(8):
                        P.op('pe', lambda e, t=t, kt=kt, s=s, g=g, h=h: e.matmul(pa[:, s, g * 128:(g + 1) * 128], lhsT=uT[:, kt, t * 128:(t + 1) * 128], rhs=wRs[:, kt, g * 256 + h * 128:g * 256 + (h + 1) * 128],
                                                                        start=(kt == 0), stop=(kt == 7)), reads=[B['uT'], B['wRs']], writes=[B['pa', s]])
                P.op('act', lambda e, s=s: e.copy(out=qk[s][:, 0, :], in_=pa[:, s, 0:128]), reads=[B['pa', s]], writes=[B['rqk', s, 0]])
                P.op('act', lambda e, s=s: e.activation(out=qk[s][:, 1, :], in_=pa[:, s, 128:256], func=AF.Copy, scale=SC), reads=[B['pa', s]], writes=[B['rqk', s, 1]])
                P.op('act', lambda e, s=s, t=t: e.copy(out=vtm[:, t, :], in_=pa[:, s, 256:384]), reads=[B['pa', s]], writes=[B['rvtm', t]])
                P.op('act', lambda e, s=s, t=t: e.activation(out=sgt[:, t, :], in_=pa[:, s, 384:512], func=AF.Silu), reads=[B['pa', s]], writes=[B['sgt', t]])
                for j in range(2):
                    if t < 32:
                        rope(qk[s][:, j, :], ('rqk', s, j), t, tmp, 'tmpr2')
                    P.op('pool', lambda e, s=s, j=j: e.tensor_copy(out=qkb[s][:, j, :], in_=qk[s][:, j, :]), reads=[B['rqk', s, j]], writes=[B['rqkb', s, j]])
                P.op('dve', lambda e, s=s, t=t: e.tensor_scalar(out=kdf[:, t, :], in0=qk[s][:, 1, :], scalar1=kdc[:, 0:1], scalar2=None, op0=ALU.mult), reads=[B['rqk', s, 1], kd[0]], writes=[B['kdf', t]])
                P.op('dve', lambda e, s=s, t=t: e.tensor_scalar(out=kdb[:, t, :], in0=qk[s][:, 1, :], scalar1=kdc[:, 1:2], scalar2=None, op0=ALU.mult), reads=[B['rqk', s, 1], kd[1]], writes=[B['kdb', t]])
                for j in range(2):
                    P.op('pe', lambda e, s=s, j=j: e.transpose(ptb[:, j * 128:(j + 1) * 128], qkb[s][:, j, :], idb[:]), reads=[B['rqkb', s, j], B['idb']], writes=[B['ptb']])
                sl = slice(t * 128, (t + 1) * 128)
                P.op('act', lambda e, sl=sl: e.copy(out=qT[:, sl], in_=ptb[:, 0:128]), reads=[B['ptb']], writes=[B['rqT', t]])
                P.op('act', lambda e, sl=sl: e.copy(out=kT[:, sl], in_=ptb[:, 128:256]), reads=[B['ptb']], writes=[B['rkT', t]])
                P.op('dve', lambda e, sl=sl: e.tensor_tensor(out=qdfT[:, sl], in0=ptb[:, 0:128], in1=qdf[:], op=ALU.mult), reads=[B['ptb'], B['qdf']], writes=[B['qdfT', t]])
                P.op('dve', lambda e, sl=sl: e.tensor_tensor(out=qdbT[:, sl], in0=ptb[:, 0:128], in1=qdb[:], op=ALU.mult), reads=[B['ptb'], B['qdb']], writes=[B['qdbT', t]])

            def chain(order, kdx, Sx, cd, ckey, name):
                cur = None
                for n_, t in enumerate(order):
                    if cur is None:
                        P.op('pool', lambda e, t=t: e.memset(Sx[:, t, :], 0.0), writes=[B[name, t]])
                    else:
                        P.op('pool', lambda e, t=t, cur=cur: e.tensor_copy(out=Sx[:, t, :], in_=stf[cur][:]), reads=[B['stf', cur]], writes=[B[name, t]])
                    if n_ == len(order) - 1:
                        break
                    P.op('pe', lambda e, t=t: e.matmul(pc[:, 0:128], lhsT=kdx[:, t, :], rhs=vtm[:, t, :], start=True, stop=True), reads=[B[name + 'k', t], B['rvtm', t]], writes=[B['pc']])
                    nxt = 0 if cur is None else 1 - cur
                    if cur is None:
                        P.op('dve', lambda e, nxt=nxt: e.tensor_copy(out=stf[nxt][:], in_=pc[:, 0:128]), reads=[B['pc']], writes=[B['stf', nxt]])
                    else:
                        P.op('dve', lambda e, nxt=nxt, cur=cur: e.scalar_tensor_tensor(out=stf[nxt][:], in0=stf[cur][:], scalar=cd, in1=pc[:, 0:128], op0=ALU.mult, op1=ALU.add),
                             reads=[B['stf', cur], B['pc'], ckey], writes=[B['stf', nxt]])
                    cur = nxt
            for t in range(NT_A):
                B.d[('Sfk', t)] = B['kdf', t]
                B.d[('Sbk', t)] = B['kdb', t]
            chain([32, 33] + list(range(32)), kdf, Sf, kdc[:, 2:3], kd[2], 'Sf')
            chain([33, 32] + list(range(31, -1, -1)), kdb, Sb, kdc[:, 3:4], kd[3], 'Sb')
            for t in range(NT_A):
                s = t % 2
                sl = slice(t * 128, (t + 1) * 128)
                P.op('pe', lambda e, sl=sl, s=s: e.matmul(pa[:, s, 0:128], lhsT=kT[:, sl], rhs=qT[:, sl], start=True, stop=True), reads=[B['rkT', t], B['rqT', t]], writes=[B['pa', s]])
                P.op('dve', lambda e, s=s: e.tensor_tensor(out=smT[s][:], in0=pa[:, s, 0:128], in1=mask[:], op=ALU.mult), reads=[B['pa', s], B['mask']], writes=[B['smT', s]])
                P.op('pe', lambda e, t=t, s=s: e.matmul(pb_[:, s, 0:128], lhsT=smT[s][:], rhs=vtm[:, t, :], start=True, stop=False), reads=[B['smT', s], B['rvtm', t]], writes=[B['pb', s, 0]])
                P.op('pe', lambda e, t=t, s=s, sl=sl: e.matmul(pb_[:, s, 0:128], lhsT=qdfT[:, sl], rhs=Sf[:, t, :], start=False, stop=False), reads=[B['qdfT', t], B['Sf', t]], writes=[B['pb', s, 0]])
                P.op('pe', lambda e, t=t, s=s, sl=sl: e.matmul(pb_[:, s, 0:128], lhsT=qdbT[:, sl], rhs=Sb[:, t, :], start=False, stop=True), reads=[B['qdbT', t], B['Sb', t]], writes=[B['pb', s, 0]])
                P.op('act', lambda e, s=s: e.copy(out=on[s][:], in_=pb_[:, s, 0:128]), reads=[B['pb', s, 0]], writes=[B['on', s]])
                P.op('dve', lambda e, s=s: e.bn_stats(out=stt[:], in_=on[s][:]), reads=[B['on', s]], writes=[B['rstt']])
                P.op('dve', lambda e: e.bn_aggr(out=mv[:], in_=stt[:]), reads=[B['rstt']], writes=[B['rmv']])
                P.op('dve', lambda e: e.tensor_scalar_add(out=rstd[:], in0=mv[:, 1:2], scalar1=1e-6), reads=[B['rmv']], writes=[B['rrstd']])
                P.op('act', lambda e: e.sqrt(out=rstd[:], in_=rstd[:]), reads=[B['rrstd']], writes=[B['rrstd']])
                P.op('dve', lambda e: e.reciprocal(out=rstd[:], in_=rstd[:]), reads=[B['rrstd']], writes=[B['rrstd']])
                P.op('dve', lambda e, s=s: e.tensor_scalar(out=on[s][:], in0=on[s][:], scalar1=mv[:, 0:1], scalar2=rstd[:, 0:1], op0=ALU.subtract, op1=ALU.mult),
                     reads=[B['on', s], B['rmv'], B['rrstd']], writes=[B['on', s]])
                P.op('pool', lambda e, s=s, t=t: e.tensor_tensor(out=ob[s][:], in0=on[s][:], in1=sgt[:, t, :], op=ALU.mult), reads=[B['on', s], B['sgt', t]], writes=[B['ob', s]])
                P.op('pe', lambda e, s=s: e.transpose(ptb[:, 512:640], ob[s][:], idb[:]), reads=[B['ob', s], B['idb']], writes=[B['ptb2']])
                g4 = t // 4
                yg = ysg[g4 % 2]
                P.op('act', lambda e, t=t, yg=yg: e.copy(out=yg[:, (t % 4) * 128:(t % 4 + 1) * 128], in_=ptb[:, 512:640]), reads=[B['ptb2']], writes=[B['rysg', g4 % 2]])
                if t % 4 == 3 or t == NT_A - 1:
                    n_ = (t % 4 + 1) * 128
                    P.dma('sp', lambda e, h=h, g4=g4, yg=yg, n_=n_: e.dma_start(out=YT[2 + h, :, g4 * 512:g4 * 512 + n_], in_=yg[:, 0:n_]), reads=[B['rysg', g4 % 2]], is_output=True)
        P.flush()


import ml_dtypes
_BF = ml_dtypes.bfloat16
_CONST = {}


def _consts():
    if _CONST:
        return _CONST
    C = _CONST
    rows = 64
    row = np.repeat(np.arange(rows, dtype=np.float32), 64)
    col = np.tile(np.arange(64, dtype=np.float32), rows)
    inv = (10000.0 ** (-np.arange(32, dtype=np.float32) / 32)).astype(np.float32)
    ang = np.stack([row[:, None] * inv, col[:, None] * inv], axis=1).astype(np.float32)
    C['ropec'] = np.ascontiguousarray(np.cos(ang).astype(np.float32).reshape(32, 128, 64).transpose(1, 0, 2))
    C['ropes'] = np.ascontiguousarray(np.sin(ang).astype(np.float32).reshape(32, 128, 64).transpose(1, 0, 2))
    j = np.arange(128, dtype=np.float32)[:, None]
    i = np.arange(128, dtype=np.float32)[None, :]
    rtab = np.stack([np.maximum(i - j, 0), (i >= j).astype(np.float32), np.maximum(j - i, 0), (j >= i).astype(np.float32),
                     np.broadcast_to(i + 1, (128, 128)), np.broadcast_to(128 - i, (128, 128))], axis=1).astype(np.float32)
    C['rtab'] = np.ascontiguousarray(rtab)
    jj = np.arange(128, dtype=np.float32)
    C['rcol'] = np.ascontiguousarray(np.stack([127 - jj, jj, np.full(128, 128.0), np.zeros(128)], 1).astype(np.float32))

    def feats(l):
        t = np.linspace(0.0, 1.0, l, dtype=np.float32)[:, None]
        w = (2.0 * np.pi * np.arange(l, dtype=np.float32) / l).astype(np.float32)
        f = np.linspace(1e-4, 15, 16, dtype=np.float32)
        a = (w[:, None] * f[None, :]).astype(np.float32)
        return np.concatenate([t, np.cos(a), -np.sin(a)], axis=-1).astype(np.float32), t[:, 0]
    f4, t4 = feats(4096)
    fc, tc = feats(256)
    C['featsT'] = np.ascontiguousarray(f4.T)
    C['featsTc'] = np.ascontiguousarray(fc.T)
    negt = np.concatenate([-t4.reshape(32, 128), -tc.reshape(2, 128)], 0).T
    C['negt'] = np.ascontiguousarray(negt.astype(np.float32))
    mx = np.log(1e-2) / 0.3
    mn = np.log(1e-2) / 1.5
    C['absdelta'] = np.abs(np.linspace(mn, mx, 2048, dtype=np.float32)).reshape(2, 2, 512)
    n = np.arange(4096, dtype=np.float64)
    k = np.arange(4096, dtype=np.float64) + 0.5
    Fm = np.empty((32, 128, 8, 1024), dtype=_BF)
    for nt in range(32):
        th = 2 * np.pi * np.outer(n[nt * 128:(nt + 1) * 128], k) / 8192.0
        Fm[nt, :, :, 0:512] = np.cos(th).reshape(128, 8, 512).astype(_BF)
        Fm[nt, :, :, 512:1024] = np.sin(th).reshape(128, 8, 512).astype(_BF)
    C['Fm'] = Fm
    Gm = np.empty((4, 64, 128, 1024), dtype=_BF)
    for kt in range(32):
        th = 2 * np.pi * np.outer(k[kt * 128:(kt + 1) * 128], n) / 8192.0
        Gm[:, kt] = np.cos(th).reshape(128, 4, 1024).transpose(1, 0, 2).astype(_BF)
        Gm[:, 32 + kt] = np.sin(th).reshape(128, 4, 1024).transpose(1, 0, 2).astype(_BF)
    C['Gm'] = Gm
    nc_ = np.arange(256, dtype=np.float64)
    kc = np.arange(256, dtype=np.float64) + 0.5
    th = 2 * np.pi * np.outer(nc_, kc) / 512.0
    Fc = np.concatenate([np.cos(th), np.sin(th)], 1).reshape(2, 128, 512).transpose(1, 0, 2)
    C['Fc'] = np.ascontiguousarray(Fc).astype(_BF)
    Gc = np.concatenate([np.cos(th.T), np.sin(th.T)], 0).reshape(4, 128, 256).transpose(1, 0, 2)
    C['Gc'] = np.ascontiguousarray(Gc).astype(_BF)
    return C


def run_A(l, x_cur, h_cur, mod, inp):
    C = _consts()
    w_in = inp['w_in'][l]
    ins = []
    for b in range(4):
        xt = np.ascontiguousarray(np.concatenate([x_cur[b], h_cur[b]], 0).reshape(NT_A, 128, 1024))
        ms = [mod[b], mod[4]]
        modcols = np.ascontiguousarray(np.stack([np.stack([_cols(m[k_ * 1024:(k_ + 1) * 1024]) for k_ in (0, 1)], 1) for m in ms], 1))
        for half in range(2):
            r256 = half * 256 + np.arange(256)
            colsA = np.concatenate([g * 512 + r256 for g in range(3)])
            colsR = np.concatenate([1536 + g * 512 + r256 for g in range(4)])
            colsH = np.concatenate([3584 + g * 512 + r256 for g in range(3)])
            colsD = np.concatenate([5120 + r256, 5120 + 512 + half * 128 + np.arange(128), 5120 + 768 + half * 128 + np.arange(128)])
            ch = half * 256 + np.arange(256)
            caw = np.stack([inp['conv_a_w'][l][0, ch], inp['conv_a_w'][l][1, ch], inp['conv_a_w'][l][2, ch], inp['conv_a_b'][l][ch]], -1)
            hch = np.concatenate([g * 512 + ch for g in range(3)])
            hcw = np.stack([inp['hy_conv_w'][l][0, hch], inp['hy_conv_w'][l][1, hch], inp['hy_conv_w'][l][2, hch], inp['hy_conv_b'][l][hch]], -1)
            d = dict(
                x=xt, modcols=modcols, ident=_IDENT,
                wA=_tile_rows(w_in[:, colsA]), wR=_tile_rows(w_in[:, colsR]), wH=_tile_rows(w_in[:, colsH]), wD=_tile_rows(w_in[:, colsD]),
                caw=np.ascontiguousarray(caw.reshape(2, 128, 4).transpose(1, 0, 2)),
                hcw=np.ascontiguousarray(hcw.reshape(6, 128, 4).transpose(1, 0, 2)),
                hsk=np.ascontiguousarray(inp['hy_skip'][l][:, ch].reshape(2, 2, 128).transpose(2, 0, 1)),
                ropec=C['ropec'], ropes=C['ropes'],
                qkn=_rep(np.stack([inp['q_norm'][l], inp['k_norm'][l]], 0)),
                rdec=_rep(inp['ret_decay'][l][:, half * 2:half * 2 + 2].reshape(4)),
                rtab=C['rtab'], rcol=C['rcol'], featsT=C['featsT'], featsTc=C['featsTc'],
                hw1=np.ascontiguousarray(inp['hy_w1'][l]), hw2=np.ascontiguousarray(inp['hy_w2'][l]),
                hcols=np.ascontiguousarray(np.stack([inp['hy_b1'][l], inp['hy_b2'][l], inp['hy_freq'][l][0], inp['hy_freq'][l][1]], 1)),
                hw3=np.ascontiguousarray(inp['hy_w3'][l].reshape(64, 2, 2, 512)[:, :, :, half * 256:(half + 1) * 256]),
                negt=C['negt'], adel=_rep(np.ascontiguousarray(C['absdelta'][:, :, half * 256:(half + 1) * 256])),
                Fm=C['Fm'], Gm=C['Gm'], Fc=C['Fc'], Gc=C['Gc'],
            )
            ins.append(d)
    res = run_bass_kernel_spmd(_prog('A', build_A), ins, core_ids=list(range(8)))
    YT = []
    for b in range(4):
        y = np.empty((4, 2, 2, 128, NTOK_A), np.float32)
        for half in range(2):
            o = res.results[b * 2 + half]['YT'].reshape(4, 2, 128, NTOK_A)
            y[:, half] = o
        YT.append(y.reshape(2048, NTOK_A))
    return YT


class _YTView:
    def __init__(self, base, half):
        self.base = base
        self.half = half

    def _m(self, i8):
        return (i8 // 2) * 4 + self.half * 2 + (i8 % 2)

    def __getitem__(self, idx):
        if isinstance(idx, tuple):
            return self.base[(self._m(idx[0]),) + tuple(idx[1:])]
        return self.base[self._m(idx)]


def emit_M2(nc, P, Dm):
    cT, wm, bcol, brow, modc, modr = Dm['cT'], Dm['wm'], Dm['bcol'], Dm['brow'], Dm['modc'], Dm['modr']
    with contextlib.ExitStack() as st:
        P.stack = st
        B = Bufs()
        cs = P.sb("cs", [128, 8, 2], F32)
        sc = P.sb("sc", [128, 8, 2], F32)
        ones = P.sb("ones", [128, 128], F32)
        scb = P.sb("scb", [128, 8, 2, 128], F32)
        wch = [P.sb(f"wch{i}", [128, 8, 1024], F32) for i in range(2)]
        bc = P.sb("bc", [128, 48], F32)
        br = P.sb("br", [128, 2, 1024], F32)
        mc = P.sb("mc", [128, 2, 48], F32)
        mr = P.sb("mr", [128, 2, 2, 1024], F32)
        pm = P.ps("pm", [128, 2, 512], F32)
        pcl = P.ps("pc", [128, 512], F32)
        P.dma('sp', lambda e: e.dma_start(out=cs[:], in_=cT), writes=[B['cs']])
        P.op('act', lambda e: e.activation(out=sc[:], in_=cs[:], func=AF.Silu), reads=[B['cs']], writes=[B['sc']])
        P.op('dve', lambda e: e.memset(ones[:], 1.0), writes=[B['ones']])
        for kt in range(8):
            for ms in range(2):
                P.op('dve', lambda e, kt=kt, ms=ms: e.tensor_scalar(out=scb[:, kt, ms, :], in0=ones[:], scalar1=sc[:, kt, ms:ms + 1], scalar2=None, op0=ALU.mult),
                     reads=[B['ones'], B['sc']], writes=[B['scb']])
        wc = 0
        for l in range(4):
            P.dma('sp', lambda e, l=l: e.dma_start(out=bc[:], in_=bcol[l]), writes=[B['bc']])
            P.dma('sp', lambda e, l=l: e.dma_start(out=br[:], in_=brow[l]), writes=[B['br']])
            for k in range(6):
                ws_ = wc % 2
                wc += 1
                P.dma('sp', lambda e, l=l, k=k, ws_=ws_: e.dma_start(out=wch[ws_][:], in_=wm[l, k]), writes=[B['wch', ws_]])
                for f in range(8):
                    for kt in range(8):
                        P.op('pe', lambda e, f=f, kt=kt, ws_=ws_: e.matmul(pcl[:, f * 2:f * 2 + 2], lhsT=wch[ws_][:, kt, f * 128:(f + 1) * 128], rhs=sc[:, kt, :], start=(kt == 0), stop=(kt == 7)),
                             reads=[B['wch', ws_], B['sc']], writes=[B['pc']])
                for ms in range(2):
                    P.op('dve', lambda e, k=k, ms=ms: e.tensor_tensor(out=mc[:, ms, k * 8:(k + 1) * 8], in0=pcl[:, 0:16].rearrange("p (f m) -> p m f", m=2)[:, ms, :], in1=bc[:, k * 8:(k + 1) * 8], op=ALU.add),
                         reads=[B['pc'], B['bc']], writes=[B['mc']])
                if k in (2, 5):
                    j = 0 if k == 2 else 1
                    for ms in range(2):
                        for nb in range(2):
                            for kt in range(8):
                                P.op('pe', lambda e, ms=ms, nb=nb, kt=kt, ws_=ws_: e.matmul(pm[:, nb, :], lhsT=scb[:, kt, ms, :], rhs=wch[ws_][:, kt, nb * 512:(nb + 1) * 512], start=(kt == 0), stop=(kt == 7)),
                                     reads=[B['scb'], B['wch', ws_]], writes=[B['pm', nb]])
                            P.op('dve', lambda e, ms=ms, nb=nb, j=j: e.tensor_tensor(out=mr[:, ms, j, nb * 512:(nb + 1) * 512], in0=pm[:, nb, :], in1=br[:, j, nb * 512:(nb + 1) * 512], op=ALU.add),
                                 reads=[B['pm', nb], B['br']], writes=[B['mr']])
            P.dma('sp', lambda e, l=l: e.dma_start(out=modc[l], in_=mc[:]), reads=[B['mc']], writes=[B['modc', l]])
            P.dma('sp', lambda e, l=l: e.dma_start(out=modr[l], in_=mr[:]), reads=[B['mr']], writes=[B['modr', l]])
        P.flush()


def emit_cast(nc, P, pairs):
    with contextlib.ExitStack() as st:
        P.stack = st
        B = Bufs()
        cb = [P.sb(f"cb{i}", [128, 4096], BF16) for i in range(3)]
        for i, (src, dst) in enumerate(pairs):
            c = cb[i % 3]
            n = src.shape[-1]
            P.dma('pool', lambda e, c=c, src=src, n=n: e.dma_start(out=c[:, 0:n], in_=src), writes=[B['cb', i % 3]])
            P.dma('sp', lambda e, c=c, dst=dst, n=n: e.dma_start(out=dst, in_=c[:, 0:n]), reads=[B['cb', i % 3]], writes=[B['dst', i]])
        P.flush()


def build_F():
    nc = bass.Bass("TRN2", target_bir_lowering=False)
    I = lambda name, shape, dt=F32: _din(nc, name, shape, dt)
    T = lambda name, shape, dt=F32: nc.dram_tensor(name, list(shape), dt, kind="Internal").ap()
    x0 = I("x0", [NT_A, 128, D])
    cT = I("cT", [128, 8, 2])
    wm = I("wm", [4, 6, 128, 8, 1024])
    bcol = I("bcol", [4, 128, 48])
    brow = I("brow", [4, 128, 2, 1024])
    ident = I("ident", [128, 128])
    wA = I("wA", [4, 2, 128, 8, 768])
    wR = I("wR", [4, 2, 128, 8, 1024])
    wH = I("wH", [4, 2, 128, 8, 768])
    wD = I("wD", [4, 2, 128, 8, 512])
    caw = I("caw", [4, 2, 128, 2, 4])
    hcw = I("hcw", [4, 2, 128, 6, 4])
    hsk = I("hsk", [4, 2, 128, 2, 2])
    ropec = I("ropec", [128, 32, 64])
    ropes = I("ropes", [128, 32, 64])
    qkn = I("qkn", [4, 128, 2, 128])
    rdec = I("rdec", [4, 2, 128, 4])
    rtab = I("rtab", [128, 6, 128])
    rcol = I("rcol", [128, 4])
    featsT = I("featsT", [33, 4096])
    featsTc = I("featsTc", [33, 256])
    hw1 = I("hw1", [4, 33, 64])
    hw2 = I("hw2", [4, 64, 64])
    hcols = I("hcols", [4, 64, 4])
    hw3 = I("hw3", [4, 2, 64, 2, 2, 256])
    negt = I("negt", [128, 34])
    adel = I("adel", [2, 128, 2, 2, 256])
    Fm = I("Fm", [32, 128, 8, 1024], BF16)
    Gm = I("Gm", [4, 64, 128, 1024], BF16)
    Fc = I("Fc", [128, 2, 512], BF16)
    Gc = I("Gc", [128, 4, 256], BF16)
    wg = I("wg", [4, 8, 128, 4, 8, 128])
    wb = I("wb", [4, 8, 128, 4, 4, 128])
    wo = I("wo", [4, 128, 8, D])
    lnrows = I("lnrows", [4, 128, 4, D])
    bgc = I("bgc", [4, 128, 4, 8])
    w1d = I("w1d", [2, 11, 128, 8, 256])
    w3d = I("w3d", [2, 11, 128, 8, 256])
    w2d = I("w2d", [2, 11, 128, 2, D])
    w1m = I("w1m", [2, 56, 128, 8, 512])
    w3m = I("w3m", [2, 56, 128, 8, 512])
    w2m = I("w2m", [2, 56, 128, 4, D])
    rt = I("rt", [2, 128, 8, 8])
    sel = I("sel", [8, 8, 128])
    out = _dout(nc, "out", [NT_A, 128, D])
    xb = [T("xbuf0", [NT_A, 128, D]), T("xbuf1", [NT_A, 128, D])]
    x1s = T("x1s", [NT_A, 128, D])
    YTs = T("YTs", [16, 128, NTOK_A])
    modc = T("modc", [4, 128, 2, 48])
    modr = T("modr", [4, 128, 2, 2, D])
    uTs = T("uTs", [128, 8, NTOK_A], BF16)
    wgb = T("wgb", [8, 128, 4, 8, 128], BF16)
    wbb = T("wbb", [8, 128, 4, 4, 128], BF16)
    wob = T("wob", [128, 8, D], BF16)
    with contextlib.ExitStack() as st0:
        P = Prog(nc, st0)
        emit_M2(nc, P, dict(cT=cT, wm=wm, bcol=bcol, brow=brow, modc=modc, modr=modr))
        import os as _os
        NL = int(_os.environ.get('F_LAYERS', '4'))
        for l in range(NL):
            xin = x0 if l == 0 else xb[l % 2]
            xout = out if l == NL - 1 else xb[(l + 1) % 2]
            mcA = modc[l][:, :, 0:16].rearrange("p m (k f) -> p m k f", k=2)
            mcB = (mcA, modc[l][:, :, 24:40].rearrange("p m (k f) -> p m k f", k=2))
            ustate = {'have': False}
            for half in range(2):
                emit_A(nc, P, dict(uTs=uTs, ustate=ustate, x=xin, modcols=mcA, ident=ident, wA=wA[l, half], wR=wR[l, half], wH=wH[l, half], wD=wD[l, half],
                                   caw=caw[l, half], hcw=hcw[l, half], hsk=hsk[l, half], ropec=ropec, ropes=ropes, qkn=qkn[l], rdec=rdec[l, half],
                                   rtab=rtab, rcol=rcol, featsT=featsT, featsTc=featsTc, hw1=hw1[l], hw2=hw2[l], hcols=hcols[l], hw3=hw3[l, half],
                                   negt=negt, adel=adel[half], Fm=Fm, Gm=Gm, Fc=Fc, Gc=Gc, YT=_YTView(YTs, half)))
            moe = (l % 2 == 1)
            i = l // 2
            pairs = []
            for fo in range(8):
                pairs.append((wg[l, fo].rearrange("p a b c -> p (a b c)"), wgb[fo].rearrange("p a b c -> p (a b c)")))
                pairs.append((wb[l, fo].rearrange("p a b c -> p (a b c)"), wbb[fo].rearrange("p a b c -> p (a b c)")))
            for kh in range(2):
                pairs.append((wo[l][:, kh * 4:(kh + 1) * 4, :].rearrange("p a b -> p (a b)"), wob[:, kh * 4:(kh + 1) * 4, :].rearrange("p a b -> p (a b)")))
            emit_cast(nc, P, pairs)
            for hb in range(2):
                Dm = dict(x=xin, yT=YTs, modcols=mcB, modrows=modr[l], lnrows=lnrows[l], bgc=bgc[l], ident=ident, wg=wgb, wb=wbb, wo=wob, wbf16=True,
                          x1o=x1s, xo=xout)
                if moe:
                    Dm.update(w1=w1m[i], w3=w3m[i], w2=w2m[i], rt=rt[i], sel=sel)
                else:
                    Dm.update(w1=w1d[i], w3=w3d[i], w2=w2d[i])
                emit_B(nc, P, Dm, moe, gt=(lambda t, hb=hb: hb * 16 + t if t < 16 else 32 + hb))
    return nc


def _pack_F(inp):
    C = _consts()
    sh = dict(ident=_IDENT, ropec=C['ropec'], ropes=C['ropes'], rtab=C['rtab'], rcol=C['rcol'], featsT=C['featsT'], featsTc=C['featsTc'],
              negt=C['negt'], Fm=C['Fm'], Gm=C['Gm'], Fc=C['Fc'], Gc=C['Gc'])
    sh['adel'] = np.stack([_rep(np.ascontiguousarray(C['absdelta'][:, :, h * 256:(h + 1) * 256])) for h in range(2)], 0)
    w_mod = inp['w_mod']
    sh['wm'] = np.ascontiguousarray(w_mod.reshape(4, 8, 128, 6, 1024).transpose(0, 3, 2, 1, 4))
    sh['bcol'] = np.ascontiguousarray(inp['b_mod'].reshape(4, 48, 128).transpose(0, 2, 1))
    sh['brow'] = np.stack([_rep(np.stack([inp['b_mod'][l, 2048:3072], inp['b_mod'][l, 5120:6144]], 0)) for l in range(4)], 0)
    wA, wR, wH, wD, caw, hcw, hsk, rdec, hw3 = [], [], [], [], [], [], [], [], []
    for l in range(4):
        w_in = inp['w_in'][l]
        rows = [[] for _ in range(9)]
        for half in range(2):
            r256 = half * 256 + np.arange(256)
            colsA = np.concatenate([g * 512 + r256 for g in range(3)])
            colsR = np.concatenate([1536 + g * 512 + r256 for g in range(4)])
            colsH = np.concatenate([3584 + g * 512 + r256 for g in range(3)])
            colsD = np.concatenate([5120 + r256, 5120 + 512 + half * 128 + np.arange(128), 5120 + 768 + half * 128 + np.arange(128)])
            ch = r256
            cawv = np.stack([inp['conv_a_w'][l][0, ch], inp['conv_a_w'][l][1, ch], inp['conv_a_w'][l][2, ch], inp['conv_a_b'][l][ch]], -1)
            hch = np.concatenate([g * 512 + ch for g in range(3)])
            hcwv = np.stack([inp['hy_conv_w'][l][0, hch], inp['hy_conv_w'][l][1, hch], inp['hy_conv_w'][l][2, hch], inp['hy_conv_b'][l][hch]], -1)
            vals = [_tile_rows(w_in[:, colsA]), _tile_rows(w_in[:, colsR]), _tile_rows(w_in[:, colsH]), _tile_rows(w_in[:, colsD]),
                    cawv.reshape(2, 128, 4).transpose(1, 0, 2), hcwv.reshape(6, 128, 4).transpose(1, 0, 2),
                    inp['hy_skip'][l][:, ch].reshape(2, 2, 128).transpose(2, 0, 1),
                    _rep(inp['ret_decay'][l][:, half * 2:half * 2 + 2].reshape(4)),
                    inp['hy_w3'][l].reshape(64, 2, 2, 512)[:, :, :, half * 256:(half + 1) * 256]]
            for r_, v_ in zip(rows, vals):
                r_.append(v_)
        for lst, r_ in zip((wA, wR, wH, wD, caw, hcw, hsk, rdec, hw3), rows):
            lst.append(np.stack(r_, 0))
    for n_, lst in zip(('wA', 'wR', 'wH', 'wD', 'caw', 'hcw', 'hsk', 'rdec', 'hw3'), (wA, wR, wH, wD, caw, hcw, hsk, rdec, hw3)):
        sh[n_] = np.ascontiguousarray(np.stack(lst, 0))
    sh['qkn'] = np.stack([_rep(np.stack([inp['q_norm'][l], inp['k_norm'][l]], 0)) for l in range(4)], 0)
    sh['hw1'] = np.ascontiguousarray(inp['hy_w1'])
    sh['hw2'] = np.ascontiguousarray(inp['hy_w2'])
    sh['hcols'] = np.ascontiguousarray(np.stack([inp['hy_b1'], inp['hy_b2'], inp['hy_freq'][:, 0], inp['hy_freq'][:, 1]], -1))
    pw = [prep_B_weights(l, inp) for l in range(4)]
    for n_ in ('wg', 'wb', 'wo', 'lnrows', 'bgc'):
        sh[n_] = np.stack([pw[l][n_] for l in range(4)], 0)
    for n_ in ('w1', 'w3', 'w2'):
        sh[n_ + 'd'] = np.stack([pw[0][n_], pw[2][n_]], 0)
        sh[n_ + 'm'] = np.stack([pw[1][n_], pw[3][n_]], 0)
    sh['rt'] = np.stack([pw[1]['rt'], pw[3]['rt']], 0)
    sh['sel'] = pw[1]['sel']
    per = []
    for b in range(4):
        cc = np.stack([inp['c'][b], inp['c_ctx']], 0)
        per.append(dict(x0=np.ascontiguousarray(np.concatenate([inp['x'][b], inp['ctx'][b]], 0).reshape(NT_A, 128, 1024)),
                        cT=np.ascontiguousarray(cc.T.reshape(8, 128, 2).transpose(1, 0, 2))))
    return sh, per


def kernel_fused(**inp):
    inp = {k_: np.asarray(v) for k_, v in inp.items()}
    sh, per = _pack_F(inp)
    ins = [dict(sh, **per[b]) for b in range(4)]
    res = run_bass_kernel_spmd(_prog('F', build_F), ins, core_ids=list(range(4)))
    out = np.stack([res.results[b]['out'].reshape(NTOK_A, 1024)[:4096] for b in range(4)], 0)
    return np.ascontiguousarray(out, dtype=np.float32)


def kernel_unfused(**inp):
    inp = {k_: np.asarray(v) for k_, v in inp.items()}
    mod = run_M(inp['c'], inp['c_ctx'], inp['w_mod'], inp['b_mod'])
    x_cur = np.ascontiguousarray(inp['x'], dtype=np.float32)
    h_cur = np.ascontiguousarray(inp['ctx'], dtype=np.float32)
    for l in range(4):
        YT = run_A(l, x_cur, h_cur, mod[:, l], inp)
        x_cur, h_cur, _ = run_B(l, x_cur, h_cur, YT, mod[:, l], inp)
    return x_cur


def kernel(**inp):
    return kernel_fused(**inp)
```

```python
import contextlib
import numpy as np
import concourse.bass as bass
import concourse.mybir as mybir

F32 = mybir.dt.float32
BF16 = mybir.dt.bfloat16
AF = mybir.ActivationFunctionType
ALU = mybir.AluOpType
AX = mybir.AxisListType

NDMA = 24
ENGS = ['pe', 'act', 'dve', 'pool', 'sp']


class Buf:
    __slots__ = ('w', 'r', 'name', 'excl')

    def __init__(self, name=''):
        self.w = None
        self.r = {}
        self.name = name
        self.excl = False


PSUM_KEYS = {'pt', 'ptb', 'pa', 'pb', 'pc', 'pg', 'pp', 'pmx', 'pg1', 'pg3', 'pf', 'pw', 'pm'}


class Bufs:
    def __init__(self, name=''):
        self.d = {}
        self.name = name

    def __getitem__(self, k):
        if k == 'ptb2':
            k = 'ptb'
        b = self.d.get(k)
        if b is None:
            b = Buf(f"{self.name}{k}")
            k0 = k[0] if isinstance(k, tuple) else k
            b.excl = k0 in PSUM_KEYS
            self.d[k] = b
        return b


class Prog:
    def __init__(self, nc, stack, same_engine_sync=True):
        self.nc = nc
        self.stack = stack
        self.q = {e: [] for e in ENGS}
        self.sem = {e: stack.enter_context(nc.semaphore(f"s_{e}")) for e in ENGS}
        self.cnt = {e: 0 for e in ENGS}
        self.seen = {e: {} for e in ENGS}
        self.dsem = [stack.enter_context(nc.semaphore(f"dq{i}")) for i in range(NDMA)]
        self.dval = [0] * NDMA
        self.dnext = 0
        self.ses = same_engine_sync
        self.out_tokens = []

    def sb(self, name, shape, dt):
        self._uid = getattr(self, '_uid', 0) + 1
        return self.stack.enter_context(self.nc.sbuf_tensor(f"{name}_u{self._uid}", list(shape), dt))

    def ps(self, name, shape, dt=F32):
        self._uid = getattr(self, '_uid', 0) + 1
        return self.stack.enter_context(self.nc.psum_tensor(f"{name}_u{self._uid}", list(shape), dt))

    def _semh(self, k):
        return self.sem[k] if isinstance(k, str) else self.dsem[k[1]]

    def _deps(self, e, reads, writes):
        need = {}
        xr = [b for b in reads if b.excl]
        if xr:
            writes = list(writes) + xr

        def add(tok):
            if tok is None:
                return
            k, v = tok
            if need.get(k, 0) < v:
                need[k] = v

        for b in reads:
            add(b.w)
        for b in writes:
            add(b.w)
            for k, v in b.r.items():
                add((k, v))
        waits = []
        for k, v in need.items():
            if k == e and (e == 'pe' or not self.ses):
                continue
            if self.seen[e].get(k, 0) >= v:
                continue
            self.seen[e][k] = v
            waits.append((k, v))
        return waits

    def _mark(self, tok, reads, writes):
        k, v = tok
        xr = [b for b in reads if b.excl]
        if xr:
            writes = list(writes) + xr
        for b in reads:
            if b.r.get(k, 0) < v:
                b.r[k] = v
        for b in writes:
            b.w = tok
            b.r = {}

    mute = False

    def op(self, e, fn, reads=(), writes=()):
        if self.mute:
            return
        waits = self._deps(e, reads, writes)
        self.cnt[e] += 1
        tok = (e, self.cnt[e])
        self._mark(tok, reads, writes)
        self.q[e].append((waits, fn, (self.sem[e], 1)))

    def dma(self, e, fn, reads=(), writes=(), is_output=False):
        if self.mute:
            return
        if e == 'pool':
            i = self._dpool = (getattr(self, '_dpool', -1) + 1) % 8
        else:
            i = 8 + self.dnext
            self.dnext = (self.dnext + 1) % (NDMA - 8)
        waits = self._deps(e, reads, writes)
        k = ('d', i)
        if self.dval[i] > 0 and self.seen[e].get(k, 0) < self.dval[i]:
            waits.append((k, self.dval[i]))
            self.seen[e][k] = self.dval[i]
        self.dval[i] += 16
        tok = (k, self.dval[i])
        self._mark(tok, reads, writes)
        self.q[e].append((waits, fn, (self.dsem[i], 16)))
        if is_output:
            self.out_tokens.append(tok)

    def flush(self):
        nc = self.nc
        q = self.q
        semh = self._semh
        ex = []
        for e in ['pe', 'act', 'dve', 'pool']:
            if self.cnt[e] > 0:
                ex.append((e, self.cnt[e]))
        for i in range(NDMA):
            if self.dval[i] > 0:
                ex.append((('d', i), self.dval[i]))

        def emit(eng, ename):
            for waits, fn, (sem, n) in q[ename]:
                for (k, v) in waits:
                    eng.wait_ge(semh(k), v)
                fn(eng).then_inc(sem, n)
            for (k, v) in ex:
                if self.seen[ename].get(k, 0) < v:
                    eng.wait_ge(semh(k), v)
                    self.seen[ename][k] = v

        with nc.Block() as block:
            @block.tensor
            def _(t):
                emit(t, 'pe')

            @block.scalar
            def _(t):
                emit(t, 'act')

            @block.vector
            def _(t):
                emit(t, 'dve')

            @block.gpsimd
            def _(t):
                emit(t, 'pool')

            @block.sync
            def _(t):
                emit(t, 'sp')
        self.q = {e: [] for e in ENGS}

from concourse.bass_utils import run_bass_kernel_spmd

D = 1024
ALPHA = (2 * 4) ** 0.25
NTILE = 17
NTOK = NTILE * 128
BLOCKS = [(0, 4), (4, 4), (8, 4), (12, 4), (16, 1)]


def _din(nc, name, shape, dt=F32):
    return nc.dram_tensor(name, list(shape), dt, kind="ExternalInput").ap()


def _dout(nc, name, shape, dt=F32):
    return nc.dram_tensor(name, list(shape), dt, kind="ExternalOutput").ap()


def build_M():
    nc = bass.Bass("TRN2", target_bir_lowering=False)
    cT = _din(nc, "cT", [128, 8, 5])
    w = _din(nc, "w", [128, 8, 3072])
    b = _din(nc, "b", [5, 3072])
    o = _dout(nc, "o", [5, 3072])
    with contextlib.ExitStack() as st:
        P = Prog(nc, st)
        B = Bufs()
        cs = P.sb("cs", [128, 8, 5], F32)
        sc = P.sb("sc", [128, 8, 5], F32)
        ws = P.sb("ws", [128, 8, 3072], F32)
        bs = P.sb("bs", [5, 3072], F32)
        os_ = P.sb("os", [5, 3072], F32)
        pm = P.ps("pm", [128, 2, 512], F32)
        P.dma('sp', lambda e: e.dma_start(out=cs[:], in_=cT), writes=[B['cs']])
        P.dma('sp', lambda e: e.dma_start(out=bs[:], in_=b), writes=[B['bs']])
        for kt in range(8):
            P.dma('sp', lambda e, kt=kt: e.dma_start(out=ws[:, kt, :], in_=w[:, kt, :]), writes=[B['ws', kt]])
        P.op('act', lambda e: e.activation(out=sc[:], in_=cs[:], func=AF.Silu), reads=[B['cs']], writes=[B['sc']])
        for nb in range(6):
            pb = B['pm', nb % 2]
            for kt in range(8):
                P.op('pe', lambda e, nb=nb, kt=kt: e.matmul(pm[0:5, nb % 2, :], lhsT=sc[:, kt, :], rhs=ws[:, kt, nb * 512:(nb + 1) * 512],
                                                          start=(kt == 0), stop=(kt == 7)),
                     reads=[B['sc'], B['ws', kt]], writes=[pb])
            P.op('dve', lambda e, nb=nb: e.tensor_tensor(out=os_[:, nb * 512:(nb + 1) * 512], in0=pm[0:5, nb % 2, :],
                                                        in1=bs[:, nb * 512:(nb + 1) * 512], op=ALU.add),
                 reads=[pb, B['bs']], writes=[B['os', nb]])
        P.dma('sp', lambda e: e.dma_start(out=o, in_=os_[:]), reads=[B['os', nb] for nb in range(6)], is_output=True)
        P.flush()
    return nc


def emit_B(nc, P, Dm, moe, gt=None):
    if gt is None:
        gt = lambda t: t
    KC = 4 if moe else 2
    NCH = 56 if moe else 11
    CPE = 7 if moe else 11
    x = Dm['x']
    yT = Dm['yT']
    modcols = Dm['modcols']
    modrows = Dm['modrows']
    lnrows = Dm['lnrows']
    bgc = Dm['bgc']
    ident = Dm['ident']
    wg = Dm['wg']
    wb = Dm['wb']
    wo = Dm['wo']
    w1 = Dm['w1']
    w3 = Dm['w3']
    w2 = Dm['w2']
    x1o = Dm['x1o']
    xo = Dm['xo']
    if moe:
        rt = Dm['rt']
        sel = Dm['sel']
    with contextlib.ExitStack() as st:
        P.stack = st
        B = Bufs()
        ids = P.sb("ids", [128, 128], F32)
        mcol = P.sb("mcol", [128, 2, 4, 8], F32)
        bgs = P.sb("bgs", [128, 4, 8], F32)
        u2T = P.sb("u2T", [128, 8, NTOK], BF16)
        P.dma('sp', lambda e: e.dma_start(out=ids[:], in_=ident), writes=[B['ids']])
        if isinstance(modcols, tuple):
            P.dma('sp', lambda e: e.dma_start(out=mcol[:, :, 0:2, :], in_=modcols[0]), writes=[B['mcol']])
            P.dma('sp', lambda e: e.dma_start(out=mcol[:, :, 2:4, :], in_=modcols[1]), writes=[B['mcol']])
        else:
            P.dma('sp', lambda e: e.dma_start(out=mcol[:], in_=modcols), writes=[B['mcol']])
        P.dma('sp', lambda e: e.dma_start(out=bgs[:], in_=bgc), writes=[B['bgs']])
        P.op('dve', lambda e: e.tensor_scalar_add(out=mcol[:, :, 1, :], in0=mcol[:, :, 1, :], scalar1=1.0), reads=[B['mcol']], writes=[B['mcol']])
        P.op('dve', lambda e: e.tensor_scalar_add(out=mcol[:, :, 3, :], in0=mcol[:, :, 3, :], scalar1=1.0), reads=[B['mcol']], writes=[B['mcol']])
        if moe:
            rts = P.sb("rts", [128, 8, 8], F32)
            sels = P.sb("sels", [8, 8, 128], F32)
            wall = P.sb("wall", [128, NTILE, 8], F32)
            WT = P.sb("WT", [8, NTOK], F32)
            P.dma('sp', lambda e: e.dma_start(out=rts[:], in_=rt), writes=[B['rts']])
            P.dma('sp', lambda e: e.dma_start(out=sels[:], in_=sel), writes=[B['sels']])

        def ln_tile(r, stt, mv, rstd, gi, key):
            rb = B[key]
            for hf in range(2):
                P.op('dve', lambda e, hf=hf: e.bn_stats(out=stt[:, hf, :], in_=r[:, hf * 512:(hf + 1) * 512]), reads=[rb], writes=[B[key, 'st', hf]])
            P.op('dve', lambda e: e.bn_aggr(out=mv[:], in_=stt[:].rearrange("p a b -> p (a b)")), reads=[B[key, 'st', 0], B[key, 'st', 1]], writes=[B[key, 'mv']])
            P.op('dve', lambda e: e.tensor_scalar_add(out=rstd[:], in0=mv[:, 1:2], scalar1=1e-6), reads=[B[key, 'mv']], writes=[B[key, 'rstd']])
            P.op('act', lambda e: e.sqrt(out=rstd[:], in_=rstd[:]), reads=[B[key, 'rstd']], writes=[B[key, 'rstd']])
            P.op('dve', lambda e: e.reciprocal(out=rstd[:], in_=rstd[:]), reads=[B[key, 'rstd']], writes=[B[key, 'rstd']])
            P.op('dve', lambda e: e.tensor_scalar(out=r[:], in0=r[:], scalar1=mv[:, 0:1], scalar2=rstd[:, 0:1], op0=ALU.subtract, op1=ALU.mult),
                 reads=[rb, B[key, 'mv'], B[key, 'rstd']], writes=[rb])
            P.op('pool', lambda e: e.tensor_tensor(out=r[:], in0=r[:], in1=lnr[:, gi, :], op=ALU.mult), reads=[rb, B['lnr']], writes=[rb])
            P.op('pool', lambda e: e.tensor_tensor(out=r[:], in0=r[:], in1=lnr[:, gi + 1, :], op=ALU.add), reads=[rb, B['lnr']], writes=[rb])

        with contextlib.ExitStack() as s1:
            P.stack = s1
            mrow = P.sb("mrow", [128, 2, 2, D], F32)
            lnr = P.sb("lnr", [128, 4, D], F32)
            P.dma('sp', lambda e: e.dma_start(out=mrow[:], in_=modrows), writes=[B['mrow']])
            P.dma('sp', lambda e: e.dma_start(out=lnr[:], in_=lnrows), writes=[B['lnr']])
            xs = P.sb("xs", [128, 4, D], F32)
            uT = P.sb("uT", [128, 8, 512], BF16)
            yTb = P.sb("yTb", [128, 16, 512], BF16)
            mT = P.sb("mT", [128, 8, 512], BF16)
            wgs = [P.sb(f"wgs{i}", [128, 4, 8, 128], BF16) for i in range(2)]
            wbs = [P.sb(f"wbs{i}", [128, 4, 4, 128], BF16) for i in range(2)]
            wos = P.sb("wos", [128, 8, D], BF16)
            sg = [P.sb(f"sg{i}", [128, 512], F32) for i in range(2)]
            macc = P.sb("macc", [128, 512], F32)
            mtmp = P.sb("mtmp", [128, 512], F32)
            rr = [P.sb(f"rr{i}", [128, D], F32) for i in range(2)]
            t1 = P.sb("t1", [128, D], F32)
            stt = P.sb("stt", [128, 2, 6], F32)
            mv = P.sb("mv", [128, 2], F32)
            rstd = P.sb("rstd", [128, 1], F32)
            u2f = P.sb("u2f", [128, 8, 128], F32)
            pt = P.ps("pt", [128, 2, 512], F32)
            pg = P.ps("pg", [128, 2, 512], F32)
            pp = P.ps("pp", [128, 2, 512], F32)
            pmx = P.ps("pmx", [128, 2, 512], F32)
            if moe:
                lg = P.sb("lg", [128, 8], F32)
                m8 = P.sb("m8", [128, 8], F32)
                msk = P.sb("msk", [128, 8], F32)
                ex = P.sb("ex", [128, 8], F32)
                sm = P.sb("sm", [128, 4], F32)

            P.dma('sp' if Dm.get('wbf16') else 'pool', lambda e: e.dma_start(out=wos[:], in_=wo), writes=[B['wos']])
            wcnt = 0
            tcnt = 0
            for bi, (t0, nt) in enumerate(BLOCKS):
                ms = 1 if bi == 4 else 0
                ntok = nt * 128
                tok0 = gt(t0) * 128
                P.dma('sp', lambda e, t0=t0, nt=nt: e.dma_start(out=xs[:, 0:nt, :], in_=x[gt(t0):gt(t0) + nt].rearrange("t p f -> p t f")),
                      writes=[B['xs', j] for j in range(nt)])
                P.dma('pool', lambda e, tok0=tok0, ntok=ntok: e.dma_start(out=yTb[:, :, 0:ntok], in_=yT[:, :, tok0:tok0 + ntok].rearrange("c p t -> p c t")),
                      writes=[B['yTb']])
                for j in range(nt):
                    for f in range(8):
                        pb = B['pt', tcnt % 2]
                        P.op('pe', lambda e, j=j, f=f, s=tcnt % 2: e.transpose(pt[:, s, 0:128], xs[:, j, f * 128:(f + 1) * 128], ids[:]),
                             reads=[B['xs', j], B['ids']], writes=[pb])
                        P.op('act', lambda e, j=j, f=f, s=tcnt % 2, ms=ms: e.activation(out=uT[:, f, j * 128:(j + 1) * 128], in_=pt[:, s, 0:128], func=AF.Identity,
                                                                                      bias=mcol[:, ms, 0, f:f + 1], scale=mcol[:, ms, 1, f:f + 1]),
                             reads=[pb, B['mcol']], writes=[B['uT', f]])
                        tcnt += 1
                for fo in range(8):
                    ws_ = wcnt % 2
                    wq = 'sp' if Dm.get('wbf16') else 'pool'
                    P.dma(wq, lambda e, fo=fo, ws_=ws_: e.dma_start(out=wgs[ws_][:], in_=wg[fo]), writes=[B['wgs', ws_]])
                    P.dma(wq, lambda e, fo=fo, ws_=ws_: e.dma_start(out=wbs[ws_][:], in_=wb[fo]), writes=[B['wbs', ws_]])
                    wcnt += 1
                    for br in range(4):
                        s = br % 2
                        for kt in range(8):
                            P.op('pe', lambda e, br=br, kt=kt, s=s, ws_=ws_, ntok=ntok: e.matmul(pg[:, s, 0:ntok], lhsT=wgs[ws_][:, br, kt, :], rhs=uT[:, kt, 0:ntok],
                                                                                               start=(kt == 0), stop=(kt == 7)),
                                 reads=[B['wgs', ws_], B['uT', kt]], writes=[B['pg', s]])
                        for kt in range(4):
                            P.op('pe', lambda e, br=br, kt=kt, s=s, ws_=ws_, ntok=ntok: e.matmul(pp[:, s, 0:ntok], lhsT=wbs[ws_][:, br, kt, :], rhs=yTb[:, br * 4 + kt, 0:ntok],
                                                                                               start=(kt == 0), stop=(kt == 3)),
                                 reads=[B['wbs', ws_], B['yTb']], writes=[B['pp', s]])
                        P.op('act', lambda e, br=br, fo=fo, s=s, ntok=ntok: e.activation(out=sg[s][:, 0:ntok], in_=pg[:, s, 0:ntok], func=AF.Sigmoid,
                                                                                       bias=bgs[:, br, fo:fo + 1], scale=1.0),
                             reads=[B['pg', s], B['bgs']], writes=[B['sg', s]])
                        if br == 0:
                            P.op('dve', lambda e, s=s, ntok=ntok: e.tensor_tensor(out=macc[:, 0:ntok], in0=sg[s][:, 0:ntok], in1=pp[:, s, 0:ntok], op=ALU.mult),
                                 reads=[B['sg', s], B['pp', s]], writes=[B['macc']])
                        else:
                            P.op('dve', lambda e, s=s, ntok=ntok: e.tensor_tensor(out=mtmp[:, 0:ntok], in0=sg[s][:, 0:ntok], in1=pp[:, s, 0:ntok], op=ALU.mult),
                                 reads=[B['sg', s], B['pp', s]], writes=[B['mtmp']])
                            if br < 3:
                                P.op('pool', lambda e, ntok=ntok: e.tensor_tensor(out=macc[:, 0:ntok], in0=macc[:, 0:ntok], in1=mtmp[:, 0:ntok], op=ALU.add),
                                     reads=[B['macc'], B['mtmp']], writes=[B['macc']])
                            else:
                                P.op('pool', lambda e, fo=fo, ntok=ntok: e.tensor_tensor(out=mT[:, fo, 0:ntok], in0=macc[:, 0:ntok], in1=mtmp[:, 0:ntok], op=ALU.add),
                                     reads=[B['macc'], B['mtmp']], writes=[B['mT', fo]])
                for j in range(nt):
                    tile = t0 + j
                    r = rr[tile % 2]
                    rk = ('rr', tile % 2)
                    for hf in range(2):
                        for kt in range(8):
                            P.op('pe', lambda e, j=j, hf=hf, kt=kt: e.matmul(pmx[:, hf, :], lhsT=mT[:, kt, j * 128:(j + 1) * 128], rhs=wos[:, kt, hf * 512:(hf + 1) * 512],
                                                                           start=(kt == 0), stop=(kt == 7)),
                                 reads=[B['mT', kt], B['wos']], writes=[B['pmx', hf]])
                        P.op('dve', lambda e, hf=hf, ms=ms: e.tensor_tensor(out=t1[:, hf * 512:(hf + 1) * 512], in0=pmx[:, hf, :], in1=mrow[:, ms, 0, hf * 512:(hf + 1) * 512], op=ALU.mult),
                             reads=[B['pmx', hf], B['mrow']], writes=[B['t1', hf]])
                    P.op('dve', lambda e, j=j, r=r: e.scalar_tensor_tensor(out=r[:], in0=xs[:, j, :], scalar=ALPHA, in1=t1[:], op0=ALU.mult, op1=ALU.add),
                         reads=[B['xs', j], B['t1', 0], B['t1', 1]], writes=[B[rk]])
                    ln_tile(r, stt, mv, rstd, 0, rk)
                    P.dma('sp', lambda e, tile=tile, r=r: e.dma_start(out=x1o[gt(tile)], in_=r[:]), reads=[B[rk]], writes=[B['x1o', tile]])
                    for f in range(8):
                        pb = B['pt', tcnt % 2]
                        P.op('pe', lambda e, r=r, f=f, s=tcnt % 2: e.transpose(pt[:, s, 0:128], r[:, f * 128:(f + 1) * 128], ids[:]),
                             reads=[B[rk], B['ids']], writes=[pb])
                        if moe:
                            P.op('act', lambda e, f=f, s=tcnt % 2, ms=ms: e.activation(out=u2f[:, f, :], in_=pt[:, s, 0:128], func=AF.Identity,
                                                                                     bias=mcol[:, ms, 2, f:f + 1], scale=mcol[:, ms, 3, f:f + 1]),
                                 reads=[pb, B['mcol']], writes=[B['u2f', f]])
                            P.op('dve', lambda e, f=f, tile=tile: e.tensor_copy(out=u2T[:, f, tile * 128:(tile + 1) * 128], in_=u2f[:, f, :]),
                                 reads=[B['u2f', f]], writes=[B['u2T', f, tile]])
                        else:
                            P.op('act', lambda e, f=f, s=tcnt % 2, ms=ms, tile=tile: e.activation(out=u2T[:, f, tile * 128:(tile + 1) * 128], in_=pt[:, s, 0:128], func=AF.Identity,
                                                                                                bias=mcol[:, ms, 2, f:f + 1], scale=mcol[:, ms, 3, f:f + 1]),
                                 reads=[pb, B['mcol']], writes=[B['u2T', f, tile]])
                        tcnt += 1
                    if moe:
                        for kt in range(8):
                            P.op('pe', lambda e, kt=kt: e.matmul(pg[:, 0, 0:8], lhsT=u2f[:, kt, :], rhs=rts[:, kt, :], start=(kt == 0), stop=(kt == 7)),
                                 reads=[B['u2f', kt], B['rts']], writes=[B['pg', 0]])
                        P.op('dve', lambda e: e.tensor_copy(out=lg[:], in_=pg[:, 0, 0:8]), reads=[B['pg', 0]], writes=[B['lg']])
                        P.op('dve', lambda e: e.max(out=m8[:], in_=lg[:]), reads=[B['lg']], writes=[B['m8']])
                        P.op('dve', lambda e: e.tensor_scalar(out=msk[:], in0=lg[:], scalar1=m8[:, 1:2], scalar2=None, op0=ALU.is_ge), reads=[B['lg'], B['m8']], writes=[B['msk']])
                        P.op('dve', lambda e: e.tensor_scalar(out=sm[:, 0:1], in0=m8[:, 0:1], scalar1=-1.0, scalar2=None, op0=ALU.mult), reads=[B['m8']], writes=[B['sm', 0]])
                        P.op('dve', lambda e: e.tensor_tensor(out=sm[:, 1:2], in0=m8[:, 1:2], in1=m8[:, 0:1], op=ALU.subtract), reads=[B['m8']], writes=[B['sm', 1]])
                        P.op('act', lambda e: e.activation(out=ex[:], in_=lg[:], func=AF.Exp, bias=sm[:, 0:1], scale=1.0), reads=[B['lg'], B['sm', 0]], writes=[B['ex']])
                        P.op('act', lambda e: e.activation(out=sm[:, 2:3], in_=sm[:, 1:2], func=AF.Exp), reads=[B['sm', 1]], writes=[B['sm', 2]])
                        P.op('dve', lambda e: e.tensor_scalar_add(out=sm[:, 2:3], in0=sm[:, 2:3], scalar1=1.0), reads=[B['sm', 2]], writes=[B['sm', 2]])
                        P.op('dve', lambda e: e.reciprocal(out=sm[:, 3:4], in_=sm[:, 2:3]), reads=[B['sm', 2]], writes=[B['sm', 3]])
                        P.op('dve', lambda e: e.tensor_tensor(out=ex[:], in0=ex[:], in1=msk[:], op=ALU.mult), reads=[B['ex'], B['msk']], writes=[B['ex']])
                        P.op('dve', lambda e, tile=tile: e.tensor_scalar(out=wall[:, tile, :], in0=ex[:], scalar1=sm[:, 3:4], scalar2=None, op0=ALU.mult),
                             reads=[B['ex'], B['sm', 3]], writes=[B['wall', tile]])
                        P.op('pe', lambda e, tile=tile: e.transpose(pp[0:8, 0, 0:128], wall[:, tile, :], ids[:]), reads=[B['wall', tile], B['ids']], writes=[B['pp', 0]])
                        P.op('act', lambda e, tile=tile: e.copy(out=WT[:, tile * 128:(tile + 1) * 128], in_=pp[0:8, 0, 0:128]), reads=[B['pp', 0]], writes=[B['WT']])
            P.flush()
        s2o = st.enter_context(contextlib.ExitStack())
        P.stack = s2o
        facc = P.sb("facc", [128, NTILE, D], F32)
        pg1 = P.ps("pg1", [128, 2, 512], F32)
        pg3 = P.ps("pg3", [128, 2, 512], F32)
        pf = P.ps("pf", [128, 2, 512], F32)
        pw = P.ps("pw", [128, 512], F32)
        with contextlib.ExitStack() as s2:
            P.stack = s2
            w1s = [P.sb(f"w1s{i}", [128, 8, KC * 128], BF16) for i in range(2)]
            w3s = [P.sb(f"w3s{i}", [128, 8, KC * 128], BF16) for i in range(2)]
            w2s = [P.sb(f"w2s{i}", [128, KC, D], BF16) for i in range(2)]
            s1b = [P.sb(f"s1b{i}", [128, 512], F32) for i in range(2)]
            hT = [P.sb(f"hT{i}", [128, KC, 512], BF16) for i in range(2)]
            gcnt = 0
            fcnt = 0
            hcnt = 0
            for ci in range(NCH):
                wsl = ci % 2
                ex_ = ci // CPE
                P.dma('pool', lambda e, ci=ci, wsl=wsl: e.dma_start(out=w1s[wsl][:], in_=w1[ci]), writes=[B['w1s', wsl]])
                P.dma('pool', lambda e, ci=ci, wsl=wsl: e.dma_start(out=w3s[wsl][:], in_=w3[ci]), writes=[B['w3s', wsl]])
                P.dma('pool', lambda e, ci=ci, wsl=wsl: e.dma_start(out=w2s[wsl][:], in_=w2[ci]), writes=[B['w2s', wsl]])
                for bi, (t0, nt) in enumerate(BLOCKS):
                    ntok = nt * 128
                    tok0 = t0 * 128
                    hs = hcnt % 2
                    hcnt += 1
                    if moe:
                        P.op('pe', lambda e, ex_=ex_, tok0=tok0, ntok=ntok: e.matmul(pw[:, 0:ntok], lhsT=sels[:, ex_, :], rhs=WT[:, tok0:tok0 + ntok], start=True, stop=True),
                             reads=[B['sels'], B['WT']], writes=[B['pw']])
                    for kk in range(KC):
                        g = gcnt % 2
                        gcnt += 1
                        for kt in range(8):
                            P.op('pe', lambda e, kk=kk, kt=kt, g=g, wsl=wsl, tok0=tok0, ntok=ntok: e.matmul(pg1[:, g, 0:ntok], lhsT=w1s[wsl][:, kt, kk * 128:(kk + 1) * 128],
                                                                                                       rhs=u2T[:, kt, tok0:tok0 + ntok], start=(kt == 0), stop=(kt == 7)),
                                 reads=[B['w1s', wsl], B['u2T']], writes=[B['pg1', g]])
                        for kt in range(8):
                            P.op('pe', lambda e, kk=kk, kt=kt, g=g, wsl=wsl, tok0=tok0, ntok=ntok: e.matmul(pg3[:, g, 0:ntok], lhsT=w3s[wsl][:, kt, kk * 128:(kk + 1) * 128],
                                                                                                       rhs=u2T[:, kt, tok0:tok0 + ntok], start=(kt == 0), stop=(kt == 7)),
                                 reads=[B['w3s', wsl], B['u2T']], writes=[B['pg3', g]])
                        P.op('act', lambda e, g=g, ntok=ntok: e.activation(out=s1b[g][:, 0:ntok], in_=pg1[:, g, 0:ntok], func=AF.Silu), reads=[B['pg1', g]], writes=[B['s1b', g]])
                        if moe:
                            P.op('dve', lambda e, g=g, ntok=ntok: e.tensor_tensor(out=s1b[g][:, 0:ntok], in0=s1b[g][:, 0:ntok], in1=pg3[:, g, 0:ntok], op=ALU.mult),
                                 reads=[B['s1b', g], B['pg3', g]], writes=[B['s1b', g]])
                            P.op('dve', lambda e, g=g, kk=kk, hs=hs, ntok=ntok: e.tensor_tensor(out=hT[hs][:, kk, 0:ntok], in0=s1b[g][:, 0:ntok], in1=pw[:, 0:ntok], op=ALU.mult),
                                 reads=[B['s1b', g], B['pw']], writes=[B['hT', hs, kk]])
                        else:
                            P.op('dve', lambda e, g=g, kk=kk, hs=hs, ntok=ntok: e.tensor_tensor(out=hT[hs][:, kk, 0:ntok], in0=s1b[g][:, 0:ntok], in1=pg3[:, g, 0:ntok], op=ALU.mult),
                                 reads=[B['s1b', g], B['pg3', g]], writes=[B['hT', hs, kk]])
                    for j in range(nt):
                        tile = t0 + j
                        for hf in range(2):
                            fs = fcnt % 2
                            fcnt += 1
                            for kk in range(KC):
                                P.op('pe', lambda e, j=j, hf=hf, kk=kk, fs=fs, hs=hs, wsl=wsl: e.matmul(pf[:, fs, :], lhsT=hT[hs][:, kk, j * 128:(j + 1) * 128],
                                                                                                    rhs=w2s[wsl][:, kk, hf * 512:(hf + 1) * 512], start=(kk == 0), stop=(kk == KC - 1)),
                                     reads=[B['hT', hs, kk], B['w2s', wsl]], writes=[B['pf', fs]])
                            fb = B['facc', tile, hf]
                            if ci == 0:
                                P.op('act', lambda e, tile=tile, hf=hf, fs=fs: e.copy(out=facc[:, tile, hf * 512:(hf + 1) * 512], in_=pf[:, fs, :]), reads=[B['pf', fs]], writes=[fb])
                            else:
                                P.op('dve', lambda e, tile=tile, hf=hf, fs=fs: e.tensor_tensor(out=facc[:, tile, hf * 512:(hf + 1) * 512], in0=facc[:, tile, hf * 512:(hf + 1) * 512],
                                                                                              in1=pf[:, fs, :], op=ALU.add), reads=[B['pf', fs], fb], writes=[fb])
            P.flush()
        with contextlib.ExitStack() as s3:
            P.stack = s3
            mrow = P.sb("mrow3", [128, 2, 2, D], F32)
            lnr = P.sb("lnr3", [128, 4, D], F32)
            P.dma('sp', lambda e: e.dma_start(out=mrow[:], in_=modrows), writes=[B['mrow']])
            P.dma('sp', lambda e: e.dma_start(out=lnr[:], in_=lnrows), writes=[B['lnr']])
            xr = [P.sb(f"xr{i}", [128, D], F32) for i in range(2)]
            t1 = P.sb("t1b", [128, D], F32)
            stt = P.sb("stt2", [128, 2, 6], F32)
            mv = P.sb("mv2", [128, 2], F32)
            rstd = P.sb("rstd2", [128, 1], F32)
            for tile in range(NTILE):
                ms = 1 if tile == 16 else 0
                r = xr[tile % 2]
                rk = ('xr', tile % 2)
                P.dma('sp', lambda e, tile=tile, r=r: e.dma_start(out=r[:], in_=x1o[gt(tile)]), reads=[B['x1o', tile]], writes=[B[rk]])
                P.op('dve', lambda e, tile=tile, ms=ms: e.tensor_tensor(out=t1[:], in0=facc[:, tile, :], in1=mrow[:, ms, 1, :], op=ALU.mult),
                     reads=[B['facc', tile, 0], B['facc', tile, 1], B['mrow']], writes=[B['t1b']])
                P.op('dve', lambda e, r=r: e.scalar_tensor_tensor(out=r[:], in0=r[:], scalar=ALPHA, in1=t1[:], op0=ALU.mult, op1=ALU.add),
                     reads=[B[rk], B['t1b']], writes=[B[rk]])
                ln_tile(r, stt, mv, rstd, 2, rk)
                P.dma('sp', lambda e, tile=tile, r=r: e.dma_start(out=xo[gt(tile)], in_=r[:]), reads=[B[rk]], is_output=True)
            P.flush()


def build_B(moe):
    KC = 4 if moe else 2
    NCH = 56 if moe else 11
    CPE = 7 if moe else 11
    nc = bass.Bass("TRN2", target_bir_lowering=False)
    x = _din(nc, "x", [NTILE, 128, D])
    yT = _din(nc, "yT", [16, 128, NTOK])
    modcols = _din(nc, "modcols", [128, 2, 4, 8])
    modrows = _din(nc, "modrows", [128, 2, 2, D])
    lnrows = _din(nc, "lnrows", [128, 4, D])
    bgc = _din(nc, "bgc", [128, 4, 8])
    ident = _din(nc, "ident", [128, 128])
    wg = _din(nc, "wg", [8, 128, 4, 8, 128])
    wb = _din(nc, "wb", [8, 128, 4, 4, 128])
    wo = _din(nc, "wo", [128, 8, D])
    w1 = _din(nc, "w1", [NCH, 128, 8, KC * 128])
    w3 = _din(nc, "w3", [NCH, 128, 8, KC * 128])
    w2 = _din(nc, "w2", [NCH, 128, KC, D])
    if moe:
        rt = _din(nc, "rt", [128, 8, 8])
        sel = _din(nc, "sel", [8, 8, 128])
    x1o = _dout(nc, "x1o", [NTILE, 128, D])
    xo = _dout(nc, "xo", [NTILE, 128, D])

    Dm = dict(x=x, yT=yT, modcols=modcols, modrows=modrows, lnrows=lnrows, bgc=bgc, ident=ident, wg=wg, wb=wb, wo=wo, w1=w1, w3=w3, w2=w2, x1o=x1o, xo=xo)
    if moe:
        Dm['rt'] = rt
        Dm['sel'] = sel
    with contextlib.ExitStack() as st0:
        P = Prog(nc, st0)
        emit_B(nc, P, Dm, moe)
    return nc


_CACHE = {}


def _prog(name, fn, *a):
    k = (name,) + a
    if k not in _CACHE:
        _CACHE[k] = fn(*a)
    return _CACHE[k]


def _tile_rows(w):
    K, N = w.shape
    return np.ascontiguousarray(w.reshape(K // 128, 128, N).transpose(1, 0, 2))


def _cols(v):
    return np.ascontiguousarray(v.reshape(-1, 128).T)


def _rep(v):
    return np.ascontiguousarray(np.broadcast_to(v[None], (128,) + v.shape))


_IDENT = np.eye(128, dtype=np.float32)


def run_M(c, c_ctx, w_mod, b_mod):
    cc = np.concatenate([c, c_ctx[None]], 0)
    cT = np.ascontiguousarray(cc.T.reshape(8, 128, 5).transpose(1, 0, 2))
    Wm = w_mod.transpose(1, 0, 2).reshape(1024, 4 * 6144)
    bm = b_mod.reshape(4 * 6144)
    ins = []
    for i in range(8):
        sl = slice(i * 3072, (i + 1) * 3072)
        ins.append(dict(cT=cT, w=_tile_rows(Wm[:, sl]), b=np.ascontiguousarray(np.broadcast_to(bm[None, sl], (5, 3072)))))
    res = run_bass_kernel_spmd(_prog('M', build_M), ins, core_ids=list(range(8)))
    mod = np.concatenate([res.results[i]['o'] for i in range(8)], axis=1)
    return mod.reshape(5, 4, 6144)


def prep_B_weights(l, inp):
    moe = (l % 2 == 1)
    i = l // 2
    d = {}
    d['wg'] = np.ascontiguousarray(inp['w_gate'][l].reshape(4, 8, 128, 8, 128).transpose(3, 2, 0, 1, 4))
    d['wb'] = np.ascontiguousarray(inp['w_branch'][l].reshape(4, 4, 128, 8, 128).transpose(3, 2, 0, 1, 4))
    d['wo'] = _tile_rows(inp['w_o'][l])
    d['lnrows'] = _rep(np.stack([inp['ln_g'][l, 0], inp['ln_b'][l, 0], inp['ln_g'][l, 1], inp['ln_b'][l, 1]], 0))
    d['bgc'] = np.ascontiguousarray(inp['b_gate'][l].reshape(4, 8, 128).transpose(2, 0, 1))
    d['ident'] = _IDENT
    if not moe:
        KC, NCH = 2, 11
        d['w1'] = np.ascontiguousarray(inp['ffn_w1'][i].reshape(8, 128, NCH, KC * 128).transpose(2, 1, 0, 3))
        d['w3'] = np.ascontiguousarray(inp['ffn_w3'][i].reshape(8, 128, NCH, KC * 128).transpose(2, 1, 0, 3))
        d['w2'] = np.ascontiguousarray(inp['ffn_w2'][i].reshape(NCH, KC, 128, 1024).transpose(0, 2, 1, 3))
    else:
        KC, CPE = 4, 7
        d['w1'] = np.ascontiguousarray(inp['moe_w1'][i].reshape(8, 8, 128, CPE, KC * 128).transpose(0, 3, 2, 1, 4)).reshape(56, 128, 8, KC * 128)
        d['w3'] = np.ascontiguousarray(inp['moe_w3'][i].reshape(8, 8, 128, CPE, KC * 128).transpose(0, 3, 2, 1, 4)).reshape(56, 128, 8, KC * 128)
        d['w2'] = np.ascontiguousarray(inp['moe_w2'][i].reshape(8, CPE, KC, 128, 1024).transpose(0, 1, 3, 2, 4)).reshape(56, 128, KC, 1024)
        d['rt'] = _tile_rows(inp['router'][i])
        sel = np.zeros((8, 8, 128), np.float32)
        for e in range(8):
            sel[e, e, :] = 1.0
        d['sel'] = sel
    return d


def run_B(l, x_cur, h_cur, YT, mod, inp):
    moe = (l % 2 == 1)
    wd = prep_B_weights(l, inp)
    ins = []
    for b in range(4):
        for half in range(2):
            d = dict(wd)
            xt = np.concatenate([x_cur[b, half * 2048:(half + 1) * 2048], h_cur[b, half * 128:(half + 1) * 128]], 0)
            d['x'] = np.ascontiguousarray(xt.reshape(17, 128, 1024))
            toks = np.concatenate([np.arange(half * 2048, (half + 1) * 2048), 4096 + np.arange(half * 128, (half + 1) * 128)])
            d['yT'] = np.ascontiguousarray(YT[b][:, toks].reshape(16, 128, NTOK))
            ms = [mod[b], mod[4]]
            d['modcols'] = np.ascontiguousarray(np.stack([np.stack([_cols(m[k * 1024:(k + 1) * 1024]) for k in (0, 1, 3, 4)], 1) for m in ms], 1))
            d['modrows'] = _rep(np.stack([np.stack([m[2048:3072], m[5120:6144]], 0) for m in ms], 0))
            ins.append(d)
    res = run_bass_kernel_spmd(_prog('B', build_B, moe), ins, core_ids=list(range(8)))
    x_new = np.empty_like(x_cur)
    h_new = np.empty_like(h_cur)
    x1 = np.empty_like(x_cur)
    for b in range(4):
        for half in range(2):
            o = res.results[b * 2 + half]['xo'].reshape(NTOK, 1024)
            x_new[b, half * 2048:(half + 1) * 2048] = o[:2048]
            h_new[b, half * 128:(half + 1) * 128] = o[2048:]
            x1[b, half * 2048:(half + 1) * 2048] = res.results[b * 2 + half]['x1o'].reshape(NTOK, 1024)[:2048]
    return x_new, h_new, x1


NT_A = 34
NFB = 6
NTOK_A = NT_A * 128
TBLK_A = [(i * 512, 512) for i in range(8)] + [(4096, 256)]
PI = float(np.pi)


def emit_A(nc, P, Dm):
    x = Dm['x']
    modcols = Dm['modcols']
    ident = Dm['ident']
    wA = Dm['wA']
    wR = Dm['wR']
    wH = Dm['wH']
    wD = Dm['wD']
    caw = Dm['caw']
    hcw = Dm['hcw']
    hsk = Dm['hsk']
    ropec = Dm['ropec']
    ropes = Dm['ropes']
    qkn = Dm['qkn']
    rdec = Dm['rdec']
    rtab = Dm['rtab']
    rcol = Dm['rcol']
    featsT = Dm['featsT']
    featsTc = Dm['featsTc']
    hw1 = Dm['hw1']
    hw2 = Dm['hw2']
    hcols = Dm['hcols']
    hw3 = Dm['hw3']
    negt = Dm['negt']
    adel = Dm['adel']
    Fm = Dm['Fm']
    Gm = Dm['Gm']
    Fc = Dm['Fc']
    Gc = Dm['Gc']
    YT = Dm['YT']
    with contextlib.ExitStack() as st:
        P.stack = st
        B = Bufs()
        ids = P.sb("ids", [128, 128], F32)
        idb = P.sb("idb", [128, 128], BF16)
        mcol = P.sb("mcol", [128, 2, 2, 8], F32)
        P.dma('sp', lambda e: e.dma_start(out=ids[:], in_=ident), writes=[B['ids']])
        P.dma('sp', lambda e: e.dma_start(out=mcol[:], in_=modcols), writes=[B['mcol']])
        P.op('dve', lambda e: e.tensor_copy(out=idb[:], in_=ids[:]), reads=[B['ids']], writes=[B['idb']])
        P.op('dve', lambda e: e.tensor_scalar_add(out=mcol[:, :, 1, :], in0=mcol[:, :, 1, :], scalar1=1.0), reads=[B['mcol']], writes=[B['mcol']])
        pt = P.ps("pt", [128, 2, 512], F32)
        ptb = P.ps("ptb", [128, 1024], BF16)
        pa = P.ps("pa", [128, 2, 512], F32)
        pb_ = P.ps("pb", [128, 2, 512], F32)
        pc = P.ps("pc", [128, 512], F32)
        cnt = {'t': 0}

        uTs = Dm.get('uTs')
        ustate = Dm.get('ustate', {'have': False})

        def build_uT(uT, stk):
            P.stack = stk
            if uTs is not None and ustate['have']:
                for f in range(8):
                    P.dma('sp', lambda e, f=f: e.dma_start(out=uT[:, f, :], in_=uTs[:, f, :]), writes=[B['uT']])
                return
            xb = [P.sb(f"xb{i}", [128, D], F32) for i in range(2)]
            for t in range(NT_A):
                ms = 1 if t >= 32 else 0
                xs = xb[t % 2]
                P.dma('sp', lambda e, t=t, xs=xs: e.dma_start(out=xs[:], in_=x[t]), writes=[B['xb', t % 2]])
                for f in range(8):
                    s = cnt['t'] % 2
                    cnt['t'] += 1
                    P.op('pe', lambda e, xs=xs, f=f, s=s: e.transpose(pt[:, s, 0:128], xs[:, f * 128:(f + 1) * 128], ids[:]),
                         reads=[B['xb', t % 2], B['ids']], writes=[B['pt', s]])
                    P.op('act', lambda e, t=t, f=f, s=s, ms=ms: e.activation(out=uT[:, f, t * 128:(t + 1) * 128], in_=pt[:, s, 0:128], func=AF.Identity,
                                                                           bias=mcol[:, ms, 0, f:f + 1], scale=mcol[:, ms, 1, f:f + 1]),
                         reads=[B['pt', s], B['mcol']], writes=[B['uT']])
            if uTs is not None:
                for f in range(8):
                    P.dma('sp', lambda e, f=f: e.dma_start(out=uTs[:, f, :], in_=uT[:, f, :]), reads=[B['uT']], writes=[B['uTs']])
                ustate['have'] = True

        def inproj_fm(uT, wsb, wkey, col0, dst, dkey):
            for bi, (tok0, ntok) in enumerate(TBLK_A):
                s = bi % 2
                for kt in range(8):
                    P.op('pe', lambda e, kt=kt, s=s, tok0=tok0, ntok=ntok: e.matmul(pa[:, s, 0:ntok], lhsT=wsb[:, kt, col0:col0 + 128], rhs=uT[:, kt, tok0:tok0 + ntok],
                                                                                  start=(kt == 0), stop=(kt == 7)),
                         reads=[B[wkey], B['uT']], writes=[B['pa', s]])
                eng = 'act' if bi % 2 == 0 else 'dve'
                if eng == 'act':
                    P.op('act', lambda e, s=s, tok0=tok0, ntok=ntok: e.copy(out=dst[:, tok0:tok0 + ntok], in_=pa[:, s, 0:ntok]), reads=[B['pa', s]], writes=[B[dkey]])
                else:
                    P.op('dve', lambda e, s=s, tok0=tok0, ntok=ntok: e.tensor_copy(out=dst[:, tok0:tok0 + ntok], in_=pa[:, s, 0:ntok]), reads=[B['pa', s]], writes=[B[dkey]])

        def dwconv(z, zkey, wc, wkey, acc, akey, out, okey):
            P.op('dve', lambda e: e.tensor_scalar(out=acc[:], in0=z, scalar1=wc[:, 1:2], scalar2=wc[:, 3:4], op0=ALU.mult, op1=ALU.add),
                 reads=[B[zkey], B[wkey]], writes=[B[akey]])
            for (a, n) in ((0, 4096), (4096, 256)):
                P.op('dve', lambda e, a=a, n=n: e.scalar_tensor_tensor(out=acc[:, a + 1:a + n], in0=z[:, a:a + n - 1], scalar=wc[:, 0:1], in1=acc[:, a + 1:a + n],
                                                                      op0=ALU.mult, op1=ALU.add), reads=[B[zkey], B[wkey], B[akey]], writes=[B[akey]])
                P.op('dve', lambda e, a=a, n=n: e.scalar_tensor_tensor(out=acc[:, a:a + n - 1], in0=z[:, a + 1:a + n], scalar=wc[:, 2:3], in1=acc[:, a:a + n - 1],
                                                                      op0=ALU.mult, op1=ALU.add), reads=[B[zkey], B[wkey], B[akey]], writes=[B[akey]])
            if out is not None:
                P.op('pool', lambda e: e.tensor_copy(out=out, in_=acc[:]), reads=[B[akey]], writes=[B[okey]])

        import os as _os
        _dbg = _os.environ.get('A_DBG', '').split(',')
        P.mute = ('noH' in _dbg)
        sH = st.enter_context(contextlib.ExitStack())
        P.stack = sH
        zH = P.sb("zH", [128, 6, NTOK_A], BF16)
        with contextlib.ExitStack() as s0:
            P.stack = s0
            uT = P.sb("uT", [128, 8, NTOK_A], BF16)
            wHs = P.sb("wHs", [128, 8, 768], BF16)
            P.dma('pool', lambda e: e.dma_start(out=wHs[:], in_=wH), writes=[B['wHs']])
            build_uT(uT, s0)
            for c in range(6):
                inproj_fm(uT, wHs, 'wHs', c * 128, zH[:, c, :], ('zH', c))
            P.flush()
        P.stack = sH
        hsks = P.sb("hsks", [128, 2, 2], F32)
        P.dma('sp', lambda e: e.dma_start(out=hsks[:], in_=hsk), writes=[B['hsks']])
        with contextlib.ExitStack() as s1:
            P.stack = s1
            hcws = P.sb("hcws", [128, 6, 4], F32)
            acc = P.sb("acc", [128, NTOK_A], F32)
            P.dma('sp', lambda e: e.dma_start(out=hcws[:], in_=hcw), writes=[B['hcws']])
            for c in range(6):
                dwconv(zH[:, c, :], ('zH', c), hcws[:, c, :], 'hcws', acc, 'acc', zH[:, c, :], ('zH', c))
            P.flush()
        P.stack = sH
        HTc = P.sb("HTc", [128, 2, 512], BF16)
        Fcs = P.sb("Fcs", [128, 2, 512], BF16)
        Gcs = P.sb("Gcs", [128, 4, 256], BF16)
        P.dma('sp', lambda e: e.dma_start(out=Fcs[:], in_=Fc), writes=[B['Fcs']])
        P.dma('sp', lambda e: e.dma_start(out=Gcs[:], in_=Gc), writes=[B['Gcs']])
        Ft = [P.sb(f"Ft{i}", [128, 1024], BF16) for i in range(NFB)]
        fcnt = {'n': 0}
        h2T = P.sb("h2T", [64, 4352], F32)
        hcs = P.sb("hcs", [64, 6], F32)
        w3s = P.sb("w3s", [64, 2, 3, 256], F32)
        negts = P.sb("negts", [128, 34], F32)
        adels = P.sb("adels", [128, 2, 3, 256], F32)
        npi = P.sb("npi", [64, 1], F32)
        P.dma('sp', lambda e: e.dma_start(out=hcs[:, 0:4], in_=hcols), writes=[B['hcs']])
        P.dma('sp', lambda e: e.dma_start(out=w3s[:, :, 0:2, :], in_=hw3), writes=[B['w3s']])
        P.dma('sp', lambda e: e.dma_start(out=negts[:], in_=negt), writes=[B['negts']])
        P.dma('sp', lambda e: e.dma_start(out=adels[:, :, 0:2, :], in_=adel), writes=[B['adels']])
        P.op('dve', lambda e: e.tensor_scalar(out=w3s[:, :, 2, :], in0=w3s[:, :, 1, :], scalar1=-1.0, scalar2=None, op0=ALU.mult), reads=[B['w3s']], writes=[B['w3s']])
        P.op('dve', lambda e: e.tensor_copy(out=adels[:, :, 2, :], in_=adels[:, :, 1, :]), reads=[B['adels']], writes=[B['adels']])
        P.op('dve', lambda e: e.tensor_tensor(out=hcs[:, 4:6], in0=hcs[:, 0:2], in1=hcs[:, 2:4], op=ALU.mult), reads=[B['hcs']], writes=[B['hcs']])
        P.op('dve', lambda e: e.memset(npi[:], -PI), writes=[B['npi']])

        def sin_layer(src, skey, wmat, wkey2, kdim, li, dst, dkey2, argb):
            for bi, (tok0, ntok) in enumerate(TBLK_A):
                s = bi % 2
                P.op('pe', lambda e, s=s, tok0=tok0, ntok=ntok: e.matmul(pa[0:64, s, 0:ntok], lhsT=wmat[0:kdim, :], rhs=src[0:kdim, tok0:tok0 + ntok], start=True, stop=True),
                     reads=[B[skey], B[wkey2]], writes=[B['pa', s]])
                ab = argb[s]
                P.op('dve', lambda e, s=s, ntok=ntok, ab=ab: e.tensor_scalar(out=ab[:, 0:ntok], in0=pa[0:64, s, 0:ntok], scalar1=hcs[:, 2 + li:3 + li], scalar2=hcs[:, 4 + li:5 + li],
                                                                            op0=ALU.mult, op1=ALU.add), reads=[B['pa', s], B['hcs']], writes=[B['argb', s]])
                m1 = argb[2 + s]
                P.op('dve', lambda e, ntok=ntok, ab=ab, m1=m1: e.tensor_scalar(out=m1[:, 0:ntok], in0=ab[:, 0:ntok], scalar1=PI, scalar2=None, op0=ALU.is_gt),
                     reads=[B['argb', s]], writes=[B['argm', s]])
                P.op('dve', lambda e, ntok=ntok, ab=ab, m1=m1: e.scalar_tensor_tensor(out=ab[:, 0:ntok], in0=m1[:, 0:ntok], scalar=-2.0 * PI, in1=ab[:, 0:ntok], op0=ALU.mult, op1=ALU.add),
                     reads=[B['argb', s], B['argm', s]], writes=[B['argb', s]])
                P.op('dve', lambda e, ntok=ntok, ab=ab, m1=m1: e.tensor_scalar(out=m1[:, 0:ntok], in0=ab[:, 0:ntok], scalar1=-PI, scalar2=None, op0=ALU.is_lt),
                     reads=[B['argb', s], B['argm', s]], writes=[B['argm', s]])
                P.op('dve', lambda e, ntok=ntok, ab=ab, m1=m1: e.scalar_tensor_tensor(out=ab[:, 0:ntok], in0=m1[:, 0:ntok], scalar=2.0 * PI, in1=ab[:, 0:ntok], op0=ALU.mult, op1=ALU.add),
                     reads=[B['argb', s], B['argm', s]], writes=[B['argb', s]])
                P.op('act', lambda e, tok0=tok0, ntok=ntok, ab=ab: e.activation(out=dst[:, tok0:tok0 + ntok], in_=ab[:, 0:ntok], func=AF.Sin),
                     reads=[B['argb', s]], writes=[B[dkey2]])

        with contextlib.ExitStack() as sm:
            P.stack = sm
            h1T = P.sb("h1T", [64, 4352], F32)
            w2s = P.sb("w2s", [64, 64], F32)
            argb = [P.sb(f"argb{i}", [64, 512], F32) for i in range(4)]
            P.dma('sp', lambda e: e.dma_start(out=w2s[:], in_=hw2), writes=[B['w2s']])
            with contextlib.ExitStack() as sm2:
                P.stack = sm2
                fT = P.sb("fT", [33, 4352], F32)
                w1s = P.sb("w1s", [33, 64], F32)
                P.dma('sp', lambda e: e.dma_start(out=fT[:, 0:4096], in_=featsT), writes=[B['fT']])
                P.dma('sp', lambda e: e.dma_start(out=fT[:, 4096:4352], in_=featsTc), writes=[B['fT']])
                P.dma('sp', lambda e: e.dma_start(out=w1s[:], in_=hw1), writes=[B['w1s']])
                sin_layer(fT, 'fT', w1s, 'w1s', 33, 0, h1T, 'h1T', argb)
                P.flush()
            sin_layer(h1T, 'h1T', w2s, 'w2s', 64, 1, h2T, 'h2T', argb)
            P.flush()

        gcnt = {'n': 0}
        for o in range(2):
          with contextlib.ExitStack() as so:
            P.stack = so
            HT = P.sb(f"HT{o}", [128, 2, 8192], BF16)
            with contextlib.ExitStack() as sf:
                P.stack = sf
                env = [P.sb(f"env{i}", [128, 768], F32) for i in range(2)]
                filt = P.sb("filt", [128, 34, 768], BF16)
                for t in range(NT_A):
                    s = t % 2
                    ev = env[s]
                    P.op('act', lambda e, t=t, o=o, ev=ev: e.activation(out=ev[:], in_=adels[:, o, :, :].rearrange("p a b -> p (a b)"), func=AF.Exp, scale=negts[:, t:t + 1]),
                         reads=[B['adels'], B['negts']], writes=[B['env', s]])
                    P.op('pe', lambda e, t=t, o=o, s=s: e.matmul(pb_[:, s, 0:512], lhsT=h2T[:, t * 128:(t + 1) * 128], rhs=w3s[:, o, 0:2, :].rearrange("p a b -> p (a b)"), start=True, stop=True),
                         reads=[B['h2T'], B['w3s']], writes=[B['pb', s, 0]])
                    P.op('dve', lambda e, t=t, s=s, ev=ev: e.tensor_tensor(out=filt[:, t, 0:512], in0=pb_[:, s, 0:512], in1=ev[:, 0:512], op=ALU.mult),
                         reads=[B['pb', s, 0], B['env', s]], writes=[B['filt', t]])
                P.op('dve', lambda e: e.memset(filt[0:1, 0, 256:512], 0.0), reads=[B['filt', 0]], writes=[B['filt', 0]])
                P.op('dve', lambda e: e.memset(filt[0:1, 32, 256:512], 0.0), reads=[B['filt', 32]], writes=[B['filt', 32]])
                for t in range(NT_A):
                    P.op('pool', lambda e, t=t: e.tensor_tensor(out=filt[:, t, 512:768], in0=filt[:, t, 0:256], in1=filt[:, t, 256:512], op=ALU.add),
                         reads=[B['filt', t]], writes=[B['filt', t]])
                    P.op('pool', lambda e, t=t: e.tensor_tensor(out=filt[:, t, 0:256], in0=filt[:, t, 0:256], in1=filt[:, t, 256:512], op=ALU.subtract),
                         reads=[B['filt', t]], writes=[B['filt', t]])
                for j in range(8):
                    for t in range(32):
                        fs = fcnt['n'] % NFB
                        fcnt['n'] += 1
                        P.dma('sp', lambda e, t=t, j=j, fs=fs: e.dma_start(out=Ft[fs][:], in_=Fm[t, :, j, :]), writes=[B['Ft', fs]])
                        for c in range(2):
                            P.op('pe', lambda e, t=t, c=c, fs=fs: e.matmul(pa[:, c, :], lhsT=filt[:, t, 512 + c * 128:512 + (c + 1) * 128], rhs=Ft[fs][:, 0:512], start=(t == 0), stop=(t == 31)),
                                 reads=[B['filt', t], B['Ft', fs]], writes=[B['pa', c]])
                            P.op('pe', lambda e, t=t, c=c, fs=fs: e.matmul(pb_[:, c, :], lhsT=filt[:, t, c * 128:(c + 1) * 128], rhs=Ft[fs][:, 512:1024], start=(t == 0), stop=(t == 31)),
                                 reads=[B['filt', t], B['Ft', fs]], writes=[B['pb', c, 0]])
                    for c in range(2):
                        P.op('act', lambda e, c=c, j=j: e.copy(out=HT[:, c, j * 1024:j * 1024 + 512], in_=pa[:, c, :]), reads=[B['pa', c]], writes=[B['HT', c]])
                        P.op('dve', lambda e, c=c, j=j: e.tensor_copy(out=HT[:, c, j * 1024 + 512:(j + 1) * 1024], in_=pb_[:, c, :]), reads=[B['pb', c, 0]], writes=[B['HT', c]])
                for c in range(2):
                    for t in range(2):
                        tt = 32 + t
                        P.op('pe', lambda e, t=t, tt=tt, c=c: e.matmul(pa[:, c, 0:256], lhsT=filt[:, tt, 512 + c * 128:512 + (c + 1) * 128], rhs=Fcs[:, t, 0:256], start=(t == 0), stop=(t == 1)),
                             reads=[B['filt', tt], B['Fcs']], writes=[B['pa', c]])
                        P.op('pe', lambda e, t=t, tt=tt, c=c: e.matmul(pb_[:, c, 0:256], lhsT=filt[:, tt, c * 128:(c + 1) * 128], rhs=Fcs[:, t, 256:512], start=(t == 0), stop=(t == 1)),
                             reads=[B['filt', tt], B['Fcs']], writes=[B['pb', c, 0]])
                    P.op('act', lambda e, c=c: e.copy(out=HTc[:, c, 0:256], in_=pa[:, c, 0:256]), reads=[B['pa', c]], writes=[B['HTc', c]])
                    P.op('dve', lambda e, c=c: e.tensor_copy(out=HTc[:, c, 256:512], in_=pb_[:, c, 0:256]), reads=[B['pb', c, 0]], writes=[B['HTc', c]])
                P.flush()
            with contextlib.ExitStack() as s3:
                P.stack = s3
                stm = P.sb("stm", [128, NT_A, 256], BF16)
                YhT = P.sb("YhT", [128, 2, 1024], BF16)
                Ytm = P.sb("Ytm", [128, 68, 256], BF16)
                Gt = [P.sb(f"Gt{i}", [128, 1024], BF16) for i in range(NFB)]
                pr1 = P.sb("pr1", [128, 512], F32)
                pr2 = P.sb("pr2", [128, 512], F32)
                yst = P.sb("yst", [128, 512], F32)
                ysk = P.sb("ysk", [128, 512], F32)
                for t in range(NT_A):
                    for c in range(2):
                        P.op('pe', lambda e, t=t, c=c: e.transpose(ptb[:, c * 128:(c + 1) * 128], zH[:, c, t * 128:(t + 1) * 128], idb[:]),
                             reads=[B['zH', c], B['idb']], writes=[B['ptb']])
                    if t % 2 == 0:
                        P.op('act', lambda e, t=t: e.copy(out=stm[:, t, :], in_=ptb[:, 0:256]), reads=[B['ptb']], writes=[B['stm', t]])
                    else:
                        P.op('dve', lambda e, t=t: e.tensor_copy(out=stm[:, t, :], in_=ptb[:, 0:256]), reads=[B['ptb']], writes=[B['stm', t]])

                def product(c, ucs, uss, hc, hs, n, keyc, keys_):
                    P.op('dve', lambda e: e.tensor_tensor(out=pr1[:, 0:n], in0=ucs, in1=hc, op=ALU.mult), reads=[B[keyc]], writes=[B['pr1']])
                    P.op('dve', lambda e: e.tensor_tensor(out=pr2[:, 0:n], in0=uss, in1=hs, op=ALU.mult), reads=[B[keys_]], writes=[B['pr2']])
                    P.op('pool', lambda e: e.tensor_tensor(out=YhT[:, c, 0:n], in0=pr1[:, 0:n], in1=pr2[:, 0:n], op=ALU.subtract), reads=[B['pr1'], B['pr2']], writes=[B['YhT', c]])
                    P.op('dve', lambda e: e.tensor_tensor(out=pr1[:, 0:n], in0=ucs, in1=hs, op=ALU.mult), reads=[B[keyc], B['pr1']], writes=[B['pr1']])
                    P.op('dve', lambda e: e.tensor_tensor(out=pr2[:, 0:n], in0=uss, in1=hc, op=ALU.mult), reads=[B[keys_], B['pr2']], writes=[B['pr2']])
                    P.op('pool', lambda e: e.tensor_tensor(out=YhT[:, c, 512:512 + n], in0=pr1[:, 0:n], in1=pr2[:, 0:n], op=ALU.add), reads=[B['pr1'], B['pr2']], writes=[B['YhT', c]])

                def ytrans(kts, cols):
                    for kt, c0 in zip(kts, cols):
                        for c in range(2):
                            P.op('pe', lambda e, c=c, c0=c0: e.transpose(ptb[:, c * 128:(c + 1) * 128], YhT[:, c, c0:c0 + 128], idb[:]),
                                 reads=[B['YhT', c], B['idb']], writes=[B['ptb']])
                        if kt % 2 == 0:
                            P.op('act', lambda e, kt=kt: e.copy(out=Ytm[:, kt, :], in_=ptb[:, 0:256]), reads=[B['ptb']], writes=[B['Ytm', kt]])
                        else:
                            P.op('dve', lambda e, kt=kt: e.tensor_copy(out=Ytm[:, kt, :], in_=ptb[:, 0:256]), reads=[B['ptb']], writes=[B['Ytm', kt]])

                for j in range(8):
                    for t in range(32):
                        fs = fcnt['n'] % NFB
                        fcnt['n'] += 1
                        P.dma('sp', lambda e, t=t, j=j, fs=fs: e.dma_start(out=Ft[fs][:], in_=Fm[t, :, j, :]), writes=[B['Ft', fs]])
                        for c in range(2):
                            P.op('pe', lambda e, t=t, c=c, fs=fs: e.matmul(pa[:, c, :], lhsT=stm[:, t, c * 128:(c + 1) * 128], rhs=Ft[fs][:, 0:512], start=(t == 0), stop=(t == 31)),
                                 reads=[B['stm', t], B['Ft', fs]], writes=[B['pa', c]])
                            P.op('pe', lambda e, t=t, c=c, fs=fs: e.matmul(pb_[:, c, :], lhsT=stm[:, t, c * 128:(c + 1) * 128], rhs=Ft[fs][:, 512:1024], start=(t == 0), stop=(t == 31)),
                                 reads=[B['stm', t], B['Ft', fs]], writes=[B['pb', c, 0]])
                    for c in range(2):
                        product(c, pa[:, c, :], pb_[:, c, :], HT[:, c, j * 1024:j * 1024 + 512], HT[:, c, j * 1024 + 512:(j + 1) * 1024], 512, ('pa', c), ('pb', c, 0))
                    ytrans([j * 4 + i for i in range(4)] + [32 + j * 4 + i for i in range(4)], [i * 128 for i in range(4)] + [512 + i * 128 for i in range(4)])
                for c in range(2):
                    for t in range(2):
                        P.op('pe', lambda e, t=t, c=c: e.matmul(pa[:, c, 0:256], lhsT=stm[:, 32 + t, c * 128:(c + 1) * 128], rhs=Fcs[:, t, 0:256], start=(t == 0), stop=(t == 1)),
                             reads=[B['stm', 32 + t], B['Fcs']], writes=[B['pa', c]])
                        P.op('pe', lambda e, t=t, c=c: e.matmul(pb_[:, c, 0:256], lhsT=stm[:, 32 + t, c * 128:(c + 1) * 128], rhs=Fcs[:, t, 256:512], start=(t == 0), stop=(t == 1)),
                             reads=[B['stm', 32 + t], B['Fcs']], writes=[B['pb', c, 0]])
                    product(c, pa[:, c, 0:256], pb_[:, c, 0:256], HTc[:, c, 0:256], HTc[:, c, 256:512], 256, ('pa', c), ('pb', c, 0))
                ytrans([64, 65, 66, 67], [0, 128, 512, 640])

                def finish_blk(c, ps_ap, pkey, tok0, n, scale):
                    P.op('pool', lambda e: e.tensor_scalar(out=ysk[:, 0:n], in0=zH[:, c, tok0:tok0 + n], scalar1=hsks[:, o, c:c + 1], scalar2=None, op0=ALU.mult),
                         reads=[B['zH', c], B['hsks']], writes=[B['ysk']])
                    P.op('dve', lambda e: e.scalar_tensor_tensor(out=yst[:, 0:n], in0=ps_ap, scalar=scale, in1=ysk[:, 0:n], op0=ALU.mult, op1=ALU.add),
                         reads=[B[pkey], B['ysk']], writes=[B['yst']])
                    if o == 0:
                        P.op('pool', lambda e: e.tensor_tensor(out=zH[:, c, tok0:tok0 + n], in0=yst[:, 0:n], in1=zH[:, 2 + c, tok0:tok0 + n], op=ALU.mult),
                             reads=[B['yst'], B['zH', 2 + c]], writes=[B['zH', c]])
                    else:
                        P.op('pool', lambda e: e.tensor_tensor(out=yst[:, 0:n], in0=yst[:, 0:n], in1=zH[:, 4 + c, tok0:tok0 + n], op=ALU.mult),
                             reads=[B['yst'], B['zH', 4 + c]], writes=[B['yst']])
                        P.dma('sp', lambda e: e.dma_start(out=YT[4 + c, :, tok0:tok0 + n], in_=yst[:, 0:n]), reads=[B['yst']], is_output=True)

                for ps_ in range(4):
                    for kt in range(64):
                        gs = gcnt['n'] % NFB
                        gcnt['n'] += 1
                        P.dma('sp', lambda e, ps_=ps_, kt=kt, gs=gs: e.dma_start(out=Gt[gs][:], in_=Gm[ps_, kt]), writes=[B['Gt', gs]])
                        for c in range(2):
                            for nb in range(2):
                                acc_ap = (pa if c == 0 else pb_)[:, nb, :]
                                P.op('pe', lambda e, kt=kt, c=c, nb=nb, gs=gs, acc_ap=acc_ap: e.matmul(acc_ap, lhsT=Ytm[:, kt, c * 128:(c + 1) * 128], rhs=Gt[gs][:, nb * 512:(nb + 1) * 512],
                                                                                                  start=(kt == 0), stop=(kt == 63)),
                                     reads=[B['Ytm', kt], B['Gt', gs]], writes=[B['pa', nb] if c == 0 else B['pb', nb, 0]])
                    for c in range(2):
                        for nb in range(2):
                            finish_blk(c, (pa if c == 0 else pb_)[:, nb, :], ('pa', nb) if c == 0 else ('pb', nb, 0), ps_ * 1024 + nb * 512, 512, 2.0 / 8192.0)
                for c in range(2):
                    for kt in range(4):
                        P.op('pe', lambda e, kt=kt, c=c: e.matmul(pc[:, 0:256], lhsT=Ytm[:, 64 + kt, c * 128:(c + 1) * 128], rhs=Gcs[:, kt, :], start=(kt == 0), stop=(kt == 3)),
                             reads=[B['Ytm', 64 + kt], B['Gcs']], writes=[B['pc']])
                    finish_blk(c, pc[:, 0:256], 'pc', 4096, 256, 2.0 / 512.0)
                P.flush()
        sH.close()
        P.stack = st
        P.mute = ('noR' in _dbg)
        build_A_rest(nc, P, B, st, x, wA, wR, wD, caw, ropec, ropes, qkn, rdec, rtab, rcol, YT, ids, idb, mcol, pt, ptb, pa, pb_, pc, build_uT, inproj_fm, dwconv)


def build_A():
    nc = bass.Bass("TRN2", target_bir_lowering=False)
    x = _din(nc, "x", [NT_A, 128, D])
    modcols = _din(nc, "modcols", [128, 2, 2, 8])
    ident = _din(nc, "ident", [128, 128])
    wA = _din(nc, "wA", [128, 8, 768])
    wR = _din(nc, "wR", [128, 8, 1024])
    wH = _din(nc, "wH", [128, 8, 768])
    wD = _din(nc, "wD", [128, 8, 512])
    caw = _din(nc, "caw", [128, 2, 4])
    hcw = _din(nc, "hcw", [128, 6, 4])
    hsk = _din(nc, "hsk", [128, 2, 2])
    ropec = _din(nc, "ropec", [128, 32, 64])
    ropes = _din(nc, "ropes", [128, 32, 64])
    qkn = _din(nc, "qkn", [128, 2, 128])
    rdec = _din(nc, "rdec", [128, 4])
    rtab = _din(nc, "rtab", [128, 6, 128])
    rcol = _din(nc, "rcol", [128, 4])
    featsT = _din(nc, "featsT", [33, 4096])
    featsTc = _din(nc, "featsTc", [33, 256])
    hw1 = _din(nc, "hw1", [33, 64])
    hw2 = _din(nc, "hw2", [64, 64])
    hcols = _din(nc, "hcols", [64, 4])
    hw3 = _din(nc, "hw3", [64, 2, 2, 256])
    negt = _din(nc, "negt", [128, 34])
    adel = _din(nc, "adel", [128, 2, 2, 256])
    Fm = _din(nc, "Fm", [32, 128, 8, 1024], BF16)
    Gm = _din(nc, "Gm", [4, 64, 128, 1024], BF16)
    Fc = _din(nc, "Fc", [128, 2, 512], BF16)
    Gc = _din(nc, "Gc", [128, 4, 256], BF16)
    YT = _dout(nc, "YT", [8, 128, NTOK_A])

    Dm = dict(x=x, modcols=modcols, ident=ident, wA=wA, wR=wR, wH=wH, wD=wD, caw=caw, hcw=hcw, hsk=hsk, ropec=ropec, ropes=ropes, qkn=qkn, rdec=rdec, rtab=rtab, rcol=rcol, featsT=featsT, featsTc=featsTc, hw1=hw1, hw2=hw2, hcols=hcols, hw3=hw3, negt=negt, adel=adel, Fm=Fm, Gm=Gm, Fc=Fc, Gc=Gc, YT=YT)
    with contextlib.ExitStack() as st0:
        P = Prog(nc, st0)
        emit_A(nc, P, Dm)
    return nc


def build_A_rest(nc, P, B, st, x, wA, wR, wD, caw, ropec, ropes, qkn, rdec, rtab, rcol, YT, ids, idb, mcol, pt, ptb, pa, pb_, pc, build_uT, inproj_fm, dwconv):
    SC = 128.0 ** -0.5
    sG = st.enter_context(contextlib.ExitStack())
    P.stack = sG
    uT = P.sb("uT2", [128, 8, NTOK_A], BF16)
    rc = P.sb("rc", [128, 32, 64], F32)
    rs = P.sb("rs", [128, 32, 64], F32)
    P.dma('sp', lambda e: e.dma_start(out=rc[:], in_=ropec), writes=[B['rope']])
    P.dma('sp', lambda e: e.dma_start(out=rs[:], in_=ropes), writes=[B['rope']])
    with contextlib.ExitStack() as s0:
        build_uT(uT, s0)
        P.flush()

    def rope(src, skey, t, tmp, tkey):
        v = src.rearrange("p (a h f) -> p a h f", a=2, h=2)
        x1 = v[:, :, 0, :]
        x2 = v[:, :, 1, :]
        c_ = rc[:, t, :].rearrange("p (a f) -> p a f", a=2)
        s_ = rs[:, t, :].rearrange("p (a f) -> p a f", a=2)
        tv = [tmp[:, i, :].rearrange("p (a f) -> p a f", a=2) for i in range(4)]
        P.op('dve', lambda e: e.tensor_tensor(out=tv[0], in0=x1, in1=c_, op=ALU.mult), reads=[B[skey], B['rope']], writes=[B[tkey, 0]])
        P.op('dve', lambda e: e.tensor_tensor(out=tv[1], in0=x2, in1=s_, op=ALU.mult), reads=[B[skey], B['rope']], writes=[B[tkey, 1]])
        P.op('pool', lambda e: e.tensor_tensor(out=tv[2], in0=x2, in1=c_, op=ALU.mult), reads=[B[skey], B['rope']], writes=[B[tkey, 2]])
        P.op('pool', lambda e: e.tensor_tensor(out=tv[3], in0=x1, in1=s_, op=ALU.mult), reads=[B[skey], B['rope']], writes=[B[tkey, 3]])
        P.op('dve', lambda e: e.tensor_tensor(out=x1, in0=tv[0], in1=tv[1], op=ALU.subtract), reads=[B[tkey, 0], B[tkey, 1], B[tkey, 3], B[skey]], writes=[B[skey]])
        P.op('pool', lambda e: e.tensor_tensor(out=x2, in0=tv[2], in1=tv[3], op=ALU.add), reads=[B[tkey, 2], B[tkey, 3], B[tkey, 1], B[skey]], writes=[B[skey]])

    import os as _os
    _dbg = _os.environ.get('A_DBG', '').split(',')
    _base = P.mute
    P.mute = _base or ('noS' in _dbg)
    with contextlib.ExitStack() as s1:
        P.stack = s1
        wAs = P.sb("wAs", [128, 8, 768], BF16)
        caws = P.sb("caws", [128, 2, 4], F32)
        P.dma('pool', lambda e: e.dma_start(out=wAs[:], in_=wA), writes=[B['wAs']])
        P.dma('sp', lambda e: e.dma_start(out=caws[:], in_=caw), writes=[B['caws']])
        zA = P.sb("zA", [128, 3, NTOK_A], BF16)
        pp_ = P.sb("pp_", [128, NTOK_A], BF16)
        acc = P.sb("accA", [128, NTOK_A], F32)
        for c in range(2):
            for g in range(3):
                inproj_fm(uT, wAs, 'wAs', g * 256 + c * 128, zA[:, g, :], ('zA', g))
            P.op('pool', lambda e: e.tensor_tensor(out=pp_[:], in0=zA[:, 1, :], in1=zA[:, 2, :], op=ALU.mult), reads=[B['zA', 1], B['zA', 2]], writes=[B['pp_']])
            dwconv(pp_[:], 'pp_', caws[:, c, :], 'caws', acc, 'accA', None, None)
            P.op('dve', lambda e: e.tensor_tensor(out=acc[:], in0=acc[:], in1=zA[:, 0, :], op=ALU.mult), reads=[B['accA'], B['zA', 0]], writes=[B['accA']])
            P.dma('sp', lambda e, c=c: e.dma_start(out=YT[0 + c], in_=acc[:]), reads=[B['accA']], is_output=True)
        P.flush()

    P.mute = _base or ('noD' in _dbg)
    with contextlib.ExitStack() as s2:
        P.stack = s2
        wDs = P.sb("wDs", [128, 8, 512], BF16)
        qkns = P.sb("qkns", [128, 2, 128], F32)
        P.dma('pool', lambda e: e.dma_start(out=wDs[:], in_=wD), writes=[B['wDs']])
        P.dma('sp', lambda e: e.dma_start(out=qkns[:], in_=qkn), writes=[B['qkns']])
        qT = P.sb("qT", [128, 2, NTOK_A], BF16)
        kT = P.sb("kT", [128, NTOK_A], BF16)
        vtm = P.sb("vtm", [128, NT_A, 128], BF16)
        onesb = P.sb("onesb", [128, 128], BF16)
        P.op('dve', lambda e: e.memset(onesb[:], 1.0), writes=[B['onesb']])
        qn = [P.sb(f"qn{i}", [128, 3, 128], F32) for i in range(2)]
        qb = [P.sb(f"qb{i}", [128, 3, 128], BF16) for i in range(2)]
        sq3s = [P.sb(f"sq3{i}", [128, 384], F32) for i in range(2)]
        ss = [P.sb(f"ss{i}", [128, 4], F32) for i in range(2)]
        tmps = [P.sb(f"tmpr{i}", [128, 4, 64], F32) for i in range(2)]
        for t in range(NT_A):
            s = t % 2
            for kt in range(8):
                P.op('pe', lambda e, t=t, kt=kt, s=s: e.matmul(pa[:, s, :], lhsT=uT[:, kt, t * 128:(t + 1) * 128], rhs=wDs[:, kt, :], start=(kt == 0), stop=(kt == 7)),
                     reads=[B['uT'], B['wDs']], writes=[B['pa', s]])
            sq3 = sq3s[s]
            P.op('act', lambda e, s=s, sq3=sq3: e.activation(out=sq3[:], in_=pa[:, s, 0:384], func=AF.Square), reads=[B['pa', s]], writes=[B['sq3', s]])
            P.op('dve', lambda e, s=s, sq3=sq3: e.reduce_sum(out=ss[s][:, 0:3], in_=sq3[:].rearrange("p (h d) -> p h d", h=3), axis=AX.X),
                 reads=[B['sq3', s]], writes=[B['ss', s, 0], B['ss', s, 1], B['ss', s, 2]])
            P.op('dve', lambda e, s=s: e.tensor_scalar(out=ss[s][:, 0:3], in0=ss[s][:, 0:3], scalar1=1.0 / 128.0, scalar2=1e-6, op0=ALU.mult, op1=ALU.add),
                 reads=[B['ss', s, h] for h in range(3)], writes=[B['ss', s, 'a']])
            P.op('act', lambda e, s=s: e.sqrt(out=ss[s][:, 0:3], in_=ss[s][:, 0:3]), reads=[B['ss', s, 'a']], writes=[B['ss', s, 'a']])
            P.op('dve', lambda e, s=s: e.reciprocal(out=ss[s][:, 0:3], in_=ss[s][:, 0:3]), reads=[B['ss', s, 'a']], writes=[B['ss', s, 'a']])
            for h in range(3):
                P.op('dve', lambda e, h=h, s=s: e.scalar_tensor_tensor(out=qn[s][:, h, :], in0=pa[:, s, h * 128:(h + 1) * 128], scalar=ss[s][:, h:h + 1], in1=qkns[:, 0 if h < 2 else 1, :],
                                                                     op0=ALU.mult, op1=ALU.mult), reads=[B['pa', s], B['ss', s, 'a'], B['qkns']], writes=[B['qn', s, h]])
                if t < 32:
                    rope(qn[s][:, h, :], ('qn', s, h), t, tmps[s], ('tmpr', s))
                P.op('pool', lambda e, h=h, s=s: e.tensor_copy(out=qb[s][:, h, :], in_=qn[s][:, h, :]), reads=[B['qn', s, h]], writes=[B['qb', s, h]])
            P.op('act', lambda e, t=t, s=s: e.copy(out=vtm[:, t, :], in_=pa[:, s, 384:512]), reads=[B['pa', s]], writes=[B['vtm', t]])
            for h in range(3):
                P.op('pe', lambda e, h=h, s=s: e.transpose(ptb[:, h * 128:(h + 1) * 128], qb[s][:, h, :], idb[:]), reads=[B['qb', s, h], B['idb']], writes=[B['ptb']])
            P.op('dve', lambda e, t=t: e.tensor_copy(out=qT[:, :, t * 128:(t + 1) * 128], in_=ptb[:, 0:256].rearrange("p (h d) -> p h d", h=2)), reads=[B['ptb']], writes=[B['qT']])
            P.op('act', lambda e, t=t: e.copy(out=kT[:, t * 128:(t + 1) * 128], in_=ptb[:, 256:384]), reads=[B['ptb']], writes=[B['kT']])
        if 'dumpQK' in _dbg:
            dq = P.sb("dq", [128, NTOK_A], F32)
            for i_, src_ in enumerate((qT[:, 0, :], qT[:, 1, :], kT[:])):
                P.op('dve', lambda e, src_=src_: e.tensor_copy(out=dq[:], in_=src_), reads=[B['qT'], B['kT']], writes=[B['dq']])
                P.dma('sp', lambda e, i_=i_: e.dma_start(out=YT[5 + i_], in_=dq[:]), reads=[B['dq']], writes=[B['dqo', i_]], is_output=True)
            P.op('dve', lambda e: e.tensor_copy(out=dq[:].rearrange("p (t d) -> p t d", d=128), in_=vtm[:]), reads=[B['vtm', t_] for t_ in range(NT_A)], writes=[B['dq']])
            P.dma('sp', lambda e: e.dma_start(out=YT[4], in_=dq[:]), reads=[B['dq']], is_output=True)
        P.mute = P.mute or ('noD2' in _dbg)
        pT = [P.sb(f"pT{i}", [128, 512], BF16) for i in range(3)]
        rd = P.sb("rd", [128, 512], F32)
        yo = [P.sb(f"yo{i}", [128, 512], F32) for i in range(2)]
        pcnt = 0
        ocnt = 0
        for h in range(2):
            for bi, (tok0, ntok) in enumerate(TBLK_A):
                if 'cOne' in _dbg and (h, bi) != (0, 0):
                    continue
                ktiles = list(range(34)) if bi < 8 else [32, 33]
                nkt = len(ktiles)
                slots = []
                for ki, kt in enumerate(ktiles):
                    slots.append((pcnt % 2, pcnt % 3))
                    pcnt += 1

                def qk_exp(ki):
                    kt = ktiles[ki]
                    s, p3 = slots[ki]
                    P.op('pe', lambda e, kt=kt, h=h, s=s, tok0=tok0, ntok=ntok: e.matmul(pa[:, s, 0:ntok], lhsT=kT[:, kt * 128:(kt + 1) * 128], rhs=qT[:, h, tok0:tok0 + ntok], start=True, stop=True),
                         reads=[B['kT'], B['qT']], writes=[B['pa', s]])
                    P.op('act', lambda e, s=s, p3=p3, ntok=ntok: e.activation(out=pT[p3][:, 0:ntok], in_=pa[:, s, 0:ntok], func=AF.Exp, scale=SC), reads=[B['pa', s]], writes=[B['pT', p3]])

                def pv(ki):
                    kt = ktiles[ki]
                    s, p3 = slots[ki]
                    P.op('pe', lambda e, kt=kt, p3=p3, ntok=ntok, ki=ki, nkt=nkt: e.matmul(pb_[:, 0, 0:ntok], lhsT=vtm[:, kt, :], rhs=pT[p3][:, 0:ntok], start=(ki == 0), stop=(ki == nkt - 1)),
                         reads=[B['vtm', kt], B['pT', p3]], writes=[B['pb', 0, 0]])
                    P.op('pe', lambda e, p3=p3, ntok=ntok, ki=ki, nkt=nkt: e.matmul(pb_[:, 1, 0:ntok], lhsT=onesb[:], rhs=pT[p3][:, 0:ntok], start=(ki == 0), stop=(ki == nkt - 1)),
                         reads=[B['onesb'], B['pT', p3]], writes=[B['pb', 1, 0]])

                qk_exp(0)
                for ki in range(1, nkt):
                    qk_exp(ki)
                    pv(ki - 1)
                pv(nkt - 1)
                if 'cQK' in _dbg:
                    continue
                P.op('dve', lambda e, ntok=ntok: e.reciprocal(out=rd[:, 0:ntok], in_=pb_[:, 1, 0:ntok]), reads=[B['pb', 1, 0]], writes=[B['rd']])
                y = yo[ocnt % 2]
                yk = ('yo', ocnt % 2)
                ocnt += 1
                P.op('dve', lambda e, ntok=ntok, y=y: e.tensor_tensor(out=y[:, 0:ntok], in0=pb_[:, 0, 0:ntok], in1=rd[:, 0:ntok], op=ALU.mult), reads=[B['pb', 0, 0], B['rd']], writes=[B[yk]])
                P.dma('sp', lambda e, h=h, tok0=tok0, ntok=ntok, y=y: e.dma_start(out=YT[6 + h, :, tok0:tok0 + ntok], in_=y[:, 0:ntok]), reads=[B[yk]], is_output=True)
        P.flush()

    P.mute = _base or ('noT' in _dbg)
    with contextlib.ExitStack() as s3:
        P.stack = s3
        wRs = P.sb("wRs", [128, 8, 1024], BF16)
        P.dma('pool', lambda e: e.dma_start(out=wRs[:], in_=wR), writes=[B['wRs']])
        rds = P.sb("rds", [128, 4], F32)
        lgam = P.sb("lgam", [128, 4], F32)
        rtabs = P.sb("rtabs", [128, 6, 128], F32)
        rcols = P.sb("rcols", [128, 4], F32)
        P.dma('sp', lambda e: e.dma_start(out=rds[:], in_=rdec), writes=[B['rds']])
        P.dma('sp', lambda e: e.dma_start(out=rtabs[:], in_=rtab), writes=[B['rtabs']])
        P.dma('sp', lambda e: e.dma_start(out=rcols[:], in_=rcol), writes=[B['rcols']])
        P.op('act', lambda e: e.activation(out=lgam[:], in_=rds[:], func=AF.Exp), reads=[B['rds']], writes=[B['lgam']])
        P.op('dve', lambda e: e.tensor_scalar(out=lgam[:], in0=lgam[:], scalar1=-1.0, scalar2=None, op0=ALU.mult), reads=[B['lgam']], writes=[B['lgam']])
        mask = P.sb("mask", [128, 128], F32)
        mtmp = P.sb("mtmpR", [128, 128], F32)
        qdf = P.sb("qdf", [128, 128], F32)
        qdb = P.sb("qdb", [128, 128], F32)
        kdc = P.sb("kdc", [128, 4], F32)
        qT = P.sb("rqT", [128, NTOK_A], BF16)
        kT = P.sb("rkT", [128, NTOK_A], BF16)
        qdfT = P.sb("qdfT", [128, NTOK_A], BF16)
        qdbT = P.sb("qdbT", [128, NTOK_A], BF16)
        kdf = P.sb("kdf", [128, NT_A, 128], BF16)
        kdb = P.sb("kdb", [128, NT_A, 128], BF16)
        vtm = P.sb("rvtm", [128, NT_A, 128], BF16)
        sgt = P.sb("sgt", [128, NT_A, 128], BF16)
        Sf = P.sb("Sf", [128, NT_A, 128], BF16)
        Sb = P.sb("Sb", [128, NT_A, 128], BF16)
        stf = [P.sb(f"stf{i}", [128, 128], F32) for i in range(2)]
        qk = [P.sb(f"rqk{i}", [128, 2, 128], F32) for i in range(2)]
        qkb = [P.sb(f"rqkb{i}", [128, 2, 128], BF16) for i in range(2)]
        tmps = [P.sb(f"tmpq{i}", [128, 4, 64], F32) for i in range(2)]
        smT = [P.sb(f"smT{i}", [128, 128], BF16) for i in range(2)]
        stts = [P.sb(f"rstt{i}", [128, 6], F32) for i in range(2)]
        mvs = [P.sb(f"rmv{i}", [128, 2], F32) for i in range(2)]
        rstds = [P.sb(f"rrstd{i}", [128, 1], F32) for i in range(2)]
        on = [P.sb(f"on{i}", [128, 128], F32) for i in range(2)]
        ob = [P.sb(f"ob{i}", [128, 128], BF16) for i in range(2)]
        ysg = [P.sb(f"rysg{i}", [128, 512], F32) for i in range(2)]
        for h in range(2):
            lf = lgam[:, h:h + 1]
            lb = lgam[:, 2 + h:3 + h]
            P.op('act', lambda e, lf=lf: e.activation(out=mask[:], in_=rtabs[:, 0, :], func=AF.Exp, scale=lf), reads=[B['rtabs'], B['lgam']], writes=[B['mask']])
            P.op('dve', lambda e: e.tensor_tensor(out=mask[:], in0=mask[:], in1=rtabs[:, 1, :], op=ALU.mult), reads=[B['mask'], B['rtabs']], writes=[B['mask']])
            P.op('act', lambda e, lb=lb: e.activation(out=mtmp[:], in_=rtabs[:, 2, :], func=AF.Exp, scale=lb), reads=[B['rtabs'], B['lgam']], writes=[B['mtmpR']])
            P.op('dve', lambda e: e.tensor_tensor(out=mtmp[:], in0=mtmp[:], in1=rtabs[:, 3, :], op=ALU.mult), reads=[B['mtmpR'], B['rtabs']], writes=[B['mtmpR']])
            P.op('dve', lambda e: e.tensor_tensor(out=mask[:], in0=mask[:], in1=mtmp[:], op=ALU.add), reads=[B['mask'], B['mtmpR']], writes=[B['mask']])
            P.op('act', lambda e, lf=lf: e.activation(out=qdf[:], in_=rtabs[:, 4, :], func=AF.Exp, scale=lf), reads=[B['rtabs'], B['lgam']], writes=[B['qdf']])
            P.op('act', lambda e, lb=lb: e.activation(out=qdb[:], in_=rtabs[:, 5, :], func=AF.Exp, scale=lb), reads=[B['rtabs'], B['lgam']], writes=[B['qdb']])
            P.op('act', lambda e, lf=lf: e.activation(out=kdc[:, 0:1], in_=rcols[:, 0:1], func=AF.Exp, scale=lf), reads=[B['rcols'], B['lgam']], writes=[B['kdc', 0]])
            P.op('act', lambda e, lb=lb: e.activation(out=kdc[:, 1:2], in_=rcols[:, 1:2], func=AF.Exp, scale=lb), reads=[B['rcols'], B['lgam']], writes=[B['kdc', 1]])
            P.op('act', lambda e, lf=lf: e.activation(out=kdc[:, 2:3], in_=rcols[:, 2:3], func=AF.Exp, scale=lf), reads=[B['rcols'], B['lgam']], writes=[B['kdc', 2]])
            P.op('act', lambda e, lb=lb: e.activation(out=kdc[:, 3:4], in_=rcols[:, 2:3], func=AF.Exp, scale=lb), reads=[B['rcols'], B['lgam']], writes=[B['kdc', 3]])
            kd = [B['kdc', i] for i in range(4)]
            for t in range(NT_A):
                s = t % 2
                for g in range(4):
                    for kt in range(8):
                        P.op('pe', lambda e, t=t, kt=kt, s=s, g=g, h=h: e.matmul(pa[:, s, g * 128:(g + 1) * 128], lhsT=uT[:, kt, t * 128:(t + 1) * 128], rhs=wRs[:, kt, g * 256 + h * 128:g * 256 + (h + 1) * 128],
                                                                        start=(kt == 0), stop=(kt == 7)), reads=[B['uT'], B['wRs']], writes=[B['pa', s]])
                P.op('act', lambda e, s=s: e.copy(out=qk[s][:, 0, :], in_=pa[:, s, 0:128]), reads=[B['pa', s]], writes=[B['rqk', s, 0]])
                P.op('act', lambda e, s=s: e.activation(out=qk[s][:, 1, :], in_=pa[:, s, 128:256], func=AF.Copy, scale=SC), reads=[B['pa', s]], writes=[B['rqk', s, 1]])
                P.op('act', lambda e, s=s, t=t: e.copy(out=vtm[:, t, :], in_=pa[:, s, 256:384]), reads=[B['pa', s]], writes=[B['rvtm', t]])
                P.op('act', lambda e, s=s, t=t: e.activation(out=sgt[:, t, :], in_=pa[:, s, 384:512], func=AF.Silu), reads=[B['pa', s]], writes=[B['sgt', t]])
                for j in range(2):
                    if t < 32:
                        rope(qk[s][:, j, :], ('rqk', s, j), t, tmps[j], ('tmpq', j))
                    P.op('pool', lambda e, s=s, j=j: e.tensor_copy(out=qkb[s][:, j, :], in_=qk[s][:, j, :]), reads=[B['rqk', s, j]], writes=[B['rqkb', s, j]])
                P.op('dve', lambda e, s=s, t=t: e.tensor_scalar(out=kdf[:, t, :], in0=qk[s][:, 1, :], scalar1=kdc[:, 0:1], scalar2=None, op0=ALU.mult), reads=[B['rqk', s, 1], kd[0]], writes=[B['kdf', t]])
                P.op('dve', lambda e, s=s, t=t: e.tensor_scalar(out=kdb[:, t, :], in0=qk[s][:, 1, :], scalar1=kdc[:, 1:2], scalar2=None, op0=ALU.mult), reads=[B['rqk', s, 1], kd[1]], writes=[B['kdb', t]])
                for j in range(2):
                    P.op('pe', lambda e, s=s, j=j: e.transpose(ptb[:, j * 128:(j + 1) * 128], qkb[s][:, j, :], idb[:]), reads=[B['rqkb', s, j], B['idb']], writes=[B['ptb']])
                sl = slice(t * 128, (t + 1) * 128)
                P.op('act', lambda e, sl=sl: e.copy(out=qT[:, sl], in_=ptb[:, 0:128]), reads=[B['ptb']], writes=[B['rqT', t]])
                P.op('act', lambda e, sl=sl: e.copy(out=kT[:, sl], in_=ptb[:, 128:256]), reads=[B['ptb']], writes=[B['rkT', t]])
                P.op('dve', lambda e, sl=sl: e.tensor_tensor(out=qdfT[:, sl], in0=ptb[:, 0:128], in1=qdf[:], op=ALU.mult), reads=[B['ptb'], B['qdf']], writes=[B['qdfT', t]])
                P.op('dve', lambda e, sl=sl: e.tensor_tensor(out=qdbT[:, sl], in0=ptb[:, 0:128], in1=qdb[:], op=ALU.mult), reads=[B['ptb'], B['qdb']], writes=[B['qdbT', t]])

            def chain(order, kdx, Sx, cd, ckey, name):
                cur = None
                for n_, t in enumerate(order):
                    if cur is None:
                        P.op('pool', lambda e, t=t: e.memset(Sx[:, t, :], 0.0), writes=[B[name, t]])
                    else:
                        P.op('pool', lambda e, t=t, cur=cur: e.tensor_copy(out=Sx[:, t, :], in_=stf[cur][:]), reads=[B['stf', cur]], writes=[B[name, t]])
                    if n_ == len(order) - 1:
                        break
                    P.op('pe', lambda e, t=t: e.matmul(pc[:, 0:128], lhsT=kdx[:, t, :], rhs=vtm[:, t, :], start=True, stop=True), reads=[B[name + 'k', t], B['rvtm', t]], writes=[B['pc']])
                    nxt = 0 if cur is None else 1 - cur
                    if cur is None:
                        P.op('dve', lambda e, nxt=nxt: e.tensor_copy(out=stf[nxt][:], in_=pc[:, 0:128]), reads=[B['pc']], writes=[B['stf', nxt]])
                    else:
                        P.op('dve', lambda e, nxt=nxt, cur=cur: e.scalar_tensor_tensor(out=stf[nxt][:], in0=stf[cur][:], scalar=cd, in1=pc[:, 0:128], op0=ALU.mult, op1=ALU.add),
                             reads=[B['stf', cur], B['pc'], ckey], writes=[B['stf', nxt]])
                    cur = nxt
            for t in range(NT_A):
                B.d[('Sfk', t)] = B['kdf', t]
                B.d[('Sbk', t)] = B['kdb', t]
            chain([32, 33] + list(range(32)), kdf, Sf, kdc[:, 2:3], kd[2], 'Sf')
            chain([33, 32] + list(range(31, -1, -1)), kdb, Sb, kdc[:, 3:4], kd[3], 'Sb')
            for t in range(NT_A):
                s = t % 2
                sl = slice(t * 128, (t + 1) * 128)
                P.op('pe', lambda e, sl=sl, s=s: e.matmul(pa[:, s, 0:128], lhsT=kT[:, sl], rhs=qT[:, sl], start=True, stop=True), reads=[B['rkT', t], B['rqT', t]], writes=[B['pa', s]])
                P.op('dve', lambda e, s=s: e.tensor_tensor(out=smT[s][:], in0=pa[:, s, 0:128], in1=mask[:], op=ALU.mult), reads=[B['pa', s], B['mask']], writes=[B['smT', s]])
                P.op('pe', lambda e, t=t, s=s: e.matmul(pb_[:, s, 0:128], lhsT=smT[s][:], rhs=vtm[:, t, :], start=True, stop=False), reads=[B['smT', s], B['rvtm', t]], writes=[B['pb', s, 0]])
                P.op('pe', lambda e, t=t, s=s, sl=sl: e.matmul(pb_[:, s, 0:128], lhsT=qdfT[:, sl], rhs=Sf[:, t, :], start=False, stop=False), reads=[B['qdfT', t], B['Sf', t]], writes=[B['pb', s, 0]])
                P.op('pe', lambda e, t=t, s=s, sl=sl: e.matmul(pb_[:, s, 0:128], lhsT=qdbT[:, sl], rhs=Sb[:, t, :], start=False, stop=True), reads=[B['qdbT', t], B['Sb', t]], writes=[B['pb', s, 0]])
                P.op('act', lambda e, s=s: e.copy(out=on[s][:], in_=pb_[:, s, 0:128]), reads=[B['pb', s, 0]], writes=[B['on', s]])
                stt, mv, rstd = stts[s], mvs[s], rstds[s]
                P.op('dve', lambda e, s=s, stt=stt: e.bn_stats(out=stt[:], in_=on[s][:]), reads=[B['on', s]], writes=[B['rstt', s]])
                P.op('dve', lambda e, stt=stt, mv=mv: e.bn_aggr(out=mv[:], in_=stt[:]), reads=[B['rstt', s]], writes=[B['rmv', s]])
                P.op('dve', lambda e, mv=mv, rstd=rstd: e.tensor_scalar_add(out=rstd[:], in0=mv[:, 1:2], scalar1=1e-6), reads=[B['rmv', s]], writes=[B['rrstd', s]])
                P.op('act', lambda e, rstd=rstd: e.sqrt(out=rstd[:], in_=rstd[:]), reads=[B['rrstd', s]], writes=[B['rrstd', s]])
                P.op('dve', lambda e, rstd=rstd: e.reciprocal(out=rstd[:], in_=rstd[:]), reads=[B['rrstd', s]], writes=[B['rrstd', s]])
                P.op('dve', lambda e, s=s, mv=mv, rstd=rstd: e.tensor_scalar(out=on[s][:], in0=on[s][:], scalar1=mv[:, 0:1], scalar2=rstd[:, 0:1], op0=ALU.subtract, op1=ALU.mult),
                     reads=[B['on', s], B['rmv', s], B['rrstd', s]], writes=[B['on', s]])
                P.op('pool', lambda e, s=s, t=t: e.tensor_tensor(out=ob[s][:], in0=on[s][:], in1=sgt[:, t, :], op=ALU.mult), reads=[B['on', s], B['sgt', t]], writes=[B['ob', s]])
                P.op('pe', lambda e, s=s: e.transpose(ptb[:, 512:640], ob[s][:], idb[:]), reads=[B['ob', s], B['idb']], writes=[B['ptb2']])
                g4 = t // 4
                yg = ysg[g4 % 2]
                P.op('act', lambda e, t=t, yg=yg: e.copy(out=yg[:, (t % 4) * 128:(t % 4 + 1) * 128], in_=ptb[:, 512:640]), reads=[B['ptb2']], writes=[B['rysg', g4 % 2]])
                if t % 4 == 3 or t == NT_A - 1:
                    n_ = (t % 4 + 1) * 128
                    P.dma('sp', lambda e, h=h, g4=g4, yg=yg, n_=n_: e.dma_start(out=YT[2 + h, :, g4 * 512:g4 * 512 + n_], in_=yg[:, 0:n_]), reads=[B['rysg', g4 % 2]], is_output=True)
        P.flush()


import ml_dtypes
_BF = ml_dtypes.bfloat16
_CONST = {}


def _consts():
    if _CONST:
        return _CONST
    C = _CONST
    rows = 64
    row = np.repeat(np.arange(rows, dtype=np.float32), 64)
    col = np.tile(np.arange(64, dtype=np.float32), rows)
    inv = (10000.0 ** (-np.arange(32, dtype=np.float32) / 32)).astype(np.float32)
    ang = np.stack([row[:, None] * inv, col[:, None] * inv], axis=1).astype(np.float32)
    C['ropec'] = np.ascontiguousarray(np.cos(ang).astype(np.float32).reshape(32, 128, 64).transpose(1, 0, 2))
    C['ropes'] = np.ascontiguousarray(np.sin(ang).astype(np.float32).reshape(32, 128, 64).transpose(1, 0, 2))
    j = np.arange(128, dtype=np.float32)[:, None]
    i = np.arange(128, dtype=np.float32)[None, :]
    rtab = np.stack([np.maximum(i - j, 0), (i >= j).astype(np.float32), np.maximum(j - i, 0), (j >= i).astype(np.float32),
                     np.broadcast_to(i + 1, (128, 128)), np.broadcast_to(128 - i, (128, 128))], axis=1).astype(np.float32)
    C['rtab'] = np.ascontiguousarray(rtab)
    jj = np.arange(128, dtype=np.float32)
    C['rcol'] = np.ascontiguousarray(np.stack([127 - jj, jj, np.full(128, 128.0), np.zeros(128)], 1).astype(np.float32))

    def feats(l):
        t = np.linspace(0.0, 1.0, l, dtype=np.float32)[:, None]
        w = (2.0 * np.pi * np.arange(l, dtype=np.float32) / l).astype(np.float32)
        f = np.linspace(1e-4, 15, 16, dtype=np.float32)
        a = (w[:, None] * f[None, :]).astype(np.float32)
        return np.concatenate([t, np.cos(a), -np.sin(a)], axis=-1).astype(np.float32), t[:, 0]
    f4, t4 = feats(4096)
    fc, tc = feats(256)
    C['featsT'] = np.ascontiguousarray(f4.T)
    C['featsTc'] = np.ascontiguousarray(fc.T)
    negt = np.concatenate([-t4.reshape(32, 128), -tc.reshape(2, 128)], 0).T
    C['negt'] = np.ascontiguousarray(negt.astype(np.float32))
    mx = np.log(1e-2) / 0.3
    mn = np.log(1e-2) / 1.5
    C['absdelta'] = np.abs(np.linspace(mn, mx, 2048, dtype=np.float32)).reshape(2, 2, 512)
    n = np.arange(4096, dtype=np.float64)
    k = np.arange(4096, dtype=np.float64) + 0.5
    Fm = np.empty((32, 128, 8, 1024), dtype=_BF)
    for nt in range(32):
        th = 2 * np.pi * np.outer(n[nt * 128:(nt + 1) * 128], k) / 8192.0
        Fm[nt, :, :, 0:512] = np.cos(th).reshape(128, 8, 512).astype(_BF)
        Fm[nt, :, :, 512:1024] = np.sin(th).reshape(128, 8, 512).astype(_BF)
    C['Fm'] = Fm
    Gm = np.empty((4, 64, 128, 1024), dtype=_BF)
    for kt in range(32):
        th = 2 * np.pi * np.outer(k[kt * 128:(kt + 1) * 128], n) / 8192.0
        Gm[:, kt] = np.cos(th).reshape(128, 4, 1024).transpose(1, 0, 2).astype(_BF)
        Gm[:, 32 + kt] = np.sin(th).reshape(128, 4, 1024).transpose(1, 0, 2).astype(_BF)
    C['Gm'] = Gm
    nc_ = np.arange(256, dtype=np.float64)
    kc = np.arange(256, dtype=np.float64) + 0.5
    th = 2 * np.pi * np.outer(nc_, kc) / 512.0
    Fc = np.concatenate([np.cos(th), np.sin(th)], 1).reshape(2, 128, 512).transpose(1, 0, 2)
    C['Fc'] = np.ascontiguousarray(Fc).astype(_BF)
    Gc = np.concatenate([np.cos(th.T), np.sin(th.T)], 0).reshape(4, 128, 256).transpose(1, 0, 2)
    C['Gc'] = np.ascontiguousarray(Gc).astype(_BF)
    return C


def run_A(l, x_cur, h_cur, mod, inp):
    C = _consts()
    w_in = inp['w_in'][l]
    ins = []
    for b in range(4):
        xt = np.ascontiguousarray(np.concatenate([x_cur[b], h_cur[b]], 0).reshape(NT_A, 128, 1024))
        ms = [mod[b], mod[4]]
        modcols = np.ascontiguousarray(np.stack([np.stack([_cols(m[k_ * 1024:(k_ + 1) * 1024]) for k_ in (0, 1)], 1) for m in ms], 1))
        for half in range(2):
            r256 = half * 256 + np.arange(256)
            colsA = np.concatenate([g * 512 + r256 for g in range(3)])
            colsR = np.concatenate([1536 + g * 512 + r256 for g in range(4)])
            colsH = np.concatenate([3584 + g * 512 + r256 for g in range(3)])
            colsD = np.concatenate([5120 + r256, 5120 + 512 + half * 128 + np.arange(128), 5120 + 768 + half * 128 + np.arange(128)])
            ch = half * 256 + np.arange(256)
            caw = np.stack([inp['conv_a_w'][l][0, ch], inp['conv_a_w'][l][1, ch], inp['conv_a_w'][l][2, ch], inp['conv_a_b'][l][ch]], -1)
            hch = np.concatenate([g * 512 + ch for g in range(3)])
            hcw = np.stack([inp['hy_conv_w'][l][0, hch], inp['hy_conv_w'][l][1, hch], inp['hy_conv_w'][l][2, hch], inp['hy_conv_b'][l][hch]], -1)
            d = dict(
                x=xt, modcols=modcols, ident=_IDENT,
                wA=_tile_rows(w_in[:, colsA]), wR=_tile_rows(w_in[:, colsR]), wH=_tile_rows(w_in[:, colsH]), wD=_tile_rows(w_in[:, colsD]),
                caw=np.ascontiguousarray(caw.reshape(2, 128, 4).transpose(1, 0, 2)),
                hcw=np.ascontiguousarray(hcw.reshape(6, 128, 4).transpose(1, 0, 2)),
                hsk=np.ascontiguousarray(inp['hy_skip'][l][:, ch].reshape(2, 2, 128).transpose(2, 0, 1)),
                ropec=C['ropec'], ropes=C['ropes'],
                qkn=_rep(np.stack([inp['q_norm'][l], inp['k_norm'][l]], 0)),
                rdec=_rep(inp['ret_decay'][l][:, half * 2:half * 2 + 2].reshape(4)),
                rtab=C['rtab'], rcol=C['rcol'], featsT=C['featsT'], featsTc=C['featsTc'],
                hw1=np.ascontiguousarray(inp['hy_w1'][l]), hw2=np.ascontiguousarray(inp['hy_w2'][l]),
                hcols=np.ascontiguousarray(np.stack([inp['hy_b1'][l], inp['hy_b2'][l], inp['hy_freq'][l][0], inp['hy_freq'][l][1]], 1)),
                hw3=np.ascontiguousarray(inp['hy_w3'][l].reshape(64, 2, 2, 512)[:, :, :, half * 256:(half + 1) * 256]),
                negt=C['negt'], adel=_rep(np.ascontiguousarray(C['absdelta'][:, :, half * 256:(half + 1) * 256])),
                Fm=C['Fm'], Gm=C['Gm'], Fc=C['Fc'], Gc=C['Gc'],
            )
            ins.append(d)
    res = run_bass_kernel_spmd(_prog('A', build_A), ins, core_ids=list(range(8)))
    YT = []
    for b in range(4):
        y = np.empty((4, 2, 2, 128, NTOK_A), np.float32)
        for half in range(2):
            o = res.results[b * 2 + half]['YT'].reshape(4, 2, 128, NTOK_A)
            y[:, half] = o
        YT.append(y.reshape(2048, NTOK_A))
    return YT


class _YTView:
    def __init__(self, base, half):
        self.base = base
        self.half = half

    def _m(self, i8):
        return (i8 // 2) * 4 + self.half * 2 + (i8 % 2)

    def __getitem__(self, idx):
        if isinstance(idx, tuple):
            return self.base[(self._m(idx[0]),) + tuple(idx[1:])]
        return self.base[self._m(idx)]


def emit_M2(nc, P, Dm):
    cT, wm, bcol, brow, modc, modr = Dm['cT'], Dm['wm'], Dm['bcol'], Dm['brow'], Dm['modc'], Dm['modr']
    with contextlib.ExitStack() as st:
        P.stack = st
        B = Bufs()
        cs = P.sb("cs", [128, 8, 2], F32)
        sc = P.sb("sc", [128, 8, 2], F32)
        ones = P.sb("ones", [128, 128], F32)
        scb = P.sb("scb", [128, 8, 2, 128], F32)
        wch = [P.sb(f"wch{i}", [128, 8, 1024], F32) for i in range(2)]
        bc = P.sb("bc", [128, 48], F32)
        br = P.sb("br", [128, 2, 1024], F32)
        mc = P.sb("mc", [128, 2, 48], F32)
        mr = P.sb("mr", [128, 2, 2, 1024], F32)
        pm = P.ps("pm", [128, 2, 512], F32)
        pcl = P.ps("pc", [128, 512], F32)
        P.dma('sp', lambda e: e.dma_start(out=cs[:], in_=cT), writes=[B['cs']])
        P.op('act', lambda e: e.activation(out=sc[:], in_=cs[:], func=AF.Silu), reads=[B['cs']], writes=[B['sc']])
        P.op('dve', lambda e: e.memset(ones[:], 1.0), writes=[B['ones']])
        for kt in range(8):
            for ms in range(2):
                P.op('dve', lambda e, kt=kt, ms=ms: e.tensor_scalar(out=scb[:, kt, ms, :], in0=ones[:], scalar1=sc[:, kt, ms:ms + 1], scalar2=None, op0=ALU.mult),
                     reads=[B['ones'], B['sc']], writes=[B['scb']])
        wc = 0
        for l in range(4):
            P.dma('sp', lambda e, l=l: e.dma_start(out=bc[:], in_=bcol[l]), writes=[B['bc']])
            P.dma('sp', lambda e, l=l: e.dma_start(out=br[:], in_=brow[l]), writes=[B['br']])
            for k in range(6):
                ws_ = wc % 2
                wc += 1
                P.dma('sp', lambda e, l=l, k=k, ws_=ws_: e.dma_start(out=wch[ws_][:], in_=wm[l, k]), writes=[B['wch', ws_]])
                for f in range(8):
                    for kt in range(8):
                        P.op('pe', lambda e, f=f, kt=kt, ws_=ws_: e.matmul(pcl[:, f * 2:f * 2 + 2], lhsT=wch[ws_][:, kt, f * 128:(f + 1) * 128], rhs=sc[:, kt, :], start=(kt == 0), stop=(kt == 7)),
                             reads=[B['wch', ws_], B['sc']], writes=[B['pc']])
                for ms in range(2):
                    P.op('dve', lambda e, k=k, ms=ms: e.tensor_tensor(out=mc[:, ms, k * 8:(k + 1) * 8], in0=pcl[:, 0:16].rearrange("p (f m) -> p m f", m=2)[:, ms, :], in1=bc[:, k * 8:(k + 1) * 8], op=ALU.add),
                         reads=[B['pc'], B['bc']], writes=[B['mc']])
                if k in (2, 5):
                    j = 0 if k == 2 else 1
                    for ms in range(2):
                        for nb in range(2):
                            for kt in range(8):
                                P.op('pe', lambda e, ms=ms, nb=nb, kt=kt, ws_=ws_: e.matmul(pm[:, nb, :], lhsT=scb[:, kt, ms, :], rhs=wch[ws_][:, kt, nb * 512:(nb + 1) * 512], start=(kt == 0), stop=(kt == 7)),
                                     reads=[B['scb'], B['wch', ws_]], writes=[B['pm', nb]])
                            P.op('dve', lambda e, ms=ms, nb=nb, j=j: e.tensor_tensor(out=mr[:, ms, j, nb * 512:(nb + 1) * 512], in0=pm[:, nb, :], in1=br[:, j, nb * 512:(nb + 1) * 512], op=ALU.add),
                                 reads=[B['pm', nb], B['br']], writes=[B['mr']])
            P.dma('sp', lambda e, l=l: e.dma_start(out=modc[l], in_=mc[:]), reads=[B['mc']], writes=[B['modc', l]])
            P.dma('sp', lambda e, l=l: e.dma_start(out=modr[l], in_=mr[:]), reads=[B['mr']], writes=[B['modr', l]])
        P.flush()


def emit_cast(nc, P, pairs):
    with contextlib.ExitStack() as st:
        P.stack = st
        B = Bufs()
        cb = [P.sb(f"cb{i}", [128, 4096], BF16) for i in range(3)]
        for i, (src, dst) in enumerate(pairs):
            c = cb[i % 3]
            n = src.shape[-1]
            P.dma('pool', lambda e, c=c, src=src, n=n: e.dma_start(out=c[:, 0:n], in_=src), writes=[B['cb', i % 3]])
            P.dma('sp', lambda e, c=c, dst=dst, n=n: e.dma_start(out=dst, in_=c[:, 0:n]), reads=[B['cb', i % 3]], writes=[B['dst', i]])
        P.flush()


def build_F():
    nc = bass.Bass("TRN2", target_bir_lowering=False)
    I = lambda name, shape, dt=F32: _din(nc, name, shape, dt)
    T = lambda name, shape, dt=F32: nc.dram_tensor(name, list(shape), dt, kind="Internal").ap()
    x0 = I("x0", [NT_A, 128, D])
    cT = I("cT", [128, 8, 2])
    wm = I("wm", [4, 6, 128, 8, 1024])
    bcol = I("bcol", [4, 128, 48])
    brow = I("brow", [4, 128, 2, 1024])
    ident = I("ident", [128, 128])
    wA = I("wA", [4, 2, 128, 8, 768])
    wR = I("wR", [4, 2, 128, 8, 1024])
    wH = I("wH", [4, 2, 128, 8, 768])
    wD = I("wD", [4, 2, 128, 8, 512])
    caw = I("caw", [4, 2, 128, 2, 4])
    hcw = I("hcw", [4, 2, 128, 6, 4])
    hsk = I("hsk", [4, 2, 128, 2, 2])
    ropec = I("ropec", [128, 32, 64])
    ropes = I("ropes", [128, 32, 64])
    qkn = I("qkn", [4, 128, 2, 128])
    rdec = I("rdec", [4, 2, 128, 4])
    rtab = I("rtab", [128, 6, 128])
    rcol = I("rcol", [128, 4])
    featsT = I("featsT", [33, 4096])
    featsTc = I("featsTc", [33, 256])
    hw1 = I("hw1", [4, 33, 64])
    hw2 = I("hw2", [4, 64, 64])
    hcols = I("hcols", [4, 64, 4])
    hw3 = I("hw3", [4, 2, 64, 2, 2, 256])
    negt = I("negt", [128, 34])
    adel = I("adel", [2, 128, 2, 2, 256])
    Fm = I("Fm", [32, 128, 8, 1024], BF16)
    Gm = I("Gm", [4, 64, 128, 1024], BF16)
    Fc = I("Fc", [128, 2, 512], BF16)
    Gc = I("Gc", [128, 4, 256], BF16)
    wg = I("wg", [4, 8, 128, 4, 8, 128])
    wb = I("wb", [4, 8, 128, 4, 4, 128])
    wo = I("wo", [4, 128, 8, D])
    lnrows = I("lnrows", [4, 128, 4, D])
    bgc = I("bgc", [4, 128, 4, 8])
    w1d = I("w1d", [2, 11, 128, 8, 256])
    w3d = I("w3d", [2, 11, 128, 8, 256])
    w2d = I("w2d", [2, 11, 128, 2, D])
    w1m = I("w1m", [2, 56, 128, 8, 512])
    w3m = I("w3m", [2, 56, 128, 8, 512])
    w2m = I("w2m", [2, 56, 128, 4, D])
    rt = I("rt", [2, 128, 8, 8])
    sel = I("sel", [8, 8, 128])
    out = _dout(nc, "out", [NT_A, 128, D])
    xb = [T("xbuf0", [NT_A, 128, D]), T("xbuf1", [NT_A, 128, D])]
    x1s = T("x1s", [NT_A, 128, D])
    YTs = T("YTs", [16, 128, NTOK_A])
    modc = T("modc", [4, 128, 2, 48])
    modr = T("modr", [4, 128, 2, 2, D])
    uTs = T("uTs", [128, 8, NTOK_A], BF16)
    wgb = T("wgb", [8, 128, 4, 8, 128], BF16)
    wbb = T("wbb", [8, 128, 4, 4, 128], BF16)
    wob = T("wob", [128, 8, D], BF16)
    with contextlib.ExitStack() as st0:
        P = Prog(nc, st0)
        emit_M2(nc, P, dict(cT=cT, wm=wm, bcol=bcol, brow=brow, modc=modc, modr=modr))
        import os as _os
        NL = int(_os.environ.get('F_LAYERS', '4'))
        for l in range(NL):
            xin = x0 if l == 0 else xb[l % 2]
            xout = out if l == NL - 1 else xb[(l + 1) % 2]
            mcA = modc[l][:, :, 0:16].rearrange("p m (k f) -> p m k f", k=2)
            mcB = (mcA, modc[l][:, :, 24:40].rearrange("p m (k f) -> p m k f", k=2))
            ustate = {'have': False}
            for half in range(2):
                emit_A(nc, P, dict(uTs=uTs, ustate=ustate, x=xin, modcols=mcA, ident=ident, wA=wA[l, half], wR=wR[l, half], wH=wH[l, half], wD=wD[l, half],
                                   caw=caw[l, half], hcw=hcw[l, half], hsk=hsk[l, half], ropec=ropec, ropes=ropes, qkn=qkn[l], rdec=rdec[l, half],
                                   rtab=rtab, rcol=rcol, featsT=featsT, featsTc=featsTc, hw1=hw1[l], hw2=hw2[l], hcols=hcols[l], hw3=hw3[l, half],
                                   negt=negt, adel=adel[half], Fm=Fm, Gm=Gm, Fc=Fc, Gc=Gc, YT=_YTView(YTs, half)))
            moe = (l % 2 == 1)
            i = l // 2
            pairs = []
            for fo in range(8):
                pairs.append((wg[l, fo].rearrange("p a b c -> p (a b c)"), wgb[fo].rearrange("p a b c -> p (a b c)")))
                pairs.append((wb[l, fo].rearrange("p a b c -> p (a b c)"), wbb[fo].rearrange("p a b c -> p (a b c)")))
            for kh in range(2):
                pairs.append((wo[l][:, kh * 4:(kh + 1) * 4, :].rearrange("p a b -> p (a b)"), wob[:, kh * 4:(kh + 1) * 4, :].rearrange("p a b -> p (a b)")))
            emit_cast(nc, P, pairs)
            for hb in range(2):
                Dm = dict(x=xin, yT=YTs, modcols=mcB, modrows=modr[l], lnrows=lnrows[l], bgc=bgc[l], ident=ident, wg=wgb, wb=wbb, wo=wob, wbf16=True,
                          x1o=x1s, xo=xout)
                if moe:
                    Dm.update(w1=w1m[i], w3=w3m[i], w2=w2m[i], rt=rt[i], sel=sel)
                else:
                    Dm.update(w1=w1d[i], w3=w3d[i], w2=w2d[i])
                emit_B(nc, P, Dm, moe, gt=(lambda t, hb=hb: hb * 16 + t if t < 16 else 32 + hb))
    return nc


def _pack_F(inp):
    C = _consts()
    sh = dict(ident=_IDENT, ropec=C['ropec'], ropes=C['ropes'], rtab=C['rtab'], rcol=C['rcol'], featsT=C['featsT'], featsTc=C['featsTc'],
              negt=C['negt'], Fm=C['Fm'], Gm=C['Gm'], Fc=C['Fc'], Gc=C['Gc'])
    sh['adel'] = np.stack([_rep(np.ascontiguousarray(C['absdelta'][:, :, h * 256:(h + 1) * 256])) for h in range(2)], 0)
    w_mod = inp['w_mod']
    sh['wm'] = np.ascontiguousarray(w_mod.reshape(4, 8, 128, 6, 1024).transpose(0, 3, 2, 1, 4))
    sh['bcol'] = np.ascontiguousarray(inp['b_mod'].reshape(4, 48, 128).transpose(0, 2, 1))
    sh['brow'] = np.stack([_rep(np.stack([inp['b_mod'][l, 2048:3072], inp['b_mod'][l, 5120:6144]], 0)) for l in range(4)], 0)
    wA, wR, wH, wD, caw, hcw, hsk, rdec, hw3 = [], [], [], [], [], [], [], [], []
    for l in range(4):
        w_in = inp['w_in'][l]
        rows = [[] for _ in range(9)]
        for half in range(2):
            r256 = half * 256 + np.arange(256)
            colsA = np.concatenate([g * 512 + r256 for g in range(3)])
            colsR = np.concatenate([1536 + g * 512 + r256 for g in range(4)])
            colsH = np.concatenate([3584 + g * 512 + r256 for g in range(3)])
            colsD = np.concatenate([5120 + r256, 5120 + 512 + half * 128 + np.arange(128), 5120 + 768 + half * 128 + np.arange(128)])
            ch = r256
            cawv = np.stack([inp['conv_a_w'][l][0, ch], inp['conv_a_w'][l][1, ch], inp['conv_a_w'][l][2, ch], inp['conv_a_b'][l][ch]], -1)
            hch = np.concatenate([g * 512 + ch for g in range(3)])
            hcwv = np.stack([inp['hy_conv_w'][l][0, hch], inp['hy_conv_w'][l][1, hch], inp['hy_conv_w'][l][2, hch], inp['hy_conv_b'][l][hch]], -1)
            vals = [_tile_rows(w_in[:, colsA]), _tile_rows(w_in[:, colsR]), _tile_rows(w_in[:, colsH]), _tile_rows(w_in[:, colsD]),
                    cawv.reshape(2, 128, 4).transpose(1, 0, 2), hcwv.reshape(6, 128, 4).transpose(1, 0, 2),
                    inp['hy_skip'][l][:, ch].reshape(2, 2, 128).transpose(2, 0, 1),
                    _rep(inp['ret_decay'][l][:, half * 2:half * 2 + 2].reshape(4)),
                    inp['hy_w3'][l].reshape(64, 2, 2, 512)[:, :, :, half * 256:(half + 1) * 256]]
            for r_, v_ in zip(rows, vals):
                r_.append(v_)
        for lst, r_ in zip((wA, wR, wH, wD, caw, hcw, hsk, rdec, hw3), rows):
            lst.append(np.stack(r_, 0))
    for n_, lst in zip(('wA', 'wR', 'wH', 'wD', 'caw', 'hcw', 'hsk', 'rdec', 'hw3'), (wA, wR, wH, wD, caw, hcw, hsk, rdec, hw3)):
        sh[n_] = np.ascontiguousarray(np.stack(lst, 0))
    sh['qkn'] = np.stack([_rep(np.stack([inp['q_norm'][l], inp['k_norm'][l]], 0)) for l in range(4)], 0)
    sh['hw1'] = np.ascontiguousarray(inp['hy_w1'])
    sh['hw2'] = np.ascontiguousarray(inp['hy_w2'])
    sh['hcols'] = np.ascontiguousarray(np.stack([inp['hy_b1'], inp['hy_b2'], inp['hy_freq'][:, 0], inp['hy_freq'][:, 1]], -1))
    pw = [prep_B_weights(l, inp) for l in range(4)]
    for n_ in ('wg', 'wb', 'wo', 'lnrows', 'bgc'):
        sh[n_] = np.stack([pw[l][n_] for l in range(4)], 0)
    for n_ in ('w1', 'w3', 'w2'):
        sh[n_ + 'd'] = np.stack([pw[0][n_], pw[2][n_]], 0)
        sh[n_ + 'm'] = np.stack([pw[1][n_], pw[3][n_]], 0)
    sh['rt'] = np.stack([pw[1]['rt'], pw[3]['rt']], 0)
    sh['sel'] = pw[1]['sel']
    per = []
    for b in range(4):
        cc = np.stack([inp['c'][b], inp['c_ctx']], 0)
        per.append(dict(x0=np.ascontiguousarray(np.concatenate([inp['x'][b], inp['ctx'][b]], 0).reshape(NT_A, 128, 1024)),
                        cT=np.ascontiguousarray(cc.T.reshape(8, 128, 2).transpose(1, 0, 2))))
    return sh, per


def kernel_fused(**inp):
    inp = {k_: np.asarray(v) for k_, v in inp.items()}
    sh, per = _pack_F(inp)
    ins = [dict(sh, **per[b]) for b in range(4)]
    res = run_bass_kernel_spmd(_prog('F', build_F), ins, core_ids=list(range(4)))
    out = np.stack([res.results[b]['out'].reshape(NTOK_A, 1024)[:4096] for b in range(4)], 0)
    return np.ascontiguousarray(out, dtype=np.float32)


def kernel_unfused(**inp):
    inp = {k_: np.asarray(v) for k_, v in inp.items()}
    mod = run_M(inp['c'], inp['c_ctx'], inp['w_mod'], inp['b_mod'])
    x_cur = np.ascontiguousarray(inp['x'], dtype=np.float32)
    h_cur = np.ascontiguousarray(inp['ctx'], dtype=np.float32)
    for l in range(4):
        YT = run_A(l, x_cur, h_cur, mod[:, l], inp)
        x_cur, h_cur, _ = run_B(l, x_cur, h_cur, YT, mod[:, l], inp)
    return x_cur


def kernel(**inp):
    return kernel_fused(**inp)
```

```python
import contextlib
import numpy as np
import concourse.bass as bass
import concourse.mybir as mybir

F32 = mybir.dt.float32
BF16 = mybir.dt.bfloat16
AF = mybir.ActivationFunctionType
ALU = mybir.AluOpType
AX = mybir.AxisListType

NDMA = 24
ENGS = ['pe', 'act', 'dve', 'pool', 'sp']


class Buf:
    __slots__ = ('w', 'r', 'name', 'excl')

    def __init__(self, name=''):
        self.w = None
        self.r = {}
        self.name = name
        self.excl = False


PSUM_KEYS = {'pt', 'ptb', 'pa', 'pb', 'pc', 'pg', 'pp', 'pmx', 'pg1', 'pg3', 'pf', 'pw', 'pm'}


class Bufs:
    def __init__(self, name=''):
        self.d = {}
        self.name = name

    def __getitem__(self, k):
        if k == 'ptb2':
            k = 'ptb'
        b = self.d.get(k)
        if b is None:
            b = Buf(f"{self.name}{k}")
            k0 = k[0] if isinstance(k, tuple) else k
            b.excl = k0 in PSUM_KEYS
            self.d[k] = b
        return b


class Prog:
    def __init__(self, nc, stack, same_engine_sync=True):
        self.nc = nc
        self.stack = stack
        self.q = {e: [] for e in ENGS}
        self.sem = {e: stack.enter_context(nc.semaphore(f"s_{e}")) for e in ENGS}
        self.cnt = {e: 0 for e in ENGS}
        self.seen = {e: {} for e in ENGS}
        self.dsem = [stack.enter_context(nc.semaphore(f"dq{i}")) for i in range(NDMA)]
        self.dval = [0] * NDMA
        self.dnext = 0
        self.ses = same_engine_sync
        self.out_tokens = []

    def sb(self, name, shape, dt):
        self._uid = getattr(self, '_uid', 0) + 1
        return self.stack.enter_context(self.nc.sbuf_tensor(f"{name}_u{self._uid}", list(shape), dt))

    def ps(self, name, shape, dt=F32):
        self._uid = getattr(self, '_uid', 0) + 1
        return self.stack.enter_context(self.nc.psum_tensor(f"{name}_u{self._uid}", list(shape), dt))

    def _semh(self, k):
        return self.sem[k] if isinstance(k, str) else self.dsem[k[1]]

    def _deps(self, e, reads, writes):
        need = {}
        xr = [b for b in reads if b.excl]
        if xr:
            writes = list(writes) + xr

        def add(tok):
            if tok is None:
                return
            k, v = tok
            if need.get(k, 0) < v:
                need[k] = v

        for b in reads:
            add(b.w)
        for b in writes:
            add(b.w)
            for k, v in b.r.items():
                add((k, v))
        waits = []
        for k, v in need.items():
            if k == e and (e == 'pe' or not self.ses):
                continue
            if self.seen[e].get(k, 0) >= v:
                continue
            self.seen[e][k] = v
            waits.append((k, v))
        return waits

    def _mark(self, tok, reads, writes):
        k, v = tok
        xr = [b for b in reads if b.excl]
        if xr:
            writes = list(writes) + xr
        for b in reads:
            if b.r.get(k, 0) < v:
                b.r[k] = v
        for b in writes:
            b.w = tok
            b.r = {}

    mute = False

    def op(self, e, fn, reads=(), writes=()):
        if self.mute:
            return
        waits = self._deps(e, reads, writes)
        self.cnt[e] += 1
        tok = (e, self.cnt[e])
        self._mark(tok, reads, writes)
        self.q[e].append((waits, fn, (self.sem[e], 1)))

    def dma(self, e, fn, reads=(), writes=(), is_output=False):
        if self.mute:
            return
        if e == 'pool':
            i = self._dpool = (getattr(self, '_dpool', -1) + 1) % 8
        else:
            i = 8 + self.dnext
            self.dnext = (self.dnext + 1) % (NDMA - 8)
        waits = self._deps(e, reads, writes)
        k = ('d', i)
        if self.dval[i] > 0 and self.seen[e].get(k, 0) < self.dval[i]:
            waits.append((k, self.dval[i]))
            self.seen[e][k] = self.dval[i]
        self.dval[i] += 16
        tok = (k, self.dval[i])
        self._mark(tok, reads, writes)
        self.q[e].append((waits, fn, (self.dsem[i], 16)))
        if is_output:
            self.out_tokens.append(tok)

    def flush(self):
        nc = self.nc
        q = self.q
        semh = self._semh
        ex = []
        for e in ['pe', 'act', 'dve', 'pool']:
            if self.cnt[e] > 0:
                ex.append((e, self.cnt[e]))
        for i in range(NDMA):
            if self.dval[i] > 0:
                ex.append((('d', i), self.dval[i]))

        def emit(eng, ename):
            for waits, fn, (sem, n) in q[ename]:
                for (k, v) in waits:
                    eng.wait_ge(semh(k), v)
                fn(eng).then_inc(sem, n)
            for (k, v) in ex:
                if self.seen[ename].get(k, 0) < v:
                    eng.wait_ge(semh(k), v)
                    self.seen[ename][k] = v

        with nc.Block() as block:
            @block.tensor
            def _(t):
                emit(t, 'pe')

            @block.scalar
            def _(t):
                emit(t, 'act')

            @block.vector
            def _(t):
                emit(t, 'dve')

            @block.gpsimd
            def _(t):
                emit(t, 'pool')

            @block.sync
            def _(t):
                emit(t, 'sp')
        self.q = {e: [] for e in ENGS}

from concourse.bass_utils import run_bass_kernel_spmd

D = 1024
ALPHA = (2 * 4) ** 0.25
NTILE = 17
NTOK = NTILE * 128
BLOCKS = [(0, 4), (4, 4), (8, 4), (12, 4), (16, 1)]


def _din(nc, name, shape, dt=F32):
    return nc.dram_tensor(name, list(shape), dt, kind="ExternalInput").ap()


def _dout(nc, name, shape, dt=F32):
    return nc.dram_tensor(name, list(shape), dt, kind="ExternalOutput").ap()


def build_M():
    nc = bass.Bass("TRN2", target_bir_lowering=False)
    cT = _din(nc, "cT", [128, 8, 5])
    w = _din(nc, "w", [128, 8, 3072])
    b = _din(nc, "b", [5, 3072])
    o = _dout(nc, "o", [5, 3072])
    with contextlib.ExitStack() as st:
        P = Prog(nc, st)
        B = Bufs()
        cs = P.sb("cs", [128, 8, 5], F32)
        sc = P.sb("sc", [128, 8, 5], F32)
        ws = P.sb("ws", [128, 8, 3072], F32)
        bs = P.sb("bs", [5, 3072], F32)
        os_ = P.sb("os", [5, 3072], F32)
        pm = P.ps("pm", [128, 2, 512], F32)
        P.dma('sp', lambda e: e.dma_start(out=cs[:], in_=cT), writes=[B['cs']])
        P.dma('sp', lambda e: e.dma_start(out=bs[:], in_=b), writes=[B['bs']])
        for kt in range(8):
            P.dma('sp', lambda e, kt=kt: e.dma_start(out=ws[:, kt, :], in_=w[:, kt, :]), writes=[B['ws', kt]])
        P.op('act', lambda e: e.activation(out=sc[:], in_=cs[:], func=AF.Silu), reads=[B['cs']], writes=[B['sc']])
        for nb in range(6):
            pb = B['pm', nb % 2]
            for kt in range(8):
                P.op('pe', lambda e, nb=nb, kt=kt: e.matmul(pm[0:5, nb % 2, :], lhsT=sc[:, kt, :], rhs=ws[:, kt, nb * 512:(nb + 1) * 512],
                                                          start=(kt == 0), stop=(kt == 7)),
                     reads=[B['sc'], B['ws', kt]], writes=[pb])
            P.op('dve', lambda e, nb=nb: e.tensor_tensor(out=os_[:, nb * 512:(nb + 1) * 512], in0=pm[0:5, nb % 2, :],
                                                        in1=bs[:, nb * 512:(nb + 1) * 512], op=ALU.add),
                 reads=[pb, B['bs']], writes=[B['os', nb]])
        P.dma('sp', lambda e: e.dma_start(out=o, in_=os_[:]), reads=[B['os', nb] for nb in range(6)], is_output=True)
        P.flush()
    return nc


def emit_B(nc, P, Dm, moe, gt=None):
    if gt is None:
        gt = lambda t: t
    KC = 4 if moe else 2
    NCH = 56 if moe else 11
    CPE = 7 if moe else 11
    x = Dm['x']
    yT = Dm['yT']
    modcols = Dm['modcols']
    modrows = Dm['modrows']
    lnrows = Dm['lnrows']
    bgc = Dm['bgc']
    ident = Dm['ident']
    wg = Dm['wg']
    wb = Dm['wb']
    wo = Dm['wo']
    w1 = Dm['w1']
    w3 = Dm['w3']
    w2 = Dm['w2']
    x1o = Dm['x1o']
    xo = Dm['xo']
    if moe:
        rt = Dm['rt']
        sel = Dm['sel']
    with contextlib.ExitStack() as st:
        P.stack = st
        B = Bufs()
        ids = P.sb("ids", [128, 128], F32)
        mcol = P.sb("mcol", [128, 2, 4, 8], F32)
        bgs = P.sb("bgs", [128, 4, 8], F32)
        u2T = P.sb("u2T", [128, 8, NTOK], BF16)
        P.dma('sp', lambda e: e.dma_start(out=ids[:], in_=ident), writes=[B['ids']])
        if isinstance(modcols, tuple):
            P.dma('sp', lambda e: e.dma_start(out=mcol[:, :, 0:2, :], in_=modcols[0]), writes=[B['mcol']])
            P.dma('sp', lambda e: e.dma_start(out=mcol[:, :, 2:4, :], in_=modcols[1]), writes=[B['mcol']])
        else:
            P.dma('sp', lambda e: e.dma_start(out=mcol[:], in_=modcols), writes=[B['mcol']])
        P.dma('sp', lambda e: e.dma_start(out=bgs[:], in_=bgc), writes=[B['bgs']])
        P.op('dve', lambda e: e.tensor_scalar_add(out=mcol[:, :, 1, :], in0=mcol[:, :, 1, :], scalar1=1.0), reads=[B['mcol']], writes=[B['mcol']])
        P.op('dve', lambda e: e.tensor_scalar_add(out=mcol[:, :, 3, :], in0=mcol[:, :, 3, :], scalar1=1.0), reads=[B['mcol']], writes=[B['mcol']])
        if moe:
            rts = P.sb("rts", [128, 8, 8], F32)
            sels = P.sb("sels", [8, 8, 128], F32)
            wall = P.sb("wall", [128, NTILE, 8], F32)
            WT = P.sb("WT", [8, NTOK], F32)
            P.dma('sp', lambda e: e.dma_start(out=rts[:], in_=rt), writes=[B['rts']])
            P.dma('sp', lambda e: e.dma_start(out=sels[:], in_=sel), writes=[B['sels']])

        def ln_tile(r, stt, mv, rstd, gi, key):
            rb = B[key]
            for hf in range(2):
                P.op('dve', lambda e, hf=hf: e.bn_stats(out=stt[:, hf, :], in_=r[:, hf * 512:(hf + 1) * 512]), reads=[rb], writes=[B[key, 'st', hf]])
            P.op('dve', lambda e: e.bn_aggr(out=mv[:], in_=stt[:].rearrange("p a b -> p (a b)")), reads=[B[key, 'st', 0], B[key, 'st', 1]], writes=[B[key, 'mv']])
            P.op('dve', lambda e: e.tensor_scalar_add(out=rstd[:], in0=mv[:, 1:2], scalar1=1e-6), reads=[B[key, 'mv']], writes=[B[key, 'rstd']])
            P.op('act', lambda e: e.sqrt(out=rstd[:], in_=rstd[:]), reads=[B[key, 'rstd']], writes=[B[key, 'rstd']])
            P.op('dve', lambda e: e.reciprocal(out=rstd[:], in_=rstd[:]), reads=[B[key, 'rstd']], writes=[B[key, 'rstd']])
            P.op('dve', lambda e: e.tensor_scalar(out=r[:], in0=r[:], scalar1=mv[:, 0:1], scalar2=rstd[:, 0:1], op0=ALU.subtract, op1=ALU.mult),
                 reads=[rb, B[key, 'mv'], B[key, 'rstd']], writes=[rb])
            P.op('pool', lambda e: e.tensor_tensor(out=r[:], in0=r[:], in1=lnr[:, gi, :], op=ALU.mult), reads=[rb, B['lnr']], writes=[rb])
            P.op('pool', lambda e: e.tensor_tensor(out=r[:], in0=r[:], in1=lnr[:, gi + 1, :], op=ALU.add), reads=[rb, B['lnr']], writes=[rb])

        with contextlib.ExitStack() as s1:
            P.stack = s1
            mrow = P.sb("mrow", [128, 2, 2, D], F32)
            lnr = P.sb("lnr", [128, 4, D], F32)
            P.dma('sp', lambda e: e.dma_start(out=mrow[:], in_=modrows), writes=[B['mrow']])
            P.dma('sp', lambda e: e.dma_start(out=lnr[:], in_=lnrows), writes=[B['lnr']])
            xs = P.sb("xs", [128, 4, D], F32)
            uT = P.sb("uT", [128, 8, 512], BF16)
            yTbs = [P.sb(f"yTb{i}", [128, 16, 512], BF16) for i in range(2)]

            def load_yT(bi_):
                t0_, nt_ = BLOCKS[bi_]
                tk0, ntk = gt(t0_) * 128, nt_ * 128
                yb = yTbs[bi_ % 2]
                P.dma('pool', lambda e: e.dma_start(out=yb[:, :, 0:ntk], in_=yT[:, :, tk0:tk0 + ntk].rearrange("c p t -> p c t")),
                      writes=[B['yTb', bi_ % 2]])
            mT = P.sb("mT", [128, 8, 512], BF16)
            wgs = [P.sb(f"wgs{i}", [128, 4, 8, 128], BF16) for i in range(2)]
            wbs = [P.sb(f"wbs{i}", [128, 4, 4, 128], BF16) for i in range(2)]
            wos = P.sb("wos", [128, 8, D], BF16)
            sg = [P.sb(f"sg{i}", [128, 512], BF16) for i in range(2)]
            macc = P.sb("macc", [128, 512], F32)
            mtmp = P.sb("mtmp", [128, 512], F32)
            rr = [P.sb(f"rr{i}", [128, D], F32) for i in range(2)]
            t1 = P.sb("t1", [128, D], F32)
            stt = P.sb("stt", [128, 2, 6], F32)
            mv = P.sb("mv", [128, 2], F32)
            rstd = P.sb("rstd", [128, 1], F32)
            u2f = P.sb("u2f", [128, 8, 128], F32)
            pt = P.ps("pt", [128, 2, 512], F32)
            pg = P.ps("pg", [128, 2, 512], F32)
            pp = P.ps("pp", [128, 2, 512], F32)
            pmx = P.ps("pmx", [128, 2, 512], F32)
            if moe:
                lg = P.sb("lg", [128, 8], F32)
                m8 = P.sb("m8", [128, 8], F32)
                msk = P.sb("msk", [128, 8], F32)
                ex = P.sb("ex", [128, 8], F32)
                sm = P.sb("sm", [128, 4], F32)

            P.dma('sp' if Dm.get('wbf16') else 'pool', lambda e: e.dma_start(out=wos[:], in_=wo), writes=[B['wos']])
            wcnt = 0
            tcnt = 0
            for bi, (t0, nt) in enumerate(BLOCKS):
                ms = 1 if bi == 4 else 0
                ntok = nt * 128
                tok0 = gt(t0) * 128
                P.dma('sp', lambda e, t0=t0, nt=nt: e.dma_start(out=xs[:, 0:nt, :], in_=x[gt(t0):gt(t0) + nt].rearrange("t p f -> p t f")),
                      writes=[B['xs', j] for j in range(nt)])
                if bi == 0:
                    load_yT(0)
                if bi + 1 < len(BLOCKS):
                    load_yT(bi + 1)
                yTb = yTbs[bi % 2]
                for j in range(nt):
                    for f in range(8):
                        pb = B['pt', tcnt % 2]
                        P.op('pe', lambda e, j=j, f=f, s=tcnt % 2: e.transpose(pt[:, s, 0:128], xs[:, j, f * 128:(f + 1) * 128], ids[:]),
                             reads=[B['xs', j], B['ids']], writes=[pb])
                        P.op('act', lambda e, j=j, f=f, s=tcnt % 2, ms=ms: e.activation(out=uT[:, f, j * 128:(j + 1) * 128], in_=pt[:, s, 0:128], func=AF.Identity,
                                                                                      bias=mcol[:, ms, 0, f:f + 1], scale=mcol[:, ms, 1, f:f + 1]),
                             reads=[pb, B['mcol']], writes=[B['uT', f]])
                        tcnt += 1
                for fo in range(8):
                    ws_ = wcnt % 2
                    wq = 'sp' if Dm.get('wbf16') else 'pool'
                    P.dma(wq, lambda e, fo=fo, ws_=ws_: e.dma_start(out=wgs[ws_][:], in_=wg[fo]), writes=[B['wgs', ws_]])
                    P.dma(wq, lambda e, fo=fo, ws_=ws_: e.dma_start(out=wbs[ws_][:], in_=wb[fo]), writes=[B['wbs', ws_]])
                    wcnt += 1
                    for br in range(4):
                        s = br % 2
                        for kt in range(8):
                            P.op('pe', lambda e, br=br, kt=kt, s=s, ws_=ws_, ntok=ntok: e.matmul(pg[:, s, 0:ntok], lhsT=wgs[ws_][:, br, kt, :], rhs=uT[:, kt, 0:ntok],
                                                                                               start=(kt == 0), stop=(kt == 7)),
                                 reads=[B['wgs', ws_], B['uT', kt]], writes=[B['pg', s]])
                        for kt in range(4):
                            P.op('pe', lambda e, br=br, kt=kt, s=s, ws_=ws_, ntok=ntok, yTb=yTb: e.matmul(pp[:, s, 0:ntok], lhsT=wbs[ws_][:, br, kt, :], rhs=yTb[:, br * 4 + kt, 0:ntok],
                                                                                               start=(kt == 0), stop=(kt == 3)),
                                 reads=[B['wbs', ws_], B['yTb', bi % 2]], writes=[B['pp', s]])
                        P.op('act', lambda e, br=br, fo=fo, s=s, ntok=ntok: e.activation(out=sg[s][:, 0:ntok], in_=pg[:, s, 0:ntok], func=AF.Sigmoid,
                                                                                       bias=bgs[:, br, fo:fo + 1], scale=1.0),
                             reads=[B['pg', s], B['bgs']], writes=[B['sg', s]])
                        if br == 0:
                            P.op('dve', lambda e, s=s, ntok=ntok: e.tensor_tensor(out=macc[:, 0:ntok], in0=sg[s][:, 0:ntok], in1=pp[:, s, 0:ntok], op=ALU.mult),
                                 reads=[B['sg', s], B['pp', s]], writes=[B['macc']])
                        else:
                            P.op('dve', lambda e, s=s, ntok=ntok: e.tensor_tensor(out=mtmp[:, 0:ntok], in0=sg[s][:, 0:ntok], in1=pp[:, s, 0:ntok], op=ALU.mult),
                                 reads=[B['sg', s], B['pp', s]], writes=[B['mtmp']])
                            if br < 3:
                                P.op('pool', lambda e, ntok=ntok: e.tensor_tensor(out=macc[:, 0:ntok], in0=macc[:, 0:ntok], in1=mtmp[:, 0:ntok], op=ALU.add),
                                     reads=[B['macc'], B['mtmp']], writes=[B['macc']])
                            else:
                                P.op('pool', lambda e, fo=fo, ntok=ntok: e.tensor_tensor(out=mT[:, fo, 0:ntok], in0=macc[:, 0:ntok], in1=mtmp[:, 0:ntok], op=ALU.add),
                                     reads=[B['macc'], B['mtmp']], writes=[B['mT', fo]])
                for j in range(nt):
                    tile = t0 + j
                    r = rr[tile % 2]
                    rk = ('rr', tile % 2)
                    for hf in range(2):
                        for kt in range(8):
                            P.op('pe', lambda e, j=j, hf=hf, kt=kt: e.matmul(pmx[:, hf, :], lhsT=mT[:, kt, j * 128:(j + 1) * 128], rhs=wos[:, kt, hf * 512:(hf + 1) * 512],
                                                                           start=(kt == 0), stop=(kt == 7)),
                                 reads=[B['mT', kt], B['wos']], writes=[B['pmx', hf]])
                        P.op('dve', lambda e, hf=hf, ms=ms: e.tensor_tensor(out=t1[:, hf * 512:(hf + 1) * 512], in0=pmx[:, hf, :], in1=mrow[:, ms, 0, hf * 512:(hf + 1) * 512], op=ALU.mult),
                             reads=[B['pmx', hf], B['mrow']], writes=[B['t1', hf]])
                    P.op('dve', lambda e, j=j, r=r: e.scalar_tensor_tensor(out=r[:], in0=xs[:, j, :], scalar=ALPHA, in1=t1[:], op0=ALU.mult, op1=ALU.add),
                         reads=[B['xs', j], B['t1', 0], B['t1', 1]], writes=[B[rk]])
                    ln_tile(r, stt, mv, rstd, 0, rk)
                    P.dma('sp', lambda e, tile=tile, r=r: e.dma_start(out=x1o[gt(tile)], in_=r[:]), reads=[B[rk]], writes=[B['x1o', tile]])
                    for f in range(8):
                        pb = B['pt', tcnt % 2]
                        P.op('pe', lambda e, r=r, f=f, s=tcnt % 2: e.transpose(pt[:, s, 0:128], r[:, f * 128:(f + 1) * 128], ids[:]),
                             reads=[B[rk], B['ids']], writes=[pb])
                        if moe:
                            P.op('act', lambda e, f=f, s=tcnt % 2, ms=ms: e.activation(out=u2f[:, f, :], in_=pt[:, s, 0:128], func=AF.Identity,
                                                                                     bias=mcol[:, ms, 2, f:f + 1], scale=mcol[:, ms, 3, f:f + 1]),
                                 reads=[pb, B['mcol']], writes=[B['u2f', f]])
                            P.op('dve', lambda e, f=f, tile=tile: e.tensor_copy(out=u2T[:, f, tile * 128:(tile + 1) * 128], in_=u2f[:, f, :]),
                                 reads=[B['u2f', f]], writes=[B['u2T', f, tile]])
                        else:
                            P.op('act', lambda e, f=f, s=tcnt % 2, ms=ms, tile=tile: e.activation(out=u2T[:, f, tile * 128:(tile + 1) * 128], in_=pt[:, s, 0:128], func=AF.Identity,
                                                                                                bias=mcol[:, ms, 2, f:f + 1], scale=mcol[:, ms, 3, f:f + 1]),
                                 reads=[pb, B['mcol']], writes=[B['u2T', f, tile]])
                        tcnt += 1
                    if moe:
                        for kt in range(8):
                            P.op('pe', lambda e, kt=kt: e.matmul(pg[:, 0, 0:8], lhsT=u2f[:, kt, :], rhs=rts[:, kt, :], start=(kt == 0), stop=(kt == 7)),
                                 reads=[B['u2f', kt], B['rts']], writes=[B['pg', 0]])
                        P.op('dve', lambda e: e.tensor_copy(out=lg[:], in_=pg[:, 0, 0:8]), reads=[B['pg', 0]], writes=[B['lg']])
                        P.op('dve', lambda e: e.max(out=m8[:], in_=lg[:]), reads=[B['lg']], writes=[B['m8']])
                        P.op('dve', lambda e: e.tensor_scalar(out=msk[:], in0=lg[:], scalar1=m8[:, 1:2], scalar2=None, op0=ALU.is_ge), reads=[B['lg'], B['m8']], writes=[B['msk']])
                        P.op('dve', lambda e: e.tensor_scalar(out=sm[:, 0:1], in0=m8[:, 0:1], scalar1=-1.0, scalar2=None, op0=ALU.mult), reads=[B['m8']], writes=[B['sm', 0]])
                        P.op('dve', lambda e: e.tensor_tensor(out=sm[:, 1:2], in0=m8[:, 1:2], in1=m8[:, 0:1], op=ALU.subtract), reads=[B['m8']], writes=[B['sm', 1]])
                        P.op('act', lambda e: e.activation(out=ex[:], in_=lg[:], func=AF.Exp, bias=sm[:, 0:1], scale=1.0), reads=[B['lg'], B['sm', 0]], writes=[B['ex']])
                        P.op('act', lambda e: e.activation(out=sm[:, 2:3], in_=sm[:, 1:2], func=AF.Exp), reads=[B['sm', 1]], writes=[B['sm', 2]])
                        P.op('dve', lambda e: e.tensor_scalar_add(out=sm[:, 2:3], in0=sm[:, 2:3], scalar1=1.0), reads=[B['sm', 2]], writes=[B['sm', 2]])
                        P.op('dve', lambda e: e.reciprocal(out=sm[:, 3:4], in_=sm[:, 2:3]), reads=[B['sm', 2]], writes=[B['sm', 3]])
                        P.op('dve', lambda e: e.tensor_tensor(out=ex[:], in0=ex[:], in1=msk[:], op=ALU.mult), reads=[B['ex'], B['msk']], writes=[B['ex']])
                        P.op('dve', lambda e, tile=tile: e.tensor_scalar(out=wall[:, tile, :], in0=ex[:], scalar1=sm[:, 3:4], scalar2=None, op0=ALU.mult),
                             reads=[B['ex'], B['sm', 3]], writes=[B['wall', tile]])
                        P.op('pe', lambda e, tile=tile: e.transpose(pp[0:8, 0, 0:128], wall[:, tile, :], ids[:]), reads=[B['wall', tile], B['ids']], writes=[B['pp', 0]])
                        P.op('act', lambda e, tile=tile: e.copy(out=WT[:, tile * 128:(tile + 1) * 128], in_=pp[0:8, 0, 0:128]), reads=[B['pp', 0]], writes=[B['WT']])
            P.flush()
        s2o = st.enter_context(contextlib.ExitStack())
        P.stack = s2o
        facc = P.sb("facc", [128, NTILE, D], F32)
        pg1 = P.ps("pg1", [128, 2, 512], F32)
        pg3 = P.ps("pg3", [128, 2, 512], F32)
        pf = P.ps("pf", [128, 2, 512], F32)
        pw = P.ps("pw", [128, 512], F32)
        with contextlib.ExitStack() as s2:
            P.stack = s2
            w1s = [P.sb(f"w1s{i}", [128, 8, KC * 128], BF16) for i in range(2)]
            w3s = [P.sb(f"w3s{i}", [128, 8, KC * 128], BF16) for i in range(2)]
            w2s = [P.sb(f"w2s{i}", [128, KC, D], BF16) for i in range(2)]
            s1b = [P.sb(f"s1b{i}", [128, 512], F32) for i in range(2)]
            hT = [P.sb(f"hT{i}", [128, KC, 512], BF16) for i in range(2)]
            gcnt = 0
            fcnt = 0
            hcnt = 0
            for ci in range(NCH):
                wsl = ci % 2
                ex_ = ci // CPE
                P.dma('pool', lambda e, ci=ci, wsl=wsl: e.dma_start(out=w1s[wsl][:], in_=w1[ci]), writes=[B['w1s', wsl]])
                P.dma('pool', lambda e, ci=ci, wsl=wsl: e.dma_start(out=w3s[wsl][:], in_=w3[ci]), writes=[B['w3s', wsl]])
                P.dma('pool', lambda e, ci=ci, wsl=wsl: e.dma_start(out=w2s[wsl][:], in_=w2[ci]), writes=[B['w2s', wsl]])
                for bi, (t0, nt) in enumerate(BLOCKS):
                    ntok = nt * 128
                    tok0 = t0 * 128
                    hs = hcnt % 2
                    hcnt += 1
                    if moe:
                        P.op('pe', lambda e, ex_=ex_, tok0=tok0, ntok=ntok: e.matmul(pw[:, 0:ntok], lhsT=sels[:, ex_, :], rhs=WT[:, tok0:tok0 + ntok], start=True, stop=True),
                             reads=[B['sels'], B['WT']], writes=[B['pw']])
                    for kk in range(KC):
                        g = gcnt % 2
                        gcnt += 1
                        for kt in range(8):
                            P.op('pe', lambda e, kk=kk, kt=kt, g=g, wsl=wsl, tok0=tok0, ntok=ntok: e.matmul(pg1[:, g, 0:ntok], lhsT=w1s[wsl][:, kt, kk * 128:(kk + 1) * 128],
                                                                                                       rhs=u2T[:, kt, tok0:tok0 + ntok], start=(kt == 0), stop=(kt == 7)),
                                 reads=[B['w1s', wsl], B['u2T']], writes=[B['pg1', g]])
                        for kt in range(8):
                            P.op('pe', lambda e, kk=kk, kt=kt, g=g, wsl=wsl, tok0=tok0, ntok=ntok: e.matmul(pg3[:, g, 0:ntok], lhsT=w3s[wsl][:, kt, kk * 128:(kk + 1) * 128],
                                                                                                       rhs=u2T[:, kt, tok0:tok0 + ntok], start=(kt == 0), stop=(kt == 7)),
                                 reads=[B['w3s', wsl], B['u2T']], writes=[B['pg3', g]])
                        P.op('act', lambda e, g=g, ntok=ntok: e.activation(out=s1b[g][:, 0:ntok], in_=pg1[:, g, 0:ntok], func=AF.Silu), reads=[B['pg1', g]], writes=[B['s1b', g]])
                        if moe:
                            P.op('dve', lambda e, g=g, ntok=ntok: e.tensor_tensor(out=s1b[g][:, 0:ntok], in0=s1b[g][:, 0:ntok], in1=pg3[:, g, 0:ntok], op=ALU.mult),
                                 reads=[B['s1b', g], B['pg3', g]], writes=[B['s1b', g]])
                            P.op('dve', lambda e, g=g, kk=kk, hs=hs, ntok=ntok: e.tensor_tensor(out=hT[hs][:, kk, 0:ntok], in0=s1b[g][:, 0:ntok], in1=pw[:, 0:ntok], op=ALU.mult),
                                 reads=[B['s1b', g], B['pw']], writes=[B['hT', hs, kk]])
                        else:
                            P.op('dve', lambda e, g=g, kk=kk, hs=hs, ntok=ntok: e.tensor_tensor(out=hT[hs][:, kk, 0:ntok], in0=s1b[g][:, 0:ntok], in1=pg3[:, g, 0:ntok], op=ALU.mult),
                                 reads=[B['s1b', g], B['pg3', g]], writes=[B['hT', hs, kk]])
                    for j in range(nt):
                        tile = t0 + j
                        for hf in range(2):
                            fs = fcnt % 2
                            fcnt += 1
                            for kk in range(KC):
                                P.op('pe', lambda e, j=j, hf=hf, kk=kk, fs=fs, hs=hs, wsl=wsl: e.matmul(pf[:, fs, :], lhsT=hT[hs][:, kk, j * 128:(j + 1) * 128],
                                                                                                    rhs=w2s[wsl][:, kk, hf * 512:(hf + 1) * 512], start=(kk == 0), stop=(kk == KC - 1)),
                                     reads=[B['hT', hs, kk], B['w2s', wsl]], writes=[B['pf', fs]])
                            fb = B['facc', tile, hf]
                            if ci == 0:
                                P.op('act', lambda e, tile=tile, hf=hf, fs=fs: e.copy(out=facc[:, tile, hf * 512:(hf + 1) * 512], in_=pf[:, fs, :]), reads=[B['pf', fs]], writes=[fb])
                            else:
                                P.op('dve', lambda e, tile=tile, hf=hf, fs=fs: e.tensor_tensor(out=facc[:, tile, hf * 512:(hf + 1) * 512], in0=facc[:, tile, hf * 512:(hf + 1) * 512],
                                                                                              in1=pf[:, fs, :], op=ALU.add), reads=[B['pf', fs], fb], writes=[fb])
            P.flush()
        with contextlib.ExitStack() as s3:
            P.stack = s3
            mrow = P.sb("mrow3", [128, 2, 2, D], F32)
            lnr = P.sb("lnr3", [128, 4, D], F32)
            P.dma('sp', lambda e: e.dma_start(out=mrow[:], in_=modrows), writes=[B['mrow']])
            P.dma('sp', lambda e: e.dma_start(out=lnr[:], in_=lnrows), writes=[B['lnr']])
            xr = [P.sb(f"xr{i}", [128, D], F32) for i in range(2)]
            t1 = P.sb("t1b", [128, D], F32)
            stt = P.sb("stt2", [128, 2, 6], F32)
            mv = P.sb("mv2", [128, 2], F32)
            rstd = P.sb("rstd2", [128, 1], F32)
            for tile in range(NTILE):
                ms = 1 if tile == 16 else 0
                r = xr[tile % 2]
                rk = ('xr', tile % 2)
                P.dma('sp', lambda e, tile=tile, r=r: e.dma_start(out=r[:], in_=x1o[gt(tile)]), reads=[B['x1o', tile]], writes=[B[rk]])
                P.op('dve', lambda e, tile=tile, ms=ms: e.tensor_tensor(out=t1[:], in0=facc[:, tile, :], in1=mrow[:, ms, 1, :], op=ALU.mult),
                     reads=[B['facc', tile, 0], B['facc', tile, 1], B['mrow']], writes=[B['t1b']])
                P.op('dve', lambda e, r=r: e.scalar_tensor_tensor(out=r[:], in0=r[:], scalar=ALPHA, in1=t1[:], op0=ALU.mult, op1=ALU.add),
                     reads=[B[rk], B['t1b']], writes=[B[rk]])
                ln_tile(r, stt, mv, rstd, 2, rk)
                P.dma('sp', lambda e, tile=tile, r=r: e.dma_start(out=xo[gt(tile)], in_=r[:]), reads=[B[rk]], is_output=True)
            P.flush()


def build_B(moe):
    KC = 4 if moe else 2
    NCH = 56 if moe else 11
    CPE = 7 if moe else 11
    nc = bass.Bass("TRN2", target_bir_lowering=False)
    x = _din(nc, "x", [NTILE, 128, D])
    yT = _din(nc, "yT", [16, 128, NTOK])
    modcols = _din(nc, "modcols", [128, 2, 4, 8])
    modrows = _din(nc, "modrows", [128, 2, 2, D])
    lnrows = _din(nc, "lnrows", [128, 4, D])
    bgc = _din(nc, "bgc", [128, 4, 8])
    ident = _din(nc, "ident", [128, 128])
    wg = _din(nc, "wg", [8, 128, 4, 8, 128])
    wb = _din(nc, "wb", [8, 128, 4, 4, 128])
    wo = _din(nc, "wo", [128, 8, D])
    w1 = _din(nc, "w1", [NCH, 128, 8, KC * 128])
    w3 = _din(nc, "w3", [NCH, 128, 8, KC * 128])
    w2 = _din(nc, "w2", [NCH, 128, KC, D])
    if moe:
        rt = _din(nc, "rt", [128, 8, 8])
        sel = _din(nc, "sel", [8, 8, 128])
    x1o = _dout(nc, "x1o", [NTILE, 128, D])
    xo = _dout(nc, "xo", [NTILE, 128, D])

    Dm = dict(x=x, yT=yT, modcols=modcols, modrows=modrows, lnrows=lnrows, bgc=bgc, ident=ident, wg=wg, wb=wb, wo=wo, w1=w1, w3=w3, w2=w2, x1o=x1o, xo=xo)
    if moe:
        Dm['rt'] = rt
        Dm['sel'] = sel
    with contextlib.ExitStack() as st0:
        P = Prog(nc, st0)
        emit_B(nc, P, Dm, moe)
    return nc


_CACHE = {}


def _prog(name, fn, *a):
    k = (name,) + a
    if k not in _CACHE:
        _CACHE[k] = fn(*a)
    return _CACHE[k]


def _tile_rows(w):
    K, N = w.shape
    return np.ascontiguousarray(w.reshape(K // 128, 128, N).transpose(1, 0, 2))


def _cols(v):
    return np.ascontiguousarray(v.reshape(-1, 128).T)


def _rep(v):
    return np.ascontiguousarray(np.broadcast_to(v[None], (128,) + v.shape))


_IDENT = np.eye(128, dtype=np.float32)


def run_M(c, c_ctx, w_mod, b_mod):
    cc = np.concatenate([c, c_ctx[None]], 0)
    cT = np.ascontiguousarray(cc.T.reshape(8, 128, 5).transpose(1, 0, 2))
    Wm = w_mod.transpose(1, 0, 2).reshape(1024, 4 * 6144)
    bm = b_mod.reshape(4 * 6144)
    ins = []
    for i in range(8):
        sl = slice(i * 3072, (i + 1) * 3072)
        ins.append(dict(cT=cT, w=_tile_rows(Wm[:, sl]), b=np.ascontiguousarray(np.broadcast_to(bm[None, sl], (5, 3072)))))
    res = run_bass_kernel_spmd(_prog('M', build_M), ins, core_ids=list(range(8)))
    mod = np.concatenate([res.results[i]['o'] for i in range(8)], axis=1)
    return mod.reshape(5, 4, 6144)


def prep_B_weights(l, inp):
    moe = (l % 2 == 1)
    i = l // 2
    d = {}
    d['wg'] = np.ascontiguousarray(inp['w_gate'][l].reshape(4, 8, 128, 8, 128).transpose(3, 2, 0, 1, 4))
    d['wb'] = np.ascontiguousarray(inp['w_branch'][l].reshape(4, 4, 128, 8, 128).transpose(3, 2, 0, 1, 4))
    d['wo'] = _tile_rows(inp['w_o'][l])
    d['lnrows'] = _rep(np.stack([inp['ln_g'][l, 0], inp['ln_b'][l, 0], inp['ln_g'][l, 1], inp['ln_b'][l, 1]], 0))
    d['bgc'] = np.ascontiguousarray(inp['b_gate'][l].reshape(4, 8, 128).transpose(2, 0, 1))
    d['ident'] = _IDENT
    if not moe:
        KC, NCH = 2, 11
        d['w1'] = np.ascontiguousarray(inp['ffn_w1'][i].reshape(8, 128, NCH, KC * 128).transpose(2, 1, 0, 3))
        d['w3'] = np.ascontiguousarray(inp['ffn_w3'][i].reshape(8, 128, NCH, KC * 128).transpose(2, 1, 0, 3))
        d['w2'] = np.ascontiguousarray(inp['ffn_w2'][i].reshape(NCH, KC, 128, 1024).transpose(0, 2, 1, 3))
    else:
        KC, CPE = 4, 7
        d['w1'] = np.ascontiguousarray(inp['moe_w1'][i].reshape(8, 8, 128, CPE, KC * 128).transpose(0, 3, 2, 1, 4)).reshape(56, 128, 8, KC * 128)
        d['w3'] = np.ascontiguousarray(inp['moe_w3'][i].reshape(8, 8, 128, CPE, KC * 128).transpose(0, 3, 2, 1, 4)).reshape(56, 128, 8, KC * 128)
        d['w2'] = np.ascontiguousarray(inp['moe_w2'][i].reshape(8, CPE, KC, 128, 1024).transpose(0, 1, 3, 2, 4)).reshape(56, 128, KC, 1024)
        d['rt'] = _tile_rows(inp['router'][i])
        sel = np.zeros((8, 8, 128), np.float32)
        for e in range(8):
            sel[e, e, :] = 1.0
        d['sel'] = sel
    return d


def run_B(l, x_cur, h_cur, YT, mod, inp):
    moe = (l % 2 == 1)
    wd = prep_B_weights(l, inp)
    ins = []
    for b in range(4):
        for half in range(2):
            d = dict(wd)
            xt = np.concatenate([x_cur[b, half * 2048:(half + 1) * 2048], h_cur[b, half * 128:(half + 1) * 128]], 0)
            d['x'] = np.ascontiguousarray(xt.reshape(17, 128, 1024))
            toks = np.concatenate([np.arange(half * 2048, (half + 1) * 2048), 4096 + np.arange(half * 128, (half + 1) * 128)])
            d['yT'] = np.ascontiguousarray(YT[b][:, toks].reshape(16, 128, NTOK))
            ms = [mod[b], mod[4]]
            d['modcols'] = np.ascontiguousarray(np.stack([np.stack([_cols(m[k * 1024:(k + 1) * 1024]) for k in (0, 1, 3, 4)], 1) for m in ms], 1))
            d['modrows'] = _rep(np.stack([np.stack([m[2048:3072], m[5120:6144]], 0) for m in ms], 0))
            ins.append(d)
    res = run_bass_kernel_spmd(_prog('B', build_B, moe), ins, core_ids=list(range(8)))
    x_new = np.empty_like(x_cur)
    h_new = np.empty_like(h_cur)
    x1 = np.empty_like(x_cur)
    for b in range(4):
        for half in range(2):
            o = res.results[b * 2 + half]['xo'].reshape(NTOK, 1024)
            x_new[b, half * 2048:(half + 1) * 2048] = o[:2048]
            h_new[b, half * 128:(half + 1) * 128] = o[2048:]
            x1[b, half * 2048:(half + 1) * 2048] = res.results[b * 2 + half]['x1o'].reshape(NTOK, 1024)[:2048]
    return x_new, h_new, x1


NT_A = 34
NFB = 6
NTOK_A = NT_A * 128
TBLK_A = [(i * 512, 512) for i in range(8)] + [(4096, 256)]
PI = float(np.pi)


def emit_A(nc, P, Dm):
    x = Dm['x']
    modcols = Dm['modcols']
    ident = Dm['ident']
    wA = Dm['wA']
    wR = Dm['wR']
    wH = Dm['wH']
    wD = Dm['wD']
    caw = Dm['caw']
    hcw = Dm['hcw']
    hsk = Dm['hsk']
    ropec = Dm['ropec']
    ropes = Dm['ropes']
    qkn = Dm['qkn']
    rdec = Dm['rdec']
    rtab = Dm['rtab']
    rcol = Dm['rcol']
    featsT = Dm['featsT']
    featsTc = Dm['featsTc']
    hw1 = Dm['hw1']
    hw2 = Dm['hw2']
    hcols = Dm['hcols']
    hw3 = Dm['hw3']
    negt = Dm['negt']
    adel = Dm['adel']
    Fm = Dm['Fm']
    Gm = Dm['Gm']
    Fc = Dm['Fc']
    Gc = Dm['Gc']
    YT = Dm['YT']
    with contextlib.ExitStack() as st:
        P.stack = st
        B = Bufs()
        ids = P.sb("ids", [128, 128], F32)
        idb = P.sb("idb", [128, 128], BF16)
        mcol = P.sb("mcol", [128, 2, 2, 8], F32)
        P.dma('sp', lambda e: e.dma_start(out=ids[:], in_=ident), writes=[B['ids']])
        P.dma('sp', lambda e: e.dma_start(out=mcol[:], in_=modcols), writes=[B['mcol']])
        P.op('dve', lambda e: e.tensor_copy(out=idb[:], in_=ids[:]), reads=[B['ids']], writes=[B['idb']])
        P.op('dve', lambda e: e.tensor_scalar_add(out=mcol[:, :, 1, :], in0=mcol[:, :, 1, :], scalar1=1.0), reads=[B['mcol']], writes=[B['mcol']])
        pt = P.ps("pt", [128, 2, 512], F32)
        ptb = P.ps("ptb", [128, 1024], BF16)
        pa = P.ps("pa", [128, 2, 512], F32)
        pb_ = P.ps("pb", [128, 2, 512], F32)
        pc = P.ps("pc", [128, 512], F32)
        cnt = {'t': 0}

        uTs = Dm.get('uTs')
        ustate = Dm.get('ustate', {'have': False})

        def build_uT(uT, stk):
            P.stack = stk
            if uTs is not None and ustate['have']:
                for f in range(8):
                    P.dma('sp', lambda e, f=f: e.dma_start(out=uT[:, f, :], in_=uTs[:, f, :]), writes=[B['uT']])
                return
            xb = [P.sb(f"xb{i}", [128, D], F32) for i in range(2)]
            for t in range(NT_A):
                ms = 1 if t >= 32 else 0
                xs = xb[t % 2]
                P.dma('sp', lambda e, t=t, xs=xs: e.dma_start(out=xs[:], in_=x[t]), writes=[B['xb', t % 2]])
                for f in range(8):
                    s = cnt['t'] % 2
                    cnt['t'] += 1
                    P.op('pe', lambda e, xs=xs, f=f, s=s: e.transpose(pt[:, s, 0:128], xs[:, f * 128:(f + 1) * 128], ids[:]),
                         reads=[B['xb', t % 2], B['ids']], writes=[B['pt', s]])
                    P.op('act', lambda e, t=t, f=f, s=s, ms=ms: e.activation(out=uT[:, f, t * 128:(t + 1) * 128], in_=pt[:, s, 0:128], func=AF.Identity,
                                                                           bias=mcol[:, ms, 0, f:f + 1], scale=mcol[:, ms, 1, f:f + 1]),
                         reads=[B['pt', s], B['mcol']], writes=[B['uT']])
            if uTs is not None:
                for f in range(8):
                    P.dma('sp', lambda e, f=f: e.dma_start(out=uTs[:, f, :], in_=uT[:, f, :]), reads=[B['uT']], writes=[B['uTs']])
                ustate['have'] = True

        def inproj_fm(uT, wsb, wkey, col0, dst, dkey):
            for bi, (tok0, ntok) in enumerate(TBLK_A):
                s = bi % 2
                for kt in range(8):
                    P.op('pe', lambda e, kt=kt, s=s, tok0=tok0, ntok=ntok: e.matmul(pa[:, s, 0:ntok], lhsT=wsb[:, kt, col0:col0 + 128], rhs=uT[:, kt, tok0:tok0 + ntok],
                                                                                  start=(kt == 0), stop=(kt == 7)),
                         reads=[B[wkey], B['uT']], writes=[B['pa', s]])
                eng = 'act' if bi % 2 == 0 else 'dve'
                if eng == 'act':
                    P.op('act', lambda e, s=s, tok0=tok0, ntok=ntok: e.copy(out=dst[:, tok0:tok0 + ntok], in_=pa[:, s, 0:ntok]), reads=[B['pa', s]], writes=[B[dkey]])
                else:
                    P.op('dve', lambda e, s=s, tok0=tok0, ntok=ntok: e.tensor_copy(out=dst[:, tok0:tok0 + ntok], in_=pa[:, s, 0:ntok]), reads=[B['pa', s]], writes=[B[dkey]])

        def dwconv(z, zkey, wc, wkey, acc, akey, out, okey):
            P.op('dve', lambda e: e.tensor_scalar(out=acc[:], in0=z, scalar1=wc[:, 1:2], scalar2=wc[:, 3:4], op0=ALU.mult, op1=ALU.add),
                 reads=[B[zkey], B[wkey]], writes=[B[akey]])
            for (a, n) in ((0, 4096), (4096, 256)):
                P.op('dve', lambda e, a=a, n=n: e.scalar_tensor_tensor(out=acc[:, a + 1:a + n], in0=z[:, a:a + n - 1], scalar=wc[:, 0:1], in1=acc[:, a + 1:a + n],
                                                                      op0=ALU.mult, op1=ALU.add), reads=[B[zkey], B[wkey], B[akey]], writes=[B[akey]])
                P.op('dve', lambda e, a=a, n=n: e.scalar_tensor_tensor(out=acc[:, a:a + n - 1], in0=z[:, a + 1:a + n], scalar=wc[:, 2:3], in1=acc[:, a:a + n - 1],
                                                                      op0=ALU.mult, op1=ALU.add), reads=[B[zkey], B[wkey], B[akey]], writes=[B[akey]])
            if out is not None:
                P.op('pool', lambda e: e.tensor_copy(out=out, in_=acc[:]), reads=[B[akey]], writes=[B[okey]])

        import os as _os
        _dbg = _os.environ.get('A_DBG', '').split(',')
        P.mute = ('noH' in _dbg)
        sH = st.enter_context(contextlib.ExitStack())
        P.stack = sH
        zH = P.sb("zH", [128, 6, NTOK_A], BF16)
        with contextlib.ExitStack() as s0:
            P.stack = s0
            uT = P.sb("uT", [128, 8, NTOK_A], BF16)
            wHs = P.sb("wHs", [128, 8, 768], BF16)
            P.dma('pool', lambda e: e.dma_start(out=wHs[:], in_=wH), writes=[B['wHs']])
            build_uT(uT, s0)
            for c in range(6):
                inproj_fm(uT, wHs, 'wHs', c * 128, zH[:, c, :], ('zH', c))
            P.flush()
        P.stack = sH
        hsks = P.sb("hsks", [128, 2, 2], F32)
        P.dma('sp', lambda e: e.dma_start(out=hsks[:], in_=hsk), writes=[B['hsks']])
        with contextlib.ExitStack() as s1:
            P.stack = s1
            hcws = P.sb("hcws", [128, 6, 4], F32)
            acc = P.sb("acc", [128, NTOK_A], F32)
            P.dma('sp', lambda e: e.dma_start(out=hcws[:], in_=hcw), writes=[B['hcws']])
            for c in range(6):
                dwconv(zH[:, c, :], ('zH', c), hcws[:, c, :], 'hcws', acc, 'acc', zH[:, c, :], ('zH', c))
            P.flush()
        P.stack = sH
        HTc = P.sb("HTc", [128, 2, 512], BF16)
        Fcs = P.sb("Fcs", [128, 2, 512], BF16)
        Gcs = P.sb("Gcs", [128, 4, 256], BF16)
        P.dma('sp', lambda e: e.dma_start(out=Fcs[:], in_=Fc), writes=[B['Fcs']])
        P.dma('sp', lambda e: e.dma_start(out=Gcs[:], in_=Gc), writes=[B['Gcs']])
        Ft = [P.sb(f"Ft{i}", [128, 1024], BF16) for i in range(NFB)]
        fcnt = {'n': 0}
        h2T = P.sb("h2T", [64, 4352], F32)
        hcs = P.sb("hcs", [64, 6], F32)
        w3s = P.sb("w3s", [64, 2, 3, 256], F32)
        negts = P.sb("negts", [128, 34], F32)
        adels = P.sb("adels", [128, 2, 3, 256], F32)
        npi = P.sb("npi", [64, 1], F32)
        P.dma('sp', lambda e: e.dma_start(out=hcs[:, 0:4], in_=hcols), writes=[B['hcs']])
        P.dma('sp', lambda e: e.dma_start(out=w3s[:, :, 0:2, :], in_=hw3), writes=[B['w3s']])
        P.dma('sp', lambda e: e.dma_start(out=negts[:], in_=negt), writes=[B['negts']])
        P.dma('sp', lambda e: e.dma_start(out=adels[:, :, 0:2, :], in_=adel), writes=[B['adels']])
        P.op('dve', lambda e: e.tensor_scalar(out=w3s[:, :, 2, :], in0=w3s[:, :, 1, :], scalar1=-1.0, scalar2=None, op0=ALU.mult), reads=[B['w3s']], writes=[B['w3s']])
        P.op('dve', lambda e: e.tensor_copy(out=adels[:, :, 2, :], in_=adels[:, :, 1, :]), reads=[B['adels']], writes=[B['adels']])
        P.op('dve', lambda e: e.tensor_tensor(out=hcs[:, 4:6], in0=hcs[:, 0:2], in1=hcs[:, 2:4], op=ALU.mult), reads=[B['hcs']], writes=[B['hcs']])
        P.op('dve', lambda e: e.memset(npi[:], -PI), writes=[B['npi']])

        def sin_layer(src, skey, wmat, wkey2, kdim, li, dst, dkey2, argb):
            for bi, (tok0, ntok) in enumerate(TBLK_A):
                s = bi % 2
                P.op('pe', lambda e, s=s, tok0=tok0, ntok=ntok: e.matmul(pa[0:64, s, 0:ntok], lhsT=wmat[0:kdim, :], rhs=src[0:kdim, tok0:tok0 + ntok], start=True, stop=True),
                     reads=[B[skey], B[wkey2]], writes=[B['pa', s]])
                ab = argb[s]
                P.op('dve', lambda e, s=s, ntok=ntok, ab=ab: e.tensor_scalar(out=ab[:, 0:ntok], in0=pa[0:64, s, 0:ntok], scalar1=hcs[:, 2 + li:3 + li], scalar2=hcs[:, 4 + li:5 + li],
                                                                            op0=ALU.mult, op1=ALU.add), reads=[B['pa', s], B['hcs']], writes=[B['argb', s]])
                m1 = argb[2 + s]
                P.op('dve', lambda e, ntok=ntok, ab=ab, m1=m1: e.tensor_scalar(out=m1[:, 0:ntok], in0=ab[:, 0:ntok], scalar1=PI, scalar2=None, op0=ALU.is_gt),
                     reads=[B['argb', s]], writes=[B['argm', s]])
                P.op('dve', lambda e, ntok=ntok, ab=ab, m1=m1: e.scalar_tensor_tensor(out=ab[:, 0:ntok], in0=m1[:, 0:ntok], scalar=-2.0 * PI, in1=ab[:, 0:ntok], op0=ALU.mult, op1=ALU.add),
                     reads=[B['argb', s], B['argm', s]], writes=[B['argb', s]])
                P.op('dve', lambda e, ntok=ntok, ab=ab, m1=m1: e.tensor_scalar(out=m1[:, 0:ntok], in0=ab[:, 0:ntok], scalar1=-PI, scalar2=None, op0=ALU.is_lt),
                     reads=[B['argb', s], B['argm', s]], writes=[B['argm', s]])
                P.op('dve', lambda e, ntok=ntok, ab=ab, m1=m1: e.scalar_tensor_tensor(out=ab[:, 0:ntok], in0=m1[:, 0:ntok], scalar=2.0 * PI, in1=ab[:, 0:ntok], op0=ALU.mult, op1=ALU.add),
                     reads=[B['argb', s], B['argm', s]], writes=[B['argb', s]])
                P.op('act', lambda e, tok0=tok0, ntok=ntok, ab=ab: e.activation(out=dst[:, tok0:tok0 + ntok], in_=ab[:, 0:ntok], func=AF.Sin),
                     reads=[B['argb', s]], writes=[B[dkey2]])

        with contextlib.ExitStack() as sm:
            P.stack = sm
            h1T = P.sb("h1T", [64, 4352], F32)
            w2s = P.sb("w2s", [64, 64], F32)
            argb = [P.sb(f"argb{i}", [64, 512], F32) for i in range(4)]
            P.dma('sp', lambda e: e.dma_start(out=w2s[:], in_=hw2), writes=[B['w2s']])
            with contextlib.ExitStack() as sm2:
                P.stack = sm2
                fT = P.sb("fT", [33, 4352], F32)
                w1s = P.sb("w1s", [33, 64], F32)
                P.dma('sp', lambda e: e.dma_start(out=fT[:, 0:4096], in_=featsT), writes=[B['fT']])
                P.dma('sp', lambda e: e.dma_start(out=fT[:, 4096:4352], in_=featsTc), writes=[B['fT']])
                P.dma('sp', lambda e: e.dma_start(out=w1s[:], in_=hw1), writes=[B['w1s']])
                sin_layer(fT, 'fT', w1s, 'w1s', 33, 0, h1T, 'h1T', argb)
                P.flush()
            sin_layer(h1T, 'h1T', w2s, 'w2s', 64, 1, h2T, 'h2T', argb)
            P.flush()

        gcnt = {'n': 0}
        for o in range(2):
          with contextlib.ExitStack() as so:
            P.stack = so
            HT = P.sb(f"HT{o}", [128, 2, 8192], BF16)
            with contextlib.ExitStack() as sf:
                P.stack = sf
                env = [P.sb(f"env{i}", [128, 768], F32) for i in range(2)]
                filt = P.sb("filt", [128, 34, 768], BF16)
                for t in range(NT_A):
                    s = t % 2
                    ev = env[s]
                    P.op('act', lambda e, t=t, o=o, ev=ev: e.activation(out=ev[:], in_=adels[:, o, :, :].rearrange("p a b -> p (a b)"), func=AF.Exp, scale=negts[:, t:t + 1]),
                         reads=[B['adels'], B['negts']], writes=[B['env', s]])
                    P.op('pe', lambda e, t=t, o=o, s=s: e.matmul(pb_[:, s, 0:512], lhsT=h2T[:, t * 128:(t + 1) * 128], rhs=w3s[:, o, 0:2, :].rearrange("p a b -> p (a b)"), start=True, stop=True),
                         reads=[B['h2T'], B['w3s']], writes=[B['pb', s, 0]])
                    P.op('dve', lambda e, t=t, s=s, ev=ev: e.tensor_tensor(out=filt[:, t, 0:512], in0=pb_[:, s, 0:512], in1=ev[:, 0:512], op=ALU.mult),
                         reads=[B['pb', s, 0], B['env', s]], writes=[B['filt', t]])
                P.op('dve', lambda e: e.memset(filt[0:1, 0, 256:512], 0.0), reads=[B['filt', 0]], writes=[B['filt', 0]])
                P.op('dve', lambda e: e.memset(filt[0:1, 32, 256:512], 0.0), reads=[B['filt', 32]], writes=[B['filt', 32]])
                for t in range(NT_A):
                    P.op('pool', lambda e, t=t: e.tensor_tensor(out=filt[:, t, 512:768], in0=filt[:, t, 0:256], in1=filt[:, t, 256:512], op=ALU.add),
                         reads=[B['filt', t]], writes=[B['filt', t]])
                    P.op('pool', lambda e, t=t: e.tensor_tensor(out=filt[:, t, 0:256], in0=filt[:, t, 0:256], in1=filt[:, t, 256:512], op=ALU.subtract),
                         reads=[B['filt', t]], writes=[B['filt', t]])
                for j in range(8):
                    for t in range(32):
                        fs = fcnt['n'] % NFB
                        fcnt['n'] += 1
                        P.dma('sp', lambda e, t=t, j=j, fs=fs: e.dma_start(out=Ft[fs][:], in_=Fm[t, :, j, :]), writes=[B['Ft', fs]])
                        for c in range(2):
                            P.op('pe', lambda e, t=t, c=c, fs=fs: e.matmul(pa[:, c, :], lhsT=filt[:, t, 512 + c * 128:512 + (c + 1) * 128], rhs=Ft[fs][:, 0:512], start=(t == 0), stop=(t == 31)),
                                 reads=[B['filt', t], B['Ft', fs]], writes=[B['pa', c]])
                            P.op('pe', lambda e, t=t, c=c, fs=fs: e.matmul(pb_[:, c, :], lhsT=filt[:, t, c * 128:(c + 1) * 128], rhs=Ft[fs][:, 512:1024], start=(t == 0), stop=(t == 31)),
                                 reads=[B['filt', t], B['Ft', fs]], writes=[B['pb', c, 0]])
                    for c in range(2):
                        P.op('act', lambda e, c=c, j=j: e.copy(out=HT[:, c, j * 1024:j * 1024 + 512], in_=pa[:, c, :]), reads=[B['pa', c]], writes=[B['HT', c]])
                        P.op('dve', lambda e, c=c, j=j: e.tensor_copy(out=HT[:, c, j * 1024 + 512:(j + 1) * 1024], in_=pb_[:, c, :]), reads=[B['pb', c, 0]], writes=[B['HT', c]])
                for c in range(2):
                    for t in range(2):
                        tt = 32 + t
                        P.op('pe', lambda e, t=t, tt=tt, c=c: e.matmul(pa[:, c, 0:256], lhsT=filt[:, tt, 512 + c * 128:512 + (c + 1) * 128], rhs=Fcs[:, t, 0:256], start=(t == 0), stop=(t == 1)),
                             reads=[B['filt', tt], B['Fcs']], writes=[B['pa', c]])
                        P.op('pe', lambda e, t=t, tt=tt, c=c: e.matmul(pb_[:, c, 0:256], lhsT=filt[:, tt, c * 128:(c + 1) * 128], rhs=Fcs[:, t, 256:512], start=(t == 0), stop=(t == 1)),
                             reads=[B['filt', tt], B['Fcs']], writes=[B['pb', c, 0]])
                    P.op('act', lambda e, c=c: e.copy(out=HTc[:, c, 0:256], in_=pa[:, c, 0:256]), reads=[B['pa', c]], writes=[B['HTc', c]])
                    P.op('dve', lambda e, c=c: e.tensor_copy(out=HTc[:, c, 256:512], in_=pb_[:, c, 0:256]), reads=[B['pb', c, 0]], writes=[B['HTc', c]])
                P.flush()
            with contextlib.ExitStack() as s3:
                P.stack = s3
                stm = P.sb("stm", [128, NT_A, 256], BF16)
                YhT = P.sb("YhT", [128, 2, 1024], BF16)
                Ytm = P.sb("Ytm", [128, 68, 256], BF16)
                Gt = [P.sb(f"Gt{i}", [128, 1024], BF16) for i in range(NFB)]
                pr1 = P.sb("pr1", [128, 512], F32)
                pr2 = P.sb("pr2", [128, 512], F32)
                yst = P.sb("yst", [128, 512], F32)
                ysk = P.sb("ysk", [128, 512], F32)
                for t in range(NT_A):
                    for c in range(2):
                        P.op('pe', lambda e, t=t, c=c: e.transpose(ptb[:, c * 128:(c + 1) * 128], zH[:, c, t * 128:(t + 1) * 128], idb[:]),
                             reads=[B['zH', c], B['idb']], writes=[B['ptb']])
                    if t % 2 == 0:
                        P.op('act', lambda e, t=t: e.copy(out=stm[:, t, :], in_=ptb[:, 0:256]), reads=[B['ptb']], writes=[B['stm', t]])
                    else:
                        P.op('dve', lambda e, t=t: e.tensor_copy(out=stm[:, t, :], in_=ptb[:, 0:256]), reads=[B['ptb']], writes=[B['stm', t]])

                def product(c, ucs, uss, hc, hs, n, keyc, keys_):
                    P.op('dve', lambda e: e.tensor_tensor(out=pr1[:, 0:n], in0=ucs, in1=hc, op=ALU.mult), reads=[B[keyc]], writes=[B['pr1']])
                    P.op('dve', lambda e: e.tensor_tensor(out=pr2[:, 0:n], in0=uss, in1=hs, op=ALU.mult), reads=[B[keys_]], writes=[B['pr2']])
                    P.op('pool', lambda e: e.tensor_tensor(out=YhT[:, c, 0:n], in0=pr1[:, 0:n], in1=pr2[:, 0:n], op=ALU.subtract), reads=[B['pr1'], B['pr2']], writes=[B['YhT', c]])
                    P.op('dve', lambda e: e.tensor_tensor(out=pr1[:, 0:n], in0=ucs, in1=hs, op=ALU.mult), reads=[B[keyc], B['pr1']], writes=[B['pr1']])
                    P.op('dve', lambda e: e.tensor_tensor(out=pr2[:, 0:n], in0=uss, in1=hc, op=ALU.mult), reads=[B[keys_], B['pr2']], writes=[B['pr2']])
                    P.op('pool', lambda e: e.tensor_tensor(out=YhT[:, c, 512:512 + n], in0=pr1[:, 0:n], in1=pr2[:, 0:n], op=ALU.add), reads=[B['pr1'], B['pr2']], writes=[B['YhT', c]])

                def ytrans(kts, cols):
                    for kt, c0 in zip(kts, cols):
                        for c in range(2):
                            P.op('pe', lambda e, c=c, c0=c0: e.transpose(ptb[:, c * 128:(c + 1) * 128], YhT[:, c, c0:c0 + 128], idb[:]),
                                 reads=[B['YhT', c], B['idb']], writes=[B['ptb']])
                        if kt % 2 == 0:
                            P.op('act', lambda e, kt=kt: e.copy(out=Ytm[:, kt, :], in_=ptb[:, 0:256]), reads=[B['ptb']], writes=[B['Ytm', kt]])
                        else:
                            P.op('dve', lambda e, kt=kt: e.tensor_copy(out=Ytm[:, kt, :], in_=ptb[:, 0:256]), reads=[B['ptb']], writes=[B['Ytm', kt]])

                for j in range(8):
                    for t in range(32):
                        fs = fcnt['n'] % NFB
                        fcnt['n'] += 1
                        P.dma('sp', lambda e, t=t, j=j, fs=fs: e.dma_start(out=Ft[fs][:], in_=Fm[t, :, j, :]), writes=[B['Ft', fs]])
                        for c in range(2):
                            P.op('pe', lambda e, t=t, c=c, fs=fs: e.matmul(pa[:, c, :], lhsT=stm[:, t, c * 128:(c + 1) * 128], rhs=Ft[fs][:, 0:512], start=(t == 0), stop=(t == 31)),
                                 reads=[B['stm', t], B['Ft', fs]], writes=[B['pa', c]])
                            P.op('pe', lambda e, t=t, c=c, fs=fs: e.matmul(pb_[:, c, :], lhsT=stm[:, t, c * 128:(c + 1) * 128], rhs=Ft[fs][:, 512:1024], start=(t == 0), stop=(t == 31)),
                                 reads=[B['stm', t], B['Ft', fs]], writes=[B['pb', c, 0]])
                    for c in range(2):
                        product(c, pa[:, c, :], pb_[:, c, :], HT[:, c, j * 1024:j * 1024 + 512], HT[:, c, j * 1024 + 512:(j + 1) * 1024], 512, ('pa', c), ('pb', c, 0))
                    ytrans([j * 4 + i for i in range(4)] + [32 + j * 4 + i for i in range(4)], [i * 128 for i in range(4)] + [512 + i * 128 for i in range(4)])
                for c in range(2):
                    for t in range(2):
                        P.op('pe', lambda e, t=t, c=c: e.matmul(pa[:, c, 0:256], lhsT=stm[:, 32 + t, c * 128:(c + 1) * 128], rhs=Fcs[:, t, 0:256], start=(t == 0), stop=(t == 1)),
                             reads=[B['stm', 32 + t], B['Fcs']], writes=[B['pa', c]])
                        P.op('pe', lambda e, t=t, c=c: e.matmul(pb_[:, c, 0:256], lhsT=stm[:, 32 + t, c * 128:(c + 1) * 128], rhs=Fcs[:, t, 256:512], start=(t == 0), stop=(t == 1)),
                             reads=[B['stm', 32 + t], B['Fcs']], writes=[B['pb', c, 0]])
                    product(c, pa[:, c, 0:256], pb_[:, c, 0:256], HTc[:, c, 0:256], HTc[:, c, 256:512], 256, ('pa', c), ('pb', c, 0))
                ytrans([64, 65, 66, 67], [0, 128, 512, 640])

                def finish_blk(c, ps_ap, pkey, tok0, n, scale):
                    P.op('pool', lambda e: e.tensor_scalar(out=ysk[:, 0:n], in0=zH[:, c, tok0:tok0 + n], scalar1=hsks[:, o, c:c + 1], scalar2=None, op0=ALU.mult),
                         reads=[B['zH', c], B['hsks']], writes=[B['ysk']])
                    P.op('dve', lambda e: e.scalar_tensor_tensor(out=yst[:, 0:n], in0=ps_ap, scalar=scale, in1=ysk[:, 0:n], op0=ALU.mult, op1=ALU.add),
                         reads=[B[pkey], B['ysk']], writes=[B['yst']])
                    if o == 0:
                        P.op('pool', lambda e: e.tensor_tensor(out=zH[:, c, tok0:tok0 + n], in0=yst[:, 0:n], in1=zH[:, 2 + c, tok0:tok0 + n], op=ALU.mult),
                             reads=[B['yst'], B['zH', 2 + c]], writes=[B['zH', c]])
                    else:
                        P.op('pool', lambda e: e.tensor_tensor(out=yst[:, 0:n], in0=yst[:, 0:n], in1=zH[:, 4 + c, tok0:tok0 + n], op=ALU.mult),
                             reads=[B['yst'], B['zH', 4 + c]], writes=[B['yst']])
                        P.dma('sp', lambda e: e.dma_start(out=YT[4 + c, :, tok0:tok0 + n], in_=yst[:, 0:n]), reads=[B['yst']], is_output=True)

                for ps_ in range(4):
                    for kt in range(64):
                        gs = gcnt['n'] % NFB
                        gcnt['n'] += 1
                        P.dma('sp', lambda e, ps_=ps_, kt=kt, gs=gs: e.dma_start(out=Gt[gs][:], in_=Gm[ps_, kt]), writes=[B['Gt', gs]])
                        for c in range(2):
                            for nb in range(2):
                                acc_ap = (pa if c == 0 else pb_)[:, nb, :]
                                P.op('pe', lambda e, kt=kt, c=c, nb=nb, gs=gs, acc_ap=acc_ap: e.matmul(acc_ap, lhsT=Ytm[:, kt, c * 128:(c + 1) * 128], rhs=Gt[gs][:, nb * 512:(nb + 1) * 512],
                                                                                                  start=(kt == 0), stop=(kt == 63)),
                                     reads=[B['Ytm', kt], B['Gt', gs]], writes=[B['pa', nb] if c == 0 else B['pb', nb, 0]])
                    for c in range(2):
                        for nb in range(2):
                            finish_blk(c, (pa if c == 0 else pb_)[:, nb, :], ('pa', nb) if c == 0 else ('pb', nb, 0), ps_ * 1024 + nb * 512, 512, 2.0 / 8192.0)
                for c in range(2):
                    for kt in range(4):
                        P.op('pe', lambda e, kt=kt, c=c: e.matmul(pc[:, 0:256], lhsT=Ytm[:, 64 + kt, c * 128:(c + 1) * 128], rhs=Gcs[:, kt, :], start=(kt == 0), stop=(kt == 3)),
                             reads=[B['Ytm', 64 + kt], B['Gcs']], writes=[B['pc']])
                    finish_blk(c, pc[:, 0:256], 'pc', 4096, 256, 2.0 / 512.0)
                P.flush()
        sH.close()
        P.stack = st
        P.mute = ('noR' in _dbg)
        build_A_rest(nc, P, B, st, x, wA, wR, wD, caw, ropec, ropes, qkn, rdec, rtab, rcol, YT, ids, idb, mcol, pt, ptb, pa, pb_, pc, build_uT, inproj_fm, dwconv)


def build_A():
    nc = bass.Bass("TRN2", target_bir_lowering=False)
    x = _din(nc, "x", [NT_A, 128, D])
    modcols = _din(nc, "modcols", [128, 2, 2, 8])
    ident = _din(nc, "ident", [128, 128])
    wA = _din(nc, "wA", [128, 8, 768])
    wR = _din(nc, "wR", [128, 8, 1024])
    wH = _din(nc, "wH", [128, 8, 768])
    wD = _din(nc, "wD", [128, 8, 512])
    caw = _din(nc, "caw", [128, 2, 4])
    hcw = _din(nc, "hcw", [128, 6, 4])
    hsk = _din(nc, "hsk", [128, 2, 2])
    ropec = _din(nc, "ropec", [128, 32, 64])
    ropes = _din(nc, "ropes", [128, 32, 64])
    qkn = _din(nc, "qkn", [128, 2, 128])
    rdec = _din(nc, "rdec", [128, 4])
    rtab = _din(nc, "rtab", [128, 6, 128])
    rcol = _din(nc, "rcol", [128, 4])
    featsT = _din(nc, "featsT", [33, 4096])
    featsTc = _din(nc, "featsTc", [33, 256])
    hw1 = _din(nc, "hw1", [33, 64])
    hw2 = _din(nc, "hw2", [64, 64])
    hcols = _din(nc, "hcols", [64, 4])
    hw3 = _din(nc, "hw3", [64, 2, 2, 256])
    negt = _din(nc, "negt", [128, 34])
    adel = _din(nc, "adel", [128, 2, 2, 256])
    Fm = _din(nc, "Fm", [32, 128, 8, 1024], BF16)
    Gm = _din(nc, "Gm", [4, 64, 128, 1024], BF16)
    Fc = _din(nc, "Fc", [128, 2, 512], BF16)
    Gc = _din(nc, "Gc", [128, 4, 256], BF16)
    YT = _dout(nc, "YT", [8, 128, NTOK_A])

    Dm = dict(x=x, modcols=modcols, ident=ident, wA=wA, wR=wR, wH=wH, wD=wD, caw=caw, hcw=hcw, hsk=hsk, ropec=ropec, ropes=ropes, qkn=qkn, rdec=rdec, rtab=rtab, rcol=rcol, featsT=featsT, featsTc=featsTc, hw1=hw1, hw2=hw2, hcols=hcols, hw3=hw3, negt=negt, adel=adel, Fm=Fm, Gm=Gm, Fc=Fc, Gc=Gc, YT=YT)
    with contextlib.ExitStack() as st0:
        P = Prog(nc, st0)
        emit_A(nc, P, Dm)
    return nc


def build_A_rest(nc, P, B, st, x, wA, wR, wD, caw, ropec, ropes, qkn, rdec, rtab, rcol, YT, ids, idb, mcol, pt, ptb, pa, pb_, pc, build_uT, inproj_fm, dwconv):
    SC = 128.0 ** -0.5
    sG = st.enter_context(contextlib.ExitStack())
    P.stack = sG
    uT = P.sb("uT2", [128, 8, NTOK_A], BF16)
    rc = P.sb("rc", [128, 32, 64], F32)
    rs = P.sb("rs", [128, 32, 64], F32)
    P.dma('sp', lambda e: e.dma_start(out=rc[:], in_=ropec), writes=[B['rope']])
    P.dma('sp', lambda e: e.dma_start(out=rs[:], in_=ropes), writes=[B['rope']])
    with contextlib.ExitStack() as s0:
        build_uT(uT, s0)
        P.flush()

    def rope(src, skey, t, tmp, tkey):
        v = src.rearrange("p (a h f) -> p a h f", a=2, h=2)
        x1 = v[:, :, 0, :]
        x2 = v[:, :, 1, :]
        c_ = rc[:, t, :].rearrange("p (a f) -> p a f", a=2)
        s_ = rs[:, t, :].rearrange("p (a f) -> p a f", a=2)
        tv = [tmp[:, i, :].rearrange("p (a f) -> p a f", a=2) for i in range(4)]
        P.op('dve', lambda e: e.tensor_tensor(out=tv[0], in0=x1, in1=c_, op=ALU.mult), reads=[B[skey], B['rope']], writes=[B[tkey, 0]])
        P.op('dve', lambda e: e.tensor_tensor(out=tv[1], in0=x2, in1=s_, op=ALU.mult), reads=[B[skey], B['rope']], writes=[B[tkey, 1]])
        P.op('pool', lambda e: e.tensor_tensor(out=tv[2], in0=x2, in1=c_, op=ALU.mult), reads=[B[skey], B['rope']], writes=[B[tkey, 2]])
        P.op('pool', lambda e: e.tensor_tensor(out=tv[3], in0=x1, in1=s_, op=ALU.mult), reads=[B[skey], B['rope']], writes=[B[tkey, 3]])
        P.op('dve', lambda e: e.tensor_tensor(out=x1, in0=tv[0], in1=tv[1], op=ALU.subtract), reads=[B[tkey, 0], B[tkey, 1], B[tkey, 3], B[skey]], writes=[B[skey]])
        P.op('pool', lambda e: e.tensor_tensor(out=x2, in0=tv[2], in1=tv[3], op=ALU.add), reads=[B[tkey, 2], B[tkey, 3], B[tkey, 1], B[skey]], writes=[B[skey]])

    import os as _os
    _dbg = _os.environ.get('A_DBG', '').split(',')
    _base = P.mute
    P.mute = _base or ('noS' in _dbg)
    with contextlib.ExitStack() as s1:
        P.stack = s1
        wAs = P.sb("wAs", [128, 8, 768], BF16)
        caws = P.sb("caws", [128, 2, 4], F32)
        P.dma('pool', lambda e: e.dma_start(out=wAs[:], in_=wA), writes=[B['wAs']])
        P.dma('sp', lambda e: e.dma_start(out=caws[:], in_=caw), writes=[B['caws']])
        zA = P.sb("zA", [128, 3, NTOK_A], BF16)
        pp_ = P.sb("pp_", [128, NTOK_A], BF16)
        acc = P.sb("accA", [128, NTOK_A], F32)
        for c in range(2):
            for g in range(3):
                inproj_fm(uT, wAs, 'wAs', g * 256 + c * 128, zA[:, g, :], ('zA', g))
            P.op('pool', lambda e: e.tensor_tensor(out=pp_[:], in0=zA[:, 1, :], in1=zA[:, 2, :], op=ALU.mult), reads=[B['zA', 1], B['zA', 2]], writes=[B['pp_']])
            dwconv(pp_[:], 'pp_', caws[:, c, :], 'caws', acc, 'accA', None, None)
            P.op('dve', lambda e: e.tensor_tensor(out=acc[:], in0=acc[:], in1=zA[:, 0, :], op=ALU.mult), reads=[B['accA'], B['zA', 0]], writes=[B['accA']])
            P.dma('sp', lambda e, c=c: e.dma_start(out=YT[0 + c], in_=acc[:]), reads=[B['accA']], is_output=True)
        P.flush()

    P.mute = _base or ('noD' in _dbg)
    with contextlib.ExitStack() as s2:
        P.stack = s2
        wDs = P.sb("wDs", [128, 8, 512], BF16)
        qkns = P.sb("qkns", [128, 2, 128], F32)
        P.dma('pool', lambda e: e.dma_start(out=wDs[:], in_=wD), writes=[B['wDs']])
        P.dma('sp', lambda e: e.dma_start(out=qkns[:], in_=qkn), writes=[B['qkns']])
        qT = P.sb("qT", [128, 2, NTOK_A], BF16)
        kT = P.sb("kT", [128, NTOK_A], BF16)
        vtm = P.sb("vtm", [128, NT_A, 128], BF16)
        onesb = P.sb("onesb", [128, 128], BF16)
        P.op('dve', lambda e: e.memset(onesb[:], 1.0), writes=[B['onesb']])
        qn = [P.sb(f"qn{i}", [128, 3, 128], F32) for i in range(2)]
        qb = [P.sb(f"qb{i}", [128, 3, 128], BF16) for i in range(2)]
        sq3s = [P.sb(f"sq3{i}", [128, 384], F32) for i in range(2)]
        ss = [P.sb(f"ss{i}", [128, 4], F32) for i in range(2)]
        tmps = [P.sb(f"tmpr{i}", [128, 4, 64], F32) for i in range(2)]
        for t in range(NT_A):
            s = t % 2
            for kt in range(8):
                P.op('pe', lambda e, t=t, kt=kt, s=s: e.matmul(pa[:, s, :], lhsT=uT[:, kt, t * 128:(t + 1) * 128], rhs=wDs[:, kt, :], start=(kt == 0), stop=(kt == 7)),
                     reads=[B['uT'], B['wDs']], writes=[B['pa', s]])
            sq3 = sq3s[s]
            P.op('act', lambda e, s=s, sq3=sq3: e.activation(out=sq3[:], in_=pa[:, s, 0:384], func=AF.Square), reads=[B['pa', s]], writes=[B['sq3', s]])
            P.op('dve', lambda e, s=s, sq3=sq3: e.reduce_sum(out=ss[s][:, 0:3], in_=sq3[:].rearrange("p (h d) -> p h d", h=3), axis=AX.X),
                 reads=[B['sq3', s]], writes=[B['ss', s, 0], B['ss', s, 1], B['ss', s, 2]])
            P.op('dve', lambda e, s=s: e.tensor_scalar(out=ss[s][:, 0:3], in0=ss[s][:, 0:3], scalar1=1.0 / 128.0, scalar2=1e-6, op0=ALU.mult, op1=ALU.add),
                 reads=[B['ss', s, h] for h in range(3)], writes=[B['ss', s, 'a']])
            P.op('act', lambda e, s=s: e.sqrt(out=ss[s][:, 0:3], in_=ss[s][:, 0:3]), reads=[B['ss', s, 'a']], writes=[B['ss', s, 'a']])
            P.op('dve', lambda e, s=s: e.reciprocal(out=ss[s][:, 0:3], in_=ss[s][:, 0:3]), reads=[B['ss', s, 'a']], writes=[B['ss', s, 'a']])
            for h in range(3):
                P.op('dve', lambda e, h=h, s=s: e.scalar_tensor_tensor(out=qn[s][:, h, :], in0=pa[:, s, h * 128:(h + 1) * 128], scalar=ss[s][:, h:h + 1], in1=qkns[:, 0 if h < 2 else 1, :],
                                                                     op0=ALU.mult, op1=ALU.mult), reads=[B['pa', s], B['ss', s, 'a'], B['qkns']], writes=[B['qn', s, h]])
                if t < 32:
                    rope(qn[s][:, h, :], ('qn', s, h), t, tmps[s], ('tmpr', s))
                P.op('pool', lambda e, h=h, s=s: e.tensor_copy(out=qb[s][:, h, :], in_=qn[s][:, h, :]), reads=[B['qn', s, h]], writes=[B['qb', s, h]])
            P.op('act', lambda e, t=t, s=s: e.copy(out=vtm[:, t, :], in_=pa[:, s, 384:512]), reads=[B['pa', s]], writes=[B['vtm', t]])
            for h in range(3):
                P.op('pe', lambda e, h=h, s=s: e.transpose(ptb[:, h * 128:(h + 1) * 128], qb[s][:, h, :], idb[:]), reads=[B['qb', s, h], B['idb']], writes=[B['ptb']])
            P.op('dve', lambda e, t=t: e.tensor_copy(out=qT[:, :, t * 128:(t + 1) * 128], in_=ptb[:, 0:256].rearrange("p (h d) -> p h d", h=2)), reads=[B['ptb']], writes=[B['qT']])
            P.op('act', lambda e, t=t: e.copy(out=kT[:, t * 128:(t + 1) * 128], in_=ptb[:, 256:384]), reads=[B['ptb']], writes=[B['kT']])
        if 'dumpQK' in _dbg:
            dq = P.sb("dq", [128, NTOK_A], F32)
            for i_, src_ in enumerate((qT[:, 0, :], qT[:, 1, :], kT[:])):
                P.op('dve', lambda e, src_=src_: e.tensor_copy(out=dq[:], in_=src_), reads=[B['qT'], B['kT']], writes=[B['dq']])
                P.dma('sp', lambda e, i_=i_: e.dma_start(out=YT[5 + i_], in_=dq[:]), reads=[B['dq']], writes=[B['dqo', i_]], is_output=True)
            P.op('dve', lambda e: e.tensor_copy(out=dq[:].rearrange("p (t d) -> p t d", d=128), in_=vtm[:]), reads=[B['vtm', t_] for t_ in range(NT_A)], writes=[B['dq']])
            P.dma('sp', lambda e: e.dma_start(out=YT[4], in_=dq[:]), reads=[B['dq']], is_output=True)
        P.mute = P.mute or ('noD2' in _dbg)
        pT = [P.sb(f"pT{i}", [128, 512], BF16) for i in range(3)]
        rd = P.sb("rd", [128, 512], F32)
        yo = [P.sb(f"yo{i}", [128, 512], F32) for i in range(2)]
        pcnt = 0
        ocnt = 0
        for h in range(2):
            for bi, (tok0, ntok) in enumerate(TBLK_A):
                if 'cOne' in _dbg and (h, bi) != (0, 0):
                    continue
                ktiles = list(range(34)) if bi < 8 else [32, 33]
                nkt = len(ktiles)
                slots = []
                for ki, kt in enumerate(ktiles):
                    slots.append((pcnt % 2, pcnt % 3))
                    pcnt += 1

                def qk_exp(ki):
                    kt = ktiles[ki]
                    s, p3 = slots[ki]
                    P.op('pe', lambda e, kt=kt, h=h, s=s, tok0=tok0, ntok=ntok: e.matmul(pa[:, s, 0:ntok], lhsT=kT[:, kt * 128:(kt + 1) * 128], rhs=qT[:, h, tok0:tok0 + ntok], start=True, stop=True),
                         reads=[B['kT'], B['qT']], writes=[B['pa', s]])
                    P.op('act', lambda e, s=s, p3=p3, ntok=ntok: e.activation(out=pT[p3][:, 0:ntok], in_=pa[:, s, 0:ntok], func=AF.Exp, scale=SC), reads=[B['pa', s]], writes=[B['pT', p3]])

                def pv(ki):
                    kt = ktiles[ki]
                    s, p3 = slots[ki]
                    P.op('pe', lambda e, kt=kt, p3=p3, ntok=ntok, ki=ki, nkt=nkt: e.matmul(pb_[:, 0, 0:ntok], lhsT=vtm[:, kt, :], rhs=pT[p3][:, 0:ntok], start=(ki == 0), stop=(ki == nkt - 1)),
                         reads=[B['vtm', kt], B['pT', p3]], writes=[B['pb', 0, 0]])
                    P.op('pe', lambda e, p3=p3, ntok=ntok, ki=ki, nkt=nkt: e.matmul(pb_[:, 1, 0:ntok], lhsT=onesb[:], rhs=pT[p3][:, 0:ntok], start=(ki == 0), stop=(ki == nkt - 1)),
                         reads=[B['onesb'], B['pT', p3]], writes=[B['pb', 1, 0]])

                qk_exp(0)
                for ki in range(1, nkt):
                    qk_exp(ki)
                    pv(ki - 1)
                pv(nkt - 1)
                if 'cQK' in _dbg:
                    continue
                P.op('dve', lambda e, ntok=ntok: e.reciprocal(out=rd[:, 0:ntok], in_=pb_[:, 1, 0:ntok]), reads=[B['pb', 1, 0]], writes=[B['rd']])
                y = yo[ocnt % 2]
                yk = ('yo', ocnt % 2)
                ocnt += 1
                P.op('dve', lambda e, ntok=ntok, y=y: e.tensor_tensor(out=y[:, 0:ntok], in0=pb_[:, 0, 0:ntok], in1=rd[:, 0:ntok], op=ALU.mult), reads=[B['pb', 0, 0], B['rd']], writes=[B[yk]])
                P.dma('sp', lambda e, h=h, tok0=tok0, ntok=ntok, y=y: e.dma_start(out=YT[6 + h, :, tok0:tok0 + ntok], in_=y[:, 0:ntok]), reads=[B[yk]], is_output=True)
        P.flush()

    P.mute = _base or ('noT' in _dbg)
    with contextlib.ExitStack() as s3:
        P.stack = s3
        wRs = P.sb("wRs", [128, 8, 1024], BF16)
        P.dma('pool', lambda e: e.dma_start(out=wRs[:], in_=wR), writes=[B['wRs']])
        rds = P.sb("rds", [128, 4], F32)
        lgam = P.sb("lgam", [128, 4], F32)
        rtabs = P.sb("rtabs", [128, 6, 128], F32)
        rcols = P.sb("rcols", [128, 4], F32)
        P.dma('sp', lambda e: e.dma_start(out=rds[:], in_=rdec), writes=[B['rds']])
        P.dma('sp', lambda e: e.dma_start(out=rtabs[:], in_=rtab), writes=[B['rtabs']])
        P.dma('sp', lambda e: e.dma_start(out=rcols[:], in_=rcol), writes=[B['rcols']])
        P.op('act', lambda e: e.activation(out=lgam[:], in_=rds[:], func=AF.Exp), reads=[B['rds']], writes=[B['lgam']])
        P.op('dve', lambda e: e.tensor_scalar(out=lgam[:], in0=lgam[:], scalar1=-1.0, scalar2=None, op0=ALU.mult), reads=[B['lgam']], writes=[B['lgam']])
        mask = P.sb("mask", [128, 128], F32)
        mtmp = P.sb("mtmpR", [128, 128], F32)
        qdf = P.sb("qdf", [128, 128], F32)
        qdb = P.sb("qdb", [128, 128], F32)
        kdc = P.sb("kdc", [128, 4], F32)
        qT = P.sb("rqT", [128, NTOK_A], BF16)
        kT = P.sb("rkT", [128, NTOK_A], BF16)
        qdfT = P.sb("qdfT", [128, NTOK_A], BF16)
        qdbT = P.sb("qdbT", [128, NTOK_A], BF16)
        kdf = P.sb("kdf", [128, NT_A, 128], BF16)
        kdb = P.sb("kdb", [128, NT_A, 128], BF16)
        vtm = P.sb("rvtm", [128, NT_A, 128], BF16)
        sgt = P.sb("sgt", [128, NT_A, 128], BF16)
        Sf = P.sb("Sf", [128, NT_A, 128], BF16)
        Sb = P.sb("Sb", [128, NT_A, 128], BF16)
        stf = [P.sb(f"stf{i}", [128, 128], F32) for i in range(2)]
        qk = [P.sb(f"rqk{i}", [128, 2, 128], F32) for i in range(2)]
        qkb = [P.sb(f"rqkb{i}", [128, 2, 128], BF16) for i in range(2)]
        tmps = [P.sb(f"tmpq{i}", [128, 4, 64], F32) for i in range(2)]
        smT = [P.sb(f"smT{i}", [128, 128], BF16) for i in range(2)]
        stts = [P.sb(f"rstt{i}", [128, 6], F32) for i in range(2)]
        mvs = [P.sb(f"rmv{i}", [128, 2], F32) for i in range(2)]
        rstds = [P.sb(f"rrstd{i}", [128, 1], F32) for i in range(2)]
        on = [P.sb(f"on{i}", [128, 128], F32) for i in range(2)]
        ob = [P.sb(f"ob{i}", [128, 128], BF16) for i in range(2)]
        ysg = [P.sb(f"rysg{i}", [128, 512], F32) for i in range(2)]
        for h in range(2):
            lf = lgam[:, h:h + 1]
            lb = lgam[:, 2 + h:3 + h]
            P.op('act', lambda e, lf=lf: e.activation(out=mask[:], in_=rtabs[:, 0, :], func=AF.Exp, scale=lf), reads=[B['rtabs'], B['lgam']], writes=[B['mask']])
            P.op('dve', lambda e: e.tensor_tensor(out=mask[:], in0=mask[:], in1=rtabs[:, 1, :], op=ALU.mult), reads=[B['mask'], B['rtabs']], writes=[B['mask']])
            P.op('act', lambda e, lb=lb: e.activation(out=mtmp[:], in_=rtabs[:, 2, :], func=AF.Exp, scale=lb), reads=[B['rtabs'], B['lgam']], writes=[B['mtmpR']])
            P.op('dve', lambda e: e.tensor_tensor(out=mtmp[:], in0=mtmp[:], in1=rtabs[:, 3, :], op=ALU.mult), reads=[B['mtmpR'], B['rtabs']], writes=[B['mtmpR']])
            P.op('dve', lambda e: e.tensor_tensor(out=mask[:], in0=mask[:], in1=mtmp[:], op=ALU.add), reads=[B['mask'], B['mtmpR']], writes=[B['mask']])
            P.op('act', lambda e, lf=lf: e.activation(out=qdf[:], in_=rtabs[:, 4, :], func=AF.Exp, scale=lf), reads=[B['rtabs'], B['lgam']], writes=[B['qdf']])
            P.op('act', lambda e, lb=lb: e.activation(out=qdb[:], in_=rtabs[:, 5, :], func=AF.Exp, scale=lb), reads=[B['rtabs'], B['lgam']], writes=[B['qdb']])
            P.op('act', lambda e, lf=lf: e.activation(out=kdc[:, 0:1], in_=rcols[:, 0:1], func=AF.Exp, scale=lf), reads=[B['rcols'], B['lgam']], writes=[B['kdc', 0]])
            P.op('act', lambda e, lb=lb: e.activation(out=kdc[:, 1:2], in_=rcols[:, 1:2], func=AF.Exp, scale=lb), reads=[B['rcols'], B['lgam']], writes=[B['kdc', 1]])
            P.op('act', lambda e, lf=lf: e.activation(out=kdc[:, 2:3], in_=rcols[:, 2:3], func=AF.Exp, scale=lf), reads=[B['rcols'], B['lgam']], writes=[B['kdc', 2]])
            P.op('act', lambda e, lb=lb: e.activation(out=kdc[:, 3:4], in_=rcols[:, 2:3], func=AF.Exp, scale=lb), reads=[B['rcols'], B['lgam']], writes=[B['kdc', 3]])
            kd = [B['kdc', i] for i in range(4)]
            for t in range(NT_A):
                s = t % 2
                for g in range(4):
                    for kt in range(8):
                        P.op('pe', lambda e, t=t, kt=kt, s=s, g=g, h=h: e.matmul(pa[:, s, g * 128:(g + 1) * 128], lhsT=uT[:, kt, t * 128:(t + 1) * 128], rhs=wRs[:, kt, g * 256 + h * 128:g * 256 + (h + 1) * 128],
                                                                        start=(kt == 0), stop=(kt == 7)), reads=[B['uT'], B['wRs']], writes=[B['pa', s]])
                P.op('act', lambda e, s=s: e.copy(out=qk[s][:, 0, :], in_=pa[:, s, 0:128]), reads=[B['pa', s]], writes=[B['rqk', s, 0]])
                P.op('act', lambda e, s=s: e.activation(out=qk[s][:, 1, :], in_=pa[:, s, 128:256], func=AF.Copy, scale=SC), reads=[B['pa', s]], writes=[B['rqk', s, 1]])
                P.op('act', lambda e, s=s, t=t: e.copy(out=vtm[:, t, :], in_=pa[:, s, 256:384]), reads=[B['pa', s]], writes=[B['rvtm', t]])
                P.op('act', lambda e, s=s, t=t: e.activation(out=sgt[:, t, :], in_=pa[:, s, 384:512], func=AF.Silu), reads=[B['pa', s]], writes=[B['sgt', t]])
                for j in range(2):
                    if t < 32:
                        rope(qk[s][:, j, :], ('rqk', s, j), t, tmps[j], ('tmpq', j))
                    P.op('pool', lambda e, s=s, j=j: e.tensor_copy(out=qkb[s][:, j, :], in_=qk[s][:, j, :]), reads=[B['rqk', s, j]], writes=[B['rqkb', s, j]])
                P.op('dve', lambda e, s=s, t=t: e.tensor_scalar(out=kdf[:, t, :], in0=qk[s][:, 1, :], scalar1=kdc[:, 0:1], scalar2=None, op0=ALU.mult), reads=[B['rqk', s, 1], kd[0]], writes=[B['kdf', t]])
                P.op('dve', lambda e, s=s, t=t: e.tensor_scalar(out=kdb[:, t, :], in0=qk[s][:, 1, :], scalar1=kdc[:, 1:2], scalar2=None, op0=ALU.mult), reads=[B['rqk', s, 1], kd[1]], writes=[B['kdb', t]])
                for j in range(2):
                    P.op('pe', lambda e, s=s, j=j: e.transpose(ptb[:, j * 128:(j + 1) * 128], qkb[s][:, j, :], idb[:]), reads=[B['rqkb', s, j], B['idb']], writes=[B['ptb']])
                sl = slice(t * 128, (t + 1) * 128)
                P.op('act', lambda e, sl=sl: e.copy(out=qT[:, sl], in_=ptb[:, 0:128]), reads=[B['ptb']], writes=[B['rqT', t]])
                P.op('act', lambda e, sl=sl: e.copy(out=kT[:, sl], in_=ptb[:, 128:256]), reads=[B['ptb']], writes=[B['rkT', t]])
                P.op('dve', lambda e, sl=sl: e.tensor_tensor(out=qdfT[:, sl], in0=ptb[:, 0:128], in1=qdf[:], op=ALU.mult), reads=[B['ptb'], B['qdf']], writes=[B['qdfT', t]])
                P.op('dve', lambda e, sl=sl: e.tensor_tensor(out=qdbT[:, sl], in0=ptb[:, 0:128], in1=qdb[:], op=ALU.mult), reads=[B['ptb'], B['qdb']], writes=[B['qdbT', t]])

            def chain(order, kdx, Sx, cd, ckey, name):
                cur = None
                for n_, t in enumerate(order):
                    if cur is None:
                        P.op('pool', lambda e, t=t: e.memset(Sx[:, t, :], 0.0), writes=[B[name, t]])
                    else:
                        P.op('pool', lambda e, t=t, cur=cur: e.tensor_copy(out=Sx[:, t, :], in_=stf[cur][:]), reads=[B['stf', cur]], writes=[B[name, t]])
                    if n_ == len(order) - 1:
                        break
                    P.op('pe', lambda e, t=t: e.matmul(pc[:, 0:128], lhsT=kdx[:, t, :], rhs=vtm[:, t, :], start=True, stop=True), reads=[B[name + 'k', t], B['rvtm', t]], writes=[B['pc']])
                    nxt = 0 if cur is None else 1 - cur
                    if cur is None:
                        P.op('dve', lambda e, nxt=nxt: e.tensor_copy(out=stf[nxt][:], in_=pc[:, 0:128]), reads=[B['pc']], writes=[B['stf', nxt]])
                    else:
                        P.op('dve', lambda e, nxt=nxt, cur=cur: e.scalar_tensor_tensor(out=stf[nxt][:], in0=stf[cur][:], scalar=cd, in1=pc[:, 0:128], op0=ALU.mult, op1=ALU.add),
                             reads=[B['stf', cur], B['pc'], ckey], writes=[B['stf', nxt]])
                    cur = nxt
            for t in range(NT_A):
                B.d[('Sfk', t)] = B['kdf', t]
                B.d[('Sbk', t)] = B['kdb', t]
            chain([32, 33] + list(range(32)), kdf, Sf, kdc[:, 2:3], kd[2], 'Sf')
            chain([33, 32] + list(range(31, -1, -1)), kdb, Sb, kdc[:, 3:4], kd[3], 'Sb')
            for t in range(NT_A):
                s = t % 2
                sl = slice(t * 128, (t + 1) * 128)
                P.op('pe', lambda e, sl=sl, s=s: e.matmul(pa[:, s, 0:128], lhsT=kT[:, sl], rhs=qT[:, sl], start=True, stop=True), reads=[B['rkT', t], B['rqT', t]], writes=[B['pa', s]])
                P.op('dve', lambda e, s=s: e.tensor_tensor(out=smT[s][:], in0=pa[:, s, 0:128], in1=mask[:], op=ALU.mult), reads=[B['pa', s], B['mask']], writes=[B['smT', s]])
                P.op('pe', lambda e, t=t, s=s: e.matmul(pb_[:, s, 0:128], lhsT=smT[s][:], rhs=vtm[:, t, :], start=True, stop=False), reads=[B['smT', s], B['rvtm', t]], writes=[B['pb', s, 0]])
                P.op('pe', lambda e, t=t, s=s, sl=sl: e.matmul(pb_[:, s, 0:128], lhsT=qdfT[:, sl], rhs=Sf[:, t, :], start=False, stop=False), reads=[B['qdfT', t], B['Sf', t]], writes=[B['pb', s, 0]])
                P.op('pe', lambda e, t=t, s=s, sl=sl: e.matmul(pb_[:, s, 0:128], lhsT=qdbT[:, sl], rhs=Sb[:, t, :], start=False, stop=True), reads=[B['qdbT', t], B['Sb', t]], writes=[B['pb', s, 0]])
                P.op('act', lambda e, s=s: e.copy(out=on[s][:], in_=pb_[:, s, 0:128]), reads=[B['pb', s, 0]], writes=[B['on', s]])
                stt, mv, rstd = stts[s], mvs[s], rstds[s]
                P.op('dve', lambda e, s=s, stt=stt: e.bn_stats(out=stt[:], in_=on[s][:]), reads=[B['on', s]], writes=[B['rstt', s]])
                P.op('dve', lambda e, stt=stt, mv=mv: e.bn_aggr(out=mv[:], in_=stt[:]), reads=[B['rstt', s]], writes=[B['rmv', s]])
                P.op('dve', lambda e, mv=mv, rstd=rstd: e.tensor_scalar_add(out=rstd[:], in0=mv[:, 1:2], scalar1=1e-6), reads=[B['rmv', s]], writes=[B['rrstd', s]])
                P.op('act', lambda e, rstd=rstd: e.sqrt(out=rstd[:], in_=rstd[:]), reads=[B['rrstd', s]], writes=[B['rrstd', s]])
                P.op('dve', lambda e, rstd=rstd: e.reciprocal(out=rstd[:], in_=rstd[:]), reads=[B['rrstd', s]], writes=[B['rrstd', s]])
                P.op('dve', lambda e, s=s, mv=mv, rstd=rstd: e.tensor_scalar(out=on[s][:], in0=on[s][:], scalar1=mv[:, 0:1], scalar2=rstd[:, 0:1], op0=ALU.subtract, op1=ALU.mult),
                     reads=[B['on', s], B['rmv', s], B['rrstd', s]], writes=[B['on', s]])
                P.op('pool', lambda e, s=s, t=t: e.tensor_tensor(out=ob[s][:], in0=on[s][:], in1=sgt[:, t, :], op=ALU.mult), reads=[B['on', s], B['sgt', t]], writes=[B['ob', s]])
                P.op('pe', lambda e, s=s: e.transpose(ptb[:, 512:640], ob[s][:], idb[:]), reads=[B['ob', s], B['idb']], writes=[B['ptb2']])
                g4 = t // 4
                yg = ysg[g4 % 2]
                P.op('act', lambda e, t=t, yg=yg: e.copy(out=yg[:, (t % 4) * 128:(t % 4 + 1) * 128], in_=ptb[:, 512:640]), reads=[B['ptb2']], writes=[B['rysg', g4 % 2]])
                if t % 4 == 3 or t == NT_A - 1:
                    n_ = (t % 4 + 1) * 128
                    P.dma('sp', lambda e, h=h, g4=g4, yg=yg, n_=n_: e.dma_start(out=YT[2 + h, :, g4 * 512:g4 * 512 + n_], in_=yg[:, 0:n_]), reads=[B['rysg', g4 % 2]], is_output=True)
        P.flush()


import ml_dtypes
_BF = ml_dtypes.bfloat16
_CONST = {}


def _consts():
    if _CONST:
        return _CONST
    C = _CONST
    rows = 64
    row = np.repeat(np.arange(rows, dtype=np.float32), 64)
    col = np.tile(np.arange(64, dtype=np.float32), rows)
    inv = (10000.0 ** (-np.arange(32, dtype=np.float32) / 32)).astype(np.float32)
    ang = np.stack([row[:, None] * inv, col[:, None] * inv], axis=1).astype(np.float32)
    C['ropec'] = np.ascontiguousarray(np.cos(ang).astype(np.float32).reshape(32, 128, 64).transpose(1, 0, 2))
    C['ropes'] = np.ascontiguousarray(np.sin(ang).astype(np.float32).reshape(32, 128, 64).transpose(1, 0, 2))
    j = np.arange(128, dtype=np.float32)[:, None]
    i = np.arange(128, dtype=np.float32)[None, :]
    rtab = np.stack([np.maximum(i - j, 0), (i >= j).astype(np.float32), np.maximum(j - i, 0), (j >= i).astype(np.float32),
                     np.broadcast_to(i + 1, (128, 128)), np.broadcast_to(128 - i, (128, 128))], axis=1).astype(np.float32)
    C['rtab'] = np.ascontiguousarray(rtab)
    jj = np.arange(128, dtype=np.float32)
    C['rcol'] = np.ascontiguousarray(np.stack([127 - jj, jj, np.full(128, 128.0), np.zeros(128)], 1).astype(np.float32))

    def feats(l):
        t = np.linspace(0.0, 1.0, l, dtype=np.float32)[:, None]
        w = (2.0 * np.pi * np.arange(l, dtype=np.float32) / l).astype(np.float32)
        f = np.linspace(1e-4, 15, 16, dtype=np.float32)
        a = (w[:, None] * f[None, :]).astype(np.float32)
        return np.concatenate([t, np.cos(a), -np.sin(a)], axis=-1).astype(np.float32), t[:, 0]
    f4, t4 = feats(4096)
    fc, tc = feats(256)
    C['featsT'] = np.ascontiguousarray(f4.T)
    C['featsTc'] = np.ascontiguousarray(fc.T)
    negt = np.concatenate([-t4.reshape(32, 128), -tc.reshape(2, 128)], 0).T
    C['negt'] = np.ascontiguousarray(negt.astype(np.float32))
    mx = np.log(1e-2) / 0.3
    mn = np.log(1e-2) / 1.5
    C['absdelta'] = np.abs(np.linspace(mn, mx, 2048, dtype=np.float32)).reshape(2, 2, 512)
    n = np.arange(4096, dtype=np.float64)
    k = np.arange(4096, dtype=np.float64) + 0.5
    Fm = np.empty((32, 128, 8, 1024), dtype=_BF)
    for nt in range(32):
        th = 2 * np.pi * np.outer(n[nt * 128:(nt + 1) * 128], k) / 8192.0
        Fm[nt, :, :, 0:512] = np.cos(th).reshape(128, 8, 512).astype(_BF)
        Fm[nt, :, :, 512:1024] = np.sin(th).reshape(128, 8, 512).astype(_BF)
    C['Fm'] = Fm
    Gm = np.empty((4, 64, 128, 1024), dtype=_BF)
    for kt in range(32):
        th = 2 * np.pi * np.outer(k[kt * 128:(kt + 1) * 128], n) / 8192.0
        Gm[:, kt] = np.cos(th).reshape(128, 4, 1024).transpose(1, 0, 2).astype(_BF)
        Gm[:, 32 + kt] = np.sin(th).reshape(128, 4, 1024).transpose(1, 0, 2).astype(_BF)
    C['Gm'] = Gm
    nc_ = np.arange(256, dtype=np.float64)
    kc = np.arange(256, dtype=np.float64) + 0.5
    th = 2 * np.pi * np.outer(nc_, kc) / 512.0
    Fc = np.concatenate([np.cos(th), np.sin(th)], 1).reshape(2, 128, 512).transpose(1, 0, 2)
    C['Fc'] = np.ascontiguousarray(Fc).astype(_BF)
    Gc = np.concatenate([np.cos(th.T), np.sin(th.T)], 0).reshape(4, 128, 256).transpose(1, 0, 2)
    C['Gc'] = np.ascontiguousarray(Gc).astype(_BF)
    return C


def run_A(l, x_cur, h_cur, mod, inp):
    C = _consts()
    w_in = inp['w_in'][l]
    ins = []
    for b in range(4):
        xt = np.ascontiguousarray(np.concatenate([x_cur[b], h_cur[b]], 0).reshape(NT_A, 128, 1024))
        ms = [mod[b], mod[4]]
        modcols = np.ascontiguousarray(np.stack([np.stack([_cols(m[k_ * 1024:(k_ + 1) * 1024]) for k_ in (0, 1)], 1) for m in ms], 1))
        for half in range(2):
            r256 = half * 256 + np.arange(256)
            colsA = np.concatenate([g * 512 + r256 for g in range(3)])
            colsR = np.concatenate([1536 + g * 512 + r256 for g in range(4)])
            colsH = np.concatenate([3584 + g * 512 + r256 for g in range(3)])
            colsD = np.concatenate([5120 + r256, 5120 + 512 + half * 128 + np.arange(128), 5120 + 768 + half * 128 + np.arange(128)])
            ch = half * 256 + np.arange(256)
            caw = np.stack([inp['conv_a_w'][l][0, ch], inp['conv_a_w'][l][1, ch], inp['conv_a_w'][l][2, ch], inp['conv_a_b'][l][ch]], -1)
            hch = np.concatenate([g * 512 + ch for g in range(3)])
            hcw = np.stack([inp['hy_conv_w'][l][0, hch], inp['hy_conv_w'][l][1, hch], inp['hy_conv_w'][l][2, hch], inp['hy_conv_b'][l][hch]], -1)
            d = dict(
                x=xt, modcols=modcols, ident=_IDENT,
                wA=_tile_rows(w_in[:, colsA]), wR=_tile_rows(w_in[:, colsR]), wH=_tile_rows(w_in[:, colsH]), wD=_tile_rows(w_in[:, colsD]),
                caw=np.ascontiguousarray(caw.reshape(2, 128, 4).transpose(1, 0, 2)),
                hcw=np.ascontiguousarray(hcw.reshape(6, 128, 4).transpose(1, 0, 2)),
                hsk=np.ascontiguousarray(inp['hy_skip'][l][:, ch].reshape(2, 2, 128).transpose(2, 0, 1)),
                ropec=C['ropec'], ropes=C['ropes'],
                qkn=_rep(np.stack([inp['q_norm'][l], inp['k_norm'][l]], 0)),
                rdec=_rep(inp['ret_decay'][l][:, half * 2:half * 2 + 2].reshape(4)),
                rtab=C['rtab'], rcol=C['rcol'], featsT=C['featsT'], featsTc=C['featsTc'],
                hw1=np.ascontiguousarray(inp['hy_w1'][l]), hw2=np.ascontiguousarray(inp['hy_w2'][l]),
                hcols=np.ascontiguousarray(np.stack([inp['hy_b1'][l], inp['hy_b2'][l], inp['hy_freq'][l][0], inp['hy_freq'][l][1]], 1)),
                hw3=np.ascontiguousarray(inp['hy_w3'][l].reshape(64, 2, 2, 512)[:, :, :, half * 256:(half + 1) * 256]),
                negt=C['negt'], adel=_rep(np.ascontiguousarray(C['absdelta'][:, :, half * 256:(half + 1) * 256])),
                Fm=C['Fm'], Gm=C['Gm'], Fc=C['Fc'], Gc=C['Gc'],
            )
            ins.append(d)
    res = run_bass_kernel_spmd(_prog('A', build_A), ins, core_ids=list(range(8)))
    YT = []
    for b in range(4):
        y = np.empty((4, 2, 2, 128, NTOK_A), np.float32)
        for half in range(2):
            o = res.results[b * 2 + half]['YT'].reshape(4, 2, 128, NTOK_A)
            y[:, half] = o
        YT.append(y.reshape(2048, NTOK_A))
    return YT


class _YTView:
    def __init__(self, base, half):
        self.base = base
        self.half = half

    def _m(self, i8):
        return (i8 // 2) * 4 + self.half * 2 + (i8 % 2)

    def __getitem__(self, idx):
        if isinstance(idx, tuple):
            return self.base[(self._m(idx[0]),) + tuple(idx[1:])]
        return self.base[self._m(idx)]


def emit_M2(nc, P, Dm):
    cT, wm, bcol, brow, modc, modr = Dm['cT'], Dm['wm'], Dm['bcol'], Dm['brow'], Dm['modc'], Dm['modr']
    with contextlib.ExitStack() as st:
        P.stack = st
        B = Bufs()
        cs = P.sb("cs", [128, 8, 2], F32)
        sc = P.sb("sc", [128, 8, 2], F32)
        ones = P.sb("ones", [128, 128], F32)
        scb = P.sb("scb", [128, 8, 2, 128], F32)
        wch = [P.sb(f"wch{i}", [128, 8, 1024], F32) for i in range(2)]
        bc = P.sb("bc", [128, 48], F32)
        br = P.sb("br", [128, 2, 1024], F32)
        mc = P.sb("mc", [128, 2, 48], F32)
        mr = P.sb("mr", [128, 2, 2, 1024], F32)
        pm = P.ps("pm", [128, 2, 512], F32)
        pcl = P.ps("pc", [128, 512], F32)
        P.dma('sp', lambda e: e.dma_start(out=cs[:], in_=cT), writes=[B['cs']])
        P.op('act', lambda e: e.activation(out=sc[:], in_=cs[:], func=AF.Silu), reads=[B['cs']], writes=[B['sc']])
        P.op('dve', lambda e: e.memset(ones[:], 1.0), writes=[B['ones']])
        for kt in range(8):
            for ms in range(2):
                P.op('dve', lambda e, kt=kt, ms=ms: e.tensor_scalar(out=scb[:, kt, ms, :], in0=ones[:], scalar1=sc[:, kt, ms:ms + 1], scalar2=None, op0=ALU.mult),
                     reads=[B['ones'], B['sc']], writes=[B['scb']])
        wc = 0
        for l in range(4):
            P.dma('sp', lambda e, l=l: e.dma_start(out=bc[:], in_=bcol[l]), writes=[B['bc']])
            P.dma('sp', lambda e, l=l: e.dma_start(out=br[:], in_=brow[l]), writes=[B['br']])
            for k in range(6):
                ws_ = wc % 2
                wc += 1
                P.dma('sp', lambda e, l=l, k=k, ws_=ws_: e.dma_start(out=wch[ws_][:], in_=wm[l, k]), writes=[B['wch', ws_]])
                for f in range(8):
                    for kt in range(8):
                        P.op('pe', lambda e, f=f, kt=kt, ws_=ws_: e.matmul(pcl[:, f * 2:f * 2 + 2], lhsT=wch[ws_][:, kt, f * 128:(f + 1) * 128], rhs=sc[:, kt, :], start=(kt == 0), stop=(kt == 7)),
                             reads=[B['wch', ws_], B['sc']], writes=[B['pc']])
                for ms in range(2):
                    P.op('dve', lambda e, k=k, ms=ms: e.tensor_tensor(out=mc[:, ms, k * 8:(k + 1) * 8], in0=pcl[:, 0:16].rearrange("p (f m) -> p m f", m=2)[:, ms, :], in1=bc[:, k * 8:(k + 1) * 8], op=ALU.add),
                         reads=[B['pc'], B['bc']], writes=[B['mc']])
                if k in (2, 5):
                    j = 0 if k == 2 else 1
                    for ms in range(2):
                        for nb in range(2):
                            for kt in range(8):
                                P.op('pe', lambda e, ms=ms, nb=nb, kt=kt, ws_=ws_: e.matmul(pm[:, nb, :], lhsT=scb[:, kt, ms, :], rhs=wch[ws_][:, kt, nb * 512:(nb + 1) * 512], start=(kt == 0), stop=(kt == 7)),
                                     reads=[B['scb'], B['wch', ws_]], writes=[B['pm', nb]])
                            P.op('dve', lambda e, ms=ms, nb=nb, j=j: e.tensor_tensor(out=mr[:, ms, j, nb * 512:(nb + 1) * 512], in0=pm[:, nb, :], in1=br[:, j, nb * 512:(nb + 1) * 512], op=ALU.add),
                                 reads=[B['pm', nb], B['br']], writes=[B['mr']])
            P.dma('sp', lambda e, l=l: e.dma_start(out=modc[l], in_=mc[:]), reads=[B['mc']], writes=[B['modc', l]])
            P.dma('sp', lambda e, l=l: e.dma_start(out=modr[l], in_=mr[:]), reads=[B['mr']], writes=[B['modr', l]])
        P.flush()


def emit_cast(nc, P, pairs):
    with contextlib.ExitStack() as st:
        P.stack = st
        B = Bufs()
        cb = [P.sb(f"cb{i}", [128, 4096], BF16) for i in range(3)]
        for i, (src, dst) in enumerate(pairs):
            c = cb[i % 3]
            n = src.shape[-1]
            P.dma('pool', lambda e, c=c, src=src, n=n: e.dma_start(out=c[:, 0:n], in_=src), writes=[B['cb', i % 3]])
            P.dma('sp', lambda e, c=c, dst=dst, n=n: e.dma_start(out=dst, in_=c[:, 0:n]), reads=[B['cb', i % 3]], writes=[B['dst', i]])
        P.flush()


def build_F():
    nc = bass.Bass("TRN2", target_bir_lowering=False)
    I = lambda name, shape, dt=F32: _din(nc, name, shape, dt)
    T = lambda name, shape, dt=F32: nc.dram_tensor(name, list(shape), dt, kind="Internal").ap()
    x0 = I("x0", [NT_A, 128, D])
    cT = I("cT", [128, 8, 2])
    wm = I("wm", [4, 6, 128, 8, 1024])
    bcol = I("bcol", [4, 128, 48])
    brow = I("brow", [4, 128, 2, 1024])
    ident = I("ident", [128, 128])
    wA = I("wA", [4, 2, 128, 8, 768])
    wR = I("wR", [4, 2, 128, 8, 1024])
    wH = I("wH", [4, 2, 128, 8, 768])
    wD = I("wD", [4, 2, 128, 8, 512])
    caw = I("caw", [4, 2, 128, 2, 4])
    hcw = I("hcw", [4, 2, 128, 6, 4])
    hsk = I("hsk", [4, 2, 128, 2, 2])
    ropec = I("ropec", [128, 32, 64])
    ropes = I("ropes", [128, 32, 64])
    qkn = I("qkn", [4, 128, 2, 128])
    rdec = I("rdec", [4, 2, 128, 4])
    rtab = I("rtab", [128, 6, 128])
    rcol = I("rcol", [128, 4])
    featsT = I("featsT", [33, 4096])
    featsTc = I("featsTc", [33, 256])
    hw1 = I("hw1", [4, 33, 64])
    hw2 = I("hw2", [4, 64, 64])
    hcols = I("hcols", [4, 64, 4])
    hw3 = I("hw3", [4, 2, 64, 2, 2, 256])
    negt = I("negt", [128, 34])
    adel = I("adel", [2, 128, 2, 2, 256])
    Fm = I("Fm", [32, 128, 8, 1024], BF16)
    Gm = I("Gm", [4, 64, 128, 1024], BF16)
    Fc = I("Fc", [128, 2, 512], BF16)
    Gc = I("Gc", [128, 4, 256], BF16)
    wg = I("wg", [4, 8, 128, 4, 8, 128])
    wb = I("wb", [4, 8, 128, 4, 4, 128])
    wo = I("wo", [4, 128, 8, D])
    lnrows = I("lnrows", [4, 128, 4, D])
    bgc = I("bgc", [4, 128, 4, 8])
    w1d = I("w1d", [2, 11, 128, 8, 256])
    w3d = I("w3d", [2, 11, 128, 8, 256])
    w2d = I("w2d", [2, 11, 128, 2, D])
    w1m = I("w1m", [2, 56, 128, 8, 512])
    w3m = I("w3m", [2, 56, 128, 8, 512])
    w2m = I("w2m", [2, 56, 128, 4, D])
    rt = I("rt", [2, 128, 8, 8])
    sel = I("sel", [8, 8, 128])
    out = _dout(nc, "out", [NT_A, 128, D])
    xb = [T("xbuf0", [NT_A, 128, D]), T("xbuf1", [NT_A, 128, D])]
    x1s = T("x1s", [NT_A, 128, D])
    YTs = T("YTs", [16, 128, NTOK_A])
    modc = T("modc", [4, 128, 2, 48])
    modr = T("modr", [4, 128, 2, 2, D])
    uTs = T("uTs", [128, 8, NTOK_A], BF16)
    wgb = T("wgb", [8, 128, 4, 8, 128], BF16)
    wbb = T("wbb", [8, 128, 4, 4, 128], BF16)
    wob = T("wob", [128, 8, D], BF16)
    with contextlib.ExitStack() as st0:
        P = Prog(nc, st0)
        emit_M2(nc, P, dict(cT=cT, wm=wm, bcol=bcol, brow=brow, modc=modc, modr=modr))
        import os as _os
        NL = int(_os.environ.get('F_LAYERS', '4'))
        for l in range(NL):
            xin = x0 if l == 0 else xb[l % 2]
            xout = out if l == NL - 1 else xb[(l + 1) % 2]
            mcA = modc[l][:, :, 0:16].rearrange("p m (k f) -> p m k f", k=2)
            mcB = (mcA, modc[l][:, :, 24:40].rearrange("p m (k f) -> p m k f", k=2))
            ustate = {'have': False}
            for half in range(2):
                emit_A(nc, P, dict(uTs=uTs, ustate=ustate, x=xin, modcols=mcA, ident=ident, wA=wA[l, half], wR=wR[l, half], wH=wH[l, half], wD=wD[l, half],
                                   caw=caw[l, half], hcw=hcw[l, half], hsk=hsk[l, half], ropec=ropec, ropes=ropes, qkn=qkn[l], rdec=rdec[l, half],
                                   rtab=rtab, rcol=rcol, featsT=featsT, featsTc=featsTc, hw1=hw1[l], hw2=hw2[l], hcols=hcols[l], hw3=hw3[l, half],
                                   negt=negt, adel=adel[half], Fm=Fm, Gm=Gm, Fc=Fc, Gc=Gc, YT=_YTView(YTs, half)))
            moe = (l % 2 == 1)
            i = l // 2
            pairs = []
            for fo in range(8):
                pairs.append((wg[l, fo].rearrange("p a b c -> p (a b c)"), wgb[fo].rearrange("p a b c -> p (a b c)")))
                pairs.append((wb[l, fo].rearrange("p a b c -> p (a b c)"), wbb[fo].rearrange("p a b c -> p (a b c)")))
            for kh in range(2):
                pairs.append((wo[l][:, kh * 4:(kh + 1) * 4, :].rearrange("p a b -> p (a b)"), wob[:, kh * 4:(kh + 1) * 4, :].rearrange("p a b -> p (a b)")))
            emit_cast(nc, P, pairs)
            for hb in range(2):
                Dm = dict(x=xin, yT=YTs, modcols=mcB, modrows=modr[l], lnrows=lnrows[l], bgc=bgc[l], ident=ident, wg=wgb, wb=wbb, wo=wob, wbf16=True,
                          x1o=x1s, xo=xout)
                if moe:
                    Dm.update(w1=w1m[i], w3=w3m[i], w2=w2m[i], rt=rt[i], sel=sel)
                else:
                    Dm.update(w1=w1d[i], w3=w3d[i], w2=w2d[i])
                emit_B(nc, P, Dm, moe, gt=(lambda t, hb=hb: hb * 16 + t if t < 16 else 32 + hb))
    return nc


def _pack_F(inp):
    C = _consts()
    sh = dict(ident=_IDENT, ropec=C['ropec'], ropes=C['ropes'], rtab=C['rtab'], rcol=C['rcol'], featsT=C['featsT'], featsTc=C['featsTc'],
              negt=C['negt'], Fm=C['Fm'], Gm=C['Gm'], Fc=C['Fc'], Gc=C['Gc'])
    sh['adel'] = np.stack([_rep(np.ascontiguousarray(C['absdelta'][:, :, h * 256:(h + 1) * 256])) for h in range(2)], 0)
    w_mod = inp['w_mod']
    sh['wm'] = np.ascontiguousarray(w_mod.reshape(4, 8, 128, 6, 1024).transpose(0, 3, 2, 1, 4))
    sh['bcol'] = np.ascontiguousarray(inp['b_mod'].reshape(4, 48, 128).transpose(0, 2, 1))
    sh['brow'] = np.stack([_rep(np.stack([inp['b_mod'][l, 2048:3072], inp['b_mod'][l, 5120:6144]], 0)) for l in range(4)], 0)
    wA, wR, wH, wD, caw, hcw, hsk, rdec, hw3 = [], [], [], [], [], [], [], [], []
    for l in range(4):
        w_in = inp['w_in'][l]
        rows = [[] for _ in range(9)]
        for half in range(2):
            r256 = half * 256 + np.arange(256)
            colsA = np.concatenate([g * 512 + r256 for g in range(3)])
            colsR = np.concatenate([1536 + g * 512 + r256 for g in range(4)])
            colsH = np.concatenate([3584 + g * 512 + r256 for g in range(3)])
            colsD = np.concatenate([5120 + r256, 5120 + 512 + half * 128 + np.arange(128), 5120 + 768 + half * 128 + np.arange(128)])
            ch = r256
            cawv = np.stack([inp['conv_a_w'][l][0, ch], inp['conv_a_w'][l][1, ch], inp['conv_a_w'][l][2, ch], inp['conv_a_b'][l][ch]], -1)
            hch = np.concatenate([g * 512 + ch for g in range(3)])
            hcwv = np.stack([inp['hy_conv_w'][l][0, hch], inp['hy_conv_w'][l][1, hch], inp['hy_conv_w'][l][2, hch], inp['hy_conv_b'][l][hch]], -1)
            vals = [_tile_rows(w_in[:, colsA]), _tile_rows(w_in[:, colsR]), _tile_rows(w_in[:, colsH]), _tile_rows(w_in[:, colsD]),
                    cawv.reshape(2, 128, 4).transpose(1, 0, 2), hcwv.reshape(6, 128, 4).transpose(1, 0, 2),
                    inp['hy_skip'][l][:, ch].reshape(2, 2, 128).transpose(2, 0, 1),
                    _rep(inp['ret_decay'][l][:, half * 2:half * 2 + 2].reshape(4)),
                    inp['hy_w3'][l].reshape(64, 2, 2, 512)[:, :, :, half * 256:(half + 1) * 256]]
            for r_, v_ in zip(rows, vals):
                r_.append(v_)
        for lst, r_ in zip((wA, wR, wH, wD, caw, hcw, hsk, rdec, hw3), rows):
            lst.append(np.stack(r_, 0))
    for n_, lst in zip(('wA', 'wR', 'wH', 'wD', 'caw', 'hcw', 'hsk', 'rdec', 'hw3'), (wA, wR, wH, wD, caw, hcw, hsk, rdec, hw3)):
        sh[n_] = np.ascontiguousarray(np.stack(lst, 0))
    sh['qkn'] = np.stack([_rep(np.stack([inp['q_norm'][l], inp['k_norm'][l]], 0)) for l in range(4)], 0)
    sh['hw1'] = np.ascontiguousarray(inp['hy_w1'])
    sh['hw2'] = np.ascontiguousarray(inp['hy_w2'])
    sh['hcols'] = np.ascontiguousarray(np.stack([inp['hy_b1'], inp['hy_b2'], inp['hy_freq'][:, 0], inp['hy_freq'][:, 1]], -1))
    pw = [prep_B_weights(l, inp) for l in range(4)]
    for n_ in ('wg', 'wb', 'wo', 'lnrows', 'bgc'):
        sh[n_] = np.stack([pw[l][n_] for l in range(4)], 0)
    for n_ in ('w1', 'w3', 'w2'):
        sh[n_ + 'd'] = np.stack([pw[0][n_], pw[2][n_]], 0)
        sh[n_ + 'm'] = np.stack([pw[1][n_], pw[3][n_]], 0)
    sh['rt'] = np.stack([pw[1]['rt'], pw[3]['rt']], 0)
    sh['sel'] = pw[1]['sel']
    per = []
    for b in range(4):
        cc = np.stack([inp['c'][b], inp['c_ctx']], 0)
        per.append(dict(x0=np.ascontiguousarray(np.concatenate([inp['x'][b], inp['ctx'][b]], 0).reshape(NT_A, 128, 1024)),
                        cT=np.ascontiguousarray(cc.T.reshape(8, 128, 2).transpose(1, 0, 2))))
    return sh, per


def kernel_fused(**inp):
    inp = {k_: np.asarray(v) for k_, v in inp.items()}
    sh, per = _pack_F(inp)
    ins = [dict(sh, **per[b]) for b in range(4)]
    res = run_bass_kernel_spmd(_prog('F', build_F), ins, core_ids=list(range(4)))
    out = np.stack([res.results[b]['out'].reshape(NTOK_A, 1024)[:4096] for b in range(4)], 0)
    return np.ascontiguousarray(out, dtype=np.float32)


def kernel_unfused(**inp):
    inp = {k_: np.asarray(v) for k_, v in inp.items()}
    mod = run_M(inp['c'], inp['c_ctx'], inp['w_mod'], inp['b_mod'])
    x_cur = np.ascontiguousarray(inp['x'], dtype=np.float32)
    h_cur = np.ascontiguousarray(inp['ctx'], dtype=np.float32)
    for l in range(4):
        YT = run_A(l, x_cur, h_cur, mod[:, l], inp)
        x_cur, h_cur, _ = run_B(l, x_cur, h_cur, YT, mod[:, l], inp)
    return x_cur


def kernel(**inp):
    return kernel_fused(**inp)
```
